# Optimizing a Trainium2 kernel written in Bass

```python
import math
import jax
import jax.numpy as jnp
from jax import lax
import numpy as np


D_MODEL = 2048
BATCH = 16
SEQ = 2048
DEPTH = 2

GRID_W = 64
CTX_LEN = 256
N_MIXERS = 4
GROUP_WIDTH = D_MODEL // N_MIXERS
MIX_WIDTH = N_MIXERS * GROUP_WIDTH
N_IN_PARTS = 12
P_S5_U = 0
P_LRU_X = 1
P_LRU_GATE = 2
P_HG_Q = 3
P_HG_FF = 4
P_HG_FB = 5
P_HG_I = 6
P_HG_G = 7
P_RET_Q = 8
P_RET_K = 9
P_RET_V = 10
P_RET_G = 11
S5_CH = 16
S5_GROUPS = GROUP_WIDTH // S5_CH
S5_STATE = 64
S5_DT_MIN = 1e-3
S5_DT_MAX = 1e-1
LRU_HEADS = 8
LRU_HEAD_DIM = GROUP_WIDTH // LRU_HEADS
LRU_CONV = 4
LRU_C = 8.0
HGRN_HEADS = 4
HGRN_DIM = GROUP_WIDTH // HGRN_HEADS
RET_HEADS = 4
RET_DIM = GROUP_WIDTH // RET_HEADS
RET_DECAY_EXP_FWD = 5.0
RET_DECAY_EXP_BWD = 5.5
ROPE_BASE = 10000.0
CHUNK = 64
N_EXPERTS = 16
N_EXPERT_GROUPS = 4
EXPERTS_PER_GROUP = N_EXPERTS // N_EXPERT_GROUPS
TOP_K = 2
D_FF_EXPERT = D_MODEL // 2
EPS = 1e-6

kernel_name = 'hybrid_headgroup_flow_block'


def rms_norm(x, g):
    xf = x.astype(jnp.float32)
    y = xf * lax.rsqrt(jnp.mean(xf * xf, axis=-1, keepdims=True) + EPS)
    return (y * g.astype(jnp.float32)).astype(x.dtype)


def head_rms(x):
    return x * lax.rsqrt(jnp.mean(x * x, axis=-1, keepdims=True) + EPS)


def modulate(h, shift, scale):
    return h * (1.0 + scale) + shift


def flip_t(t):
    return jnp.flip(t, axis=1)


def gated_head_norm(o, g):
    return head_rms(o).reshape(g.shape) * jax.nn.silu(g.astype(jnp.float32))


def linear_scan(a, b, h0, reverse):
    def combine(e1, e2):
        return e1[0] * e2[0], e2[0] * e1[1] + e2[1]
    a_cum, h = lax.associative_scan(combine, (a, b), reverse=reverse, axis=1)
    return h + a_cum * h0[:, None]


def s5_states(u, h0_re, h0_im, lam_re, lam_im, log_dt, b_re, b_im, reverse):
    bsz, n, _ = u.shape
    ug = u.reshape(bsz, n, S5_GROUPS, S5_CH)
    dt = jnp.exp(log_dt.astype(jnp.float32))[:, None]
    lr, li = lam_re.astype(jnp.float32), lam_im.astype(jnp.float32)
    mag = jnp.exp(lr * dt)
    abar_re, abar_im = mag * jnp.cos(li * dt), mag * jnp.sin(li * dt)
    den = lr * lr + li * li
    zr = abar_re - 1.0
    w_re = (zr * lr + abar_im * li) / den
    w_im = (abar_im * lr - zr * li) / den
    bu_re = jnp.einsum('blgc,gcp->blgp', ug, b_re.astype(jnp.float32))
    bu_im = jnp.einsum('blgc,gcp->blgp', ug, b_im.astype(jnp.float32))
    drive_re = w_re * bu_re - w_im * bu_im
    drive_im = w_re * bu_im + w_im * bu_re
    a_re = jnp.broadcast_to(abar_re, (1, n) + abar_re.shape)
    a_im = jnp.broadcast_to(abar_im, (1, n) + abar_im.shape)

    def combine(e1, e2):
        a1r, a1i, b1r, b1i = e1
        a2r, a2i, b2r, b2i = e2
        return (a1r * a2r - a1i * a2i, a1r * a2i + a1i * a2r,
                a2r * b1r - a2i * b1i + b2r, a2r * b1i + a2i * b1r + b2i)

    p_re, p_im, h_re, h_im = lax.associative_scan(
        combine, (a_re, a_im, drive_re, drive_im), reverse=reverse, axis=1)
    h0r, h0i = h0_re[:, None], h0_im[:, None]
    return h_re + p_re * h0r - p_im * h0i, h_im + p_re * h0i + p_im * h0r


def s5_readout(h_re, h_im, c_re, c_im):
    bsz, n = h_re.shape[:2]
    y = (jnp.einsum('blgp,gpc->blgc', h_re, c_re.astype(jnp.float32))
         - jnp.einsum('blgp,gpc->blgc', h_im, c_im.astype(jnp.float32)))
    return y.reshape(bsz, n, GROUP_WIDTH)


def s5_glu(y, glu_w, glu_b):
    y = jax.nn.gelu(y)
    return y * jax.nn.sigmoid(y @ glu_w.astype(jnp.float32) + glu_b.astype(jnp.float32))


def s5_mixer(pc, pl, lam_re, lam_im, log_dt, b_re, b_im, c_re, c_im, d, glu_w, glu_b, ctx_out):
    u_c = pc[P_S5_U].astype(jnp.float32)
    u_l = pl[P_S5_U].astype(jnp.float32)
    d = d.astype(jnp.float32)
    zero = jnp.zeros((u_c.shape[0], S5_GROUPS, S5_STATE), jnp.float32)
    y_l = d * u_l
    y_c = d * u_c if ctx_out else None
    for dr in (0, 1):
        rev = dr == 1
        end = 0 if rev else -1
        prm = (lam_re[dr], lam_im[dr], log_dt[dr], b_re[dr], b_im[dr])
        hc_re, hc_im = s5_states(u_c, zero, zero, *prm, rev)
        hl_re, hl_im = s5_states(u_l, hc_re[:, end], hc_im[:, end], *prm, rev)
        y_l = y_l + s5_readout(hl_re, hl_im, c_re[dr], c_im[dr])
        if ctx_out:
            y_c = y_c + s5_readout(hc_re, hc_im, c_re[dr], c_im[dr])
    out_c = s5_glu(y_c, glu_w, glu_b) if ctx_out else None
    return out_c, s5_glu(y_l, glu_w, glu_b)


def short_conv(x, w, b):
    k = w.shape[0]
    left = k // 2
    y = lax.conv_general_dilated(x, w[:, None, :].astype(x.dtype), window_strides=(1,),
                                 padding=[(left, k - 1 - left)],
                                 dimension_numbers=('NWC', 'WIO', 'NWC'),
                                 feature_group_count=x.shape[-1])
    return y + b.astype(x.dtype)


def rglru_coefficients(x, wa, ba, wx, bx, lam):
    bsz, n, w = x.shape
    xh = x.reshape(bsz, n, LRU_HEADS, LRU_HEAD_DIM)
    r = jax.nn.sigmoid(jnp.einsum('blhi,hij->blhj', xh, wa.astype(jnp.float32)).reshape(bsz, n, w)
                       + ba.astype(jnp.float32))
    i = jax.nn.sigmoid(jnp.einsum('blhi,hij->blhj', xh, wx.astype(jnp.float32)).reshape(bsz, n, w)
                       + bx.astype(jnp.float32))
    log_a = -LRU_C * r * jax.nn.softplus(-lam.astype(jnp.float32))
    return jnp.exp(log_a), jnp.sqrt(-jnp.expm1(2.0 * log_a)) * (i * x)


def rglru_mixer(pc, pl, conv_w, conv_b, wa, ba, wx, bx, lam, ctx_out):
    x_c = short_conv(pc[P_LRU_X], conv_w, conv_b).astype(jnp.float32)
    x_l = short_conv(pl[P_LRU_X], conv_w, conv_b).astype(jnp.float32)
    zero = jnp.zeros((x_c.shape[0], GROUP_WIDTH), jnp.float32)
    h_c_sum, h_l_sum = 0.0, 0.0
    for dr in (0, 1):
        rev = dr == 1
        prm = (wa[dr], ba[dr], wx[dr], bx[dr], lam[dr])
        h_c = linear_scan(*rglru_coefficients(x_c, *prm), zero, rev)
        h_l = linear_scan(*rglru_coefficients(x_l, *prm), h_c[:, 0 if rev else -1], rev)
        h_l_sum = h_l_sum + h_l
        if ctx_out:
            h_c_sum = h_c_sum + h_c
    out_l = h_l_sum * jax.nn.gelu(pl[P_LRU_GATE].astype(jnp.float32))
    out_c = h_c_sum * jax.nn.gelu(pc[P_LRU_GATE].astype(jnp.float32)) if ctx_out else None
    return out_c, out_l


def chunk_gla(q, k, v, log_f, s0, with_out):
    bsz, n, h, _ = q.shape
    dv = v.shape[-1]
    nc = n // CHUNK

    def blocks(t):
        return t.reshape(bsz, nc, CHUNK, h, t.shape[-1]).transpose(1, 0, 3, 2, 4)

    lower = jnp.tril(jnp.ones((CHUNK, CHUNK), bool))[:, :, None]

    def step(s, blk):
        qb, kb, vb, gb = blk
        cum = jnp.cumsum(gb, axis=2)
        last = cum[:, :, -1:]
        s_new = (jnp.exp(last[:, :, 0])[..., None] * s
                 + jnp.einsum('bhsd,bhsv->bhdv', kb * jnp.exp(last - cum), vb))
        if not with_out:
            return s_new, None
        rel = jnp.where(lower, cum[:, :, :, None] - cum[:, :, None], -jnp.inf)
        scores = jnp.einsum('bhtd,bhsd,bhtsd->bhts', qb, kb, jnp.exp(rel))
        o = (jnp.einsum('bhts,bhsv->bhtv', scores, vb)
             + jnp.einsum('bhtd,bhdv->bhtv', qb * jnp.exp(cum), s))
        return s_new, o

    s_fin, o = lax.scan(step, s0, (blocks(q), blocks(k), blocks(v), blocks(log_f)))
    if not with_out:
        return None, s_fin
    return o.transpose(1, 0, 3, 2, 4).reshape(bsz, n, h, dv), s_fin


def gla_two_stream(q_c, v_c, kg_c, q_l, v_l, kg_l, ctx_out):
    s0 = jnp.zeros((q_c.shape[0], q_c.shape[2], q_c.shape[3], v_c.shape[3]), jnp.float32)
    o_c, o_l = 0.0, 0.0
    for dr in (0, 1):
        orient = flip_t if dr == 1 else (lambda t: t)
        k_c, g_c = kg_c[dr]
        k_l, g_l = kg_l[dr]
        oc, s_ctx = chunk_gla(orient(q_c), orient(k_c), orient(v_c), orient(g_c), s0, ctx_out)
        ol, _ = chunk_gla(orient(q_l), orient(k_l), orient(v_l), orient(g_l), s_ctx, True)
        o_l = o_l + orient(ol)
        if ctx_out:
            o_c = o_c + orient(oc)
    return (o_c if ctx_out else None), o_l


def hgrn_lower_bound(lb_logits, layer):
    cum = jnp.cumsum(jax.nn.softmax(lb_logits.astype(jnp.float32), axis=0), axis=0)
    return cum[layer] - cum[0]


def hgrn2_mixer(pc, pl, lbs, ctx_out):
    def prepare(p):
        bsz, n, _ = p[P_HG_Q].shape

        def heads(t):
            return t.astype(jnp.float32).reshape(bsz, n, HGRN_HEADS, HGRN_DIM)

        q = heads(p[P_HG_Q]) * HGRN_DIM ** -0.5
        v = jax.nn.silu(heads(p[P_HG_I]))
        kg = []
        for z, lb in zip((heads(p[P_HG_FF]), heads(p[P_HG_FB])), lbs):
            lb = lb.reshape(HGRN_HEADS, HGRN_DIM)
            f = lb + (1.0 - lb) * jax.nn.sigmoid(z)
            kg.append(((1.0 - lb) * jax.nn.sigmoid(-z), jnp.log(f)))
        return q, v, kg

    q_c, v_c, kg_c = prepare(pc)
    q_l, v_l, kg_l = prepare(pl)
    o_c, o_l = gla_two_stream(q_c, v_c, kg_c, q_l, v_l, kg_l, ctx_out)
    out_c = gated_head_norm(o_c, pc[P_HG_G]) if ctx_out else None
    return out_c, gated_head_norm(o_l, pl[P_HG_G])


def retention_log_decay(offset):
    return jnp.log1p(-jnp.exp2(-(offset + jnp.arange(RET_HEADS, dtype=jnp.float32))))


def axial_rotary(x):
    n = x.shape[1]
    rows = n // GRID_W
    row = jnp.repeat(jnp.arange(rows, dtype=jnp.float32), GRID_W)
    col = jnp.tile(jnp.arange(GRID_W, dtype=jnp.float32), rows)
    half = x.shape[-1] // 2
    quarter = half // 2
    inv_freq = ROPE_BASE ** (-jnp.arange(quarter, dtype=jnp.float32) / quarter)
    ang = jnp.concatenate([row[:, None] * inv_freq, col[:, None] * inv_freq], axis=-1)[None, :, None, :]
    cos, sin = jnp.cos(ang), jnp.sin(ang)
    x1, x2 = x[..., :half], x[..., half:]
    return jnp.concatenate([x1 * cos - x2 * sin, x1 * sin + x2 * cos], axis=-1)


def retention_mixer(pc, pl, ctx_out):
    def heads(t):
        bsz, n, _ = t.shape
        return t.astype(jnp.float32).reshape(bsz, n, RET_HEADS, RET_DIM)

    scale = RET_DIM ** -0.5
    q_c, k_c, v_c = heads(pc[P_RET_Q]), heads(pc[P_RET_K]) * scale, heads(pc[P_RET_V])
    q_l = axial_rotary(heads(pl[P_RET_Q]))
    k_l = axial_rotary(heads(pl[P_RET_K])) * scale
    v_l = heads(pl[P_RET_V])
    decays = (retention_log_decay(RET_DECAY_EXP_FWD), retention_log_decay(RET_DECAY_EXP_BWD))
    kg_c = [(k_c, jnp.broadcast_to(dec[:, None], k_c.shape)) for dec in decays]
    kg_l = [(k_l, jnp.broadcast_to(dec[:, None], k_l.shape)) for dec in decays]
    o_c, o_l = gla_two_stream(q_c, v_c, kg_c, q_l, v_l, kg_l, ctx_out)
    out_c = gated_head_norm(o_c, pc[P_RET_G]) if ctx_out else None
    return out_c, gated_head_norm(o_l, pl[P_RET_G])


def moe_ffn(h, router_w, router_bias, w_gate, w_up, w_down):
    t = h.shape[0]
    scores = jax.nn.sigmoid(h.astype(jnp.float32) @ router_w.astype(jnp.float32))
    biased = scores + router_bias.astype(jnp.float32)
    grouped = biased.reshape(t, N_EXPERT_GROUPS, EXPERTS_PER_GROUP)
    group_score = jnp.sum(lax.top_k(grouped, TOP_K)[0], axis=-1)
    best = jnp.argmax(group_score, axis=-1)
    in_group = (jnp.arange(N_EXPERTS) // EXPERTS_PER_GROUP)[None, :] == best[:, None]
    _, idx = lax.top_k(jnp.where(in_group, biased, -jnp.inf), TOP_K)
    w = jnp.take_along_axis(scores, idx, axis=-1)
    w = w / jnp.sum(w, axis=-1, keepdims=True)
    gates = jnp.sum(jax.nn.one_hot(idx, N_EXPERTS, dtype=jnp.float32) * w[..., None], axis=1)
    y = jnp.zeros(h.shape, jnp.float32)
    for e in range(N_EXPERTS):
        he = jax.nn.silu(h @ w_gate[e]) * (h @ w_up[e])
        y = y + gates[:, e:e + 1] * (he @ w_down[e]).astype(jnp.float32)
    return y.astype(h.dtype)


def setup_inputs(seed: int = 0) -> dict:
    key = jax.random.key(seed)
    keys = iter(jax.random.split(key, 40))

    def normal(shape, scale):
        return jax.random.normal(next(keys), shape, jnp.float32) * scale

    def uniform(shape, lo, hi):
        return jax.random.uniform(next(keys), shape, jnp.float32, lo, hi)

    d, w, nl = D_MODEL, GROUP_WIDTH, DEPTH
    inp = {}
    inp['x'] = normal((BATCH, SEQ, d), 1.0)
    inp['c'] = normal((BATCH, d), 1.0)
    inp['ctx'] = normal((BATCH, CTX_LEN, d), 1.0)
    inp['c_ctx'] = normal((d,), 1.0)
    inp['norm_mix_g'] = 1.0 + normal((nl, d), 0.01)
    inp['norm_ffn_g'] = 1.0 + normal((nl, d), 0.01)
    inp['w_mod'] = normal((nl, d, 6 * d), 0.5 * d ** -0.5)
    inp['b_mod'] = normal((nl, 6 * d), 0.02)
    inp['w_in'] = normal((nl, d, N_IN_PARTS * w), d ** -0.5)
    inp['w_out'] = normal((nl, MIX_WIDTH, d), MIX_WIDTH ** -0.5)
    inp['s5_lam_re'] = -0.5 + normal((nl, 2, S5_GROUPS, S5_STATE), 0.01)
    inp['s5_lam_im'] = jnp.pi * jnp.arange(S5_STATE, dtype=jnp.float32) + normal((nl, 2, S5_GROUPS, S5_STATE), 0.01)
    inp['s5_log_dt'] = uniform((nl, 2, S5_GROUPS), math.log(S5_DT_MIN), math.log(S5_DT_MAX))
    inp['s5_b_re'] = normal((nl, 2, S5_GROUPS, S5_CH, S5_STATE), (2 * S5_CH) ** -0.5)
    inp['s5_b_im'] = normal((nl, 2, S5_GROUPS, S5_CH, S5_STATE), (2 * S5_CH) ** -0.5)
    inp['s5_c_re'] = normal((nl, 2, S5_GROUPS, S5_STATE, S5_CH), 0.5)
    inp['s5_c_im'] = normal((nl, 2, S5_GROUPS, S5_STATE, S5_CH), 0.5)
    inp['s5_d'] = normal((nl, w), 1.0)
    inp['s5_glu_w'] = normal((nl, w, w), w ** -0.5)
    inp['s5_glu_b'] = normal((nl, w), 0.01)
    inp['lru_conv_w'] = normal((nl, LRU_CONV, w), LRU_CONV ** -0.5)
    inp['lru_conv_b'] = normal((nl, w), 0.01)
    inp['lru_wa'] = normal((nl, 2, LRU_HEADS, LRU_HEAD_DIM, LRU_HEAD_DIM), LRU_HEAD_DIM ** -0.5)
    inp['lru_ba'] = normal((nl, 2, w), 0.01)
    inp['lru_wx'] = normal((nl, 2, LRU_HEADS, LRU_HEAD_DIM, LRU_HEAD_DIM), LRU_HEAD_DIM ** -0.5)
    inp['lru_bx'] = normal((nl, 2, w), 0.01)
    sig = uniform((nl, 2, w), 0.9, 0.999) ** (1.0 / LRU_C)
    inp['lru_lam'] = jnp.log(sig) - jnp.log1p(-sig)
    inp['hgrn_lb_logits'] = normal((2, nl, w), 1.0)
    inp['router_w'] = normal((d, N_EXPERTS), d ** -0.5)
    inp['router_bias'] = normal((N_EXPERTS,), 0.01)
    inp['moe_w_gate'] = normal((nl, N_EXPERTS, d, D_FF_EXPERT), d ** -0.5)
    inp['moe_w_up'] = normal((nl, N_EXPERTS, d, D_FF_EXPERT), d ** -0.5)
    inp['moe_w_down'] = normal((nl, N_EXPERTS, D_FF_EXPERT, d), D_FF_EXPERT ** -0.5)
    inp['final_norm_g'] = 1.0 + normal((d,), 0.01)
    return inp


def reference(x, c, ctx, c_ctx, norm_mix_g, norm_ffn_g, w_mod, b_mod, w_in, w_out,
              s5_lam_re, s5_lam_im, s5_log_dt, s5_b_re, s5_b_im, s5_c_re, s5_c_im, s5_d,
              s5_glu_w, s5_glu_b, lru_conv_w, lru_conv_b, lru_wa, lru_ba, lru_wx, lru_bx, lru_lam,
              hgrn_lb_logits, router_w, router_bias, moe_w_gate, moe_w_up, moe_w_down, final_norm_g):
    bsz, n, dm = x.shape
    dtype = x.dtype
    xl = x
    xc = ctx.astype(dtype)
    for layer in range(DEPTH):
        ctx_out = layer < DEPTH - 1
        mod_l = (jax.nn.silu(c) @ w_mod[layer] + b_mod[layer]).reshape(bsz, 6, 1, dm)
        mod_c = (jax.nn.silu(c_ctx) @ w_mod[layer] + b_mod[layer]).reshape(6, dm)

        hl = modulate(rms_norm(xl, norm_mix_g[layer]), mod_l[:, 0], mod_l[:, 1])
        hc = modulate(rms_norm(xc, norm_mix_g[layer]), mod_c[0], mod_c[1])
        pl = jnp.split(hl @ w_in[layer], N_IN_PARTS, axis=-1)
        pc = jnp.split(hc @ w_in[layer], N_IN_PARTS, axis=-1)
        a_c, a_l = s5_mixer(pc, pl, s5_lam_re[layer], s5_lam_im[layer], s5_log_dt[layer],
                            s5_b_re[layer], s5_b_im[layer], s5_c_re[layer], s5_c_im[layer],
                            s5_d[layer], s5_glu_w[layer], s5_glu_b[layer], ctx_out)
        b_c, b_l = rglru_mixer(pc, pl, lru_conv_w[layer], lru_conv_b[layer], lru_wa[layer],
                               lru_ba[layer], lru_wx[layer], lru_bx[layer], lru_lam[layer], ctx_out)
        lbs = (hgrn_lower_bound(hgrn_lb_logits[0], layer), hgrn_lower_bound(hgrn_lb_logits[1], layer))
        h_c, h_l = hgrn2_mixer(pc, pl, lbs, ctx_out)
        r_c, r_l = retention_mixer(pc, pl, ctx_out)
        mix_l = jnp.concatenate([a_l, b_l, h_l, r_l], axis=-1).astype(dtype) @ w_out[layer]
        xl = xl + mod_l[:, 2] * mix_l
        if ctx_out:
            mix_c = jnp.concatenate([a_c, b_c, h_c, r_c], axis=-1).astype(dtype) @ w_out[layer]
            xc = xc + mod_c[2] * mix_c

        hl = modulate(rms_norm(xl, norm_ffn_g[layer]), mod_l[:, 3], mod_l[:, 4])
        if ctx_out:
            hc = modulate(rms_norm(xc, norm_ffn_g[layer]), mod_c[3], mod_c[4])
            tokens = jnp.concatenate([hl.reshape(-1, dm), hc.reshape(-1, dm)], axis=0)
        else:
            tokens = hl.reshape(-1, dm)
        y = moe_ffn(tokens, router_w, router_bias, moe_w_gate[layer], moe_w_up[layer], moe_w_down[layer])
        xl = xl + mod_l[:, 5] * y[: bsz * n].reshape(bsz, n, dm)
        if ctx_out:
            xc = xc + mod_c[5] * y[bsz * n:].reshape(bsz, -1, dm)
    return rms_norm(xl, final_norm_g)
```

```python
import math
from contextlib import ExitStack
import numpy as np
import concourse.bass as bass
import concourse.mybir as mybir
from concourse.bass_utils import run_bass_kernel_spmd

F32 = mybir.dt.float32
BF16 = mybir.dt.bfloat16
I32 = mybir.dt.int32
ALU = mybir.AluOpType
AF = mybir.ActivationFunctionType
AX = mybir.AxisListType

D = 2048
KC = 16
W = 512
NPARTS = 14
FM_PARTS = [0, 1, 2, 3, 4, 5, 8, 9, 12, 13]
TM_PARTS = [6, 7, 10, 11]
NE = 16
DFF = 1024
EPS = 1e-6
TWO_PI = 2.0 * math.pi


class Buf:
    __slots__ = ("w", "r")

    def __init__(self):
        self.w = None
        self.r = {}


class Eng:
    def __init__(self, name, h, sem):
        self.name, self.h, self.sem = name, h, sem
        self.n = 0
        self.seen = {}
        self.dsems, self.dcnt, self.dnext = [], [], 0


class Sched:
    def __init__(self, nc, stack, n_dma_sems=16):
        self.nc = nc
        self.E = {}
        for name, h in (("pe", nc.tensor), ("dve", nc.vector), ("act", nc.scalar),
                        ("pool", nc.gpsimd), ("sp", nc.sync)):
            self.E[name] = Eng(name, h, stack.enter_context(nc.semaphore("s_" + name)))
        for name in ("sp", "act", "pool"):
            e = self.E[name]
            for i in range(n_dma_sems):
                e.dsems.append(stack.enter_context(nc.semaphore(f"d_{name}{i}")))
                e.dcnt.append(0)
        self.ninstr = 0

    def _wait(self, e, sem, val):
        if e.seen.get(sem, 0) < val:
            e.h.wait_ge(sem, val)
            e.seen[sem] = val

    def _deps(self, e, reads, writes):
        for b in reads:
            if b.w is not None:
                self._wait(e, *b.w)
        for b in writes:
            if b.w is not None:
                self._wait(e, *b.w)
            for s, v in b.r.items():
                self._wait(e, s, v)

    @staticmethod
    def _mark(tok, reads, writes):
        s, v = tok
        for b in reads:
            if b.r.get(s, 0) < v:
                b.r[s] = v
        for b in writes:
            b.w = tok
            b.r = {}

    def op(self, eng, fn, reads=(), writes=()):
        e = self.E[eng]
        self._deps(e, reads, writes)
        ins = fn(e.h)
        e.n += 1
        ins.then_inc(e.sem, 1)
        self._mark((e.sem, e.n), reads, writes)
        self.ninstr += 1
        return ins

    def dma(self, eng, out, in_, reads=(), writes=(), **kw):
        e = self.E[eng]
        self._deps(e, reads, writes)
        i = e.dnext
        e.dnext = (i + 1) % len(e.dsems)
        sem = e.dsems[i]
        if e.dcnt[i]:
            self._wait(e, sem, e.dcnt[i])
        ins = e.h.dma_start(out=out, in_=in_, **kw)
        e.dcnt[i] += 16
        ins.then_inc(sem, 16)
        self._mark((sem, e.dcnt[i]), reads, writes)
        self.ninstr += 1
        return ins

    def barrier(self):
        for e in self.E.values():
            for f in self.E.values():
                if f is not e and f.n:
                    self._wait(e, f.sem, f.n)
            for q in ("sp", "act", "pool"):
                qe = self.E[q]
                for sem, cnt in zip(qe.dsems, qe.dcnt):
                    if cnt:
                        self._wait(e, sem, cnt)


class Cfg:
    def __init__(self, NB=2, CTXL=256, SEQL=2048, L=2, dbg=False):
        self.NB, self.CTXL, self.SEQL, self.L, self.dbg = NB, CTXL, SEQL, L, dbg
        self.TT = CTXL + SEQL
        self.NT = self.TT // 128
        self.NTC = CTXL // 128
        self.NCH = self.TT // 64
        self.NCHC = CTXL // 64


def tok_groups(T, g=512):
    out, t = [], 0
    while t < T:
        n = min(g, T - t)
        out.append((t, n))
        t += n
    return out


class Prog:
    def __init__(self, cfg):
        self.c = cfg
        self.nc = bass.Bass("TRN2", target_bir_lowering=False)
        self.inp = {}

    def din(self, name, shape, dt=F32):
        t = self.nc.dram_tensor(name, list(shape), dt, kind="ExternalInput").ap()
        self.inp[name] = t
        return t

    def dscr(self, name, shape, dt=F32):
        kind = "ExternalOutput" if self.c.dbg else "Internal"
        return self.nc.dram_tensor(name, list(shape), dt, kind=kind).ap()

    def sb(self, st, name, shape, dt=F32):
        self._uid = getattr(self, "_uid", 0) + 1
        return st.enter_context(self.nc.sbuf_tensor(f"{name}_{self._uid}", list(shape), dt))

    def V(self, fn, R=(), Wr=()):
        return self.S.op("dve", fn, R, Wr)

    def A(self, fn, R=(), Wr=()):
        return self.S.op("act", fn, R, Wr)

    def G(self, fn, R=(), Wr=()):
        return self.S.op("pool", fn, R, Wr)

    def PE(self, fn, R=(), Wr=()):
        return self.S.op("pe", fn, R, Wr)

    def ld(self, out, in_, Wr, q="sp", R=()):
        return self.S.dma(q, out, in_, reads=R, writes=Wr)

    def ldc(self, out, in_, Wr, R=()):
        return self.S.dma("pool", out, in_, reads=R, writes=Wr)

    def st(self, out, in_, R, q="sp"):
        return self.S.dma(q, out, in_, reads=R, writes=())

    def build(self):
        c, nc = self.c, self.nc
        NB, TT, NT, L = c.NB, c.TT, c.NT, c.L
        i_ = self.din
        self.xin = i_("xin", [NB, TT, D])
        self.cT = i_("cT", [128, KC, 3])
        self.gmix = i_("gmix", [L, 128, KC])
        self.gffn = i_("gffn", [L, 128, KC])
        self.gfin = i_("gfin", [1, D])
        self.w_mod = i_("w_mod", [L, D, 6 * D])
        self.b_mod = i_("b_mod", [L, 6 * D])
        self.w_in = i_("w_in", [L, D, NPARTS * W])
        self.w_out = i_("w_out", [L, D, D])
        self.s5B = i_("s5B", [L, 2, 16, 128, 2, 128])
        self.s5C = i_("s5C", [L, 2, 16, 128, 2, 128])
        self.s5lam = i_("s5lam", [L, 128, 3, 2, 16])
        self.s5d = i_("s5d", [L, 128, 4])
        self.gluw = i_("gluw", [L, W, W])
        self.glub = i_("glub", [L, 128, 4])
        self.kidx = i_("kidx", [2, 128, TT])
        self.convw = i_("convw", [L, 128, 4, 4])
        self.lruv = i_("lruv", [L, 128, 4, 7])
        self.lruw = i_("lruw", [L, 2, 2, 4, 128, 128])
        self.hglb = i_("hglb", [2, L, 128, 4])
        self.rang = i_("rang", [128, c.SEQL])
        self.rw = i_("rw", [128, KC, NE])
        self.rbias = i_("rbias", [1, NE])
        self.wg = i_("wg", [L, NE, D, DFF])
        self.wu = i_("wu", [L, NE, D, DFF])
        self.wd = i_("wd", [L, NE, DFF, D])
        self.ident = i_("ident", [128, 128])
        self.masks = i_("masks", [2, 128, 4, 64])
        self.rst = i_("rst", [128, TT + 1])
        self.rst32 = i_("rst32", [128, TT + 1])
        self.masks32 = i_("masks32", [2, 128, 4, 32])
        self.sel3 = i_("sel3", [3, 3, 128])
        self.out = nc.dram_tensor("out", [NB, c.SEQL, D], F32, kind="ExternalOutput").ap()

        self.xres = self.dscr("xres", [NB, TT, D])
        self.Pfm = self.dscr("Pfm", [NB, len(FM_PARTS) * W, TT])
        self.Ptm = self.dscr("Ptm", [NB, TT, len(TM_PARTS) * W])
        self.mixT = self.dscr("mixT", [NB, D, TT], BF16)
        self.h2T = self.dscr("h2T", [NB, D, TT], BF16)
        self.gates = self.dscr("gates", [NB, TT, NE])
        self.ygD = self.dscr("ygD", [NB, W, TT])
        self.modD = self.dscr("modD", [3, 2, D])

        with ExitStack() as top:
            self.S = Sched(nc, top)
            S = self.S
            self.ps = [top.enter_context(nc.psum_tensor(f"ps{i}", [128, 512], F32)) for i in range(8)]
            self.pb = [Buf() for _ in range(8)]
            self.idf = self.sb(top, "idf", [128, 128]); self.idb = self.sb(top, "idb", [128, 128], BF16)
            self.bconst = Buf()
            self.ld(self.idf[:], self.ident[:, :], [self.bconst])
            self.ldc(self.idb[:], self.ident[:, :], [self.bconst])
            self.cmask = self.sb(top, "cmask", [128, 2, 4, 64])
            self.ld(self.cmask[:], self.masks.rearrange("a p h j -> p a h j"), [self.bconst])
            self.cmask32 = self.sb(top, "cmask32", [128, 2, 4, 32])
            self.ld(self.cmask32[:], self.masks32.rearrange("a p h j -> p a h j"), [self.bconst])
            self.epsc = self.sb(top, "epsc", [128, 1])
            self.V(lambda h: h.memset(self.epsc[:], EPS), [], [self.bconst])
            self.sel3t = self.sb(top, "sel3t", [3, 3, 128])
            self.ld(self.sel3t[:], self.sel3[:, :, :], [self.bconst])
            self.modP = self.sb(top, "modP", [128, 6, KC, 3])
            self.AB = self.sb(top, "AB", [128, 4, KC, 3])
            self.bmod = Buf()
            S.barrier()
            for l in range(L):
                last = (l == L - 1)
                self.phase_mod(l)
                S.barrier()
                for b in range(NB):
                    xsrc = self.xin if l == 0 else self.xres
                    self.phase_proj(l, b, xsrc)
                    S.barrier()
                    self.phase_lru(l, b)
                    S.barrier()
                    self.phase_gla(l, b, 0)
                    S.barrier()
                    self.phase_gla(l, b, 1)
                    S.barrier()
                    self.phase_s5(l, b)
                    S.barrier()
                    self.phase_out(l, b, xsrc, last)
                    S.barrier()
                    self.phase_moe(l, b, last)
                    S.barrier()
            self.phase_final()
            S.barrier()
        return nc

    def phase_mod(self, l):
        c, S = self.c, self.S
        with ExitStack() as st:
            cT = self.sb(st, "cT", [128, KC, 3]); cs = self.sb(st, "cs", [128, KC, 3], BF16)
            bc = Buf()
            self.ld(cT[:], self.cT[:, :, :], [bc])
            self.A(lambda h: h.activation(cs[:], cT[:], AF.Silu), [bc], [bc])
            modrow = self.sb(st, "modrow", [3, 6 * D]); bmr = Buf()
            wm = [self.sb(st, f"wm{i}", [128, KC, 512], BF16) for i in range(2)]; bwm = [Buf(), Buf()]
            bt = [self.sb(st, f"bt{i}", [3, 512]) for i in range(2)]; bbt = [Buf(), Buf()]
            wsrc = self.w_mod[l].rearrange("(kc kp) n -> kp kc n", kp=128)
            for cg in range(24):
                j = cg % 2
                self.ldc(wm[j][:], wsrc[:, :, cg * 512:(cg + 1) * 512], [bwm[j]])
                self.ld(bt[j][:], self.b_mod[l:l + 1, cg * 512:(cg + 1) * 512].partition_broadcast(3)[:, 0, :], [bbt[j]], q="act")
                p = self.ps[cg % 2]; pb = self.pb[cg % 2]
                for kc in range(KC):
                    self.PE(lambda h: h.matmul(p[0:3, :], cs[:, kc, :], wm[j][:, kc, :], start=(kc == 0), stop=(kc == KC - 1)),
                            [bc, bwm[j]], [pb])
                self.V(lambda h: h.tensor_add(modrow[:, cg * 512:(cg + 1) * 512], p[0:3, :], bt[j][:]), [pb, bbt[j]], [bmr])
            pT = self.ps[2]; pTb = self.pb[2]
            for v in range(6):
                for kc in range(KC):
                    k = v * KC + kc
                    self.PE(lambda h: h.transpose(pT[:, k * 3:k * 3 + 3], modrow[:, v * D + kc * 128: v * D + (kc + 1) * 128], self.idf[0:3, 0:3]),
                            [bmr, self.bconst], [pTb])
            self.V(lambda h: h.tensor_copy(self.modP[:].rearrange("p a k s -> p (a k s)"), pT[:, 0:288]), [pTb], [self.bmod])
            self.st(self.modD[:, 0, :], modrow[:, 2 * D:3 * D], [bmr])
            self.st(self.modD[:, 1, :], modrow[:, 5 * D:6 * D], [bmr])
            g1 = self.sb(st, "g1", [128, KC]); g2 = self.sb(st, "g2", [128, KC]); bg = Buf()
            self.ld(g1[:], self.gmix[l], [bg]); self.ld(g2[:], self.gffn[l], [bg])
            for (gi, gt, vs, vsh) in ((0, g1, 1, 0), (2, g2, 4, 3)):
                self.V(lambda h: h.tensor_scalar(self.AB[:, gi], self.modP[:, vs], 1.0, None, ALU.add), [self.bmod], [self.bmod])
                self.V(lambda h: h.tensor_mul(self.AB[:, gi], self.AB[:, gi], gt[:].unsqueeze(2).to_broadcast([128, KC, 3])), [self.bmod, bg], [self.bmod])
                self.V(lambda h: h.tensor_copy(self.AB[:, gi + 1], self.modP[:, vsh]), [self.bmod], [self.bmod])

    def bcast_row(self, st, which, src, name):
        t = self.sb(st, name, [128, D]); b = Buf()
        self.ld(t[:], self.modD[src:src + 1, which, :].partition_broadcast(128)[:, 0, :], [b])
        return t, b

    def norm_T(self, st_tiles, xt, bx, which, src, dst_fn, bdst, fp32):
        sq, ss, xs = st_tiles["sq"], st_tiles["ss"], (st_tiles["xsf"] if fp32 else st_tiles["xsb"])
        bsq, bss, bxs = st_tiles["bsq"], st_tiles["bss"], st_tiles["bxs"]
        self.A(lambda h: h.activation(sq[:], xt[:], AF.Square, accum_out=ss[:, 0:1]), [bx], [bsq, bss])
        self.A(lambda h: h.activation(ss[:, 1:2], ss[:, 0:1], AF.Sqrt, scale=1.0 / D, bias=self.epsc[:, 0:1]), [bss, self.bconst], [bss])
        self.V(lambda h: h.reciprocal(ss[:, 2:3], ss[:, 1:2]), [bss], [bss])
        self.A(lambda h: h.activation(xs[:], xt[:], AF.Identity, scale=ss[:, 2:3]), [bx, bss], [bxs])
        a_i, b_i = (0, 1) if which == 0 else (2, 3)
        idm = self.idf if fp32 else self.idb
        ngrp = 4 if fp32 else 2
        per = KC // ngrp
        for g in range(ngrp):
            bank = 4 + g
            p = self.ps[bank]; pb = self.pb[bank]
            pv = p[:] if fp32 else p[:].bitcast(BF16)
            for j in range(per):
                kc = g * per + j
                self.PE(lambda h: h.transpose(pv[:, j * 128:(j + 1) * 128], xs[:, kc * 128:(kc + 1) * 128], idm[:]),
                        [bxs, self.bconst], [pb])
            for j in range(per):
                kc = g * per + j
                sc, bi = self.AB[:, a_i, kc, src:src + 1], self.AB[:, b_i, kc, src:src + 1]
                if kc % 2 == 0:
                    self.V(lambda h: h.tensor_scalar(dst_fn(kc), pv[:, j * 128:(j + 1) * 128], sc, bi, ALU.mult, ALU.add),
                           [pb, self.bmod], [bdst])
                else:
                    self.A(lambda h: h.activation(dst_fn(kc), pv[:, j * 128:(j + 1) * 128], AF.Identity, bias=bi, scale=sc),
                           [pb, self.bmod], [bdst])

    def norm_tiles(self, st):
        d = {"sq": self.sb(st, "sq", [128, D], BF16), "ss": self.sb(st, "ss", [128, 4]),
             "xsf": None, "xsb": None, "bsq": Buf(), "bss": Buf(), "bxs": Buf()}
        return d

    def phase_proj(self, l, b, xsrc):
        c, S = self.c, self.S
        TT, NT = c.TT, c.NT
        with ExitStack() as st:
            hT = self.sb(st, "hT", [128, KC, TT], BF16); bh = Buf()
            nt = self.norm_tiles(st); nt["xsb"] = self.sb(st, "xsb", [128, D], BF16)
            xt = [self.sb(st, f"xt{i}", [128, D]) for i in range(2)]; bx = [Buf(), Buf()]
            for i in range(NT):
                j = i % 2
                self.ld(xt[j][:], xsrc[b, i * 128:(i + 1) * 128, :], [bx[j]], q=("sp" if j == 0 else "act"))
                src = 2 if i < c.NTC else b
                self.norm_T(nt, xt[j], bx[j], 0, src, lambda kc: hT[:, kc, i * 128:(i + 1) * 128], bh, fp32=False)
            wp = [self.sb(st, f"wp{i}", [128, KC, W], BF16) for i in range(2)]; bw = [Buf(), Buf()]
            stg = [self.sb(st, f"stg{i}", [128, TT]) for i in range(2)]; bs = [Buf(), Buf()]
            stg2 = [self.sb(st, f"stgb{i}", [128, W]) for i in range(2)]; bs2 = [Buf(), Buf()]
            wsrc = self.w_in[l].rearrange("(kc kp) n -> kp kc n", kp=128)
            tgs = tok_groups(TT)
            ev = 0
            for pi, p in enumerate(FM_PARTS + TM_PARTS):
                j = pi % 2
                self.ldc(wp[j][:], wsrc[:, :, p * W:(p + 1) * W], [bw[j]])
                if p in FM_PARTS:
                    fi = FM_PARTS.index(p)
                    for cb in range(4):
                        sj = cb % 2
                        for (t0, n) in tgs:
                            bank = ev % 4; ev += 1
                            ps, pb = self.ps[bank], self.pb[bank]
                            for kc in range(KC):
                                self.PE(lambda h: h.matmul(ps[:, 0:n], wp[j][:, kc, cb * 128:(cb + 1) * 128], hT[:, kc, t0:t0 + n],
                                                           start=(kc == 0), stop=(kc == KC - 1)), [bw[j], bh], [pb])
                            if ev % 2:
                                self.A(lambda h: h.copy(stg[sj][:, t0:t0 + n], ps[:, 0:n]), [pb], [bs[sj]])
                            else:
                                self.V(lambda h: h.tensor_copy(stg[sj][:, t0:t0 + n], ps[:, 0:n]), [pb], [bs[sj]])
                        self.st(self.Pfm[b, fi * W + cb * 128: fi * W + (cb + 1) * 128, :], stg[sj][:], [bs[sj]], q=("sp" if sj == 0 else "act"))
                else:
                    ti = TM_PARTS.index(p)
                    for i in range(NT):
                        sj = i % 2
                        bank = ev % 4; ev += 1
                        ps, pb = self.ps[bank], self.pb[bank]
                        for kc in range(KC):
                            self.PE(lambda h: h.matmul(ps[:, :], hT[:, kc, i * 128:(i + 1) * 128], wp[j][:, kc, :],
                                                       start=(kc == 0), stop=(kc == KC - 1)), [bw[j], bh], [pb])
                        if ev % 2:
                            self.A(lambda h: h.copy(stg2[sj][:], ps[:, :]), [pb], [bs2[sj]])
                        else:
                            self.V(lambda h: h.tensor_copy(stg2[sj][:], ps[:, :]), [pb], [bs2[sj]])
                        self.st(self.Ptm[b, i * 128:(i + 1) * 128, ti * W:(ti + 1) * W], stg2[sj][:], [bs2[sj]], q=("sp" if sj == 0 else "act"))

    def pfm(self, b, part, r0, n=128):
        fi = FM_PARTS.index(part)
        return self.Pfm[b, fi * W + r0: fi * W + r0 + n, :]

    def ptm(self, b, part):
        ti = TM_PARTS.index(part)
        return self.Ptm[b, :, ti * W:(ti + 1) * W].rearrange("(i p) w -> p i w", p=128)

    def scan_bidir(self, out_f, out_b, a_f, x_f, a_b, x_b, R, Wf, Wb, eng="dve"):
        c = self.c
        CL, TT = c.CTXL, c.TT
        op = self.V if eng == "dve" else self.G
        op(lambda h: h.tensor_tensor_scan(out_f[:, 0:TT], a_f[:, 0:TT], x_f[:, 0:TT], 0.0, ALU.mult, ALU.add), R, [Wf])
        op(lambda h: h.tensor_tensor_scan(out_b[:, 0:CL][:, ::-1], a_b[:, 0:CL][:, ::-1], x_b[:, 0:CL][:, ::-1], 0.0, ALU.mult, ALU.add), R, [Wb])
        op(lambda h: h.tensor_tensor_scan(out_b[:, CL:TT][:, ::-1], a_b[:, CL:TT][:, ::-1], x_b[:, CL:TT][:, ::-1], out_b[:, 0:1], ALU.mult, ALU.add),
           list(R) + [Wb], [Wb])

    def phase_lru(self, l, b):
        c = self.c
        TT, CL = c.TT, c.CTXL
        tgs = tok_groups(TT)
        with ExitStack() as st:
            cw = self.sb(st, "cw", [128, 4, 4]); lv = self.sb(st, "lv", [128, 4, 7]); bp = Buf()
            self.ld(cw[:], self.convw[l], [bp]); self.ld(lv[:], self.lruv[l], [bp])
            lw = self.sb(st, "lw", [128, 2, 2, 4, 128], BF16)
            self.ldc(lw[:], self.lruw[l].rearrange("a d c p j -> p a d c j"), [bp])
            c1 = self.sb(st, "c1", [128, 4, 2]); c2 = self.sb(st, "c2", [128, 4, 2])
            self.A(lambda h: h.activation(c1[:], lv[:, :, 5:7], AF.Exp, scale=-1.0), [bp], [bp])
            self.A(lambda h: h.activation(c1[:], c1[:], AF.Ln, bias=1.0), [bp], [bp])
            self.V(lambda h: h.tensor_scalar(c2[:], c1[:], -16.0, None, ALU.mult), [bp], [bp])
            self.V(lambda h: h.tensor_scalar(c1[:], c1[:], -8.0, None, ALU.mult), [bp], [bp])
            T = lambda n, dt=F32: self.sb(st, n, [128, TT], dt)
            xr, gt, xc, xcb = T("xr"), T("gt"), T("xc"), T("xcb", BF16)
            rr, ii, aa, bb = [T("rr0"), T("rr1")], [T("ii0"), T("ii1")], [T("aa0"), T("aa1")], [T("bb0"), T("bb1")]
            hf, hb, ob = T("hf"), T("hb"), T("ob", BF16)
            bxr, bgt, bxc, bxcb, bo = Buf(), Buf(), Buf(), Buf(), Buf()
            br, bi, ba, bbb = [Buf(), Buf()], [Buf(), Buf()], [Buf(), Buf()], [Buf(), Buf()]
            bhf, bhb = Buf(), Buf()
            for cc in range(4):
                self.ld(xr[:], self.pfm(b, 1, cc * 128), [bxr])
                self.ld(gt[:], self.pfm(b, 2, cc * 128), [bgt], q="act")
                self.V(lambda h: h.tensor_scalar(xc[:], xr[:], cw[:, cc, 2:3], lv[:, cc, 0:1], ALU.mult, ALU.add), [bxr, bp], [bxc])
                for (s0, s1) in ((0, CL), (CL, TT)):
                    for k, off in ((0, -2), (1, -1), (3, 1)):
                        o0, o1 = max(s0, s0 - off), min(s1, s1 - off)
                        self.V(lambda h: h.scalar_tensor_tensor(xc[:, o0:o1], xr[:, o0 + off:o1 + off], cw[:, cc, k:k + 1], xc[:, o0:o1], ALU.mult, ALU.add),
                               [bxr, bp, bxc], [bxc])
                self.A(lambda h: h.copy(xcb[:], xc[:]), [bxc], [bxcb])
                for d in range(2):
                    for (wi, dst, bd, bias_col) in ((0, rr[d], br[d], 1 + d), (1, ii[d], bi[d], 3 + d)):
                        for gi, (t0, n) in enumerate(tgs):
                            ps, pb = self.ps[gi % 4], self.pb[gi % 4]
                            self.PE(lambda h: h.matmul(ps[:, 0:n], lw[:, wi, d, cc, :], xcb[:, t0:t0 + n], start=True, stop=True), [bp, bxcb], [pb])
                            self.A(lambda h: h.activation(dst[:, t0:t0 + n], ps[:, 0:n], AF.Sigmoid, bias=lv[:, cc, bias_col:bias_col + 1]), [pb, bp], [bd])
                    self.A(lambda h: h.activation(aa[d][:], rr[d][:], AF.Exp, scale=c1[:, cc, d:d + 1]), [br[d], bp], [ba[d]])
                    self.A(lambda h: h.activation(bb[d][:], rr[d][:], AF.Exp, scale=c2[:, cc, d:d + 1]), [br[d], bp], [bbb[d]])
                    self.A(lambda h: h.activation(bb[d][:], bb[d][:], AF.Sqrt, scale=-1.0, bias=1.0), [bbb[d]], [bbb[d]])
                    self.V(lambda h: h.tensor_mul(ii[d][:], ii[d][:], xc[:]), [bi[d], bxc], [bi[d]])
                    self.V(lambda h: h.tensor_mul(bb[d][:], bb[d][:], ii[d][:]), [bbb[d], bi[d]], [bbb[d]])
                self.scan_bidir(hf, hb, aa[0], bb[0], aa[1], bb[1], [ba[0], ba[1], bbb[0], bbb[1]], bhf, bhb)
                self.V(lambda h: h.tensor_add(hf[:], hf[:], hb[:]), [bhf, bhb], [bhf])
                self.gelu(gt[:], gt[:], hb[:], [bgt], bgt, bhb)
                self.V(lambda h: h.tensor_mul(ob[:], hf[:], gt[:]), [bhf, bgt], [bo])
                self.st(self.mixT[b, W + cc * 128: W + (cc + 1) * 128, :], ob[:], [bo])

    def sincos(self, st, ang, bang, n, name, share=None):
        T = lambda nm, dt=F32: self.sb(st, f"{name}_{nm}", [128, n], dt)
        y, yi, sn, cs = T("y"), T("yi", I32), T("sn"), T("cs")
        b = Buf(); bs = Buf(); bcs = Buf()
        for (shift, dst, bd) in ((0.0, sn, bs), (0.25, cs, bcs)):
            self.V(lambda h: h.tensor_scalar(y[:], ang[:], 1.0 / TWO_PI, shift, ALU.mult, ALU.add), [bang, b], [b])
            self.wrap_frac(y, yi, dst, b, extra=[bd])
            self.A(lambda h: h.activation(dst[:], y[:], AF.Sin, scale=TWO_PI), [b], [bd, b])
        return sn, bs, cs, bcs

    def wrap_frac(self, y, yi, yf, b, extra=()):
        Wb = [b] + list(extra)
        self.V(lambda h: h.tensor_copy(yi[:], y[:]), [b], Wb)
        self.V(lambda h: h.tensor_copy(yf[:], yi[:]), [b], Wb)
        self.V(lambda h: h.tensor_sub(y[:], y[:], yf[:]), [b], Wb)
        self.V(lambda h: h.tensor_scalar(yf[:], y[:], 0.5, -1.0, ALU.is_gt, ALU.mult), [b], Wb)
        self.V(lambda h: h.tensor_add(y[:], y[:], yf[:]), [b], Wb)
        self.V(lambda h: h.tensor_scalar(yf[:], y[:], -0.5, None, ALU.is_lt), [b], Wb)
        self.V(lambda h: h.tensor_add(y[:], y[:], yf[:]), [b], Wb)

    def gelu(self, dst, src, tmp, R, Wd, Wt):
        self.V(lambda h: h.tensor_mul(tmp, src, src), R, [Wt])
        self.V(lambda h: h.tensor_scalar(tmp, tmp, 0.044715, 1.0, ALU.mult, ALU.add), [Wt], [Wt])
        self.V(lambda h: h.tensor_mul(tmp, tmp, src), list(R) + [Wt], [Wt])
        self.A(lambda h: h.activation(tmp, tmp, AF.Sigmoid, scale=1.5957691216057308), [Wt], [Wt])
        self.V(lambda h: h.tensor_mul(dst, src, tmp), list(R) + [Wt], [Wd])

    def phase_gla(self, l, b, mixer):
        c = self.c
        TT, NT, CL = c.TT, c.NT, c.CTXL
        C = 64
        NPT = 128 // C
        NCH, NCHC = TT // C, CL // C
        cmask = self.cmask32 if C == 32 else self.cmask
        rsrc = self.rst32 if C == 32 else self.rst
        SQ = 128 ** -0.5
        with ExitStack() as st:
            T = lambda s_, n, dt=F32: self.sb(s_, n, [128, TT], dt)
            vbf = self.sb(st, "vbf", [128, NT, W], BF16); bv = Buf()
            oacc = self.sb(st, "oacc", [128, NT, W]); bo = Buf()
            vst = [self.sb(st, f"vst{i}", [128, W]) for i in range(2)]; bvs = [Buf(), Buf()]
            vsrc = self.ptm(b, 6 if mixer == 0 else 10)
            for i in range(NT):
                j = i % 2
                self.ld(vst[j][:], vsrc[:, i, :], [bvs[j]], q=("sp" if j == 0 else "act"))
                self.A(lambda h: h.activation(vbf[:, i, :], vst[j][:], AF.Silu if mixer == 0 else AF.Identity), [bvs[j]], [bv])
            bprm = Buf()
            if mixer == 0:
                lbr = self.sb(st, "lbr", [128, 2, c.L, 4]); lbe = self.sb(st, "lbe", [128, 2, c.L, 4])
                lb = self.sb(st, "lb", [128, 2, 4]); lbs = self.sb(st, "lbs", [128, 2, 4]); oml = self.sb(st, "oml", [128, 2, 4])
                self.ld(lbr[:], self.hglb.rearrange("d l p h -> p d l h"), [bprm])
                self.A(lambda h: h.activation(lbe[:], lbr[:], AF.Exp), [bprm], [bprm])
                self.V(lambda h: h.tensor_copy(lbs[:], lbe[:, :, 0, :]), [bprm], [bprm])
                for ll in range(1, c.L):
                    self.V(lambda h: h.tensor_add(lbs[:], lbs[:], lbe[:, :, ll, :]), [bprm], [bprm])
                self.V(lambda h: h.memset(lb[:], 0.0), [], [bprm])
                for ll in range(1, l + 1):
                    self.V(lambda h: h.tensor_add(lb[:], lb[:], lbe[:, :, ll, :]), [bprm], [bprm])
                self.V(lambda h: h.reciprocal(lbs[:], lbs[:]), [bprm], [bprm])
                self.V(lambda h: h.tensor_mul(lb[:], lb[:], lbs[:]), [bprm], [bprm])
                self.V(lambda h: h.tensor_scalar(oml[:], lb[:], -1.0, 1.0, ALU.mult, ALU.add), [bprm], [bprm])
            for d in range(2):
                with ExitStack() as s2:
                    rst = self.sb(s2, "rst", [128, TT + 1]); brs = Buf()
                    self.ld(rst[:], rsrc[:, :], [brs])
                    if mixer == 1:
                        ang = self.sb(s2, "ang", [128, c.SEQL]); bang = Buf()
                        self.ld(ang[:], self.rang[:, :], [bang])
                        sn, bsn, cs, bcs = self.sincos(s2, ang, bang, c.SEQL, "rot", share=ang)
                        self.V(lambda h: h.tensor_scalar(sn[0:64, :], sn[0:64, :], -1.0, None, ALU.mult), [bsn], [bsn])
                    q_, k_, z_, cum, e1 = T(s2, "q_"), T(s2, "k_"), T(s2, "z_"), T(s2, "cum"), T(s2, "e1")
                    tmp = z_ if mixer == 1 else T(s2, "tmp")
                    bq, bk, bz, bcum, be1 = (Buf() for _ in range(5))
                    btmp = bz if mixer == 1 else Buf()
                    qt = [T(s2, f"qt{h}", BF16) for h in range(4)]; kt = [T(s2, f"kt{h}", BF16) for h in range(4)]
                    bqt = [Buf() for _ in range(4)]; bkt = [Buf() for _ in range(4)]
                    dec = self.sb(s2, "dec", [128, NCH, 4]); em = self.sb(s2, "em", [128, NCH, 4]); elm = self.sb(s2, "elm", [128, NCH, 4]); bdec = Buf()
                    S_ = self.sb(s2, "S_", [128, 4, 128]); Sb = self.sb(s2, "Sb", [128, 4, 128], BF16); Ut = self.sb(s2, "Ut", [128, 4, 128]); bS, bSb, bUt = Buf(), Buf(), Buf()
                    sT = [self.sb(s2, f"sT{i}", [128, 4, C], BF16) for i in range(2)]; bsT = [Buf(), Buf()]
                    khT = [self.sb(s2, f"khT{i}", [128, 4, 128], BF16) for i in range(2)]; bkhT = [Buf(), Buf()]
                    c3 = lambda t: t[:].rearrange("p (c j) -> p c j", j=C)
                    for hd in range(4):
                        r0 = hd * 128
                        if mixer == 0:
                            self.ld(q_[:], self.pfm(b, 3, r0), [bq])
                            self.ld(z_[:], self.pfm(b, 4 + d, r0), [bz], q="act")
                            self.A(lambda h: h.activation(k_[:], z_[:], AF.Sigmoid, scale=-1.0), [bz], [bk])
                            self.A(lambda h: h.activation(z_[:], z_[:], AF.Sigmoid), [bz, bk], [bz])
                            self.A(lambda h: h.activation(z_[:], z_[:], AF.Ln, scale=oml[:, d, hd:hd + 1], bias=lb[:, d, hd:hd + 1]), [bz, bprm], [bz])
                            self.V(lambda h: h.tensor_scalar(k_[:], k_[:], oml[:, d, hd:hd + 1], None, ALU.mult), [bk, bprm], [bk])
                            gsrc, bgs = z_, bz
                            qscale, kscale = SQ, 1.0
                        else:
                            for (x_, bx_, pa, pb_) in ((q_, bq, 8, 12), (k_, bk, 9, 13)):
                                self.ld(x_[:], self.pfm(b, pa, r0), [bx_])
                                self.ld(e1[:], self.pfm(b, pb_, r0), [be1], q="act")
                                self.V(lambda h: h.tensor_mul(x_[:, CL:TT], x_[:, CL:TT], cs[:]), [bx_, bcs], [bx_])
                                self.V(lambda h: h.tensor_mul(e1[:, CL:TT], e1[:, CL:TT], sn[:]), [be1, bsn], [be1])
                                self.V(lambda h: h.tensor_add(x_[:, CL:TT], x_[:, CL:TT], e1[:, CL:TT]), [bx_, be1], [bx_])
                            gdec = math.log1p(-2.0 ** (-((5.0 if d == 0 else 5.5) + hd)))
                            gsrc, bgs = None, None
                            qscale, kscale = 1.0, SQ
                        if mixer == 1:
                            self.V(lambda h: h.memset(e1[:], gdec), [be1], [be1])
                            gsrc, bgs = e1, be1
                        if d == 0:
                            self.V(lambda h: h.tensor_tensor_scan(cum[:], rst[:, 0:TT], gsrc[:], 0.0, ALU.mult, ALU.add), [brs, bgs], [bcum])
                            mcol, lcol = C // 2 - 1, C - 1
                        else:
                            self.V(lambda h: h.tensor_tensor_scan(cum[:, ::-1], rst[:, 1:TT + 1][:, ::-1], gsrc[:, ::-1], 0.0, ALU.mult, ALU.add), [brs, bgs], [bcum])
                            mcol, lcol = C // 2, 0
                        cum3 = c3(cum)
                        mB = cum3[:, :, mcol:mcol + 1].to_broadcast([128, NCH, C])
                        self.V(lambda h: h.tensor_sub(c3(tmp), cum3, mB), [bcum, btmp], [btmp])
                        self.A(lambda h: h.activation(e1[:], tmp[:], AF.Exp), [btmp, be1], [be1])
                        self.V(lambda h: h.scalar_tensor_tensor(qt[hd][:], q_[:], qscale, e1[:], ALU.mult, ALU.mult), [bq, be1], [bqt[hd]])
                        self.A(lambda h: h.activation(e1[:], tmp[:], AF.Exp, scale=-1.0), [btmp, be1], [be1])
                        self.V(lambda h: h.scalar_tensor_tensor(kt[hd][:], k_[:], kscale, e1[:], ALU.mult, ALU.mult), [bk, be1], [bkt[hd]])
                        self.A(lambda h: h.activation(dec[:, :, hd:hd + 1], cum3[:, :, lcol:lcol + 1], AF.Exp), [bcum], [bdec])
                        self.A(lambda h: h.activation(em[:, :, hd:hd + 1], cum3[:, :, mcol:mcol + 1], AF.Exp), [bcum], [bdec])
                        self.V(lambda h: h.tensor_sub(elm[:, :, hd:hd + 1], cum3[:, :, lcol:lcol + 1], cum3[:, :, mcol:mcol + 1]), [bcum], [bdec])
                        self.A(lambda h: h.activation(elm[:, :, hd:hd + 1], elm[:, :, hd:hd + 1], AF.Exp), [bdec], [bdec])
                    self.V(lambda h: h.memset(S_[:], 0.0), [bS], [bS])
                    self.V(lambda h: h.memset(Sb[:], 0.0), [bSb], [bSb])
                    order = list(range(NCH)) if d == 0 else (list(range(NCHC - 1, -1, -1)) + list(range(NCH - 1, NCHC - 1, -1)))
                    for ci, ch in enumerate(order):
                        i, hh = ch // NPT, ch % NPT
                        P0 = C * hh
                        cs_ = slice(ch * C, ch * C + C)
                        j = ci % 2
                        pS, pO, pK, pU = self.ps[0 + j], self.ps[2 + j], self.ps[4 + j], self.ps[6 + j]
                        bpS, bpO, bpK, bpU = self.pb[0 + j], self.pb[2 + j], self.pb[4 + j], self.pb[6 + j]
                        for hd in range(4):
                            self.PE(lambda h: h.matmul(pS[P0:P0 + C, hd * C:(hd + 1) * C], kt[hd][:, cs_], qt[hd][:, cs_], start=True, stop=True),
                                    [bkt[hd], bqt[hd]], [bpS])
                        self.G(lambda h: h.memset(sT[j][P0:P0 + C], 0.0), [bsT[j]], [bsT[j]])
                        self.V(lambda h: h.copy_predicated(sT[j][P0:P0 + C], cmask[P0:P0 + C, d].bitcast(mybir.dt.uint32), pS[P0:P0 + C, 0:4 * C].rearrange("p (h t) -> p h t", t=C)),
                               [bpS, self.bconst, bsT[j]], [bsT[j]])
                        self.V(lambda h: h.tensor_mul(Sb[:], S_[:], em[:, ch, :].unsqueeze(2).to_broadcast([128, 4, 128])), [bS, bdec, bSb], [bSb])
                        for hd in range(4):
                            osl = pO[P0:P0 + C, hd * 128:(hd + 1) * 128]
                            self.PE(lambda h: h.matmul(osl, sT[j][P0:P0 + C, hd, :], vbf[P0:P0 + C, i, hd * 128:(hd + 1) * 128], start=True, stop=False),
                                    [bsT[j], bv], [bpO])
                            self.PE(lambda h: h.matmul(osl, qt[hd][:, cs_], Sb[:, hd, :], start=False, stop=True), [bqt[hd], bSb], [bpO])
                        if d == 0:
                            self.A(lambda h: h.copy(oacc[P0:P0 + C, i, :], pO[P0:P0 + C, :]), [bpO], [bo])
                        else:
                            self.V(lambda h: h.tensor_add(oacc[P0:P0 + C, i, :], oacc[P0:P0 + C, i, :], pO[P0:P0 + C, :]), [bpO, bo], [bo])
                        pKb = pK[:].bitcast(BF16)
                        for hd in range(4):
                            self.PE(lambda h: h.transpose(pKb[P0:P0 + C, hd * 128:(hd + 1) * 128], kt[hd][:, cs_], self.idb[:]), [bkt[hd], self.bconst], [bpK])
                        self.A(lambda h: h.copy(khT[j][P0:P0 + C].rearrange("p h d -> p (h d)"), pKb[P0:P0 + C, 0:512]), [bpK], [bkhT[j]])
                        for hd in range(4):
                            self.PE(lambda h: h.matmul(pU[:, hd * 128:(hd + 1) * 128], khT[j][P0:P0 + C, hd, :], vbf[P0:P0 + C, i, hd * 128:(hd + 1) * 128], start=True, stop=True),
                                    [bkhT[j], bv], [bpU])
                        self.V(lambda h: h.tensor_mul(Ut[:], pU[:, :].rearrange("p (h d) -> p h d", d=128), elm[:, ch, :].unsqueeze(2).to_broadcast([128, 4, 128])), [bpU, bdec, bUt], [bUt])
                        self.V(lambda h: h.tensor_mul(S_[:], S_[:], dec[:, ch, :].unsqueeze(2).to_broadcast([128, 4, 128])), [bS, bdec], [bS])
                        self.V(lambda h: h.tensor_add(S_[:], S_[:], Ut[:]), [bS, bUt], [bS])
                self.S.barrier()
            with ExitStack() as s3:
                graw = self.sb(s3, "graw", [128, NT, W]); bg = Buf()
                self.ld(graw[:], self.ptm(b, 7 if mixer == 0 else 11), [bg], q="act")
                self.A(lambda h: h.activation(graw[:], graw[:], AF.Silu), [bg], [bg])
                sq = self.sb(s3, "gsq", [128, NT, W]); ssq = self.sb(s3, "ssq", [128, NT * 4]); bsq = Buf()
                o4 = oacc[:].rearrange("p i (h d) -> p (i h) d", d=128)
                self.V(lambda h: h.tensor_mul(sq[:], oacc[:], oacc[:]), [bo], [bsq])
                self.V(lambda h: h.tensor_reduce(ssq[:], sq[:].rearrange("p i (h d) -> p (i h) d", d=128), AX.X, ALU.add), [bsq], [bsq])
                self.A(lambda h: h.activation(ssq[:], ssq[:], AF.Sqrt, scale=1.0 / 128, bias=self.epsc[:, 0:1]), [bsq, self.bconst], [bsq])
                self.V(lambda h: h.reciprocal(ssq[:], ssq[:]), [bsq], [bsq])
                self.V(lambda h: h.tensor_mul(o4, o4, ssq[:].unsqueeze(2).to_broadcast([128, NT * 4, 128])), [bo, bsq], [bo])
                self.V(lambda h: h.tensor_mul(vbf[:], oacc[:], graw[:]), [bo, bg, bv], [bv])
                mst = self.sb(s3, "mst", [128, 4, TT], BF16); bm = Buf()
                for i in range(NT):
                    p, pb = self.ps[i % 4], self.pb[i % 4]
                    pv = p[:].bitcast(BF16)
                    for hd in range(4):
                        self.PE(lambda h: h.transpose(pv[:, hd * 128:(hd + 1) * 128], vbf[:, i, hd * 128:(hd + 1) * 128], self.idb[:]), [bv, self.bconst], [pb])
                    self.A(lambda h: h.copy(mst[:, :, i * 128:(i + 1) * 128], pv[:, 0:512].rearrange("p (h t) -> p h t", t=128)), [pb], [bm])
                base = 2 * W + mixer * W
                self.st(self.mixT[b, base:base + W, :].rearrange("(h p) t -> p h t", p=128), mst[:], [bm])
                self.S.barrier()

    def phase_s5(self, l, b):
        c = self.c
        TT, CL = c.TT, c.CTXL
        tgs = tok_groups(TT)
        with ExitStack() as st:
            T = lambda n, dt=F32: self.sb(st, n, [128, TT], dt)
            lam = self.sb(st, "lam", [128, 3, 2, 16]); bp = Buf()
            self.ld(lam[:], self.s5lam[l], [bp])
            P = lambda n: self.sb(st, n, [128, 32])
            dt_, mag, th, thc, are, aim, den, wre, wim, t1, t2 = (P(f"p{i}") for i in range(11))
            lr = lam[:, 0].rearrange("p d s -> p (d s)"); li = lam[:, 1].rearrange("p d s -> p (d s)"); ldt = lam[:, 2].rearrange("p d s -> p (d s)")
            self.A(lambda h: h.activation(dt_[:], ldt, AF.Exp), [bp], [bp])
            self.V(lambda h: h.tensor_mul(t1[:], lr, dt_[:]), [bp], [bp])
            self.A(lambda h: h.activation(mag[:], t1[:], AF.Exp), [bp], [bp])
            self.V(lambda h: h.tensor_mul(th[:], li, dt_[:]), [bp], [bp])
            sn, bsn, cs, bcs = self.sincos(st, th, bp, 32, "ab")
            self.V(lambda h: h.tensor_mul(are[:], mag[:], cs[:]), [bp, bcs], [bp])
            self.V(lambda h: h.tensor_mul(aim[:], mag[:], sn[:]), [bp, bsn], [bp])
            self.V(lambda h: h.tensor_mul(den[:], lr, lr), [bp], [bp])
            self.V(lambda h: h.tensor_mul(t1[:], li, li), [bp], [bp])
            self.V(lambda h: h.tensor_add(den[:], den[:], t1[:]), [bp], [bp])
            self.V(lambda h: h.reciprocal(den[:], den[:]), [bp], [bp])
            self.V(lambda h: h.tensor_scalar(t2[:], are[:], -1.0, None, ALU.add), [bp], [bp])
            self.V(lambda h: h.tensor_mul(wre[:], t2[:], lr), [bp], [bp])
            self.V(lambda h: h.tensor_mul(t1[:], aim[:], li), [bp], [bp])
            self.V(lambda h: h.tensor_add(wre[:], wre[:], t1[:]), [bp], [bp])
            self.V(lambda h: h.tensor_mul(wre[:], wre[:], den[:]), [bp], [bp])
            self.V(lambda h: h.tensor_mul(wim[:], aim[:], lr), [bp], [bp])
            self.V(lambda h: h.tensor_mul(t1[:], t2[:], li), [bp], [bp])
            self.V(lambda h: h.tensor_sub(wim[:], wim[:], t1[:]), [bp], [bp])
            self.V(lambda h: h.tensor_mul(wim[:], wim[:], den[:]), [bp], [bp])
            nwim = P("nwim")
            self.V(lambda h: h.tensor_scalar(nwim[:], wim[:], -1.0, None, ALU.mult), [bp], [bp])
            thi = self.sb(st, "thi", [128, 32], I32); thf = P("thf")
            self.V(lambda h: h.tensor_scalar(thc[:], th[:], 1.0 / TWO_PI, None, ALU.mult), [bp], [bp])
            self.wrap_frac(thc, thi, thf, bp)
            dsk = self.sb(st, "dsk", [128, 4]); self.ld(dsk[:], self.s5d[l], [bp])
            with ExitStack() as s2:
                T = lambda n, dt=F32: self.sb(s2, n, [128, TT], dt)
                kid = [T("kid0"), T("kid1")]; bkid = Buf()
                self.ld(kid[0][:], self.kidx[0], [bkid]); self.ld(kid[1][:], self.kidx[1], [bkid], q="act")
                ub = self.sb(s2, "ub", [128, 4, TT], BF16); bu = Buf()
                self.ldc(ub[:], self.Pfm[b, 0:W, :].rearrange("(c p) t -> p c t", p=128), [bu])
                uf = T("uf"); buf_ = Buf()
                Bt = [self.sb(s2, f"Bt{i}", [128, 2, 128], BF16) for i in range(2)]; bB = [Buf(), Buf()]
                Cf = [self.sb(s2, f"Cf{i}", [128, 2, 128]) for i in range(2)]; bC = [Buf(), Buf()]
                Cw = [self.sb(s2, f"Cw{i}", [128, 2, 128], BF16) for i in range(2)]; bCw = [Buf(), Buf()]
                ct = self.sb(s2, "ct", [128, 128]); bct = Buf()
                y, yi = T("ty"), T("tyi", I32)
                sn_, cs_ = T("tsn"), T("tcs"); by, bsn_, bcs_ = Buf(), Buf(), Buf()
                bur, bui, gre, gim, t3 = T("bur"), T("bui"), T("gre"), T("gim"), T("t3")
                bbur, bbui, bgre, bgim, bt3 = (Buf() for _ in range(5))
                hre = [T(f"hre{i}", BF16) for i in range(2)]; him = [T(f"him{i}", BF16) for i in range(2)]
                bhre = [Buf(), Buf()]; bhim = [Buf(), Buf()]
                ytmp = T("ytmp"); bytmp = Buf()
                yo = T("yo"); byo = Buf()
                k = 0
                for cc in range(4):
                    self.ld(uf[:], self.Pfm[b, cc * 128:(cc + 1) * 128, :], [buf_])
                    pY = [self.ps[4 + g] for g in range(4)]; bpY = [self.pb[4 + g] for g in range(4)]
                    first = True
                    for d in range(2):
                        for s4 in range(4):
                            sc = cc * 4 + s4
                            col = d * 16 + sc
                            j = k % 2; k += 1
                            lastone = (d == 1 and s4 == 3)
                            self.ldc(Bt[j][:], self.s5B[l, d, sc], [bB[j]])
                            self.ld(Cf[j][:], self.s5C[l, d, sc], [bC[j]], q="act")
                            self.V(lambda h: h.tensor_scalar(ct[:], Cf[j][:, 1, :], nwim[:, col:col + 1], None, ALU.mult), [bC[j], bp, bct], [bct])
                            self.V(lambda h: h.scalar_tensor_tensor(Cw[j][:, 0, :], Cf[j][:, 0, :], wre[:, col:col + 1], ct[:], ALU.mult, ALU.add), [bC[j], bp, bct], [bCw[j]])
                            self.V(lambda h: h.tensor_scalar(ct[:], Cf[j][:, 1, :], wre[:, col:col + 1], -1.0, ALU.mult, ALU.mult), [bC[j], bp, bct], [bct])
                            self.V(lambda h: h.scalar_tensor_tensor(Cw[j][:, 1, :], Cf[j][:, 0, :], nwim[:, col:col + 1], ct[:], ALU.mult, ALU.add), [bC[j], bp, bct], [bCw[j]])
                            for (shift, dst, bd) in ((0.0, sn_, bsn_), (0.25, cs_, bcs_)):
                                self.V(lambda h: h.tensor_scalar(y[:], kid[d][:], thc[:, col:col + 1], shift, ALU.mult, ALU.add), [bkid, bp, by], [by])
                                self.wrap_frac(y, yi, dst, by, extra=[bd])
                                self.A(lambda h: h.activation(dst[:], y[:], AF.Sin, scale=TWO_PI), [by], [bd, by])
                            for (ri, dst, bd) in ((0, bur, bbur), (1, bui, bbui)):
                                for gi, (t0, n) in enumerate(tgs):
                                    ps, pb = self.ps[gi % 4], self.pb[gi % 4]
                                    self.PE(lambda h: h.matmul(ps[:, 0:n], Bt[j][:, ri, :], ub[:, cc, t0:t0 + n], start=True, stop=True), [bB[j], bu], [pb])
                                    self.A(lambda h: h.copy(dst[:, t0:t0 + n], ps[:, 0:n]), [pb], [bd])
                            self.V(lambda h: h.tensor_mul(gre[:], bur[:], cs_[:]), [bbur, bcs_], [bgre])
                            self.V(lambda h: h.tensor_mul(t3[:], bui[:], sn_[:]), [bbui, bsn_], [bt3])
                            self.V(lambda h: h.tensor_add(gre[:], gre[:], t3[:]), [bgre, bt3], [bgre])
                            self.V(lambda h: h.tensor_mul(gim[:], bui[:], cs_[:]), [bbui, bcs_], [bgim])
                            self.V(lambda h: h.tensor_mul(t3[:], bur[:], sn_[:]), [bbur, bsn_], [bt3])
                            self.V(lambda h: h.tensor_sub(gim[:], gim[:], t3[:]), [bgim, bt3], [bgim])
                            for (src, dst, bs_, bd) in ((gre, bur, bgre, bbur), (gim, bui, bgim, bbui)):
                                if d == 0:
                                    self.V(lambda h: h.tensor_tensor_scan(dst[:], mag[:, col:col + 1].to_broadcast([128, TT]), src[:], 0.0, ALU.mult, ALU.add), [bs_, bp], [bd])
                                else:
                                    mC = mag[:, col:col + 1].to_broadcast([128, CL]); mL = mag[:, col:col + 1].to_broadcast([128, TT - CL])
                                    self.V(lambda h: h.tensor_tensor_scan(dst[:, 0:CL][:, ::-1], mC, src[:, 0:CL][:, ::-1], 0.0, ALU.mult, ALU.add), [bs_, bp], [bd])
                                    self.V(lambda h: h.tensor_tensor_scan(dst[:, CL:TT][:, ::-1], mL, src[:, CL:TT][:, ::-1], dst[:, 0:1], ALU.mult, ALU.add), [bs_, bp, bd], [bd])
                            self.V(lambda h: h.tensor_mul(gre[:], bur[:], cs_[:]), [bbur, bcs_], [bgre])
                            self.V(lambda h: h.tensor_mul(t3[:], bui[:], sn_[:]), [bbui, bsn_], [bt3])
                            self.V(lambda h: h.tensor_sub(hre[j][:], gre[:], t3[:]), [bgre, bt3], [bhre[j]])
                            self.V(lambda h: h.tensor_mul(gim[:], bur[:], sn_[:]), [bbur, bsn_], [bgim])
                            self.V(lambda h: h.tensor_mul(t3[:], bui[:], cs_[:]), [bbui, bcs_], [bt3])
                            self.V(lambda h: h.tensor_add(him[j][:], gim[:], t3[:]), [bgim, bt3], [bhim[j]])
                            for gi, (t0, n) in enumerate(tgs):
                                if gi < 4:
                                    self.PE(lambda h: h.matmul(pY[gi][:, 0:n], Cw[j][:, 0, :], hre[j][:, t0:t0 + n], start=first, stop=False), [bCw[j], bhre[j]], [bpY[gi]])
                                    self.PE(lambda h: h.matmul(pY[gi][:, 0:n], Cw[j][:, 1, :], him[j][:, t0:t0 + n], start=False, stop=lastone), [bCw[j], bhim[j]], [bpY[gi]])
                                else:
                                    ps, pb = self.ps[gi % 4], self.pb[gi % 4]
                                    self.PE(lambda h: h.matmul(ps[:, 0:n], Cw[j][:, 0, :], hre[j][:, t0:t0 + n], start=True, stop=False), [bCw[j], bhre[j]], [pb])
                                    self.PE(lambda h: h.matmul(ps[:, 0:n], Cw[j][:, 1, :], him[j][:, t0:t0 + n], start=False, stop=True), [bCw[j], bhim[j]], [pb])
                                    if first:
                                        self.V(lambda h: h.tensor_copy(ytmp[:, t0:t0 + n], ps[:, 0:n]), [pb], [bytmp])
                                    else:
                                        self.V(lambda h: h.tensor_add(ytmp[:, t0:t0 + n], ytmp[:, t0:t0 + n], ps[:, 0:n]), [pb, bytmp], [bytmp])
                            first = False
                    for gi, (t0, n) in enumerate(tgs):
                        if gi < 4:
                            self.V(lambda h: h.scalar_tensor_tensor(yo[:, t0:t0 + n], uf[:, t0:t0 + n], dsk[:, cc:cc + 1], pY[gi][:, 0:n], ALU.mult, ALU.add), [buf_, bp, bpY[gi]], [byo])
                        else:
                            self.V(lambda h: h.scalar_tensor_tensor(yo[:, t0:t0 + n], uf[:, t0:t0 + n], dsk[:, cc:cc + 1], ytmp[:, t0:t0 + n], ALU.mult, ALU.add), [buf_, bp, bytmp], [byo])
                    self.gelu(yo[:], yo[:], t3[:], [byo], byo, bt3)
                    self.st(self.ygD[b, cc * 128:(cc + 1) * 128, :], yo[:], [byo])
                self.S.barrier()
            with ExitStack() as s3:
                T = lambda n, dt=F32: self.sb(s3, n, [128, TT], dt)
                yg = self.sb(s3, "yg", [128, 4, TT]); ygb = self.sb(s3, "ygb", [128, 4, TT], BF16); byg = Buf()
                self.ld(yg[:], self.ygD[b].rearrange("(c p) t -> p c t", p=128), [byg])
                self.A(lambda h: h.copy(ygb[:], yg[:]), [byg], [byg])
                gw = self.sb(s3, "gw", [128, 4, W], BF16); gb = self.sb(s3, "gb", [128, 4]); bgw = Buf()
                self.ldc(gw[:], self.gluw[l].rearrange("(kc kp) n -> kp kc n", kp=128), [bgw])
                self.ld(gb[:], self.glub[l], [bgw])
                ao = [T("ao0", BF16), T("ao1", BF16)]; bao = [Buf(), Buf()]
                sg = T("sg"); bsg = Buf()
                for oc in range(4):
                    j = oc % 2
                    for gi, (t0, n) in enumerate(tgs):
                        ps, pb = self.ps[gi % 4], self.pb[gi % 4]
                        for kc in range(4):
                            self.PE(lambda h: h.matmul(ps[:, 0:n], gw[:, kc, oc * 128:(oc + 1) * 128], ygb[:, kc, t0:t0 + n], start=(kc == 0), stop=(kc == 3)), [bgw, byg], [pb])
                        self.A(lambda h: h.activation(sg[:, t0:t0 + n], ps[:, 0:n], AF.Sigmoid, bias=gb[:, oc:oc + 1]), [pb, bgw], [bsg])
                    self.V(lambda h: h.tensor_mul(ao[j][:], yg[:, oc, :], sg[:]), [byg, bsg], [bao[j]])
                    self.st(self.mixT[b, oc * 128:(oc + 1) * 128, :], ao[j][:], [bao[j]])
                self.S.barrier()

    def phase_out(self, l, b, xsrc, last):
        c = self.c
        TT, NT = c.TT, c.NT
        with ExitStack() as st:
            wo = self.sb(st, "wo", [128, KC, D], BF16); bwo = Buf()
            wsrc = self.w_out[l].rearrange("(kc kp) n -> kp kc n", kp=128)
            for g in range(4):
                self.ldc(wo[:, :, g * 512:(g + 1) * 512], wsrc[:, :, g * 512:(g + 1) * 512], [bwo])
            gB = {}
            for src in ([b] if last else [b, 2]):
                gB[src] = self.bcast_row(st, 0, src, f"gmsa{src}")
            rwt = self.sb(st, "rwt", [128, KC, NE]); rb = self.sb(st, "rb", [128, NE]); brw = Buf()
            self.ld(rwt[:], self.rw[:, :, :], [brw])
            self.ld(rb[:], self.rbias[0:1, :].partition_broadcast(128)[:, 0, :], [brw])
            nt = self.norm_tiles(st); nt["xsf"] = self.sb(st, "xsf", [128, D])
            mt = [self.sb(st, f"mt{i}", [128, KC, 128], BF16) for i in range(2)]; bmt = [Buf(), Buf()]
            xt = [self.sb(st, f"xo{i}", [128, D]) for i in range(2)]; bx = [Buf(), Buf()]
            h2f = self.sb(st, "h2f", [128, KC, 128]); h2b = [self.sb(st, f"h2b{i}", [128, KC, 128], BF16) for i in range(2)]
            bh2f = Buf(); bh2b = [Buf(), Buf()]
            R = lambda n, w=NE: self.sb(st, n, [128, w])
            scr, bia, m1, m2, gs, gmx, ing, sel, tmp4, tmp16, gsum = R("scr"), R("bia"), R("m1", 4), R("m2", 4), R("gs", 4), R("gmx", 1), R("ing", 4), R("sel"), R("tmp4", 4), R("tmp16"), R("gsum", 1)
            gout = [R("gout0"), R("gout1")]; bgo = [Buf(), Buf()]
            br_ = Buf()
            tmo = [self.sb(st, f"tmo{i}", [128, 512]) for i in range(2)]; btmo = [Buf(), Buf()]
            tiles = range(c.NTC, NT) if last else range(NT)
            for i in tiles:
                j = i % 2
                src = 2 if i < c.NTC else b
                self.ld(mt[j][:], self.mixT[b, :, i * 128:(i + 1) * 128].rearrange("(kc kp) t -> kp kc t", kp=128), [bmt[j]], q="act")
                self.ld(xt[j][:], xsrc[b, i * 128:(i + 1) * 128, :], [bx[j]])
                for g in range(4):
                    ps, pb = self.ps[g], self.pb[g]
                    for kc in range(KC):
                        self.PE(lambda h: h.matmul(ps[:, :], mt[j][:, kc, :], wo[:, kc, g * 512:(g + 1) * 512], start=(kc == 0), stop=(kc == KC - 1)), [bmt[j], bwo], [pb])
                    gt_, bgt_ = gB[src]
                    sl = slice(g * 512, (g + 1) * 512)
                    self.V(lambda h: h.tensor_tensor(tmo[g % 2][:], ps[:, :], gt_[:, sl], ALU.mult), [pb, bgt_], [btmo[g % 2]])
                    self.V(lambda h: h.tensor_add(xt[j][:, sl], xt[j][:, sl], tmo[g % 2][:]), [btmo[g % 2], bx[j]], [bx[j]])
                self.st(self.xres[b, i * 128:(i + 1) * 128, :], xt[j][:], [bx[j]])
                self.norm_T(nt, xt[j], bx[j], 1, src, lambda kc: h2f[:, kc, :], bh2f, fp32=True)
                self.A(lambda h: h.copy(h2b[j][:], h2f[:]), [bh2f], [bh2b[j]])
                self.st(self.h2T[b, :, i * 128:(i + 1) * 128].rearrange("(kc kp) t -> kp kc t", kp=128), h2b[j][:], [bh2b[j]], q="act")
                pr, bpr = self.ps[4], self.pb[4]
                for kc in range(KC):
                    self.PE(lambda h: h.matmul(pr[:, 0:NE], h2f[:, kc, :], rwt[:, kc, :], start=(kc == 0), stop=(kc == KC - 1)), [bh2f, brw], [bpr])
                self.A(lambda h: h.activation(scr[:], pr[:, 0:NE], AF.Sigmoid), [bpr], [br_])
                self.V(lambda h: h.tensor_add(bia[:], scr[:], rb[:]), [br_, brw], [br_])
                b4 = bia[:].rearrange("p (g e) -> p g e", e=4)
                self.V(lambda h: h.tensor_reduce(m1[:], b4, AX.X, ALU.max), [br_], [br_])
                self.V(lambda h: h.tensor_tensor(tmp16[:].rearrange("p (g e) -> p g e", e=4), b4, m1[:].unsqueeze(2).to_broadcast([128, 4, 4]), ALU.is_equal), [br_], [br_])
                self.V(lambda h: h.scalar_tensor_tensor(tmp16[:], tmp16[:], -1e9, bia[:], ALU.mult, ALU.add), [br_], [br_])
                self.V(lambda h: h.tensor_reduce(m2[:], tmp16[:].rearrange("p (g e) -> p g e", e=4), AX.X, ALU.max), [br_], [br_])
                self.V(lambda h: h.tensor_add(gs[:], m1[:], m2[:]), [br_], [br_])
                self.V(lambda h: h.tensor_reduce(gmx[:], gs[:], AX.X, ALU.max), [br_], [br_])
                self.V(lambda h: h.tensor_tensor(ing[:], gs[:], gmx[:].to_broadcast([128, 4]), ALU.is_equal), [br_], [br_])
                self.V(lambda h: h.tensor_tensor(sel[:].rearrange("p (g e) -> p g e", e=4), b4, m2[:].unsqueeze(2).to_broadcast([128, 4, 4]), ALU.is_ge), [br_], [br_])
                self.V(lambda h: h.tensor_mul(sel[:].rearrange("p (g e) -> p g e", e=4), sel[:].rearrange("p (g e) -> p g e", e=4), ing[:].unsqueeze(2).to_broadcast([128, 4, 4])), [br_], [br_])
                self.V(lambda h: h.tensor_mul(sel[:], sel[:], scr[:]), [br_], [br_])
                self.V(lambda h: h.tensor_reduce(gsum[:], sel[:], AX.X, ALU.add), [br_], [br_])
                self.V(lambda h: h.reciprocal(gsum[:], gsum[:]), [br_], [br_])
                self.V(lambda h: h.tensor_scalar(gout[j][:], sel[:], gsum[:, 0:1], None, ALU.mult), [br_], [bgo[j]])
                self.st(self.gates[b, i * 128:(i + 1) * 128, :], gout[j][:], [bgo[j]], q="act")

    def phase_moe(self, l, b, last):
        c = self.c
        NT = c.NT
        tiles = list(range(c.NTC, NT)) if last else list(range(NT))
        STM = 6
        supers = [tiles[i:i + STM] for i in range(0, len(tiles), STM)]
        with ExitStack() as st:
            h2 = self.sb(st, "h2s", [128, KC, STM * 128], BF16); bh2 = Buf()
            gt = self.sb(st, "gts", [128, STM, NE]); bgt = Buf()
            yacc = self.sb(st, "yacc", [128, STM, D]); bya = Buf()
            he = self.sb(st, "he", [128, 8, STM * 128], BF16); bhe = Buf()
            wdn = [self.sb(st, f"wdn{i}", [128, 8, D], BF16) for i in range(2)]; bwd = [Buf(), Buf()]
            wgu = [self.sb(st, f"wgu{i}", [128, 2, KC, 128], BF16) for i in range(2)]; bwgu = [Buf() for _ in range(2)]
            sg = self.sb(st, "sgm", [128, STM * 128]); bsg = Buf()
            gB = {}
            for src in ([b] if last else [b, 2]):
                gB[src] = self.bcast_row(st, 1, src, f"gmlp{src}")
            xt = [self.sb(st, "xm0", [128, D])] * 2; bx = [Buf()] * 2
            ew = 0
            for sup in supers:
                n_t = len(sup); ntok = n_t * 128
                t0 = sup[0] * 128
                self.ld(h2[:, :, 0:ntok], self.h2T[b, :, t0:t0 + ntok].rearrange("(kc kp) t -> kp kc t", kp=128), [bh2])
                self.ld(gt[:, 0:n_t, :], self.gates[b, t0:t0 + ntok, :].rearrange("(i p) e -> p i e", p=128), [bgt], q="act")
                tg = tok_groups(ntok)
                for e in range(NE):
                    jd = e % 2
                    wds = self.wd[l, e].rearrange("(fc fp) n -> fp fc n", fp=128)
                    for g in range(2):
                        self.ldc(wdn[jd][:, :, g * 1024:(g + 1) * 1024], wds[:, :, g * 1024:(g + 1) * 1024], [bwd[jd]])
                    for fc in range(8):
                        jw = ew % 2; ew += 1
                        self.ldc(wgu[jw][:, 0], self.wg[l, e][:, fc * 128:(fc + 1) * 128].rearrange("(kc kp) f -> kp kc f", kp=128), [bwgu[jw]])
                        self.ldc(wgu[jw][:, 1], self.wu[l, e][:, fc * 128:(fc + 1) * 128].rearrange("(kc kp) f -> kp kc f", kp=128), [bwgu[jw]])
                        for gi, (s0, n) in enumerate(tg):
                            pG, pU = self.ps[gi], self.ps[2 + gi]; bpG, bpU = self.pb[gi], self.pb[2 + gi]
                            for kc in range(KC):
                                self.PE(lambda h: h.matmul(pG[:, 0:n], wgu[jw][:, 0, kc, :], h2[:, kc, s0:s0 + n], start=(kc == 0), stop=(kc == KC - 1)), [bwgu[jw], bh2], [bpG])
                            for kc in range(KC):
                                self.PE(lambda h: h.matmul(pU[:, 0:n], wgu[jw][:, 1, kc, :], h2[:, kc, s0:s0 + n], start=(kc == 0), stop=(kc == KC - 1)), [bwgu[jw], bh2], [bpU])
                            self.A(lambda h: h.activation(sg[:, s0:s0 + n], pG[:, 0:n], AF.Silu), [bpG], [bsg])
                            self.V(lambda h: h.tensor_mul(he[:, fc, s0:s0 + n], sg[:, s0:s0 + n], pU[:, 0:n]), [bsg, bpU], [bhe])
                    for ti in range(n_t):
                        for g in range(4):
                            ps, pb = self.ps[4 + g], self.pb[4 + g]
                            for fc in range(8):
                                self.PE(lambda h: h.matmul(ps[:, :], he[:, fc, ti * 128:(ti + 1) * 128], wdn[jd][:, fc, g * 512:(g + 1) * 512], start=(fc == 0), stop=(fc == 7)), [bhe, bwd[jd]], [pb])
                            ysl = yacc[:, ti, g * 512:(g + 1) * 512]
                            if e == 0:
                                self.V(lambda h: h.tensor_scalar(ysl, ps[:, :], gt[:, ti, e:e + 1], None, ALU.mult), [pb, bgt], [bya])
                            else:
                                self.V(lambda h: h.scalar_tensor_tensor(ysl, ps[:, :], gt[:, ti, e:e + 1], ysl, ALU.mult, ALU.add), [pb, bgt, bya], [bya])
                for ti, i in enumerate(sup):
                    j = i % 2
                    src = 2 if i < c.NTC else b
                    gt_, bgt_ = gB[src]
                    self.ld(xt[j][:], self.xres[b, i * 128:(i + 1) * 128, :], [bx[j]])
                    self.V(lambda h: h.tensor_mul(yacc[:, ti, :], yacc[:, ti, :], gt_[:]), [bya, bgt_], [bya])
                    self.V(lambda h: h.tensor_add(xt[j][:], xt[j][:], yacc[:, ti, :]), [bya, bx[j]], [bx[j]])
                    self.st(self.xres[b, i * 128:(i + 1) * 128, :], xt[j][:], [bx[j]])

    def phase_final(self):
        c = self.c
        with ExitStack() as st:
            gB = self.sb(st, "gfinB", [128, D]); bg = Buf()
            self.ld(gB[:], self.gfin[0:1, :].partition_broadcast(128)[:, 0, :], [bg])
            xt = [self.sb(st, f"xf{i}", [128, D]) for i in range(2)]; bx = [Buf(), Buf()]
            sq = self.sb(st, "fsq", [128, D], BF16); ss = self.sb(st, "fss", [128, 4]); bs = Buf()
            for b in range(c.NB):
                for i in range(c.NTC, c.NT):
                    j = i % 2
                    self.ld(xt[j][:], self.xres[b, i * 128:(i + 1) * 128, :], [bx[j]], q=("sp" if j == 0 else "act"))
                    self.A(lambda h: h.activation(sq[:], xt[j][:], AF.Square, accum_out=ss[:, 0:1]), [bx[j]], [bs])
                    self.A(lambda h: h.activation(ss[:, 1:2], ss[:, 0:1], AF.Sqrt, scale=1.0 / D, bias=self.epsc[:, 0:1]), [bs, self.bconst], [bs])
                    self.V(lambda h: h.reciprocal(ss[:, 2:3], ss[:, 1:2]), [bs], [bs])
                    self.V(lambda h: h.scalar_tensor_tensor(xt[j][:], xt[j][:], ss[:, 2:3], gB[:], ALU.mult, ALU.mult), [bx[j], bs, bg], [bx[j]])
                    self.st(self.out[b, (i - c.NTC) * 128:(i - c.NTC + 1) * 128, :], xt[j][:], [bx[j]], q=("sp" if j == 0 else "act"))


def host_shared(inp, cfg):
    L = cfg.L
    f = lambda a: np.ascontiguousarray(np.asarray(a, dtype=np.float32))
    pk = lambda v: f(np.asarray(v).reshape(v.shape[:-1] + (v.shape[-1] // 128, 128)).swapaxes(-1, -2))
    sh = {}
    sh["gmix"] = pk(inp["norm_mix_g"]); sh["gffn"] = pk(inp["norm_ffn_g"])
    sh["gfin"] = f(inp["final_norm_g"]).reshape(1, D)
    sh["w_mod"] = f(inp["w_mod"]); sh["b_mod"] = f(inp["b_mod"])
    w_in = np.asarray(inp["w_in"], np.float32).reshape(L, D, 12, W)
    def swap(p):
        x = w_in[:, :, p].reshape(L, D, 4, 2, 64)
        return x[:, :, :, ::-1, :].reshape(L, D, W)
    sh["w_in"] = f(np.concatenate([w_in.reshape(L, D, 12 * W), swap(8), swap(9)], axis=-1))
    sh["w_out"] = f(inp["w_out"])
    bre, bim = np.asarray(inp["s5_b_re"], np.float32), np.asarray(inp["s5_b_im"], np.float32)
    cre, cim = np.asarray(inp["s5_c_re"], np.float32), np.asarray(inp["s5_c_im"], np.float32)
    s5B = np.zeros((L, 2, 16, 128, 2, 128), np.float32)
    s5C = np.zeros((L, 2, 16, 128, 2, 128), np.float32)
    for sc in range(16):
        for g2 in range(2):
            g = 2 * sc + g2
            r0 = 16 * (g % 8)
            s5B[:, :, sc, r0:r0 + 16, 0, g2 * 64:(g2 + 1) * 64] = bre[:, :, g]
            s5B[:, :, sc, r0:r0 + 16, 1, g2 * 64:(g2 + 1) * 64] = bim[:, :, g]
            s5C[:, :, sc, g2 * 64:(g2 + 1) * 64, 0, r0:r0 + 16] = cre[:, :, g]
            s5C[:, :, sc, g2 * 64:(g2 + 1) * 64, 1, r0:r0 + 16] = cim[:, :, g]
    sh["s5B"], sh["s5C"] = s5B, s5C
    lam = np.zeros((L, 128, 3, 2, 16), np.float32)
    lre, lim, ldt = (np.asarray(inp[k], np.float32) for k in ("s5_lam_re", "s5_lam_im", "s5_log_dt"))
    for sc in range(16):
        for g2 in range(2):
            g = 2 * sc + g2
            lam[:, g2 * 64:(g2 + 1) * 64, 0, :, sc] = lre[:, :, g, :].transpose(0, 2, 1)
            lam[:, g2 * 64:(g2 + 1) * 64, 1, :, sc] = lim[:, :, g, :].transpose(0, 2, 1)
            lam[:, g2 * 64:(g2 + 1) * 64, 2, :, sc] = ldt[:, :, g][:, None, :]
    sh["s5lam"] = lam
    sh["s5d"] = pk(inp["s5_d"]); sh["gluw"] = f(inp["s5_glu_w"]); sh["glub"] = pk(inp["s5_glu_b"])
    TT, CL = cfg.TT, cfg.CTXL
    kf = np.arange(TT, dtype=np.float32)
    kb = np.concatenate([CL - 1 - np.arange(CL), CL + (TT - CL) - 1 - np.arange(TT - CL)]).astype(np.float32)
    sh["kidx"] = f(np.stack([np.broadcast_to(kf, (128, TT)), np.broadcast_to(kb, (128, TT))]))
    cw = np.asarray(inp["lru_conv_w"], np.float32)
    sh["convw"] = f(cw.reshape(L, 4, 4, 128).transpose(0, 3, 2, 1))
    lv = np.stack([np.asarray(inp["lru_conv_b"], np.float32)] +
                  [np.asarray(inp[k], np.float32)[:, d] for k in ("lru_ba", "lru_bx", "lru_lam") for d in range(2)], axis=-1)
    sh["lruv"] = f(lv.reshape(L, 4, 128, 7).transpose(0, 2, 1, 3))
    lw = np.zeros((L, 2, 2, 4, 128, 128), np.float32)
    for a, k in enumerate(("lru_wa", "lru_wx")):
        wsrc = np.asarray(inp[k], np.float32)
        for hd in range(8):
            cc, h2 = hd // 2, hd % 2
            lw[:, a, :, cc, h2 * 64:(h2 + 1) * 64, h2 * 64:(h2 + 1) * 64] = wsrc[:, :, hd]
    sh["lruw"] = lw
    hl = np.asarray(inp["hgrn_lb_logits"], np.float32)
    sh["hglb"] = f(hl.reshape(2, L, 4, 128).transpose(0, 1, 3, 2))
    n = cfg.SEQL
    rows = n // 64
    row = np.repeat(np.arange(rows, dtype=np.float32), 64); col = np.tile(np.arange(64, dtype=np.float32), rows)
    inv = (np.float32(10000.0) ** (-np.arange(32, dtype=np.float32) / np.float32(32))).astype(np.float32)
    ang = np.concatenate([row[:, None] * inv, col[:, None] * inv], axis=-1).astype(np.float32)
    sh["rang"] = f(np.concatenate([ang.T, ang.T], axis=0))
    sh["rw"] = f(np.asarray(inp["router_w"], np.float32).reshape(KC, 128, NE).transpose(1, 0, 2))
    sh["rbias"] = f(inp["router_bias"]).reshape(1, NE)
    sh["wg"], sh["wu"], sh["wd"] = f(inp["moe_w_gate"]), f(inp["moe_w_up"]), f(inp["moe_w_down"])
    sh["ident"] = np.eye(128, dtype=np.float32)
    s_, t_ = np.meshgrid(np.arange(64), np.arange(64), indexing="ij")
    m = np.stack([(s_ <= t_), (s_ >= t_)]).astype(np.float32)
    mm = np.concatenate([m, m], axis=1)
    sh["masks"] = f(np.broadcast_to(mm[:, :, None, :], (2, 128, 4, 64)))
    rst = np.ones((128, TT + 1), np.float32); rst[:, 0::64] = 0.0
    sh["rst"] = rst
    rst32 = np.ones((128, TT + 1), np.float32); rst32[:, 0::32] = 0.0
    sh["rst32"] = rst32
    s_, t_ = np.meshgrid(np.arange(32), np.arange(32), indexing="ij")
    m32 = np.stack([(s_ <= t_), (s_ >= t_)]).astype(np.float32)
    sh["masks32"] = f(np.broadcast_to(np.concatenate([m32] * 4, axis=1)[:, :, None, :], (2, 128, 4, 32)))
    sel = np.zeros((3, 3, 128), np.float32)
    for s in range(3):
        sel[s, s, :] = 1.0
    sh["sel3"] = sel
    return sh


def host_core(inp, cfg, b0):
    NB = cfg.NB
    x = np.asarray(inp["x"], np.float32)[b0:b0 + NB]
    ctx = np.asarray(inp["ctx"], np.float32)[b0:b0 + NB]
    cvec = np.concatenate([np.asarray(inp["c"], np.float32)[b0:b0 + NB], np.asarray(inp["c_ctx"], np.float32)[None]], axis=0)
    if NB == 1:
        cvec = np.concatenate([cvec[0:1], cvec[0:1], cvec[1:2]], axis=0)
    return {"xin": np.ascontiguousarray(np.concatenate([ctx, x], axis=1)),
            "cT": np.ascontiguousarray(cvec.reshape(3, KC, 128).transpose(2, 1, 0))}


_CACHE = {}


def kernel(**inputs):
    cfg = Cfg()
    n_cores = 8
    if "nc" not in _CACHE:
        _CACHE["nc"] = Prog(cfg).build()
    nc = _CACHE["nc"]
    sh = host_shared(inputs, cfg)
    in_maps = []
    for core in range(n_cores):
        m = dict(sh)
        m.update(host_core(inputs, cfg, core * cfg.NB))
        in_maps.append(m)
    res = run_bass_kernel_spmd(nc, in_maps, core_ids=list(range(n_cores)))
    return np.concatenate([r["out"] for r in res.results], axis=0).astype(np.float32)
```

```python
import math
from contextlib import ExitStack
import numpy as np
import concourse.bass as bass
import concourse.mybir as mybir
from concourse.bass_utils import run_bass_kernel_spmd

F32 = mybir.dt.float32
BF16 = mybir.dt.bfloat16
I32 = mybir.dt.int32
ALU = mybir.AluOpType
AF = mybir.ActivationFunctionType
AX = mybir.AxisListType

D = 2048
KC = 16
W = 512
NPARTS = 14
FM_PARTS = [0, 1, 2, 3, 4, 5, 8, 9, 12, 13]
TM_PARTS = [6, 7, 10, 11]
NE = 16
DFF = 1024
EPS = 1e-6
TWO_PI = 2.0 * math.pi


class Buf:
    __slots__ = ("w", "r")

    def __init__(self):
        self.w = None
        self.r = {}


class Eng:
    def __init__(self, name, h, sem):
        self.name, self.h, self.sem = name, h, sem
        self.n = 0
        self.seen = {}
        self.dsems, self.dcnt, self.dnext = [], [], 0


class Sched:
    def __init__(self, nc, stack, n_dma_sems=16):
        self.nc = nc
        self.E = {}
        for name, h in (("pe", nc.tensor), ("dve", nc.vector), ("act", nc.scalar),
                        ("pool", nc.gpsimd), ("sp", nc.sync)):
            self.E[name] = Eng(name, h, stack.enter_context(nc.semaphore("s_" + name)))
        for name in ("sp", "act", "pool"):
            e = self.E[name]
            for i in range(n_dma_sems):
                e.dsems.append(stack.enter_context(nc.semaphore(f"d_{name}{i}")))
                e.dcnt.append(0)
        self.ninstr = 0

    def _wait(self, e, sem, val):
        if sem is e.sem and e.name == "pe":
            return
        if e.seen.get(sem, 0) < val:
            e.h.wait_ge(sem, val)
            e.seen[sem] = val

    def _deps(self, e, reads, writes):
        for b in reads:
            if b.w is not None:
                self._wait(e, *b.w)
        for b in writes:
            if b.w is not None:
                self._wait(e, *b.w)
            for s, v in b.r.items():
                self._wait(e, s, v)

    @staticmethod
    def _mark(tok, reads, writes):
        s, v = tok
        for b in reads:
            if b.r.get(s, 0) < v:
                b.r[s] = v
        for b in writes:
            b.w = tok
            b.r = {}

    def op(self, eng, fn, reads=(), writes=(), inc=True):
        e = self.E[eng]
        self._deps(e, reads, writes)
        ins = fn(e.h)
        if inc:
            e.n += 1
            ins.then_inc(e.sem, 1)
            tok = (e.sem, e.n)
            e.pend = False
        else:
            tok = (e.sem, e.n + 1)
            e.pend = True
        self._mark(tok, reads, writes)
        self.ninstr += 1
        return ins

    def dma(self, eng, out, in_, reads=(), writes=(), **kw):
        e = self.E[eng]
        self._deps(e, reads, writes)
        i = e.dnext
        e.dnext = (i + 1) % len(e.dsems)
        sem = e.dsems[i]
        if e.dcnt[i]:
            self._wait(e, sem, e.dcnt[i])
        ins = e.h.dma_start(out=out, in_=in_, **kw)
        e.dcnt[i] += 16
        ins.then_inc(sem, 16)
        self._mark((sem, e.dcnt[i]), reads, writes)
        self.ninstr += 1
        return ins

    def idma(self, reads=(), writes=(), **kw):
        e = self.E["pool"]
        self._deps(e, reads, writes)
        i = e.dnext
        e.dnext = (i + 1) % len(e.dsems)
        sem = e.dsems[i]
        if e.dcnt[i]:
            self._wait(e, sem, e.dcnt[i])
        ins = e.h.indirect_dma_start(**kw)
        e.dcnt[i] += 16
        ins.then_inc(sem, 16)
        self._mark((sem, e.dcnt[i]), reads, writes)
        self.ninstr += 1
        return ins

    def barrier(self):
        assert not any(getattr(e, "pend", False) for e in self.E.values())
        for e in self.E.values():
            for f in self.E.values():
                if f is not e and f.n:
                    self._wait(e, f.sem, f.n)
            for q in ("sp", "act", "pool"):
                qe = self.E[q]
                for sem, cnt in zip(qe.dsems, qe.dcnt):
                    if cnt:
                        self._wait(e, sem, cnt)


class Cfg:
    def __init__(self, NB=2, CTXL=256, SEQL=2048, L=2, dbg=False):
        self.NB, self.CTXL, self.SEQL, self.L, self.dbg = NB, CTXL, SEQL, L, dbg
        self.TT = CTXL + SEQL
        self.NT = self.TT // 128
        self.NTC = CTXL // 128
        self.NCH = self.TT // 64
        self.NCHC = CTXL // 64
        self.sparse = True
        self.SLOT = 512


def tok_groups(T, g=512):
    out, t = [], 0
    while t < T:
        n = min(g, T - t)
        out.append((t, n))
        t += n
    return out


class Prog:
    def __init__(self, cfg):
        self.c = cfg
        self.nc = bass.Bass("TRN2", target_bir_lowering=False)
        self.inp = {}

    def din(self, name, shape, dt=F32):
        t = self.nc.dram_tensor(name, list(shape), dt, kind="ExternalInput").ap()
        self.inp[name] = t
        return t

    def dscr(self, name, shape, dt=F32):
        kind = "ExternalOutput" if self.c.dbg else "Internal"
        return self.nc.dram_tensor(name, list(shape), dt, kind=kind).ap()

    def sb(self, st, name, shape, dt=F32):
        self._uid = getattr(self, "_uid", 0) + 1
        return st.enter_context(self.nc.sbuf_tensor(f"{name}_{self._uid}", list(shape), dt))

    def V(self, fn, R=(), Wr=()):
        return self.S.op("dve", fn, R, Wr)

    def A(self, fn, R=(), Wr=()):
        return self.S.op("act", fn, R, Wr)

    def G(self, fn, R=(), Wr=()):
        return self.S.op("pool", fn, R, Wr)

    def PE(self, fn, R=(), Wr=(), inc=True):
        return self.S.op("pe", fn, R, Wr, inc=inc)

    def ld(self, out, in_, Wr, q="sp", R=()):
        return self.S.dma(q, out, in_, reads=R, writes=Wr)

    def ldc(self, out, in_, Wr, R=()):
        return self.S.dma("pool", out, in_, reads=R, writes=Wr)

    def st(self, out, in_, R, q="sp"):
        return self.S.dma(q, out, in_, reads=R, writes=())

    def build(self):
        c, nc = self.c, self.nc
        NB, TT, NT, L = c.NB, c.TT, c.NT, c.L
        i_ = self.din
        self.xin = i_("xin", [NB, TT, D])
        self.cT = i_("cT", [128, KC, 3])
        self.gmix = i_("gmix", [L, 128, KC])
        self.gffn = i_("gffn", [L, 128, KC])
        self.gfin = i_("gfin", [1, D])
        self.w_mod = i_("w_mod", [L, D, 6 * D])
        self.b_mod = i_("b_mod", [L, 6 * D])
        self.w_in = i_("w_in", [L, D, NPARTS * W])
        self.w_out = i_("w_out", [L, D, D])
        self.s5B = i_("s5B", [L, 2, 16, 128, 2, 128])
        self.s5C = i_("s5C", [L, 2, 16, 128, 2, 128])
        self.s5lam = i_("s5lam", [L, 128, 3, 2, 16])
        self.s5d = i_("s5d", [L, 128, 4])
        self.gluw = i_("gluw", [L, W, W])
        self.glub = i_("glub", [L, 128, 4])
        self.kidx = i_("kidx", [2, 128, TT])
        self.convw = i_("convw", [L, 128, 4, 4])
        self.lruv = i_("lruv", [L, 128, 4, 7])
        self.lruw = i_("lruw", [L, 2, 2, 4, 128, 128])
        self.hglb = i_("hglb", [2, L, 128, 4])
        self.rang = i_("rang", [128, c.SEQL])
        self.rw = i_("rw", [128, KC, NE])
        self.rbias = i_("rbias", [1, NE])
        self.wg = i_("wg", [L, NE, D, DFF])
        self.wu = i_("wu", [L, NE, D, DFF])
        self.wd = i_("wd", [L, NE, DFF, D])
        self.ident = i_("ident", [128, 128])
        self.masks = i_("masks", [2, 128, 4, 64])
        self.rst = i_("rst", [128, TT + 1])
        self.rst32 = i_("rst32", [128, TT + 1])
        self.masks32 = i_("masks32", [2, 128, 4, 32])
        self.sel3 = i_("sel3", [3, 3, 128])
        self.gffn_row = i_("gffn_row", [L, D])
        self.ltri = i_("ltri", [2, 128, 128])
        self.kio = i_("kio", [128, 24])
        NSMAX = (2 * NB * TT + c.SLOT - 1) // c.SLOT + NE
        self.NSMAX = NSMAX
        self.siota = i_("siota", [128, NSMAX])
        self.out = nc.dram_tensor("out", [NB, c.SEQL, D], F32, kind="ExternalOutput").ap()

        self.xres = self.dscr("xres", [NB, TT, D])
        self.Pfm = self.dscr("Pfm", [NB, len(FM_PARTS) * W, TT])
        self.Ptm = self.dscr("Ptm", [NB, TT, len(TM_PARTS) * W])
        self.mixT = self.dscr("mixT", [NB, D, TT], BF16)
        self.h2T = self.dscr("h2T", [NB, D, TT], BF16)
        self.gates = self.dscr("gates", [NB, TT, NE])
        self.ygD = self.dscr("ygD", [NB, W, TT])
        self.modD = self.dscr("modD", [3, 4, D])
        RM = self.NSMAX * c.SLOT
        self.h2tok = self.dscr("h2tok", [NB * TT, D], BF16)
        self.selD = self.dscr("selD", [NB * TT, NE])
        self.rankD = self.dscr("rankD", [NB * TT, NE])
        self.hs = self.dscr("hs", [RM, D], BF16)
        self.gsD = self.dscr("gsD", [RM, 1])
        self.ysD = self.dscr("ysD", [RM, D])

        with ExitStack() as top:
            self.S = Sched(nc, top)
            S = self.S
            self.ps = [top.enter_context(nc.psum_tensor(f"ps{i}", [128, 512], F32)) for i in range(8)]
            self.pb = [Buf() for _ in range(8)]
            self.idf = self.sb(top, "idf", [128, 128]); self.idb = self.sb(top, "idb", [128, 128], BF16)
            self.bconst = Buf()
            self.ld(self.idf[:], self.ident[:, :], [self.bconst])
            self.ldc(self.idb[:], self.ident[:, :], [self.bconst])
            self.cmask = self.sb(top, "cmask", [128, 2, 4, 64])
            self.ld(self.cmask[:], self.masks.rearrange("a p h j -> p a h j"), [self.bconst])
            self.cmask32 = self.sb(top, "cmask32", [128, 2, 4, 32])
            self.ld(self.cmask32[:], self.masks32.rearrange("a p h j -> p a h j"), [self.bconst])
            self.epsc = self.sb(top, "epsc", [128, 1])
            self.V(lambda h: h.memset(self.epsc[:], EPS), [], [self.bconst])
            self.sel3t = self.sb(top, "sel3t", [3, 3, 128])
            self.ld(self.sel3t[:], self.sel3[:, :, :], [self.bconst])
            self.modP = self.sb(top, "modP", [128, 6, KC, 3])
            self.AB = self.sb(top, "AB", [128, 4, KC, 3])
            self.bmod = Buf()
            self.run = self.sb(top, "run", [128, NE]); self.brun = Buf()
            self.pidx = self.sb(top, "pidx", [128, NB * NT, 2], I32); self.bpidx = Buf()
            self.ltt = self.sb(top, "ltt", [128, 2, 128])
            self.ld(self.ltt[:], self.ltri.rearrange("a p j -> p a j"), [self.bconst])
            self.es2 = self.sb(top, "es2", [128, 2, self.NSMAX]); self.bes = Buf()
            self.kiot = self.sb(top, "kiot", [128, 24])
            self.ld(self.kiot[:], self.kio[:, :], [self.bconst])
            if c.sparse:
                with ExitStack() as zst:
                    z = self.sb(zst, "zz", [128, D], BF16); bz = Buf()
                    self.V(lambda h: h.memset(z[:], 0.0), [], [bz])
                    for r0 in range(0, self.NSMAX * c.SLOT, 128):
                        self.st(self.hs[r0:r0 + 128, :], z[:], [bz], q=("sp" if (r0 // 128) % 2 == 0 else "act"))
                    S.barrier()
            S.barrier()
            for l in range(L):
                last = (l == L - 1)
                self.phase_mod(l)
                S.barrier()
                for b in range(NB):
                    xsrc = self.xin if l == 0 else self.xres
                    self.phase_proj(l, b, xsrc)
                    S.barrier()
                    self.phase_lru(l, b)
                    S.barrier()
                    self.phase_gla(l, b, 0)
                    S.barrier()
                    self.phase_gla(l, b, 1)
                    S.barrier()
                    self.phase_s5(l, b)
                    S.barrier()
                    self.phase_out(l, b, xsrc, last)
                    S.barrier()
                    if not c.sparse:
                        self.phase_moe(l, b, last)
                        S.barrier()
                if c.sparse:
                    self.phase_route(l, last)
                    S.barrier()
                    self.phase_moe_sparse(l, last)
                    S.barrier()
                    self.phase_unsort(l, last)
                    S.barrier()
            self.phase_final()
            S.barrier()
        return nc

    def phase_mod(self, l):
        c, S = self.c, self.S
        with ExitStack() as st:
            cT = self.sb(st, "cT", [128, KC, 3]); cs = self.sb(st, "cs", [128, KC, 3], BF16)
            bc = Buf()
            self.ld(cT[:], self.cT[:, :, :], [bc])
            self.A(lambda h: h.activation(cs[:], cT[:], AF.Silu), [bc], [bc])
            modrow = self.sb(st, "modrow", [3, 6 * D]); bmr = Buf()
            wm = [self.sb(st, f"wm{i}", [128, KC, 512], BF16) for i in range(2)]; bwm = [Buf(), Buf()]
            bt = [self.sb(st, f"bt{i}", [3, 512]) for i in range(2)]; bbt = [Buf(), Buf()]
            wsrc = self.w_mod[l].rearrange("(kc kp) n -> kp kc n", kp=128)
            for cg in range(24):
                j = cg % 2
                self.ldc(wm[j][:], wsrc[:, :, cg * 512:(cg + 1) * 512], [bwm[j]])
                self.ld(bt[j][:], self.b_mod[l:l + 1, cg * 512:(cg + 1) * 512].partition_broadcast(3)[:, 0, :], [bbt[j]], q="act")
                p = self.ps[cg % 2]; pb = self.pb[cg % 2]
                for kc in range(KC):
                    self.PE(lambda h: h.matmul(p[0:3, :], cs[:, kc, :], wm[j][:, kc, :], start=(kc == 0), stop=(kc == KC - 1)),
                            [bc, bwm[j]], [pb], inc=(kc == KC - 1))
                self.V(lambda h: h.tensor_add(modrow[:, cg * 512:(cg + 1) * 512], p[0:3, :], bt[j][:]), [pb, bbt[j]], [bmr])
            pT = self.ps[2]; pTb = self.pb[2]
            for v in range(6):
                for kc in range(KC):
                    k = v * KC + kc
                    self.PE(lambda h: h.transpose(pT[:, k * 3:k * 3 + 3], modrow[:, v * D + kc * 128: v * D + (kc + 1) * 128], self.idf[0:3, 0:3]),
                            [bmr, self.bconst], [pTb], inc=(k == 6 * KC - 1))
            self.V(lambda h: h.tensor_copy(self.modP[:].rearrange("p a k s -> p (a k s)"), pT[:, 0:288]), [pTb], [self.bmod])
            self.st(self.modD[:, 0, :], modrow[:, 2 * D:3 * D], [bmr])
            self.st(self.modD[:, 1, :], modrow[:, 5 * D:6 * D], [bmr])
            self.st(self.modD[:, 3, :], modrow[:, 3 * D:4 * D], [bmr])
            grow = self.sb(st, "grow", [3, D]); bgr = Buf()
            self.ld(grow[:], self.gffn_row[l:l + 1, :].partition_broadcast(3)[:, 0, :], [bgr])
            self.V(lambda h: h.scalar_tensor_tensor(grow[:], modrow[:, 4 * D:5 * D], 1.0, grow[:], ALU.add, ALU.mult), [bmr, bgr], [bgr])
            self.st(self.modD[:, 2, :], grow[:], [bgr])
            self.V(lambda h: h.memset(self.run[:], 0.0), [self.brun], [self.brun])
            g1 = self.sb(st, "g1", [128, KC]); g2 = self.sb(st, "g2", [128, KC]); bg = Buf()
            self.ld(g1[:], self.gmix[l], [bg]); self.ld(g2[:], self.gffn[l], [bg])
            for (gi, gt, vs, vsh) in ((0, g1, 1, 0), (2, g2, 4, 3)):
                self.V(lambda h: h.tensor_scalar(self.AB[:, gi], self.modP[:, vs], 1.0, None, ALU.add), [self.bmod], [self.bmod])
                self.V(lambda h: h.tensor_mul(self.AB[:, gi], self.AB[:, gi], gt[:].unsqueeze(2).to_broadcast([128, KC, 3])), [self.bmod, bg], [self.bmod])
                self.V(lambda h: h.tensor_copy(self.AB[:, gi + 1], self.modP[:, vsh]), [self.bmod], [self.bmod])

    def bcast_row(self, st, which, src, name):
        t = self.sb(st, name, [128, D]); b = Buf()
        self.ld(t[:], self.modD[src:src + 1, which, :].partition_broadcast(128)[:, 0, :], [b])
        return t, b

    def norm_T(self, st_tiles, xt, bx, which, src, dst_fn, bdst, fp32):
        sq, ss, xs = st_tiles["sq"], st_tiles["ss"], (st_tiles["xsf"] if fp32 else st_tiles["xsb"])
        bsq, bss, bxs = st_tiles["bsq"], st_tiles["bss"], st_tiles["bxs"]
        self.A(lambda h: h.activation(sq[:], xt[:], AF.Square, accum_out=ss[:, 0:1]), [bx], [bsq, bss])
        self.A(lambda h: h.activation(ss[:, 1:2], ss[:, 0:1], AF.Sqrt, scale=1.0 / D, bias=self.epsc[:, 0:1]), [bss, self.bconst], [bss])
        self.V(lambda h: h.reciprocal(ss[:, 2:3], ss[:, 1:2]), [bss], [bss])
        self.A(lambda h: h.activation(xs[:], xt[:], AF.Identity, scale=ss[:, 2:3]), [bx, bss], [bxs])
        a_i, b_i = (0, 1) if which == 0 else (2, 3)
        idm = self.idf if fp32 else self.idb
        ngrp = 4 if fp32 else 2
        per = KC // ngrp
        for g in range(ngrp):
            bank = 4 + g
            p = self.ps[bank]; pb = self.pb[bank]
            pv = p[:] if fp32 else p[:].bitcast(BF16)
            for j in range(per):
                kc = g * per + j
                self.PE(lambda h: h.transpose(pv[:, j * 128:(j + 1) * 128], xs[:, kc * 128:(kc + 1) * 128], idm[:]),
                        [bxs, self.bconst], [pb], inc=(j == per - 1))
            for j in range(per):
                kc = g * per + j
                sc, bi = self.AB[:, a_i, kc, src:src + 1], self.AB[:, b_i, kc, src:src + 1]
                if kc % 2 == 0:
                    self.V(lambda h: h.tensor_scalar(dst_fn(kc), pv[:, j * 128:(j + 1) * 128], sc, bi, ALU.mult, ALU.add),
                           [pb, self.bmod], [bdst])
                else:
                    self.A(lambda h: h.activation(dst_fn(kc), pv[:, j * 128:(j + 1) * 128], AF.Identity, bias=bi, scale=sc),
                           [pb, self.bmod], [bdst])

    def norm_tiles(self, st):
        d = {"sq": self.sb(st, "sq", [128, D], BF16), "ss": self.sb(st, "ss", [128, 4]),
             "xsf": None, "xsb": None, "bsq": Buf(), "bss": Buf(), "bxs": Buf()}
        return d

    def phase_proj(self, l, b, xsrc):
        c, S = self.c, self.S
        TT, NT = c.TT, c.NT
        with ExitStack() as st:
            hT = self.sb(st, "hT", [128, KC, TT], BF16); bh = Buf()
            nt = self.norm_tiles(st); nt["xsb"] = self.sb(st, "xsb", [128, D], BF16)
            xt = [self.sb(st, f"xt{i}", [128, D]) for i in range(2)]; bx = [Buf(), Buf()]
            for i in range(NT):
                j = i % 2
                self.ld(xt[j][:], xsrc[b, i * 128:(i + 1) * 128, :], [bx[j]], q=("sp" if j == 0 else "act"))
                src = 2 if i < c.NTC else b
                self.norm_T(nt, xt[j], bx[j], 0, src, lambda kc: hT[:, kc, i * 128:(i + 1) * 128], bh, fp32=False)
            wp = [self.sb(st, f"wp{i}", [128, KC, W], BF16) for i in range(2)]; bw = [Buf(), Buf()]
            stg = [self.sb(st, f"stg{i}", [128, TT]) for i in range(2)]; bs = [Buf(), Buf()]
            stg2 = [self.sb(st, f"stgb{i}", [128, W]) for i in range(2)]; bs2 = [Buf(), Buf()]
            wsrc = self.w_in[l].rearrange("(kc kp) n -> kp kc n", kp=128)
            tgs = tok_groups(TT)
            ev = 0
            for pi, p in enumerate(FM_PARTS + TM_PARTS):
                j = pi % 2
                self.ldc(wp[j][:], wsrc[:, :, p * W:(p + 1) * W], [bw[j]])
                if p in FM_PARTS:
                    fi = FM_PARTS.index(p)
                    for cb in range(4):
                        sj = cb % 2
                        for (t0, n) in tgs:
                            bank = ev % 4; ev += 1
                            ps, pb = self.ps[bank], self.pb[bank]
                            for kc in range(KC):
                                self.PE(lambda h: h.matmul(ps[:, 0:n], wp[j][:, kc, cb * 128:(cb + 1) * 128], hT[:, kc, t0:t0 + n],
                                                           start=(kc == 0), stop=(kc == KC - 1)), [bw[j], bh], [pb], inc=(kc == KC - 1))
                            if ev % 2:
                                self.A(lambda h: h.copy(stg[sj][:, t0:t0 + n], ps[:, 0:n]), [pb], [bs[sj]])
                            else:
                                self.V(lambda h: h.tensor_copy(stg[sj][:, t0:t0 + n], ps[:, 0:n]), [pb], [bs[sj]])
                        self.st(self.Pfm[b, fi * W + cb * 128: fi * W + (cb + 1) * 128, :], stg[sj][:], [bs[sj]], q=("sp" if sj == 0 else "act"))
                else:
                    ti = TM_PARTS.index(p)
                    for i in range(NT):
                        sj = i % 2
                        bank = ev % 4; ev += 1
                        ps, pb = self.ps[bank], self.pb[bank]
                        for kc in range(KC):
                            self.PE(lambda h: h.matmul(ps[:, :], hT[:, kc, i * 128:(i + 1) * 128], wp[j][:, kc, :],
                                                       start=(kc == 0), stop=(kc == KC - 1)), [bw[j], bh], [pb], inc=(kc == KC - 1))
                        if ev % 2:
                            self.A(lambda h: h.copy(stg2[sj][:], ps[:, :]), [pb], [bs2[sj]])
                        else:
                            self.V(lambda h: h.tensor_copy(stg2[sj][:], ps[:, :]), [pb], [bs2[sj]])
                        self.st(self.Ptm[b, i * 128:(i + 1) * 128, ti * W:(ti + 1) * W], stg2[sj][:], [bs2[sj]], q=("sp" if sj == 0 else "act"))

    def pfm(self, b, part, r0, n=128):
        fi = FM_PARTS.index(part)
        return self.Pfm[b, fi * W + r0: fi * W + r0 + n, :]

    def ptm(self, b, part):
        ti = TM_PARTS.index(part)
        return self.Ptm[b, :, ti * W:(ti + 1) * W].rearrange("(i p) w -> p i w", p=128)

    def scan_bidir(self, out_f, out_b, a_f, x_f, a_b, x_b, R, Wf, Wb, eng="dve"):
        c = self.c
        CL, TT = c.CTXL, c.TT
        op = self.V if eng == "dve" else self.G
        op(lambda h: h.tensor_tensor_scan(out_f[:, 0:TT], a_f[:, 0:TT], x_f[:, 0:TT], 0.0, ALU.mult, ALU.add), R, [Wf])
        op(lambda h: h.tensor_tensor_scan(out_b[:, 0:CL][:, ::-1], a_b[:, 0:CL][:, ::-1], x_b[:, 0:CL][:, ::-1], 0.0, ALU.mult, ALU.add), R, [Wb])
        op(lambda h: h.tensor_tensor_scan(out_b[:, CL:TT][:, ::-1], a_b[:, CL:TT][:, ::-1], x_b[:, CL:TT][:, ::-1], out_b[:, 0:1], ALU.mult, ALU.add),
           list(R) + [Wb], [Wb])

    def phase_lru(self, l, b):
        c = self.c
        TT, CL = c.TT, c.CTXL
        tgs = tok_groups(TT)
        with ExitStack() as st:
            cw = self.sb(st, "cw", [128, 4, 4]); lv = self.sb(st, "lv", [128, 4, 7]); bp = Buf()
            self.ld(cw[:], self.convw[l], [bp]); self.ld(lv[:], self.lruv[l], [bp])
            lw = self.sb(st, "lw", [128, 2, 2, 4, 128], BF16)
            self.ldc(lw[:], self.lruw[l].rearrange("a d c p j -> p a d c j"), [bp])
            c1 = self.sb(st, "c1", [128, 4, 2]); c2 = self.sb(st, "c2", [128, 4, 2])
            self.A(lambda h: h.activation(c1[:], lv[:, :, 5:7], AF.Exp, scale=-1.0), [bp], [bp])
            self.A(lambda h: h.activation(c1[:], c1[:], AF.Ln, bias=1.0), [bp], [bp])
            self.V(lambda h: h.tensor_scalar(c2[:], c1[:], -16.0, None, ALU.mult), [bp], [bp])
            self.V(lambda h: h.tensor_scalar(c1[:], c1[:], -8.0, None, ALU.mult), [bp], [bp])
            T = lambda n, dt=F32: self.sb(st, n, [128, TT], dt)
            xr, gt, xc, xcb = T("xr"), T("gt"), T("xc"), T("xcb", BF16)
            rr, ii, aa, bb = [T("rr0"), T("rr1")], [T("ii0"), T("ii1")], [T("aa0"), T("aa1")], [T("bb0"), T("bb1")]
            hf, hb, ob = T("hf"), T("hb"), T("ob", BF16)
            bxr, bgt, bxc, bxcb, bo = Buf(), Buf(), Buf(), Buf(), Buf()
            br, bi, ba, bbb = [Buf(), Buf()], [Buf(), Buf()], [Buf(), Buf()], [Buf(), Buf()]
            bhf, bhb = Buf(), Buf()
            for cc in range(4):
                self.ld(xr[:], self.pfm(b, 1, cc * 128), [bxr])
                self.ld(gt[:], self.pfm(b, 2, cc * 128), [bgt], q="act")
                self.V(lambda h: h.tensor_scalar(xc[:], xr[:], cw[:, cc, 2:3], lv[:, cc, 0:1], ALU.mult, ALU.add), [bxr, bp], [bxc])
                for (s0, s1) in ((0, CL), (CL, TT)):
                    for k, off in ((0, -2), (1, -1), (3, 1)):
                        o0, o1 = max(s0, s0 - off), min(s1, s1 - off)
                        self.V(lambda h: h.scalar_tensor_tensor(xc[:, o0:o1], xr[:, o0 + off:o1 + off], cw[:, cc, k:k + 1], xc[:, o0:o1], ALU.mult, ALU.add),
                               [bxr, bp, bxc], [bxc])
                self.A(lambda h: h.copy(xcb[:], xc[:]), [bxc], [bxcb])
                for d in range(2):
                    for (wi, dst, bd, bias_col) in ((0, rr[d], br[d], 1 + d), (1, ii[d], bi[d], 3 + d)):
                        for gi, (t0, n) in enumerate(tgs):
                            ps, pb = self.ps[gi % 4], self.pb[gi % 4]
                            self.PE(lambda h: h.matmul(ps[:, 0:n], lw[:, wi, d, cc, :], xcb[:, t0:t0 + n], start=True, stop=True), [bp, bxcb], [pb])
                            self.A(lambda h: h.activation(dst[:, t0:t0 + n], ps[:, 0:n], AF.Sigmoid, bias=lv[:, cc, bias_col:bias_col + 1]), [pb, bp], [bd])
                    self.A(lambda h: h.activation(aa[d][:], rr[d][:], AF.Exp, scale=c1[:, cc, d:d + 1]), [br[d], bp], [ba[d]])
                    self.A(lambda h: h.activation(bb[d][:], rr[d][:], AF.Exp, scale=c2[:, cc, d:d + 1]), [br[d], bp], [bbb[d]])
                    self.A(lambda h: h.activation(bb[d][:], bb[d][:], AF.Sqrt, scale=-1.0, bias=1.0), [bbb[d]], [bbb[d]])
                    self.V(lambda h: h.tensor_mul(ii[d][:], ii[d][:], xc[:]), [bi[d], bxc], [bi[d]])
                    self.V(lambda h: h.tensor_mul(bb[d][:], bb[d][:], ii[d][:]), [bbb[d], bi[d]], [bbb[d]])
                self.scan_bidir(hf, hb, aa[0], bb[0], aa[1], bb[1], [ba[0], ba[1], bbb[0], bbb[1]], bhf, bhb)
                self.V(lambda h: h.tensor_add(hf[:], hf[:], hb[:]), [bhf, bhb], [bhf])
                self.gelu(gt[:], gt[:], hb[:], [bgt], bgt, bhb)
                self.V(lambda h: h.tensor_mul(ob[:], hf[:], gt[:]), [bhf, bgt], [bo])
                self.st(self.mixT[b, W + cc * 128: W + (cc + 1) * 128, :], ob[:], [bo])

    def sincos(self, st, ang, bang, n, name, share=None):
        T = lambda nm, dt=F32: self.sb(st, f"{name}_{nm}", [128, n], dt)
        y, yi, sn, cs = T("y"), T("yi", I32), T("sn"), T("cs")
        b = Buf(); bs = Buf(); bcs = Buf()
        for (shift, dst, bd) in ((0.0, sn, bs), (0.25, cs, bcs)):
            self.V(lambda h: h.tensor_scalar(y[:], ang[:], 1.0 / TWO_PI, shift, ALU.mult, ALU.add), [bang, b], [b])
            self.wrap_frac(y, yi, dst, b, extra=[bd])
            self.A(lambda h: h.activation(dst[:], y[:], AF.Sin, scale=TWO_PI), [b], [bd, b])
        return sn, bs, cs, bcs

    def wrap_frac(self, y, yi, yf, b, extra=()):
        Wb = [b] + list(extra)
        self.V(lambda h: h.tensor_copy(yi[:], y[:]), [b], Wb)
        self.V(lambda h: h.tensor_copy(yf[:], yi[:]), [b], Wb)
        self.V(lambda h: h.tensor_sub(y[:], y[:], yf[:]), [b], Wb)
        self.V(lambda h: h.tensor_scalar(yf[:], y[:], 0.5, -1.0, ALU.is_gt, ALU.mult), [b], Wb)
        self.V(lambda h: h.tensor_add(y[:], y[:], yf[:]), [b], Wb)
        self.V(lambda h: h.tensor_scalar(yf[:], y[:], -0.5, None, ALU.is_lt), [b], Wb)
        self.V(lambda h: h.tensor_add(y[:], y[:], yf[:]), [b], Wb)

    def gelu(self, dst, src, tmp, R, Wd, Wt):
        self.V(lambda h: h.tensor_mul(tmp, src, src), R, [Wt])
        self.V(lambda h: h.tensor_scalar(tmp, tmp, 0.044715, 1.0, ALU.mult, ALU.add), [Wt], [Wt])
        self.V(lambda h: h.tensor_mul(tmp, tmp, src), list(R) + [Wt], [Wt])
        self.A(lambda h: h.activation(tmp, tmp, AF.Sigmoid, scale=1.5957691216057308), [Wt], [Wt])
        self.V(lambda h: h.tensor_mul(dst, src, tmp), list(R) + [Wt], [Wd])

    def phase_gla(self, l, b, mixer):
        c = self.c
        TT, NT, CL = c.TT, c.NT, c.CTXL
        C = 64
        NPT = 128 // C
        NCH, NCHC = TT // C, CL // C
        cmask = self.cmask32 if C == 32 else self.cmask
        rsrc = self.rst32 if C == 32 else self.rst
        SQ = 128 ** -0.5
        with ExitStack() as st:
            T = lambda s_, n, dt=F32: self.sb(s_, n, [128, TT], dt)
            vbf = self.sb(st, "vbf", [128, NT, W], BF16); bv = Buf()
            oacc = self.sb(st, "oacc", [128, NT, W]); bo = Buf()
            vst = [self.sb(st, f"vst{i}", [128, W]) for i in range(2)]; bvs = [Buf(), Buf()]
            vsrc = self.ptm(b, 6 if mixer == 0 else 10)
            for i in range(NT):
                j = i % 2
                self.ld(vst[j][:], vsrc[:, i, :], [bvs[j]], q=("sp" if j == 0 else "act"))
                self.A(lambda h: h.activation(vbf[:, i, :], vst[j][:], AF.Silu if mixer == 0 else AF.Identity), [bvs[j]], [bv])
            bprm = Buf()
            if mixer == 0:
                lbr = self.sb(st, "lbr", [128, 2, c.L, 4]); lbe = self.sb(st, "lbe", [128, 2, c.L, 4])
                lb = self.sb(st, "lb", [128, 2, 4]); lbs = self.sb(st, "lbs", [128, 2, 4]); oml = self.sb(st, "oml", [128, 2, 4])
                self.ld(lbr[:], self.hglb.rearrange("d l p h -> p d l h"), [bprm])
                self.A(lambda h: h.activation(lbe[:], lbr[:], AF.Exp), [bprm], [bprm])
                self.V(lambda h: h.tensor_copy(lbs[:], lbe[:, :, 0, :]), [bprm], [bprm])
                for ll in range(1, c.L):
                    self.V(lambda h: h.tensor_add(lbs[:], lbs[:], lbe[:, :, ll, :]), [bprm], [bprm])
                self.V(lambda h: h.memset(lb[:], 0.0), [], [bprm])
                for ll in range(1, l + 1):
                    self.V(lambda h: h.tensor_add(lb[:], lb[:], lbe[:, :, ll, :]), [bprm], [bprm])
                self.V(lambda h: h.reciprocal(lbs[:], lbs[:]), [bprm], [bprm])
                self.V(lambda h: h.tensor_mul(lb[:], lb[:], lbs[:]), [bprm], [bprm])
                self.V(lambda h: h.tensor_scalar(oml[:], lb[:], -1.0, 1.0, ALU.mult, ALU.add), [bprm], [bprm])
            for d in range(2):
                with ExitStack() as s2:
                    rst = self.sb(s2, "rst", [128, TT + 1]); brs = Buf()
                    self.ld(rst[:], rsrc[:, :], [brs])
                    if mixer == 1:
                        ang = self.sb(s2, "ang", [128, c.SEQL]); bang = Buf()
                        self.ld(ang[:], self.rang[:, :], [bang])
                        sn, bsn, cs, bcs = self.sincos(s2, ang, bang, c.SEQL, "rot", share=ang)
                        self.V(lambda h: h.tensor_scalar(sn[0:64, :], sn[0:64, :], -1.0, None, ALU.mult), [bsn], [bsn])
                    q_, k_, z_, cum, e1 = T(s2, "q_"), T(s2, "k_"), T(s2, "z_"), T(s2, "cum"), T(s2, "e1")
                    tmp = z_ if mixer == 1 else T(s2, "tmp")
                    bq, bk, bz, bcum, be1 = (Buf() for _ in range(5))
                    btmp = bz if mixer == 1 else Buf()
                    qt = [T(s2, f"qt{h}", BF16) for h in range(4)]; kt = [T(s2, f"kt{h}", BF16) for h in range(4)]
                    bqt = [Buf() for _ in range(4)]; bkt = [Buf() for _ in range(4)]
                    dec = self.sb(s2, "dec", [128, NCH, 4]); em = self.sb(s2, "em", [128, NCH, 4]); elm = self.sb(s2, "elm", [128, NCH, 4]); bdec = Buf()
                    S_ = self.sb(s2, "S_", [128, 4, 128]); Sb = self.sb(s2, "Sb", [128, 4, 128], BF16); Ut = self.sb(s2, "Ut", [128, 4, 128]); bS, bSb, bUt = Buf(), Buf(), Buf()
                    sT = [self.sb(s2, f"sT{i}", [128, 4, C], BF16) for i in range(2)]; bsT = [Buf(), Buf()]
                    khT = [self.sb(s2, f"khT{i}", [128, 4, 128], BF16) for i in range(2)]; bkhT = [Buf(), Buf()]
                    c3 = lambda t: t[:].rearrange("p (c j) -> p c j", j=C)
                    for hd in range(4):
                        r0 = hd * 128
                        if mixer == 0:
                            self.ld(q_[:], self.pfm(b, 3, r0), [bq])
                            self.ld(z_[:], self.pfm(b, 4 + d, r0), [bz], q="act")
                            self.A(lambda h: h.activation(k_[:], z_[:], AF.Sigmoid, scale=-1.0), [bz], [bk])
                            self.A(lambda h: h.activation(z_[:], z_[:], AF.Sigmoid), [bz, bk], [bz])
                            self.A(lambda h: h.activation(z_[:], z_[:], AF.Ln, scale=oml[:, d, hd:hd + 1], bias=lb[:, d, hd:hd + 1]), [bz, bprm], [bz])
                            self.V(lambda h: h.tensor_scalar(k_[:], k_[:], oml[:, d, hd:hd + 1], None, ALU.mult), [bk, bprm], [bk])
                            gsrc, bgs = z_, bz
                            qscale, kscale = SQ, 1.0
                        else:
                            for (x_, bx_, pa, pb_) in ((q_, bq, 8, 12), (k_, bk, 9, 13)):
                                self.ld(x_[:], self.pfm(b, pa, r0), [bx_])
                                self.ld(e1[:], self.pfm(b, pb_, r0), [be1], q="act")
                                self.V(lambda h: h.tensor_mul(x_[:, CL:TT], x_[:, CL:TT], cs[:]), [bx_, bcs], [bx_])
                                self.V(lambda h: h.tensor_mul(e1[:, CL:TT], e1[:, CL:TT], sn[:]), [be1, bsn], [be1])
                                self.V(lambda h: h.tensor_add(x_[:, CL:TT], x_[:, CL:TT], e1[:, CL:TT]), [bx_, be1], [bx_])
                            gdec = math.log1p(-2.0 ** (-((5.0 if d == 0 else 5.5) + hd)))
                            gsrc, bgs = None, None
                            qscale, kscale = 1.0, SQ
                        if mixer == 1:
                            self.V(lambda h: h.memset(e1[:], gdec), [be1], [be1])
                            gsrc, bgs = e1, be1
                        if d == 0:
                            self.V(lambda h: h.tensor_tensor_scan(cum[:], rst[:, 0:TT], gsrc[:], 0.0, ALU.mult, ALU.add), [brs, bgs], [bcum])
                            mcol, lcol = C // 2 - 1, C - 1
                        else:
                            self.V(lambda h: h.tensor_tensor_scan(cum[:, ::-1], rst[:, 1:TT + 1][:, ::-1], gsrc[:, ::-1], 0.0, ALU.mult, ALU.add), [brs, bgs], [bcum])
                            mcol, lcol = C // 2, 0
                        cum3 = c3(cum)
                        mB = cum3[:, :, mcol:mcol + 1].to_broadcast([128, NCH, C])
                        self.V(lambda h: h.tensor_sub(c3(tmp), cum3, mB), [bcum, btmp], [btmp])
                        self.A(lambda h: h.activation(e1[:], tmp[:], AF.Exp), [btmp, be1], [be1])
                        self.V(lambda h: h.scalar_tensor_tensor(qt[hd][:], q_[:], qscale, e1[:], ALU.mult, ALU.mult), [bq, be1], [bqt[hd]])
                        self.A(lambda h: h.activation(e1[:], tmp[:], AF.Exp, scale=-1.0), [btmp, be1], [be1])
                        self.V(lambda h: h.scalar_tensor_tensor(kt[hd][:], k_[:], kscale, e1[:], ALU.mult, ALU.mult), [bk, be1], [bkt[hd]])
                        self.A(lambda h: h.activation(dec[:, :, hd:hd + 1], cum3[:, :, lcol:lcol + 1], AF.Exp), [bcum], [bdec])
                        self.A(lambda h: h.activation(em[:, :, hd:hd + 1], cum3[:, :, mcol:mcol + 1], AF.Exp), [bcum], [bdec])
                        self.V(lambda h: h.tensor_sub(elm[:, :, hd:hd + 1], cum3[:, :, lcol:lcol + 1], cum3[:, :, mcol:mcol + 1]), [bcum], [bdec])
                        self.A(lambda h: h.activation(elm[:, :, hd:hd + 1], elm[:, :, hd:hd + 1], AF.Exp), [bdec], [bdec])
                    self.V(lambda h: h.memset(S_[:], 0.0), [bS], [bS])
                    self.V(lambda h: h.memset(Sb[:], 0.0), [bSb], [bSb])
                    order = list(range(NCH)) if d == 0 else (list(range(NCHC - 1, -1, -1)) + list(range(NCH - 1, NCHC - 1, -1)))
                    for ci, ch in enumerate(order):
                        i, hh = ch // NPT, ch % NPT
                        P0 = C * hh
                        cs_ = slice(ch * C, ch * C + C)
                        j = ci % 2
                        pS, pO, pK, pU = self.ps[0 + j], self.ps[2 + j], self.ps[4 + j], self.ps[6 + j]
                        bpS, bpO, bpK, bpU = self.pb[0 + j], self.pb[2 + j], self.pb[4 + j], self.pb[6 + j]
                        for hd in range(4):
                            self.PE(lambda h: h.matmul(pS[P0:P0 + C, hd * C:(hd + 1) * C], kt[hd][:, cs_], qt[hd][:, cs_], start=True, stop=True),
                                    [bkt[hd], bqt[hd]], [bpS], inc=(hd == 3))
                        self.G(lambda h: h.memset(sT[j][P0:P0 + C], 0.0), [bsT[j]], [bsT[j]])
                        self.V(lambda h: h.copy_predicated(sT[j][P0:P0 + C], cmask[P0:P0 + C, d].bitcast(mybir.dt.uint32), pS[P0:P0 + C, 0:4 * C].rearrange("p (h t) -> p h t", t=C)),
                               [bpS, self.bconst, bsT[j]], [bsT[j]])
                        self.V(lambda h: h.tensor_mul(Sb[:], S_[:], em[:, ch, :].unsqueeze(2).to_broadcast([128, 4, 128])), [bS, bdec, bSb], [bSb])
                        for hd in range(4):
                            osl = pO[P0:P0 + C, hd * 128:(hd + 1) * 128]
                            self.PE(lambda h: h.matmul(osl, sT[j][P0:P0 + C, hd, :], vbf[P0:P0 + C, i, hd * 128:(hd + 1) * 128], start=True, stop=False),
                                    [bsT[j], bv], [bpO], inc=False)
                            self.PE(lambda h: h.matmul(osl, qt[hd][:, cs_], Sb[:, hd, :], start=False, stop=True), [bqt[hd], bSb], [bpO], inc=(hd == 3))
                        if d == 0:
                            self.A(lambda h: h.copy(oacc[P0:P0 + C, i, :], pO[P0:P0 + C, :]), [bpO], [bo])
                        else:
                            self.V(lambda h: h.tensor_add(oacc[P0:P0 + C, i, :], oacc[P0:P0 + C, i, :], pO[P0:P0 + C, :]), [bpO, bo], [bo])
                        pKb = pK[:].bitcast(BF16)
                        for hd in range(4):
                            self.PE(lambda h: h.transpose(pKb[P0:P0 + C, hd * 128:(hd + 1) * 128], kt[hd][:, cs_], self.idb[:]), [bkt[hd], self.bconst], [bpK], inc=(hd == 3))
                        self.A(lambda h: h.copy(khT[j][P0:P0 + C].rearrange("p h d -> p (h d)"), pKb[P0:P0 + C, 0:512]), [bpK], [bkhT[j]])
                        for hd in range(4):
                            self.PE(lambda h: h.matmul(pU[:, hd * 128:(hd + 1) * 128], khT[j][P0:P0 + C, hd, :], vbf[P0:P0 + C, i, hd * 128:(hd + 1) * 128], start=True, stop=True),
                                    [bkhT[j], bv], [bpU], inc=(hd == 3))
                        self.V(lambda h: h.tensor_mul(Ut[:], pU[:, :].rearrange("p (h d) -> p h d", d=128), elm[:, ch, :].unsqueeze(2).to_broadcast([128, 4, 128])), [bpU, bdec, bUt], [bUt])
                        self.V(lambda h: h.tensor_mul(S_[:], S_[:], dec[:, ch, :].unsqueeze(2).to_broadcast([128, 4, 128])), [bS, bdec], [bS])
                        self.V(lambda h: h.tensor_add(S_[:], S_[:], Ut[:]), [bS, bUt], [bS])
                self.S.barrier()
            with ExitStack() as s3:
                graw = self.sb(s3, "graw", [128, NT, W]); bg = Buf()
                self.ld(graw[:], self.ptm(b, 7 if mixer == 0 else 11), [bg], q="act")
                self.A(lambda h: h.activation(graw[:], graw[:], AF.Silu), [bg], [bg])
                sq = self.sb(s3, "gsq", [128, NT, W]); ssq = self.sb(s3, "ssq", [128, NT * 4]); bsq = Buf()
                o4 = oacc[:].rearrange("p i (h d) -> p (i h) d", d=128)
                self.V(lambda h: h.tensor_mul(sq[:], oacc[:], oacc[:]), [bo], [bsq])
                self.V(lambda h: h.tensor_reduce(ssq[:], sq[:].rearrange("p i (h d) -> p (i h) d", d=128), AX.X, ALU.add), [bsq], [bsq])
                self.A(lambda h: h.activation(ssq[:], ssq[:], AF.Sqrt, scale=1.0 / 128, bias=self.epsc[:, 0:1]), [bsq, self.bconst], [bsq])
                self.V(lambda h: h.reciprocal(ssq[:], ssq[:]), [bsq], [bsq])
                self.V(lambda h: h.tensor_mul(o4, o4, ssq[:].unsqueeze(2).to_broadcast([128, NT * 4, 128])), [bo, bsq], [bo])
                self.V(lambda h: h.tensor_mul(vbf[:], oacc[:], graw[:]), [bo, bg, bv], [bv])
                mst = self.sb(s3, "mst", [128, 4, TT], BF16); bm = Buf()
                for i in range(NT):
                    p, pb = self.ps[i % 4], self.pb[i % 4]
                    pv = p[:].bitcast(BF16)
                    for hd in range(4):
                        self.PE(lambda h: h.transpose(pv[:, hd * 128:(hd + 1) * 128], vbf[:, i, hd * 128:(hd + 1) * 128], self.idb[:]), [bv, self.bconst], [pb], inc=(hd == 3))
                    self.A(lambda h: h.copy(mst[:, :, i * 128:(i + 1) * 128], pv[:, 0:512].rearrange("p (h t) -> p h t", t=128)), [pb], [bm])
                base = 2 * W + mixer * W
                self.st(self.mixT[b, base:base + W, :].rearrange("(h p) t -> p h t", p=128), mst[:], [bm])
                self.S.barrier()

    def phase_s5(self, l, b):
        c = self.c
        TT, CL = c.TT, c.CTXL
        tgs = tok_groups(TT)
        with ExitStack() as st:
            T = lambda n, dt=F32: self.sb(st, n, [128, TT], dt)
            lam = self.sb(st, "lam", [128, 3, 2, 16]); bp = Buf()
            self.ld(lam[:], self.s5lam[l], [bp])
            P = lambda n: self.sb(st, n, [128, 32])
            dt_, mag, th, thc, are, aim, den, wre, wim, t1, t2 = (P(f"p{i}") for i in range(11))
            lr = lam[:, 0].rearrange("p d s -> p (d s)"); li = lam[:, 1].rearrange("p d s -> p (d s)"); ldt = lam[:, 2].rearrange("p d s -> p (d s)")
            self.A(lambda h: h.activation(dt_[:], ldt, AF.Exp), [bp], [bp])
            self.V(lambda h: h.tensor_mul(t1[:], lr, dt_[:]), [bp], [bp])
            self.A(lambda h: h.activation(mag[:], t1[:], AF.Exp), [bp], [bp])
            self.V(lambda h: h.tensor_mul(th[:], li, dt_[:]), [bp], [bp])
            sn, bsn, cs, bcs = self.sincos(st, th, bp, 32, "ab")
            self.V(lambda h: h.tensor_mul(are[:], mag[:], cs[:]), [bp, bcs], [bp])
            self.V(lambda h: h.tensor_mul(aim[:], mag[:], sn[:]), [bp, bsn], [bp])
            self.V(lambda h: h.tensor_mul(den[:], lr, lr), [bp], [bp])
            self.V(lambda h: h.tensor_mul(t1[:], li, li), [bp], [bp])
            self.V(lambda h: h.tensor_add(den[:], den[:], t1[:]), [bp], [bp])
            self.V(lambda h: h.reciprocal(den[:], den[:]), [bp], [bp])
            self.V(lambda h: h.tensor_scalar(t2[:], are[:], -1.0, None, ALU.add), [bp], [bp])
            self.V(lambda h: h.tensor_mul(wre[:], t2[:], lr), [bp], [bp])
            self.V(lambda h: h.tensor_mul(t1[:], aim[:], li), [bp], [bp])
            self.V(lambda h: h.tensor_add(wre[:], wre[:], t1[:]), [bp], [bp])
            self.V(lambda h: h.tensor_mul(wre[:], wre[:], den[:]), [bp], [bp])
            self.V(lambda h: h.tensor_mul(wim[:], aim[:], lr), [bp], [bp])
            self.V(lambda h: h.tensor_mul(t1[:], t2[:], li), [bp], [bp])
            self.V(lambda h: h.tensor_sub(wim[:], wim[:], t1[:]), [bp], [bp])
            self.V(lambda h: h.tensor_mul(wim[:], wim[:], den[:]), [bp], [bp])
            nwim = P("nwim")
            self.V(lambda h: h.tensor_scalar(nwim[:], wim[:], -1.0, None, ALU.mult), [bp], [bp])
            thi = self.sb(st, "thi", [128, 32], I32); thf = P("thf")
            self.V(lambda h: h.tensor_scalar(thc[:], th[:], 1.0 / TWO_PI, None, ALU.mult), [bp], [bp])
            self.wrap_frac(thc, thi, thf, bp)
            dsk = self.sb(st, "dsk", [128, 4]); self.ld(dsk[:], self.s5d[l], [bp])
            with ExitStack() as s2:
                T = lambda n, dt=F32: self.sb(s2, n, [128, TT], dt)
                kid = [T("kid0"), T("kid1")]; bkid = Buf()
                self.ld(kid[0][:], self.kidx[0], [bkid]); self.ld(kid[1][:], self.kidx[1], [bkid], q="act")
                ub = self.sb(s2, "ub", [128, 4, TT], BF16); bu = Buf()
                self.ldc(ub[:], self.Pfm[b, 0:W, :].rearrange("(c p) t -> p c t", p=128), [bu])
                uf = T("uf"); buf_ = Buf()
                Bt = [self.sb(s2, f"Bt{i}", [128, 2, 128], BF16) for i in range(2)]; bB = [Buf(), Buf()]
                Cf = [self.sb(s2, f"Cf{i}", [128, 2, 128]) for i in range(2)]; bC = [Buf(), Buf()]
                Cw = [self.sb(s2, f"Cw{i}", [128, 2, 128], BF16) for i in range(2)]; bCw = [Buf(), Buf()]
                ct = self.sb(s2, "ct", [128, 128]); bct = Buf()
                y, yi = T("ty"), T("tyi", I32)
                sn_, cs_ = T("tsn"), T("tcs"); by, bsn_, bcs_ = Buf(), Buf(), Buf()
                bur, bui, gre, gim, t3 = T("bur"), T("bui"), T("gre"), T("gim"), T("t3")
                bbur, bbui, bgre, bgim, bt3 = (Buf() for _ in range(5))
                hre = [T(f"hre{i}", BF16) for i in range(2)]; him = [T(f"him{i}", BF16) for i in range(2)]
                bhre = [Buf(), Buf()]; bhim = [Buf(), Buf()]
                ytmp = T("ytmp"); bytmp = Buf()
                yo = T("yo"); byo = Buf()
                k = 0
                for cc in range(4):
                    self.ld(uf[:], self.Pfm[b, cc * 128:(cc + 1) * 128, :], [buf_])
                    pY = [self.ps[4 + g] for g in range(4)]; bpY = [self.pb[4 + g] for g in range(4)]
                    first = True
                    for d in range(2):
                        for s4 in range(4):
                            sc = cc * 4 + s4
                            col = d * 16 + sc
                            j = k % 2; k += 1
                            lastone = (d == 1 and s4 == 3)
                            self.ldc(Bt[j][:], self.s5B[l, d, sc], [bB[j]])
                            self.ld(Cf[j][:], self.s5C[l, d, sc], [bC[j]], q="act")
                            self.V(lambda h: h.tensor_scalar(ct[:], Cf[j][:, 1, :], nwim[:, col:col + 1], None, ALU.mult), [bC[j], bp, bct], [bct])
                            self.V(lambda h: h.scalar_tensor_tensor(Cw[j][:, 0, :], Cf[j][:, 0, :], wre[:, col:col + 1], ct[:], ALU.mult, ALU.add), [bC[j], bp, bct], [bCw[j]])
                            self.V(lambda h: h.tensor_scalar(ct[:], Cf[j][:, 1, :], wre[:, col:col + 1], -1.0, ALU.mult, ALU.mult), [bC[j], bp, bct], [bct])
                            self.V(lambda h: h.scalar_tensor_tensor(Cw[j][:, 1, :], Cf[j][:, 0, :], nwim[:, col:col + 1], ct[:], ALU.mult, ALU.add), [bC[j], bp, bct], [bCw[j]])
                            for (shift, dst, bd) in ((0.0, sn_, bsn_), (0.25, cs_, bcs_)):
                                self.V(lambda h: h.tensor_scalar(y[:], kid[d][:], thc[:, col:col + 1], shift, ALU.mult, ALU.add), [bkid, bp, by], [by])
                                self.wrap_frac(y, yi, dst, by, extra=[bd])
                                self.A(lambda h: h.activation(dst[:], y[:], AF.Sin, scale=TWO_PI), [by], [bd, by])
                            for (ri, dst, bd) in ((0, bur, bbur), (1, bui, bbui)):
                                for gi, (t0, n) in enumerate(tgs):
                                    ps, pb = self.ps[gi % 4], self.pb[gi % 4]
                                    self.PE(lambda h: h.matmul(ps[:, 0:n], Bt[j][:, ri, :], ub[:, cc, t0:t0 + n], start=True, stop=True), [bB[j], bu], [pb])
                                    self.A(lambda h: h.copy(dst[:, t0:t0 + n], ps[:, 0:n]), [pb], [bd])
                            self.V(lambda h: h.tensor_mul(gre[:], bur[:], cs_[:]), [bbur, bcs_], [bgre])
                            self.V(lambda h: h.tensor_mul(t3[:], bui[:], sn_[:]), [bbui, bsn_], [bt3])
                            self.V(lambda h: h.tensor_add(gre[:], gre[:], t3[:]), [bgre, bt3], [bgre])
                            self.V(lambda h: h.tensor_mul(gim[:], bui[:], cs_[:]), [bbui, bcs_], [bgim])
                            self.V(lambda h: h.tensor_mul(t3[:], bur[:], sn_[:]), [bbur, bsn_], [bt3])
                            self.V(lambda h: h.tensor_sub(gim[:], gim[:], t3[:]), [bgim, bt3], [bgim])
                            for (src, dst, bs_, bd) in ((gre, bur, bgre, bbur), (gim, bui, bgim, bbui)):
                                if d == 0:
                                    self.V(lambda h: h.tensor_tensor_scan(dst[:], mag[:, col:col + 1].to_broadcast([128, TT]), src[:], 0.0, ALU.mult, ALU.add), [bs_, bp], [bd])
                                else:
                                    mC = mag[:, col:col + 1].to_broadcast([128, CL]); mL = mag[:, col:col + 1].to_broadcast([128, TT - CL])
                                    self.V(lambda h: h.tensor_tensor_scan(dst[:, 0:CL][:, ::-1], mC, src[:, 0:CL][:, ::-1], 0.0, ALU.mult, ALU.add), [bs_, bp], [bd])
                                    self.V(lambda h: h.tensor_tensor_scan(dst[:, CL:TT][:, ::-1], mL, src[:, CL:TT][:, ::-1], dst[:, 0:1], ALU.mult, ALU.add), [bs_, bp, bd], [bd])
                            self.V(lambda h: h.tensor_mul(gre[:], bur[:], cs_[:]), [bbur, bcs_], [bgre])
                            self.V(lambda h: h.tensor_mul(t3[:], bui[:], sn_[:]), [bbui, bsn_], [bt3])
                            self.V(lambda h: h.tensor_sub(hre[j][:], gre[:], t3[:]), [bgre, bt3], [bhre[j]])
                            self.V(lambda h: h.tensor_mul(gim[:], bur[:], sn_[:]), [bbur, bsn_], [bgim])
                            self.V(lambda h: h.tensor_mul(t3[:], bui[:], cs_[:]), [bbui, bcs_], [bt3])
                            self.V(lambda h: h.tensor_add(him[j][:], gim[:], t3[:]), [bgim, bt3], [bhim[j]])
                            for gi, (t0, n) in enumerate(tgs):
                                if gi < 4:
                                    self.PE(lambda h: h.matmul(pY[gi][:, 0:n], Cw[j][:, 0, :], hre[j][:, t0:t0 + n], start=first, stop=False), [bCw[j], bhre[j]], [bpY[gi]], inc=False)
                                    self.PE(lambda h: h.matmul(pY[gi][:, 0:n], Cw[j][:, 1, :], him[j][:, t0:t0 + n], start=False, stop=lastone), [bCw[j], bhim[j]], [bpY[gi]])
                                else:
                                    ps, pb = self.ps[gi % 4], self.pb[gi % 4]
                                    self.PE(lambda h: h.matmul(ps[:, 0:n], Cw[j][:, 0, :], hre[j][:, t0:t0 + n], start=True, stop=False), [bCw[j], bhre[j]], [pb], inc=False)
                                    self.PE(lambda h: h.matmul(ps[:, 0:n], Cw[j][:, 1, :], him[j][:, t0:t0 + n], start=False, stop=True), [bCw[j], bhim[j]], [pb])
                                    if first:
                                        self.V(lambda h: h.tensor_copy(ytmp[:, t0:t0 + n], ps[:, 0:n]), [pb], [bytmp])
                                    else:
                                        self.V(lambda h: h.tensor_add(ytmp[:, t0:t0 + n], ytmp[:, t0:t0 + n], ps[:, 0:n]), [pb, bytmp], [bytmp])
                            first = False
                    for gi, (t0, n) in enumerate(tgs):
                        if gi < 4:
                            self.V(lambda h: h.scalar_tensor_tensor(yo[:, t0:t0 + n], uf[:, t0:t0 + n], dsk[:, cc:cc + 1], pY[gi][:, 0:n], ALU.mult, ALU.add), [buf_, bp, bpY[gi]], [byo])
                        else:
                            self.V(lambda h: h.scalar_tensor_tensor(yo[:, t0:t0 + n], uf[:, t0:t0 + n], dsk[:, cc:cc + 1], ytmp[:, t0:t0 + n], ALU.mult, ALU.add), [buf_, bp, bytmp], [byo])
                    self.gelu(yo[:], yo[:], t3[:], [byo], byo, bt3)
                    self.st(self.ygD[b, cc * 128:(cc + 1) * 128, :], yo[:], [byo])
                self.S.barrier()
            with ExitStack() as s3:
                T = lambda n, dt=F32: self.sb(s3, n, [128, TT], dt)
                yg = self.sb(s3, "yg", [128, 4, TT]); ygb = self.sb(s3, "ygb", [128, 4, TT], BF16); byg = Buf()
                self.ld(yg[:], self.ygD[b].rearrange("(c p) t -> p c t", p=128), [byg])
                self.A(lambda h: h.copy(ygb[:], yg[:]), [byg], [byg])
                gw = self.sb(s3, "gw", [128, 4, W], BF16); gb = self.sb(s3, "gb", [128, 4]); bgw = Buf()
                self.ldc(gw[:], self.gluw[l].rearrange("(kc kp) n -> kp kc n", kp=128), [bgw])
                self.ld(gb[:], self.glub[l], [bgw])
                ao = [T("ao0", BF16), T("ao1", BF16)]; bao = [Buf(), Buf()]
                sg = T("sg"); bsg = Buf()
                for oc in range(4):
                    j = oc % 2
                    for gi, (t0, n) in enumerate(tgs):
                        ps, pb = self.ps[gi % 4], self.pb[gi % 4]
                        for kc in range(4):
                            self.PE(lambda h: h.matmul(ps[:, 0:n], gw[:, kc, oc * 128:(oc + 1) * 128], ygb[:, kc, t0:t0 + n], start=(kc == 0), stop=(kc == 3)), [bgw, byg], [pb], inc=(kc == 3))
                        self.A(lambda h: h.activation(sg[:, t0:t0 + n], ps[:, 0:n], AF.Sigmoid, bias=gb[:, oc:oc + 1]), [pb, bgw], [bsg])
                    self.V(lambda h: h.tensor_mul(ao[j][:], yg[:, oc, :], sg[:]), [byg, bsg], [bao[j]])
                    self.st(self.mixT[b, oc * 128:(oc + 1) * 128, :], ao[j][:], [bao[j]])
                self.S.barrier()

    def phase_out(self, l, b, xsrc, last):
        c = self.c
        TT, NT = c.TT, c.NT
        with ExitStack() as st:
            wo = self.sb(st, "wo", [128, KC, D], BF16); bwo = Buf()
            wsrc = self.w_out[l].rearrange("(kc kp) n -> kp kc n", kp=128)
            for g in range(4):
                self.ldc(wo[:, :, g * 512:(g + 1) * 512], wsrc[:, :, g * 512:(g + 1) * 512], [bwo])
            gB = {}
            for src in ([b] if last else [b, 2]):
                gB[src] = self.bcast_row(st, 0, src, f"gmsa{src}")
            A2 = {}; B2 = {}
            if c.sparse:
                for src in ([b] if last else [b, 2]):
                    A2[src] = self.bcast_row(st, 2, src, f"a2r{src}")
                    B2[src] = self.bcast_row(st, 3, src, f"b2r{src}")
            htk = [self.sb(st, f"htk{i}", [128, D], BF16) for i in range(2)]; bhtk = [Buf(), Buf()]
            htf = self.sb(st, "htf", [128, D]); bhtf = Buf()
            s01 = [self.sb(st, f"s01{i}", [128, NE]) for i in range(2)]; bs01 = [Buf(), Buf()]
            rk = [self.sb(st, f"rk{i}", [128, NE]) for i in range(2)]; brk = [Buf(), Buf()]
            rwt = self.sb(st, "rwt", [128, KC, NE]); rb = self.sb(st, "rb", [128, NE]); brw = Buf()
            self.ld(rwt[:], self.rw[:, :, :], [brw])
            self.ld(rb[:], self.rbias[0:1, :].partition_broadcast(128)[:, 0, :], [brw])
            nt = self.norm_tiles(st); nt["xsf"] = self.sb(st, "xsf", [128, D])
            mt = [self.sb(st, f"mt{i}", [128, KC, 128], BF16) for i in range(2)]; bmt = [Buf(), Buf()]
            xt = [self.sb(st, f"xo{i}", [128, D]) for i in range(2)]; bx = [Buf(), Buf()]
            h2f = self.sb(st, "h2f", [128, KC, 128]); h2b = [self.sb(st, f"h2b{i}", [128, KC, 128], BF16) for i in range(2)]
            bh2f = Buf(); bh2b = [Buf(), Buf()]
            R = lambda n, w=NE: self.sb(st, n, [128, w])
            scr, bia, m1, m2, gs, gmx, ing, sel, tmp4, tmp16, gsum = R("scr"), R("bia"), R("m1", 4), R("m2", 4), R("gs", 4), R("gmx", 1), R("ing", 4), R("sel"), R("tmp4", 4), R("tmp16"), R("gsum", 1)
            gout = [R("gout0"), R("gout1")]; bgo = [Buf(), Buf()]
            br_ = Buf()
            tmo = [self.sb(st, f"tmo{i}", [128, 512]) for i in range(2)]; btmo = [Buf(), Buf()]
            tiles = range(c.NTC, NT) if last else range(NT)
            for i in tiles:
                j = i % 2
                src = 2 if i < c.NTC else b
                self.ld(mt[j][:], self.mixT[b, :, i * 128:(i + 1) * 128].rearrange("(kc kp) t -> kp kc t", kp=128), [bmt[j]], q="act")
                self.ld(xt[j][:], xsrc[b, i * 128:(i + 1) * 128, :], [bx[j]])
                for g in range(4):
                    ps, pb = self.ps[g], self.pb[g]
                    for kc in range(KC):
                        self.PE(lambda h: h.matmul(ps[:, :], mt[j][:, kc, :], wo[:, kc, g * 512:(g + 1) * 512], start=(kc == 0), stop=(kc == KC - 1)), [bmt[j], bwo], [pb], inc=(kc == KC - 1))
                    gt_, bgt_ = gB[src]
                    sl = slice(g * 512, (g + 1) * 512)
                    self.V(lambda h: h.tensor_tensor(tmo[g % 2][:], ps[:, :], gt_[:, sl], ALU.mult), [pb, bgt_], [btmo[g % 2]])
                    self.V(lambda h: h.tensor_add(xt[j][:, sl], xt[j][:, sl], tmo[g % 2][:]), [btmo[g % 2], bx[j]], [bx[j]])
                self.st(self.xres[b, i * 128:(i + 1) * 128, :], xt[j][:], [bx[j]])
                self.norm_T(nt, xt[j], bx[j], 1, src, lambda kc: h2f[:, kc, :], bh2f, fp32=True)
                if not c.sparse:
                    self.A(lambda h: h.copy(h2b[j][:], h2f[:]), [bh2f], [bh2b[j]])
                    self.st(self.h2T[b, :, i * 128:(i + 1) * 128].rearrange("(kc kp) t -> kp kc t", kp=128), h2b[j][:], [bh2b[j]], q="act")
                else:
                    xsf = nt["xsf"]
                    self.V(lambda h: h.tensor_mul(htf[:], xsf[:], A2[src][0][:]), [nt["bxs"], A2[src][1], bhtf], [bhtf])
                    self.V(lambda h: h.tensor_add(htk[j][:], htf[:], B2[src][0][:]), [bhtf, B2[src][1]], [bhtk[j]])
                    self.st(self.h2tok[b * TT + i * 128: b * TT + (i + 1) * 128, :], htk[j][:], [bhtk[j]], q="act")
                pr, bpr = self.ps[4], self.pb[4]
                for kc in range(KC):
                    self.PE(lambda h: h.matmul(pr[:, 0:NE], h2f[:, kc, :], rwt[:, kc, :], start=(kc == 0), stop=(kc == KC - 1)), [bh2f, brw], [bpr], inc=(kc == KC - 1))
                self.A(lambda h: h.activation(scr[:], pr[:, 0:NE], AF.Sigmoid), [bpr], [br_])
                self.V(lambda h: h.tensor_add(bia[:], scr[:], rb[:]), [br_, brw], [br_])
                b4 = bia[:].rearrange("p (g e) -> p g e", e=4)
                self.V(lambda h: h.tensor_reduce(m1[:], b4, AX.X, ALU.max), [br_], [br_])
                self.V(lambda h: h.tensor_tensor(tmp16[:].rearrange("p (g e) -> p g e", e=4), b4, m1[:].unsqueeze(2).to_broadcast([128, 4, 4]), ALU.is_equal), [br_], [br_])
                self.V(lambda h: h.scalar_tensor_tensor(tmp16[:], tmp16[:], -1e9, bia[:], ALU.mult, ALU.add), [br_], [br_])
                self.V(lambda h: h.tensor_reduce(m2[:], tmp16[:].rearrange("p (g e) -> p g e", e=4), AX.X, ALU.max), [br_], [br_])
                self.V(lambda h: h.tensor_add(gs[:], m1[:], m2[:]), [br_], [br_])
                self.V(lambda h: h.tensor_reduce(gmx[:], gs[:], AX.X, ALU.max), [br_], [br_])
                self.V(lambda h: h.tensor_tensor(ing[:], gs[:], gmx[:].to_broadcast([128, 4]), ALU.is_equal), [br_], [br_])
                self.V(lambda h: h.tensor_tensor(sel[:].rearrange("p (g e) -> p g e", e=4), b4, m2[:].unsqueeze(2).to_broadcast([128, 4, 4]), ALU.is_ge), [br_], [br_])
                self.V(lambda h: h.tensor_mul(sel[:].rearrange("p (g e) -> p g e", e=4), sel[:].rearrange("p (g e) -> p g e", e=4), ing[:].unsqueeze(2).to_broadcast([128, 4, 4])), [br_], [br_])
                if c.sparse:
                    self.V(lambda h: h.tensor_copy(s01[j][:], sel[:]), [br_], [bs01[j]])
                    pk, bpk = self.ps[5], self.pb[5]
                    self.PE(lambda h: h.matmul(pk[:, 0:NE], self.ltt[:, 0, :], s01[j][:], start=True, stop=True), [bs01[j], self.bconst], [bpk], inc=False)
                    self.PE(lambda h: h.matmul(pk[:, NE:2 * NE], self.ltt[:, 1, :], s01[j][:], start=True, stop=True), [bs01[j], self.bconst], [bpk])
                    self.V(lambda h: h.tensor_add(rk[j][:], pk[:, 0:NE], self.run[:]), [bpk, self.brun], [brk[j]])
                    self.V(lambda h: h.tensor_add(self.run[:], self.run[:], pk[:, NE:2 * NE]), [bpk, self.brun], [self.brun])
                    self.st(self.selD[b * TT + i * 128: b * TT + (i + 1) * 128, :], s01[j][:], [bs01[j]])
                    self.st(self.rankD[b * TT + i * 128: b * TT + (i + 1) * 128, :], rk[j][:], [brk[j]], q="act")
                self.V(lambda h: h.tensor_mul(sel[:], sel[:], scr[:]), [br_], [br_])
                self.V(lambda h: h.tensor_reduce(gsum[:], sel[:], AX.X, ALU.add), [br_], [br_])
                self.V(lambda h: h.reciprocal(gsum[:], gsum[:]), [br_], [br_])
                self.V(lambda h: h.tensor_scalar(gout[j][:], sel[:], gsum[:, 0:1], None, ALU.mult), [br_], [bgo[j]])
                self.st(self.gates[b, i * 128:(i + 1) * 128, :], gout[j][:], [bgo[j]], q="act")

    def phase_moe(self, l, b, last):
        c = self.c
        NT = c.NT
        tiles = list(range(c.NTC, NT)) if last else list(range(NT))
        STM = 6
        supers = [tiles[i:i + STM] for i in range(0, len(tiles), STM)]
        with ExitStack() as st:
            h2 = self.sb(st, "h2s", [128, KC, STM * 128], BF16); bh2 = Buf()
            gt = self.sb(st, "gts", [128, STM, NE]); bgt = Buf()
            yacc = self.sb(st, "yacc", [128, STM, D]); bya = Buf()
            he = self.sb(st, "he", [128, 8, STM * 128], BF16); bhe = Buf()
            wdn = [self.sb(st, f"wdn{i}", [128, 8, D], BF16) for i in range(2)]; bwd = [Buf(), Buf()]
            wgu = [self.sb(st, f"wgu{i}", [128, 2, KC, 128], BF16) for i in range(2)]; bwgu = [Buf() for _ in range(2)]
            sg = self.sb(st, "sgm", [128, STM * 128]); bsg = Buf()
            gB = {}
            for src in ([b] if last else [b, 2]):
                gB[src] = self.bcast_row(st, 1, src, f"gmlp{src}")
            xt = [self.sb(st, "xm0", [128, D])] * 2; bx = [Buf()] * 2
            ew = 0
            for sup in supers:
                n_t = len(sup); ntok = n_t * 128
                t0 = sup[0] * 128
                self.ld(h2[:, :, 0:ntok], self.h2T[b, :, t0:t0 + ntok].rearrange("(kc kp) t -> kp kc t", kp=128), [bh2])
                self.ld(gt[:, 0:n_t, :], self.gates[b, t0:t0 + ntok, :].rearrange("(i p) e -> p i e", p=128), [bgt], q="act")
                tg = tok_groups(ntok)
                for e in range(NE):
                    jd = e % 2
                    wds = self.wd[l, e].rearrange("(fc fp) n -> fp fc n", fp=128)
                    for g in range(2):
                        self.ldc(wdn[jd][:, :, g * 1024:(g + 1) * 1024], wds[:, :, g * 1024:(g + 1) * 1024], [bwd[jd]])
                    for fc in range(8):
                        jw = ew % 2; ew += 1
                        self.ldc(wgu[jw][:, 0], self.wg[l, e][:, fc * 128:(fc + 1) * 128].rearrange("(kc kp) f -> kp kc f", kp=128), [bwgu[jw]])
                        self.ldc(wgu[jw][:, 1], self.wu[l, e][:, fc * 128:(fc + 1) * 128].rearrange("(kc kp) f -> kp kc f", kp=128), [bwgu[jw]])
                        for gi, (s0, n) in enumerate(tg):
                            pG, pU = self.ps[gi], self.ps[2 + gi]; bpG, bpU = self.pb[gi], self.pb[2 + gi]
                            for kc in range(KC):
                                self.PE(lambda h: h.matmul(pG[:, 0:n], wgu[jw][:, 0, kc, :], h2[:, kc, s0:s0 + n], start=(kc == 0), stop=(kc == KC - 1)), [bwgu[jw], bh2], [bpG], inc=(kc == KC - 1))
                            for kc in range(KC):
                                self.PE(lambda h: h.matmul(pU[:, 0:n], wgu[jw][:, 1, kc, :], h2[:, kc, s0:s0 + n], start=(kc == 0), stop=(kc == KC - 1)), [bwgu[jw], bh2], [bpU], inc=(kc == KC - 1))
                            self.A(lambda h: h.activation(sg[:, s0:s0 + n], pG[:, 0:n], AF.Silu), [bpG], [bsg])
                            self.V(lambda h: h.tensor_mul(he[:, fc, s0:s0 + n], sg[:, s0:s0 + n], pU[:, 0:n]), [bsg, bpU], [bhe])
                    for ti in range(n_t):
                        for g in range(4):
                            ps, pb = self.ps[4 + g], self.pb[4 + g]
                            for fc in range(8):
                                self.PE(lambda h: h.matmul(ps[:, :], he[:, fc, ti * 128:(ti + 1) * 128], wdn[jd][:, fc, g * 512:(g + 1) * 512], start=(fc == 0), stop=(fc == 7)), [bhe, bwd[jd]], [pb], inc=(fc == 7))
                            ysl = yacc[:, ti, g * 512:(g + 1) * 512]
                            if e == 0:
                                self.V(lambda h: h.tensor_scalar(ysl, ps[:, :], gt[:, ti, e:e + 1], None, ALU.mult), [pb, bgt], [bya])
                            else:
                                self.V(lambda h: h.scalar_tensor_tensor(ysl, ps[:, :], gt[:, ti, e:e + 1], ysl, ALU.mult, ALU.add), [pb, bgt, bya], [bya])
                for ti, i in enumerate(sup):
                    j = i % 2
                    src = 2 if i < c.NTC else b
                    gt_, bgt_ = gB[src]
                    self.ld(xt[j][:], self.xres[b, i * 128:(i + 1) * 128, :], [bx[j]])
                    self.V(lambda h: h.tensor_mul(yacc[:, ti, :], yacc[:, ti, :], gt_[:]), [bya, bgt_], [bya])
                    self.V(lambda h: h.tensor_add(xt[j][:], xt[j][:], yacc[:, ti, :]), [bya, bx[j]], [bx[j]])
                    self.st(self.xres[b, i * 128:(i + 1) * 128, :], xt[j][:], [bx[j]])


    def moe_tiles(self, last):
        c = self.c
        tl = range(c.NTC, c.NT) if last else range(c.NT)
        return [(b, i) for b in range(c.NB) for i in tl]

    def n_slots(self, last):
        c = self.c
        return (2 * len(self.moe_tiles(last)) * 128 + c.SLOT - 1) // c.SLOT + NE

    def phase_route(self, l, last):
        c = self.c
        TT = c.TT
        NS = self.n_slots(last)
        SL = float(c.SLOT)
        with ExitStack() as st:
            R = lambda n, w=NE, dt=F32: self.sb(st, n, [128, w], dt)
            x, xi, xf, nsl, cum, base, one = R("rx"), R("rxi", NE, I32), R("rxf"), R("nsl"), R("cum"), R("base"), R("one")
            bq = Buf()
            self.V(lambda h: h.tensor_scalar(x[:], self.run[:], SL - 1.0, 1.0 / SL, ALU.add, ALU.mult), [self.brun], [bq])
            self.V(lambda h: h.tensor_copy(xi[:], x[:]), [bq], [bq])
            self.V(lambda h: h.tensor_copy(xf[:], xi[:]), [bq], [bq])
            self.V(lambda h: h.tensor_tensor(nsl[:], xf[:], x[:], ALU.is_gt), [bq], [bq])
            self.V(lambda h: h.tensor_sub(nsl[:], xf[:], nsl[:]), [bq], [bq])
            self.V(lambda h: h.memset(one[:], 1.0), [], [bq])
            self.V(lambda h: h.tensor_tensor_scan(cum[:], one[:], nsl[:], 0.0, ALU.mult, ALU.add), [bq], [bq])
            self.V(lambda h: h.tensor_sub(base[:], cum[:], nsl[:]), [bq], [bq])
            self.V(lambda h: h.tensor_scalar(base[:], base[:], SL, None, ALU.mult), [bq], [bq])
            sio = R("sio", NS); ge = self.sb(st, "ge", [128, NS, NE]); es = R("es", NS)
            self.ld(sio[:], self.siota[:, 0:NS], [bq])
            self.V(lambda h: h.tensor_tensor(ge[:], sio[:].unsqueeze(2).to_broadcast([128, NS, NE]), cum[:].unsqueeze(1).to_broadcast([128, NS, NE]), ALU.is_ge), [bq], [bq])
            self.V(lambda h: h.tensor_reduce(es[:], ge[:], AX.X, ALU.add), [bq], [bq])
            self.V(lambda h: h.tensor_scalar(es[:], es[:], float(NE - 1), None, ALU.min), [bq], [bq])
            self.V(lambda h: h.tensor_scalar(self.es2[:, 0, 0:NS], es[:], float(D), None, ALU.mult), [bq], [self.bes])
            self.V(lambda h: h.tensor_scalar(self.es2[:, 1, 0:NS], es[:], float(DFF), None, ALU.mult), [bq], [self.bes])
            zg = R("zg", NS * c.SLOT // 128); bgs = Buf()
            self.V(lambda h: h.memset(zg[:], 0.0), [], [bq])
            self.st(self.gsD[0:NS * c.SLOT, :].rearrange("(p r) o -> p (r o)", p=128), zg[:], [bq])
            self.S.barrier()
            sl = [R("sl0"), R("sl1")]; gl = [R("gl0"), R("gl1")]; rl = [R("rl0"), R("rl1")]; bl = [Buf(), Buf()]
            pos, pm, t1, eq = R("pos"), R("pm"), R("t1"), R("eq")
            pp = R("pp", 2); gg = [R("gg0", 2), R("gg1", 2)]; bgg = [Buf(), Buf()]; gsum = R("gsm", 1)
            ht = [self.sb(st, f"rht{i}", [128, D], BF16) for i in range(2)]; bht = [Buf(), Buf()]
            bw_ = Buf()
            for ti, (b, i) in enumerate(self.moe_tiles(last)):
                j = ti % 2
                r0 = b * TT + i * 128
                self.ld(sl[j][:], self.selD[r0:r0 + 128, :], [bl[j]])
                self.ld(gl[j][:], self.gates[b, i * 128:(i + 1) * 128, :], [bl[j]], q="act")
                self.ld(rl[j][:], self.rankD[r0:r0 + 128, :], [bl[j]])
                self.ld(ht[j][:], self.h2tok[r0:r0 + 128, :], [bht[j]], q="act")
                self.V(lambda h: h.tensor_add(pos[:], rl[j][:], base[:]), [bl[j], bq], [bw_])
                self.V(lambda h: h.tensor_scalar(t1[:], sl[j][:], -1e6, 1e6, ALU.mult, ALU.add), [bl[j]], [bw_])
                self.V(lambda h: h.tensor_mul(pm[:], pos[:], sl[j][:]), [bw_, bl[j]], [bw_])
                self.V(lambda h: h.tensor_add(t1[:], t1[:], pm[:]), [bw_], [bw_])
                self.V(lambda h: h.tensor_reduce(pp[:, 0:1], t1[:], AX.X, ALU.min), [bw_], [bw_])
                self.V(lambda h: h.tensor_reduce(pp[:, 1:2], pm[:], AX.X, ALU.max), [bw_], [bw_])
                self.V(lambda h: h.tensor_copy(self.pidx[:, ti, :], pp[:]), [bw_], [self.bpidx])
                self.V(lambda h: h.tensor_scalar(eq[:], t1[:], pp[:, 0:1], None, ALU.is_equal), [bw_], [bw_])
                self.V(lambda h: h.tensor_mul(eq[:], eq[:], gl[j][:]), [bw_, bl[j]], [bw_])
                self.V(lambda h: h.tensor_reduce(gg[j][:, 0:1], eq[:], AX.X, ALU.add), [bw_, bgg[j]], [bgg[j]])
                self.V(lambda h: h.tensor_reduce(gsum[:], gl[j][:], AX.X, ALU.add), [bl[j]], [bw_])
                self.V(lambda h: h.tensor_sub(gg[j][:, 1:2], gsum[:], gg[j][:, 0:1]), [bw_, bgg[j]], [bgg[j]])
                for k in range(2):
                    self.S.idma(reads=[bht[j], self.bpidx], writes=[], out=self.hs[:, :], out_offset=bass.IndirectOffsetOnAxis(ap=self.pidx[:, ti, k:k + 1], axis=0),
                                in_=ht[j][:, :], in_offset=None)
                    self.S.idma(reads=[bgg[j], self.bpidx], writes=[], out=self.gsD[:, :], out_offset=bass.IndirectOffsetOnAxis(ap=self.pidx[:, ti, k:k + 1], axis=0),
                                in_=gg[j][:, k:k + 1], in_offset=None)

    def phase_moe_sparse(self, l, last):
        c = self.c
        NS = self.n_slots(last)
        SLT = c.SLOT // 128
        wgf = self.wg.rearrange("l e k (h f) -> (l e k h) f", h=2); wuf = self.wu.rearrange("l e k (h f) -> (l e k h) f", h=2)
        wdf = self.wd.rearrange("l e f n -> (l e f) n")
        with ExitStack() as st:
            wg_ = [self.sb(st, f"swg{i}", [128, KC, 512], BF16) for i in range(2)]
            wu_ = [self.sb(st, f"swu{i}", [128, KC, 512], BF16) for i in range(2)]
            wd_ = [self.sb(st, f"swd{i}", [128, 4, D], BF16) for i in range(2)]
            bwg, bwu, bwd = [Buf(), Buf()], [Buf(), Buf()], [Buf(), Buf()]
            h2s = self.sb(st, "h2s", [128, KC, c.SLOT], BF16); bh2 = Buf()
            hr = [self.sb(st, f"hr{i}", [128, D], BF16) for i in range(2)]; bhr = [Buf(), Buf()]
            he = [self.sb(st, f"she{i}", [128, 4, c.SLOT], BF16) for i in range(2)]; bhe = [Buf(), Buf()]
            ys = self.sb(st, "ys", [128, SLT, D]); bys = Buf()
            sg = self.sb(st, "ssg", [128, c.SLOT]); bsg = Buf()
            gs = [self.sb(st, f"sgs{i}", [128, SLT]) for i in range(2)]; bgs = [Buf(), Buf()]
            wi = [self.sb(st, f"swi{i}", [128, 40], I32) for i in range(2)]; bwi = [Buf(), Buf()]
            wif = self.sb(st, "swif", [128, 16]); bwif = Buf()
            hh = 0
            for s_ in range(NS):
                js = s_ % 2
                self.V(lambda h: h.tensor_scalar(wif[:], self.kiot[:, 0:16], self.es2[:, 0, s_:s_ + 1], float(l * NE * D), ALU.add, ALU.add), [self.bes, self.bconst, bwif], [bwif])
                for hf in range(2):
                    self.V(lambda h: h.tensor_scalar(wi[js][:, hf * 16:(hf + 1) * 16], wif[:], 2.0, float(hf), ALU.mult, ALU.add), [bwif, bwi[js]], [bwi[js]])
                self.V(lambda h: h.tensor_scalar(wi[js][:, 32:40], self.kiot[:, 16:24], self.es2[:, 1, s_:s_ + 1], float(l * NE * DFF), ALU.add, ALU.add), [self.bes, self.bconst, bwi[js]], [bwi[js]])
                self.S.dma("act", gs[js][:], self.gsD[s_ * c.SLOT:(s_ + 1) * c.SLOT, :].rearrange("(t p) o -> p (t o)", p=128), writes=[bgs[js]], allow_slow_non_contiguous=True)
                for t in range(SLT):
                    jr = t % 2
                    self.ld(hr[jr][:], self.hs[s_ * c.SLOT + t * 128: s_ * c.SLOT + (t + 1) * 128, :], [bhr[jr]], q=("sp" if jr == 0 else "act"))
                    for g in range(2):
                        p, pb = self.ps[4 + g], self.pb[4 + g]
                        pv = p[:].bitcast(BF16)
                        for k8 in range(8):
                            kc = g * 8 + k8
                            self.PE(lambda h: h.transpose(pv[:, k8 * 128:(k8 + 1) * 128], hr[jr][:, kc * 128:(kc + 1) * 128], self.idb[:]), [bhr[jr], self.bconst], [pb], inc=(k8 == 7))
                        dst = h2s[:, g * 8:(g + 1) * 8, t * 128:(t + 1) * 128]
                        if g == 0:
                            self.A(lambda h: h.copy(dst, pv[:, 0:1024].rearrange("p (k t) -> p k t", t=128)), [pb], [bh2])
                        else:
                            self.V(lambda h: h.tensor_copy(dst, pv[:, 0:1024].rearrange("p (k t) -> p k t", t=128)), [pb], [bh2])
                for half in range(2):
                    jw = hh % 2; hh += 1
                    cs = slice(half * 512, (half + 1) * 512)
                    for kc in range(KC):
                        self.S.idma(reads=[bwi[js]], writes=[bwg[jw]], out=wg_[jw][:, kc, :], out_offset=None, in_=wgf[:, :],
                                    in_offset=bass.IndirectOffsetOnAxis(ap=wi[js][:, half * 16 + kc:half * 16 + kc + 1], axis=0))
                        self.S.idma(reads=[bwi[js]], writes=[bwu[jw]], out=wu_[jw][:, kc, :], out_offset=None, in_=wuf[:, :],
                                    in_offset=bass.IndirectOffsetOnAxis(ap=wi[js][:, half * 16 + kc:half * 16 + kc + 1], axis=0))
                    for fc in range(4):
                        self.S.idma(reads=[bwi[js]], writes=[bwd[jw]], out=wd_[jw][:, fc, :], out_offset=None, in_=wdf[:, :],
                                    in_offset=bass.IndirectOffsetOnAxis(ap=wi[js][:, 32 + half * 4 + fc:33 + half * 4 + fc], axis=0))
                    for fc in range(4):
                        pG, pU = self.ps[fc % 2], self.ps[2 + fc % 2]; bpG, bpU = self.pb[fc % 2], self.pb[2 + fc % 2]
                        for kc in range(KC):
                            self.PE(lambda h: h.matmul(pG[:, :], wg_[jw][:, kc, fc * 128:(fc + 1) * 128], h2s[:, kc, :], start=(kc == 0), stop=(kc == KC - 1)), [bwg[jw], bh2], [bpG], inc=(kc == KC - 1))
                        for kc in range(KC):
                            self.PE(lambda h: h.matmul(pU[:, :], wu_[jw][:, kc, fc * 128:(fc + 1) * 128], h2s[:, kc, :], start=(kc == 0), stop=(kc == KC - 1)), [bwu[jw], bh2], [bpU], inc=(kc == KC - 1))
                        self.A(lambda h: h.activation(sg[:], pG[:, :], AF.Silu), [bpG, bsg], [bsg])
                        self.V(lambda h: h.tensor_mul(he[jw][:, fc, :], sg[:], pU[:, :]), [bsg, bpU], [bhe[jw]])
                    for t in range(SLT):
                        for g in range(4):
                            ps, pb = self.ps[4 + g], self.pb[4 + g]
                            for fc in range(4):
                                self.PE(lambda h: h.matmul(ps[:, :], he[jw][:, fc, t * 128:(t + 1) * 128], wd_[jw][:, fc, g * 512:(g + 1) * 512], start=(fc == 0), stop=(fc == 3)), [bhe[jw], bwd[jw]], [pb], inc=(fc == 3))
                            ysl = ys[:, t, g * 512:(g + 1) * 512]
                            if half == 0:
                                self.V(lambda h: h.tensor_scalar(ysl, ps[:, :], gs[js][:, t:t + 1], None, ALU.mult), [pb, bgs[js], bys], [bys])
                            else:
                                self.V(lambda h: h.scalar_tensor_tensor(ysl, ps[:, :], gs[js][:, t:t + 1], ysl, ALU.mult, ALU.add), [pb, bgs[js], bys], [bys])
                self.st(self.ysD[s_ * c.SLOT:(s_ + 1) * c.SLOT, :].rearrange("(t p) n -> p t n", p=128), ys[:], [bys])

    def phase_unsort(self, l, last):
        c = self.c
        TT = c.TT
        with ExitStack() as st:
            gB = {}
            for src in range(c.NB):
                gB[src] = self.bcast_row(st, 1, src, f"ugm{src}")
            if not last:
                gB[2] = self.bcast_row(st, 1, 2, "ugm2")
            ya = [self.sb(st, f"ya{i}", [128, D]) for i in range(2)]; yb = [self.sb(st, f"yb{i}", [128, D]) for i in range(2)]
            xt = [self.sb(st, f"ux{i}", [128, D]) for i in range(2)]
            bya, byb, bx = [Buf(), Buf()], [Buf(), Buf()], [Buf(), Buf()]
            for ti, (b, i) in enumerate(self.moe_tiles(last)):
                j = ti % 2
                src = 2 if i < c.NTC else b
                self.S.idma(reads=[self.bpidx], writes=[bya[j]], out=ya[j][:, :], out_offset=None, in_=self.ysD[:, :],
                            in_offset=bass.IndirectOffsetOnAxis(ap=self.pidx[:, ti, 0:1], axis=0))
                self.S.idma(reads=[self.bpidx], writes=[byb[j]], out=yb[j][:, :], out_offset=None, in_=self.ysD[:, :],
                            in_offset=bass.IndirectOffsetOnAxis(ap=self.pidx[:, ti, 1:2], axis=0))
                self.ld(xt[j][:], self.xres[b, i * 128:(i + 1) * 128, :], [bx[j]])
                self.V(lambda h: h.tensor_add(ya[j][:], ya[j][:], yb[j][:]), [bya[j], byb[j]], [bya[j]])
                self.V(lambda h: h.tensor_mul(ya[j][:], ya[j][:], gB[src][0][:]), [bya[j], gB[src][1]], [bya[j]])
                self.V(lambda h: h.tensor_add(xt[j][:], xt[j][:], ya[j][:]), [bya[j], bx[j]], [bx[j]])
                self.st(self.xres[b, i * 128:(i + 1) * 128, :], xt[j][:], [bx[j]], q="act")

    def phase_final(self):
        c = self.c
        with ExitStack() as st:
            gB = self.sb(st, "gfinB", [128, D]); bg = Buf()
            self.ld(gB[:], self.gfin[0:1, :].partition_broadcast(128)[:, 0, :], [bg])
            xt = [self.sb(st, f"xf{i}", [128, D]) for i in range(2)]; bx = [Buf(), Buf()]
            sq = self.sb(st, "fsq", [128, D], BF16); ss = self.sb(st, "fss", [128, 4]); bs = Buf()
            for b in range(c.NB):
                for i in range(c.NTC, c.NT):
                    j = i % 2
                    self.ld(xt[j][:], self.xres[b, i * 128:(i + 1) * 128, :], [bx[j]], q=("sp" if j == 0 else "act"))
                    self.A(lambda h: h.activation(sq[:], xt[j][:], AF.Square, accum_out=ss[:, 0:1]), [bx[j]], [bs])
                    self.A(lambda h: h.activation(ss[:, 1:2], ss[:, 0:1], AF.Sqrt, scale=1.0 / D, bias=self.epsc[:, 0:1]), [bs, self.bconst], [bs])
                    self.V(lambda h: h.reciprocal(ss[:, 2:3], ss[:, 1:2]), [bs], [bs])
                    self.V(lambda h: h.scalar_tensor_tensor(xt[j][:], xt[j][:], ss[:, 2:3], gB[:], ALU.mult, ALU.mult), [bx[j], bs, bg], [bx[j]])
                    self.st(self.out[b, (i - c.NTC) * 128:(i - c.NTC + 1) * 128, :], xt[j][:], [bx[j]], q=("sp" if j == 0 else "act"))


def host_shared(inp, cfg):
    L = cfg.L
    f = lambda a: np.ascontiguousarray(np.asarray(a, dtype=np.float32))
    pk = lambda v: f(np.asarray(v).reshape(v.shape[:-1] + (v.shape[-1] // 128, 128)).swapaxes(-1, -2))
    sh = {}
    sh["gmix"] = pk(inp["norm_mix_g"]); sh["gffn"] = pk(inp["norm_ffn_g"])
    sh["gfin"] = f(inp["final_norm_g"]).reshape(1, D)
    sh["w_mod"] = f(inp["w_mod"]); sh["b_mod"] = f(inp["b_mod"])
    w_in = np.asarray(inp["w_in"], np.float32).reshape(L, D, 12, W)
    def swap(p):
        x = w_in[:, :, p].reshape(L, D, 4, 2, 64)
        return x[:, :, :, ::-1, :].reshape(L, D, W)
    sh["w_in"] = f(np.concatenate([w_in.reshape(L, D, 12 * W), swap(8), swap(9)], axis=-1))
    sh["w_out"] = f(inp["w_out"])
    bre, bim = np.asarray(inp["s5_b_re"], np.float32), np.asarray(inp["s5_b_im"], np.float32)
    cre, cim = np.asarray(inp["s5_c_re"], np.float32), np.asarray(inp["s5_c_im"], np.float32)
    s5B = np.zeros((L, 2, 16, 128, 2, 128), np.float32)
    s5C = np.zeros((L, 2, 16, 128, 2, 128), np.float32)
    for sc in range(16):
        for g2 in range(2):
            g = 2 * sc + g2
            r0 = 16 * (g % 8)
            s5B[:, :, sc, r0:r0 + 16, 0, g2 * 64:(g2 + 1) * 64] = bre[:, :, g]
            s5B[:, :, sc, r0:r0 + 16, 1, g2 * 64:(g2 + 1) * 64] = bim[:, :, g]
            s5C[:, :, sc, g2 * 64:(g2 + 1) * 64, 0, r0:r0 + 16] = cre[:, :, g]
            s5C[:, :, sc, g2 * 64:(g2 + 1) * 64, 1, r0:r0 + 16] = cim[:, :, g]
    sh["s5B"], sh["s5C"] = s5B, s5C
    lam = np.zeros((L, 128, 3, 2, 16), np.float32)
    lre, lim, ldt = (np.asarray(inp[k], np.float32) for k in ("s5_lam_re", "s5_lam_im", "s5_log_dt"))
    for sc in range(16):
        for g2 in range(2):
            g = 2 * sc + g2
            lam[:, g2 * 64:(g2 + 1) * 64, 0, :, sc] = lre[:, :, g, :].transpose(0, 2, 1)
            lam[:, g2 * 64:(g2 + 1) * 64, 1, :, sc] = lim[:, :, g, :].transpose(0, 2, 1)
            lam[:, g2 * 64:(g2 + 1) * 64, 2, :, sc] = ldt[:, :, g][:, None, :]
    sh["s5lam"] = lam
    sh["s5d"] = pk(inp["s5_d"]); sh["gluw"] = f(inp["s5_glu_w"]); sh["glub"] = pk(inp["s5_glu_b"])
    TT, CL = cfg.TT, cfg.CTXL
    kf = np.arange(TT, dtype=np.float32)
    kb = np.concatenate([CL - 1 - np.arange(CL), CL + (TT - CL) - 1 - np.arange(TT - CL)]).astype(np.float32)
    sh["kidx"] = f(np.stack([np.broadcast_to(kf, (128, TT)), np.broadcast_to(kb, (128, TT))]))
    cw = np.asarray(inp["lru_conv_w"], np.float32)
    sh["convw"] = f(cw.reshape(L, 4, 4, 128).transpose(0, 3, 2, 1))
    lv = np.stack([np.asarray(inp["lru_conv_b"], np.float32)] +
                  [np.asarray(inp[k], np.float32)[:, d] for k in ("lru_ba", "lru_bx", "lru_lam") for d in range(2)], axis=-1)
    sh["lruv"] = f(lv.reshape(L, 4, 128, 7).transpose(0, 2, 1, 3))
    lw = np.zeros((L, 2, 2, 4, 128, 128), np.float32)
    for a, k in enumerate(("lru_wa", "lru_wx")):
        wsrc = np.asarray(inp[k], np.float32)
        for hd in range(8):
            cc, h2 = hd // 2, hd % 2
            lw[:, a, :, cc, h2 * 64:(h2 + 1) * 64, h2 * 64:(h2 + 1) * 64] = wsrc[:, :, hd]
    sh["lruw"] = lw
    hl = np.asarray(inp["hgrn_lb_logits"], np.float32)
    sh["hglb"] = f(hl.reshape(2, L, 4, 128).transpose(0, 1, 3, 2))
    n = cfg.SEQL
    rows = n // 64
    row = np.repeat(np.arange(rows, dtype=np.float32), 64); col = np.tile(np.arange(64, dtype=np.float32), rows)
    inv = (np.float32(10000.0) ** (-np.arange(32, dtype=np.float32) / np.float32(32))).astype(np.float32)
    ang = np.concatenate([row[:, None] * inv, col[:, None] * inv], axis=-1).astype(np.float32)
    sh["rang"] = f(np.concatenate([ang.T, ang.T], axis=0))
    sh["rw"] = f(np.asarray(inp["router_w"], np.float32).reshape(KC, 128, NE).transpose(1, 0, 2))
    sh["rbias"] = f(inp["router_bias"]).reshape(1, NE)
    sh["wg"], sh["wu"], sh["wd"] = f(inp["moe_w_gate"]), f(inp["moe_w_up"]), f(inp["moe_w_down"])
    sh["ident"] = np.eye(128, dtype=np.float32)
    s_, t_ = np.meshgrid(np.arange(64), np.arange(64), indexing="ij")
    m = np.stack([(s_ <= t_), (s_ >= t_)]).astype(np.float32)
    mm = np.concatenate([m, m], axis=1)
    sh["masks"] = f(np.broadcast_to(mm[:, :, None, :], (2, 128, 4, 64)))
    rst = np.ones((128, TT + 1), np.float32); rst[:, 0::64] = 0.0
    sh["rst"] = rst
    rst32 = np.ones((128, TT + 1), np.float32); rst32[:, 0::32] = 0.0
    sh["rst32"] = rst32
    s_, t_ = np.meshgrid(np.arange(32), np.arange(32), indexing="ij")
    m32 = np.stack([(s_ <= t_), (s_ >= t_)]).astype(np.float32)
    sh["masks32"] = f(np.broadcast_to(np.concatenate([m32] * 4, axis=1)[:, :, None, :], (2, 128, 4, 32)))
    sh["gffn_row"] = f(inp["norm_ffn_g"])
    tp_, t_ = np.meshgrid(np.arange(128), np.arange(128), indexing="ij")
    sh["ltri"] = f(np.stack([(tp_ < t_).astype(np.float32), np.ones((128, 128), np.float32)]))
    p_ = np.arange(128)[:, None]
    sh["kio"] = f(np.concatenate([np.arange(16)[None, :] * 128 + p_, np.arange(8)[None, :] * 128 + p_], axis=1))
    nsmax = (2 * cfg.NB * cfg.TT + cfg.SLOT - 1) // cfg.SLOT + NE
    sh["siota"] = f(np.broadcast_to(np.arange(nsmax, dtype=np.float32), (128, nsmax)))
    sel = np.zeros((3, 3, 128), np.float32)
    for s in range(3):
        sel[s, s, :] = 1.0
    sh["sel3"] = sel
    return sh


def host_core(inp, cfg, b0):
    NB = cfg.NB
    x = np.asarray(inp["x"], np.float32)[b0:b0 + NB]
    ctx = np.asarray(inp["ctx"], np.float32)[b0:b0 + NB]
    cvec = np.concatenate([np.asarray(inp["c"], np.float32)[b0:b0 + NB], np.asarray(inp["c_ctx"], np.float32)[None]], axis=0)
    if NB == 1:
        cvec = np.concatenate([cvec[0:1], cvec[0:1], cvec[1:2]], axis=0)
    return {"xin": np.ascontiguousarray(np.concatenate([ctx, x], axis=1)),
            "cT": np.ascontiguousarray(cvec.reshape(3, KC, 128).transpose(2, 1, 0))}


_CACHE = {}


def kernel(**inputs):
    cfg = Cfg()
    n_cores = 8
    if "nc" not in _CACHE:
        _CACHE["nc"] = Prog(cfg).build()
    nc = _CACHE["nc"]
    sh = host_shared(inputs, cfg)
    in_maps = []
    for core in range(n_cores):
        m = dict(sh)
        m.update(host_core(inputs, cfg, core * cfg.NB))
        in_maps.append(m)
    res = run_bass_kernel_spmd(nc, in_maps, core_ids=list(range(n_cores)))
    return np.concatenate([r["out"] for r in res.results], axis=0).astype(np.float32)
```

```python
import math
from contextlib import ExitStack
import numpy as np
import concourse.bass as bass
import concourse.mybir as mybir
from concourse.bass_utils import run_bass_kernel_spmd

F32 = mybir.dt.float32
BF16 = mybir.dt.bfloat16
I32 = mybir.dt.int32
ALU = mybir.AluOpType
AF = mybir.ActivationFunctionType
AX = mybir.AxisListType

D = 2048
KC = 16
W = 512
NPARTS = 14
FM_PARTS = [0, 1, 2, 3, 4, 5, 8, 9, 12, 13]
TM_PARTS = [6, 7, 10, 11]
NE = 16
DFF = 1024
EPS = 1e-6
TWO_PI = 2.0 * math.pi


class Buf:
    __slots__ = ("w", "r")

    def __init__(self):
        self.w = None
        self.r = {}


class Eng:
    def __init__(self, name, h, sem):
        self.name, self.h, self.sem = name, h, sem
        self.n = 0
        self.seen = {}
        self.dsems, self.dcnt, self.dnext = [], [], 0


class Sched:
    def __init__(self, nc, stack, n_dma_sems=16):
        self.nc = nc
        self.E = {}
        for name, h in (("pe", nc.tensor), ("dve", nc.vector), ("act", nc.scalar),
                        ("pool", nc.gpsimd), ("sp", nc.sync)):
            self.E[name] = Eng(name, h, stack.enter_context(nc.semaphore("s_" + name)))
        for name in ("sp", "act", "pool"):
            e = self.E[name]
            for i in range(n_dma_sems):
                e.dsems.append(stack.enter_context(nc.semaphore(f"d_{name}{i}")))
                e.dcnt.append(0)
        self.ninstr = 0

    def _wait(self, e, sem, val):
        if sem is e.sem and e.name == "pe":
            return
        if e.seen.get(sem, 0) < val:
            e.h.wait_ge(sem, val)
            e.seen[sem] = val

    def _deps(self, e, reads, writes):
        for b in reads:
            if b.w is not None:
                self._wait(e, *b.w)
        for b in writes:
            if b.w is not None:
                self._wait(e, *b.w)
            for s, v in b.r.items():
                self._wait(e, s, v)

    @staticmethod
    def _mark(tok, reads, writes):
        s, v = tok
        for b in reads:
            if b.r.get(s, 0) < v:
                b.r[s] = v
        for b in writes:
            b.w = tok
            b.r = {}

    def op(self, eng, fn, reads=(), writes=(), inc=True):
        e = self.E[eng]
        self._deps(e, reads, writes)
        ins = fn(e.h)
        if inc:
            e.n += 1
            ins.then_inc(e.sem, 1)
            tok = (e.sem, e.n)
            e.pend = False
        else:
            tok = (e.sem, e.n + 1)
            e.pend = True
        self._mark(tok, reads, writes)
        self.ninstr += 1
        return ins

    def dma(self, eng, out, in_, reads=(), writes=(), **kw):
        e = self.E[eng]
        self._deps(e, reads, writes)
        i = e.dnext
        e.dnext = (i + 1) % len(e.dsems)
        sem = e.dsems[i]
        if e.dcnt[i]:
            self._wait(e, sem, e.dcnt[i])
        ins = e.h.dma_start(out=out, in_=in_, **kw)
        e.dcnt[i] += 16
        ins.then_inc(sem, 16)
        self._mark((sem, e.dcnt[i]), reads, writes)
        self.ninstr += 1
        return ins

    def idma(self, reads=(), writes=(), **kw):
        e = self.E["pool"]
        self._deps(e, reads, writes)
        i = e.dnext
        e.dnext = (i + 1) % len(e.dsems)
        sem = e.dsems[i]
        if e.dcnt[i]:
            self._wait(e, sem, e.dcnt[i])
        ins = e.h.indirect_dma_start(**kw)
        e.dcnt[i] += 16
        ins.then_inc(sem, 16)
        self._mark((sem, e.dcnt[i]), reads, writes)
        self.ninstr += 1
        return ins

    def barrier(self):
        assert not any(getattr(e, "pend", False) for e in self.E.values())
        for e in self.E.values():
            for f in self.E.values():
                if f is not e and f.n:
                    self._wait(e, f.sem, f.n)
            for q in ("sp", "act", "pool"):
                qe = self.E[q]
                for sem, cnt in zip(qe.dsems, qe.dcnt):
                    if cnt:
                        self._wait(e, sem, cnt)


class Cfg:
    def __init__(self, NB=2, CTXL=256, SEQL=2048, L=2, dbg=False):
        self.NB, self.CTXL, self.SEQL, self.L, self.dbg = NB, CTXL, SEQL, L, dbg
        self.TT = CTXL + SEQL
        self.NT = self.TT // 128
        self.NTC = CTXL // 128
        self.NCH = self.TT // 64
        self.NCHC = CTXL // 64
        self.sparse = True
        self.SLOT = 512


def tok_groups(T, g=512):
    out, t = [], 0
    while t < T:
        n = min(g, T - t)
        out.append((t, n))
        t += n
    return out


class Prog:
    def __init__(self, cfg):
        self.c = cfg
        self.nc = bass.Bass("TRN2", target_bir_lowering=False)
        self.inp = {}

    def din(self, name, shape, dt=F32):
        t = self.nc.dram_tensor(name, list(shape), dt, kind="ExternalInput").ap()
        self.inp[name] = t
        return t

    def dscr(self, name, shape, dt=F32):
        kind = "ExternalOutput" if self.c.dbg else "Internal"
        return self.nc.dram_tensor(name, list(shape), dt, kind=kind).ap()

    def sb(self, st, name, shape, dt=F32):
        self._uid = getattr(self, "_uid", 0) + 1
        return st.enter_context(self.nc.sbuf_tensor(f"{name}_{self._uid}", list(shape), dt))

    def V(self, fn, R=(), Wr=()):
        return self.S.op("dve", fn, R, Wr)

    def A(self, fn, R=(), Wr=()):
        return self.S.op("act", fn, R, Wr)

    def G(self, fn, R=(), Wr=()):
        return self.S.op("pool", fn, R, Wr)

    def PE(self, fn, R=(), Wr=(), inc=True):
        return self.S.op("pe", fn, R, Wr, inc=inc)

    def ld(self, out, in_, Wr, q="sp", R=()):
        return self.S.dma(q, out, in_, reads=R, writes=Wr)

    def ldc(self, out, in_, Wr, R=()):
        return self.S.dma("pool", out, in_, reads=R, writes=Wr)

    def st(self, out, in_, R, q="sp"):
        return self.S.dma(q, out, in_, reads=R, writes=())

    def build(self):
        c, nc = self.c, self.nc
        NB, TT, NT, L = c.NB, c.TT, c.NT, c.L
        i_ = self.din
        self.xin = i_("xin", [NB, TT, D])
        self.cT = i_("cT", [128, KC, 3])
        self.gmix = i_("gmix", [L, 128, KC])
        self.gffn = i_("gffn", [L, 128, KC])
        self.gfin = i_("gfin", [1, D])
        self.w_mod = i_("w_mod", [L, D, 6 * D])
        self.b_mod = i_("b_mod", [L, 6 * D])
        self.w_in = i_("w_in", [L, D, NPARTS * W])
        self.w_out = i_("w_out", [L, D, D])
        self.s5B = i_("s5B", [L, 2, 16, 128, 2, 128])
        self.s5C = i_("s5C", [L, 2, 16, 128, 2, 128])
        self.s5lam = i_("s5lam", [L, 128, 3, 2, 16])
        self.s5d = i_("s5d", [L, 128, 4])
        self.gluw = i_("gluw", [L, W, W])
        self.glub = i_("glub", [L, 128, 4])
        self.kidx = i_("kidx", [2, 128, TT])
        self.convw = i_("convw", [L, 128, 4, 4])
        self.lruv = i_("lruv", [L, 128, 4, 7])
        self.lruw = i_("lruw", [L, 2, 2, 4, 128, 128])
        self.hglb = i_("hglb", [2, L, 128, 4])
        self.rang = i_("rang", [128, c.SEQL])
        self.rw = i_("rw", [128, KC, NE])
        self.rbias = i_("rbias", [1, NE])
        self.wg = i_("wg", [L, NE, D, DFF])
        self.wu = i_("wu", [L, NE, D, DFF])
        self.wd = i_("wd", [L, NE, DFF, D])
        self.ident = i_("ident", [128, 128])
        self.masks = i_("masks", [2, 128, 4, 64])
        self.rst = i_("rst", [128, TT + 1])
        self.rst32 = i_("rst32", [128, TT + 1])
        self.masks32 = i_("masks32", [2, 128, 4, 32])
        self.sel3 = i_("sel3", [3, 3, 128])
        self.gffn_row = i_("gffn_row", [L, D])
        self.ltri = i_("ltri", [2, 128, 128])
        self.kio = i_("kio", [128, 24])
        NSMAX = (2 * NB * TT + c.SLOT - 1) // c.SLOT + NE
        self.NSMAX = NSMAX
        self.siota = i_("siota", [128, NSMAX])
        self.out = nc.dram_tensor("out", [NB, c.SEQL, D], F32, kind="ExternalOutput").ap()

        self.xres = self.dscr("xres", [NB, TT, D])
        self.Pfm = self.dscr("Pfm", [NB, len(FM_PARTS) * W, TT])
        self.Ptm = self.dscr("Ptm", [NB, TT, len(TM_PARTS) * W])
        self.mixT = self.dscr("mixT", [NB, D, TT], BF16)
        self.h2T = self.dscr("h2T", [NB, D, TT], BF16)
        self.gates = self.dscr("gates", [NB, TT, NE])
        self.ygD = self.dscr("ygD", [NB, W, TT])
        self.modD = self.dscr("modD", [3, 4, D])
        RM = self.NSMAX * c.SLOT
        self.h2tok = self.dscr("h2tok", [NB * TT, D], BF16)
        self.selD = self.dscr("selD", [NB * TT, NE])
        self.rankD = self.dscr("rankD", [NB * TT, NE])
        self.hs = self.dscr("hs", [RM, D], BF16)
        self.gsD = self.dscr("gsD", [RM, 1])
        self.ysD = self.dscr("ysD", [RM, D])

        with ExitStack() as top:
            self.S = Sched(nc, top)
            S = self.S
            self.ps = [top.enter_context(nc.psum_tensor(f"ps{i}", [128, 512], F32)) for i in range(8)]
            self.pb = [Buf() for _ in range(8)]
            self.idf = self.sb(top, "idf", [128, 128]); self.idb = self.sb(top, "idb", [128, 128], BF16)
            self.bconst = Buf()
            self.ld(self.idf[:], self.ident[:, :], [self.bconst])
            self.ldc(self.idb[:], self.ident[:, :], [self.bconst])
            self.cmask = self.sb(top, "cmask", [128, 2, 4, 64])
            self.ld(self.cmask[:], self.masks.rearrange("a p h j -> p a h j"), [self.bconst])
            self.cmask32 = self.sb(top, "cmask32", [128, 2, 4, 32])
            self.ld(self.cmask32[:], self.masks32.rearrange("a p h j -> p a h j"), [self.bconst])
            self.epsc = self.sb(top, "epsc", [128, 1])
            self.V(lambda h: h.memset(self.epsc[:], EPS), [], [self.bconst])
            self.hpi = self.sb(top, "hpi", [128, 1])
            self.V(lambda h: h.memset(self.hpi[:], math.pi / 2), [], [self.bconst])
            self.sel3t = self.sb(top, "sel3t", [3, 3, 128])
            self.ld(self.sel3t[:], self.sel3[:, :, :], [self.bconst])
            self.modP = self.sb(top, "modP", [128, 6, KC, 3])
            self.AB = self.sb(top, "AB", [128, 4, KC, 3])
            self.bmod = Buf()
            self.run = self.sb(top, "run", [128, NE]); self.brun = Buf()
            self.pidx = self.sb(top, "pidx", [128, NB * NT, 2], I32); self.bpidx = Buf()
            self.ltt = self.sb(top, "ltt", [128, 2, 128])
            self.ld(self.ltt[:], self.ltri.rearrange("a p j -> p a j"), [self.bconst])
            self.es2 = self.sb(top, "es2", [128, 2, self.NSMAX]); self.bes = Buf()
            self.kiot = self.sb(top, "kiot", [128, 24])
            self.ld(self.kiot[:], self.kio[:, :], [self.bconst])
            if c.sparse:
                with ExitStack() as zst:
                    z = self.sb(zst, "zz", [128, D], BF16); bz = Buf()
                    self.V(lambda h: h.memset(z[:], 0.0), [], [bz])
                    for r0 in range(0, self.NSMAX * c.SLOT, 128):
                        self.st(self.hs[r0:r0 + 128, :], z[:], [bz], q=("sp" if (r0 // 128) % 2 == 0 else "act"))
                    S.barrier()
            S.barrier()
            for l in range(L):
                last = (l == L - 1)
                self.phase_mod(l)
                S.barrier()
                for b in range(NB):
                    xsrc = self.xin if l == 0 else self.xres
                    self.phase_proj(l, b, xsrc)
                    S.barrier()
                    self.phase_lru(l, b)
                    S.barrier()
                    self.phase_gla(l, b, 0)
                    S.barrier()
                    self.phase_gla(l, b, 1)
                    S.barrier()
                    self.phase_s5(l, b)
                    S.barrier()
                    self.phase_out(l, b, xsrc, last)
                    S.barrier()
                    if not c.sparse:
                        self.phase_moe(l, b, last)
                        S.barrier()
                if c.sparse:
                    self.phase_route(l, last)
                    S.barrier()
                    self.phase_moe_sparse(l, last)
                    S.barrier()
                    self.phase_unsort(l, last)
                    S.barrier()
            self.phase_final()
            S.barrier()
        return nc

    def phase_mod(self, l):
        c, S = self.c, self.S
        with ExitStack() as st:
            cT = self.sb(st, "cT", [128, KC, 3]); cs = self.sb(st, "cs", [128, KC, 3], BF16)
            bc = Buf()
            self.ld(cT[:], self.cT[:, :, :], [bc])
            self.A(lambda h: h.activation(cs[:], cT[:], AF.Silu), [bc], [bc])
            modrow = self.sb(st, "modrow", [3, 6 * D]); bmr = Buf()
            wm = [self.sb(st, f"wm{i}", [128, KC, 512], BF16) for i in range(2)]; bwm = [Buf(), Buf()]
            bt = [self.sb(st, f"bt{i}", [3, 512]) for i in range(2)]; bbt = [Buf(), Buf()]
            wsrc = self.w_mod[l].rearrange("(kc kp) n -> kp kc n", kp=128)
            for cg in range(24):
                j = cg % 2
                self.ldc(wm[j][:], wsrc[:, :, cg * 512:(cg + 1) * 512], [bwm[j]])
                self.ld(bt[j][:], self.b_mod[l:l + 1, cg * 512:(cg + 1) * 512].partition_broadcast(3)[:, 0, :], [bbt[j]], q="act")
                p = self.ps[cg % 2]; pb = self.pb[cg % 2]
                for kc in range(KC):
                    self.PE(lambda h: h.matmul(p[0:3, :], cs[:, kc, :], wm[j][:, kc, :], start=(kc == 0), stop=(kc == KC - 1)),
                            [bc, bwm[j]], [pb], inc=(kc == KC - 1))
                self.V(lambda h: h.tensor_add(modrow[:, cg * 512:(cg + 1) * 512], p[0:3, :], bt[j][:]), [pb, bbt[j]], [bmr])
            pT = self.ps[2]; pTb = self.pb[2]
            for v in range(6):
                for kc in range(KC):
                    k = v * KC + kc
                    self.PE(lambda h: h.transpose(pT[:, k * 3:k * 3 + 3], modrow[:, v * D + kc * 128: v * D + (kc + 1) * 128], self.idf[0:3, 0:3]),
                            [bmr, self.bconst], [pTb], inc=(k == 6 * KC - 1))
            self.V(lambda h: h.tensor_copy(self.modP[:].rearrange("p a k s -> p (a k s)"), pT[:, 0:288]), [pTb], [self.bmod])
            self.st(self.modD[:, 0, :], modrow[:, 2 * D:3 * D], [bmr])
            self.st(self.modD[:, 1, :], modrow[:, 5 * D:6 * D], [bmr])
            self.st(self.modD[:, 3, :], modrow[:, 3 * D:4 * D], [bmr])
            grow = self.sb(st, "grow", [3, D]); bgr = Buf()
            self.ld(grow[:], self.gffn_row[l:l + 1, :].partition_broadcast(3)[:, 0, :], [bgr])
            self.V(lambda h: h.scalar_tensor_tensor(grow[:], modrow[:, 4 * D:5 * D], 1.0, grow[:], ALU.add, ALU.mult), [bmr, bgr], [bgr])
            self.st(self.modD[:, 2, :], grow[:], [bgr])
            self.V(lambda h: h.memset(self.run[:], 0.0), [self.brun], [self.brun])
            g1 = self.sb(st, "g1", [128, KC]); g2 = self.sb(st, "g2", [128, KC]); bg = Buf()
            self.ld(g1[:], self.gmix[l], [bg]); self.ld(g2[:], self.gffn[l], [bg])
            for (gi, gt, vs, vsh) in ((0, g1, 1, 0), (2, g2, 4, 3)):
                self.V(lambda h: h.tensor_scalar(self.AB[:, gi], self.modP[:, vs], 1.0, None, ALU.add), [self.bmod], [self.bmod])
                self.V(lambda h: h.tensor_mul(self.AB[:, gi], self.AB[:, gi], gt[:].unsqueeze(2).to_broadcast([128, KC, 3])), [self.bmod, bg], [self.bmod])
                self.V(lambda h: h.tensor_copy(self.AB[:, gi + 1], self.modP[:, vsh]), [self.bmod], [self.bmod])

    def bcast_row(self, st, which, src, name):
        t = self.sb(st, name, [128, D]); b = Buf()
        self.ld(t[:], self.modD[src:src + 1, which, :].partition_broadcast(128)[:, 0, :], [b])
        return t, b

    def norm_T(self, st_tiles, xt, bx, which, src, dst_fn, bdst, fp32):
        sq, ss, xs = st_tiles["sq"], st_tiles["ss"], (st_tiles["xsf"] if fp32 else st_tiles["xsb"])
        bsq, bss, bxs = st_tiles["bsq"], st_tiles["bss"], st_tiles["bxs"]
        self.A(lambda h: h.activation(sq[:], xt[:], AF.Square, accum_out=ss[:, 0:1]), [bx], [bsq, bss])
        self.A(lambda h: h.activation(ss[:, 1:2], ss[:, 0:1], AF.Sqrt, scale=1.0 / D, bias=self.epsc[:, 0:1]), [bss, self.bconst], [bss])
        self.V(lambda h: h.reciprocal(ss[:, 2:3], ss[:, 1:2]), [bss], [bss])
        self.A(lambda h: h.activation(xs[:], xt[:], AF.Identity, scale=ss[:, 2:3]), [bx, bss], [bxs])
        a_i, b_i = (0, 1) if which == 0 else (2, 3)
        idm = self.idf if fp32 else self.idb
        ngrp = 4 if fp32 else 2
        per = KC // ngrp
        for g in range(ngrp):
            bank = 4 + g
            p = self.ps[bank]; pb = self.pb[bank]
            pv = p[:] if fp32 else p[:].bitcast(BF16)
            for j in range(per):
                kc = g * per + j
                self.PE(lambda h: h.transpose(pv[:, j * 128:(j + 1) * 128], xs[:, kc * 128:(kc + 1) * 128], idm[:]),
                        [bxs, self.bconst], [pb], inc=(j == per - 1))
            for j in range(per):
                kc = g * per + j
                sc, bi = self.AB[:, a_i, kc, src:src + 1], self.AB[:, b_i, kc, src:src + 1]
                if kc % 2 == 0:
                    self.V(lambda h: h.tensor_scalar(dst_fn(kc), pv[:, j * 128:(j + 1) * 128], sc, bi, ALU.mult, ALU.add),
                           [pb, self.bmod], [bdst])
                else:
                    self.A(lambda h: h.activation(dst_fn(kc), pv[:, j * 128:(j + 1) * 128], AF.Identity, bias=bi, scale=sc),
                           [pb, self.bmod], [bdst])

    def norm_tiles(self, st):
        d = {"sq": self.sb(st, "sq", [128, D], BF16), "ss": self.sb(st, "ss", [128, 4]),
             "xsf": None, "xsb": None, "bsq": Buf(), "bss": Buf(), "bxs": Buf()}
        return d

    def phase_proj(self, l, b, xsrc):
        c, S = self.c, self.S
        TT, NT = c.TT, c.NT
        with ExitStack() as st:
            hT = self.sb(st, "hT", [128, KC, TT], BF16); bh = Buf()
            nt = self.norm_tiles(st); nt["xsb"] = self.sb(st, "xsb", [128, D], BF16)
            xt = [self.sb(st, f"xt{i}", [128, D]) for i in range(2)]; bx = [Buf(), Buf()]
            for i in range(NT):
                j = i % 2
                self.ld(xt[j][:], xsrc[b, i * 128:(i + 1) * 128, :], [bx[j]], q=("sp" if j == 0 else "act"))
                src = 2 if i < c.NTC else b
                self.norm_T(nt, xt[j], bx[j], 0, src, lambda kc: hT[:, kc, i * 128:(i + 1) * 128], bh, fp32=False)
            wp = [self.sb(st, f"wp{i}", [128, KC, W], BF16) for i in range(2)]; bw = [Buf(), Buf()]
            stg = [self.sb(st, f"stg{i}", [128, TT]) for i in range(2)]; bs = [Buf(), Buf()]
            stg2 = [self.sb(st, f"stgb{i}", [128, W]) for i in range(2)]; bs2 = [Buf(), Buf()]
            wsrc = self.w_in[l].rearrange("(kc kp) n -> kp kc n", kp=128)
            tgs = tok_groups(TT)
            ev = 0
            for pi, p in enumerate(FM_PARTS + TM_PARTS):
                j = pi % 2
                self.ldc(wp[j][:], wsrc[:, :, p * W:(p + 1) * W], [bw[j]])
                if p in FM_PARTS:
                    fi = FM_PARTS.index(p)
                    for cb in range(4):
                        sj = cb % 2
                        for (t0, n) in tgs:
                            bank = ev % 4; ev += 1
                            ps, pb = self.ps[bank], self.pb[bank]
                            for kc in range(KC):
                                self.PE(lambda h: h.matmul(ps[:, 0:n], wp[j][:, kc, cb * 128:(cb + 1) * 128], hT[:, kc, t0:t0 + n],
                                                           start=(kc == 0), stop=(kc == KC - 1)), [bw[j], bh], [pb], inc=(kc == KC - 1))
                            if ev % 2:
                                self.A(lambda h: h.copy(stg[sj][:, t0:t0 + n], ps[:, 0:n]), [pb], [bs[sj]])
                            else:
                                self.V(lambda h: h.tensor_copy(stg[sj][:, t0:t0 + n], ps[:, 0:n]), [pb], [bs[sj]])
                        self.st(self.Pfm[b, fi * W + cb * 128: fi * W + (cb + 1) * 128, :], stg[sj][:], [bs[sj]], q=("sp" if sj == 0 else "act"))
                else:
                    ti = TM_PARTS.index(p)
                    for i in range(NT):
                        sj = i % 2
                        bank = ev % 4; ev += 1
                        ps, pb = self.ps[bank], self.pb[bank]
                        for kc in range(KC):
                            self.PE(lambda h: h.matmul(ps[:, :], hT[:, kc, i * 128:(i + 1) * 128], wp[j][:, kc, :],
                                                       start=(kc == 0), stop=(kc == KC - 1)), [bw[j], bh], [pb], inc=(kc == KC - 1))
                        if ev % 2:
                            self.A(lambda h: h.copy(stg2[sj][:], ps[:, :]), [pb], [bs2[sj]])
                        else:
                            self.V(lambda h: h.tensor_copy(stg2[sj][:], ps[:, :]), [pb], [bs2[sj]])
                        self.st(self.Ptm[b, i * 128:(i + 1) * 128, ti * W:(ti + 1) * W], stg2[sj][:], [bs2[sj]], q=("sp" if sj == 0 else "act"))

    def pfm(self, b, part, r0, n=128):
        fi = FM_PARTS.index(part)
        return self.Pfm[b, fi * W + r0: fi * W + r0 + n, :]

    def ptm(self, b, part):
        ti = TM_PARTS.index(part)
        return self.Ptm[b, :, ti * W:(ti + 1) * W].rearrange("(i p) w -> p i w", p=128)

    def scan_bidir(self, out_f, out_b, a_f, x_f, a_b, x_b, R, Wf, Wb, eng="dve"):
        c = self.c
        CL, TT = c.CTXL, c.TT
        op = self.V if eng == "dve" else self.G
        op(lambda h: h.tensor_tensor_scan(out_f[:, 0:TT], a_f[:, 0:TT], x_f[:, 0:TT], 0.0, ALU.mult, ALU.add), R, [Wf])
        op(lambda h: h.tensor_tensor_scan(out_b[:, 0:CL][:, ::-1], a_b[:, 0:CL][:, ::-1], x_b[:, 0:CL][:, ::-1], 0.0, ALU.mult, ALU.add), R, [Wb])
        op(lambda h: h.tensor_tensor_scan(out_b[:, CL:TT][:, ::-1], a_b[:, CL:TT][:, ::-1], x_b[:, CL:TT][:, ::-1], out_b[:, 0:1], ALU.mult, ALU.add),
           list(R) + [Wb], [Wb])

    def phase_lru(self, l, b):
        c = self.c
        TT, CL = c.TT, c.CTXL
        tgs = tok_groups(TT)
        with ExitStack() as st:
            cw = self.sb(st, "cw", [128, 4, 4]); lv = self.sb(st, "lv", [128, 4, 7]); bp = Buf()
            self.ld(cw[:], self.convw[l], [bp]); self.ld(lv[:], self.lruv[l], [bp])
            lw = self.sb(st, "lw", [128, 2, 2, 4, 128], BF16)
            self.ldc(lw[:], self.lruw[l].rearrange("a d c p j -> p a d c j"), [bp])
            c1 = self.sb(st, "c1", [128, 4, 2]); c2 = self.sb(st, "c2", [128, 4, 2])
            self.A(lambda h: h.activation(c1[:], lv[:, :, 5:7], AF.Exp, scale=-1.0), [bp], [bp])
            self.A(lambda h: h.activation(c1[:], c1[:], AF.Ln, bias=1.0), [bp], [bp])
            self.V(lambda h: h.tensor_scalar(c2[:], c1[:], -16.0, None, ALU.mult), [bp], [bp])
            self.V(lambda h: h.tensor_scalar(c1[:], c1[:], -8.0, None, ALU.mult), [bp], [bp])
            T = lambda n, dt=F32: self.sb(st, n, [128, TT], dt)
            xr, gt, xc, xcb = T("xr"), T("gt"), T("xc"), T("xcb", BF16)
            rr, ii, aa, bb = [T("rr0"), T("rr1")], [T("ii0"), T("ii1")], [T("aa0"), T("aa1")], [T("bb0"), T("bb1")]
            hf, hb, ob = T("hf"), T("hb"), T("ob", BF16)
            bxr, bgt, bxc, bxcb, bo = Buf(), Buf(), Buf(), Buf(), Buf()
            br, bi, ba, bbb = [Buf(), Buf()], [Buf(), Buf()], [Buf(), Buf()], [Buf(), Buf()]
            bhf, bhb = Buf(), Buf()
            for cc in range(4):
                self.ld(xr[:], self.pfm(b, 1, cc * 128), [bxr])
                self.ld(gt[:], self.pfm(b, 2, cc * 128), [bgt], q="act")
                self.V(lambda h: h.tensor_scalar(xc[:], xr[:], cw[:, cc, 2:3], lv[:, cc, 0:1], ALU.mult, ALU.add), [bxr, bp], [bxc])
                for (s0, s1) in ((0, CL), (CL, TT)):
                    for k, off in ((0, -2), (1, -1), (3, 1)):
                        o0, o1 = max(s0, s0 - off), min(s1, s1 - off)
                        self.V(lambda h: h.scalar_tensor_tensor(xc[:, o0:o1], xr[:, o0 + off:o1 + off], cw[:, cc, k:k + 1], xc[:, o0:o1], ALU.mult, ALU.add),
                               [bxr, bp, bxc], [bxc])
                self.A(lambda h: h.copy(xcb[:], xc[:]), [bxc], [bxcb])
                for d in range(2):
                    for (wi, dst, bd, bias_col) in ((0, rr[d], br[d], 1 + d), (1, ii[d], bi[d], 3 + d)):
                        for gi, (t0, n) in enumerate(tgs):
                            ps, pb = self.ps[gi % 4], self.pb[gi % 4]
                            self.PE(lambda h: h.matmul(ps[:, 0:n], lw[:, wi, d, cc, :], xcb[:, t0:t0 + n], start=True, stop=True), [bp, bxcb], [pb])
                            self.A(lambda h: h.activation(dst[:, t0:t0 + n], ps[:, 0:n], AF.Sigmoid, bias=lv[:, cc, bias_col:bias_col + 1]), [pb, bp], [bd])
                    self.A(lambda h: h.activation(aa[d][:], rr[d][:], AF.Exp, scale=c1[:, cc, d:d + 1]), [br[d], bp], [ba[d]])
                    self.A(lambda h: h.activation(bb[d][:], rr[d][:], AF.Exp, scale=c2[:, cc, d:d + 1]), [br[d], bp], [bbb[d]])
                    self.A(lambda h: h.activation(bb[d][:], bb[d][:], AF.Sqrt, scale=-1.0, bias=1.0), [bbb[d]], [bbb[d]])
                    self.V(lambda h: h.tensor_mul(ii[d][:], ii[d][:], xc[:]), [bi[d], bxc], [bi[d]])
                    self.V(lambda h: h.tensor_mul(bb[d][:], bb[d][:], ii[d][:]), [bbb[d], bi[d]], [bbb[d]])
                self.scan_bidir(hf, hb, aa[0], bb[0], aa[1], bb[1], [ba[0], ba[1], bbb[0], bbb[1]], bhf, bhb)
                self.V(lambda h: h.tensor_add(hf[:], hf[:], hb[:]), [bhf, bhb], [bhf])
                self.gelu(gt[:], gt[:], hb[:], [bgt], bgt, bhb)
                self.V(lambda h: h.tensor_mul(ob[:], hf[:], gt[:]), [bhf, bgt], [bo])
                self.st(self.mixT[b, W + cc * 128: W + (cc + 1) * 128, :], ob[:], [bo])

    def sincos(self, st, ang, bang, n, name, share=None):
        T = lambda nm, dt=F32: self.sb(st, f"{name}_{nm}", [128, n], dt)
        y, yi, sn, cs = T("y"), T("yi", I32), T("sn"), T("cs")
        b = Buf(); bs = Buf(); bcs = Buf()
        for (shift, dst, bd) in ((0.0, sn, bs), (0.25, cs, bcs)):
            self.V(lambda h: h.tensor_scalar(y[:], ang[:], 1.0 / TWO_PI, shift, ALU.mult, ALU.add), [bang, b], [b])
            self.wrap_frac(y, yi, dst, b, extra=[bd])
            self.A(lambda h: h.activation(dst[:], y[:], AF.Sin, scale=TWO_PI), [b], [bd, b])
        return sn, bs, cs, bcs

    def wrap_frac(self, y, yi, yf, b, extra=()):
        Wb = [b] + list(extra)
        self.V(lambda h: h.tensor_copy(yi[:], y[:]), [b], Wb)
        self.V(lambda h: h.tensor_copy(yf[:], yi[:]), [b], Wb)
        self.V(lambda h: h.tensor_sub(y[:], y[:], yf[:]), [b], Wb)
        self.V(lambda h: h.tensor_scalar(yf[:], y[:], 0.5, -1.0, ALU.is_gt, ALU.mult), [b], Wb)
        self.V(lambda h: h.tensor_add(y[:], y[:], yf[:]), [b], Wb)
        self.V(lambda h: h.tensor_scalar(yf[:], y[:], -0.5, None, ALU.is_lt), [b], Wb)
        self.V(lambda h: h.tensor_add(y[:], y[:], yf[:]), [b], Wb)

    def gelu(self, dst, src, tmp, R, Wd, Wt):
        self.V(lambda h: h.tensor_mul(tmp, src, src), R, [Wt])
        self.V(lambda h: h.tensor_scalar(tmp, tmp, 0.044715, 1.0, ALU.mult, ALU.add), [Wt], [Wt])
        self.V(lambda h: h.tensor_mul(tmp, tmp, src), list(R) + [Wt], [Wt])
        self.A(lambda h: h.activation(tmp, tmp, AF.Sigmoid, scale=1.5957691216057308), [Wt], [Wt])
        self.V(lambda h: h.tensor_mul(dst, src, tmp), list(R) + [Wt], [Wd])

    def phase_gla(self, l, b, mixer):
        c = self.c
        TT, NT, CL = c.TT, c.NT, c.CTXL
        C = 64
        NPT = 128 // C
        NCH, NCHC = TT // C, CL // C
        cmask = self.cmask32 if C == 32 else self.cmask
        rsrc = self.rst32 if C == 32 else self.rst
        SQ = 128 ** -0.5
        with ExitStack() as st:
            T = lambda s_, n, dt=F32: self.sb(s_, n, [128, TT], dt)
            vbf = self.sb(st, "vbf", [128, NT, W], BF16); bv = Buf()
            oacc = self.sb(st, "oacc", [128, NT, W]); bo = Buf()
            vst = [self.sb(st, f"vst{i}", [128, W]) for i in range(2)]; bvs = [Buf(), Buf()]
            vsrc = self.ptm(b, 6 if mixer == 0 else 10)
            for i in range(NT):
                j = i % 2
                self.ld(vst[j][:], vsrc[:, i, :], [bvs[j]], q=("sp" if j == 0 else "act"))
                self.A(lambda h: h.activation(vbf[:, i, :], vst[j][:], AF.Silu if mixer == 0 else AF.Identity), [bvs[j]], [bv])
            bprm = Buf()
            if mixer == 0:
                lbr = self.sb(st, "lbr", [128, 2, c.L, 4]); lbe = self.sb(st, "lbe", [128, 2, c.L, 4])
                lb = self.sb(st, "lb", [128, 2, 4]); lbs = self.sb(st, "lbs", [128, 2, 4]); oml = self.sb(st, "oml", [128, 2, 4])
                self.ld(lbr[:], self.hglb.rearrange("d l p h -> p d l h"), [bprm])
                self.A(lambda h: h.activation(lbe[:], lbr[:], AF.Exp), [bprm], [bprm])
                self.V(lambda h: h.tensor_copy(lbs[:], lbe[:, :, 0, :]), [bprm], [bprm])
                for ll in range(1, c.L):
                    self.V(lambda h: h.tensor_add(lbs[:], lbs[:], lbe[:, :, ll, :]), [bprm], [bprm])
                self.V(lambda h: h.memset(lb[:], 0.0), [], [bprm])
                for ll in range(1, l + 1):
                    self.V(lambda h: h.tensor_add(lb[:], lb[:], lbe[:, :, ll, :]), [bprm], [bprm])
                self.V(lambda h: h.reciprocal(lbs[:], lbs[:]), [bprm], [bprm])
                self.V(lambda h: h.tensor_mul(lb[:], lb[:], lbs[:]), [bprm], [bprm])
                self.V(lambda h: h.tensor_scalar(oml[:], lb[:], -1.0, 1.0, ALU.mult, ALU.add), [bprm], [bprm])
            for d in range(2):
                with ExitStack() as s2:
                    rst = self.sb(s2, "rst", [128, TT + 1]); brs = Buf()
                    self.ld(rst[:], rsrc[:, :], [brs])
                    if mixer == 1:
                        ang = self.sb(s2, "ang", [128, c.SEQL]); bang = Buf()
                        self.ld(ang[:], self.rang[:, :], [bang])
                        sn, bsn, cs, bcs = self.sincos(s2, ang, bang, c.SEQL, "rot", share=ang)
                        self.V(lambda h: h.tensor_scalar(sn[0:64, :], sn[0:64, :], -1.0, None, ALU.mult), [bsn], [bsn])
                    q_, k_, z_, cum, e1 = T(s2, "q_"), T(s2, "k_"), T(s2, "z_"), T(s2, "cum"), T(s2, "e1")
                    tmp = z_ if mixer == 1 else T(s2, "tmp")
                    bq, bk, bz, bcum, be1 = (Buf() for _ in range(5))
                    btmp = bz if mixer == 1 else Buf()
                    qt = [T(s2, f"qt{h}", BF16) for h in range(4)]; kt = [T(s2, f"kt{h}", BF16) for h in range(4)]
                    bqt = [Buf() for _ in range(4)]; bkt = [Buf() for _ in range(4)]
                    dec = self.sb(s2, "dec", [128, NCH, 4]); em = self.sb(s2, "em", [128, NCH, 4]); elm = self.sb(s2, "elm", [128, NCH, 4]); bdec = Buf()
                    S_ = self.sb(s2, "S_", [128, 4, 128]); Sb = self.sb(s2, "Sb", [128, 4, 128], BF16); Ut = self.sb(s2, "Ut", [128, 4, 128]); bS, bSb, bUt = Buf(), Buf(), Buf()
                    sT = [self.sb(s2, f"sT{i}", [128, 4, C], BF16) for i in range(2)]; bsT = [Buf(), Buf()]
                    khT = [self.sb(s2, f"khT{i}", [128, 4, 128], BF16) for i in range(2)]; bkhT = [Buf(), Buf()]
                    c3 = lambda t: t[:].rearrange("p (c j) -> p c j", j=C)
                    for hd in range(4):
                        r0 = hd * 128
                        if mixer == 0:
                            self.ld(q_[:], self.pfm(b, 3, r0), [bq])
                            self.ld(z_[:], self.pfm(b, 4 + d, r0), [bz], q="act")
                            self.A(lambda h: h.activation(k_[:], z_[:], AF.Sigmoid, scale=-1.0), [bz], [bk])
                            self.A(lambda h: h.activation(z_[:], z_[:], AF.Sigmoid), [bz, bk], [bz])
                            self.A(lambda h: h.activation(z_[:], z_[:], AF.Ln, scale=oml[:, d, hd:hd + 1], bias=lb[:, d, hd:hd + 1]), [bz, bprm], [bz])
                            self.V(lambda h: h.tensor_scalar(k_[:], k_[:], oml[:, d, hd:hd + 1], None, ALU.mult), [bk, bprm], [bk])
                            gsrc, bgs = z_, bz
                            qscale, kscale = SQ, 1.0
                        else:
                            for (x_, bx_, pa, pb_) in ((q_, bq, 8, 12), (k_, bk, 9, 13)):
                                self.ld(x_[:], self.pfm(b, pa, r0), [bx_])
                                self.ld(e1[:], self.pfm(b, pb_, r0), [be1], q="act")
                                self.V(lambda h: h.tensor_mul(x_[:, CL:TT], x_[:, CL:TT], cs[:]), [bx_, bcs], [bx_])
                                self.V(lambda h: h.tensor_mul(e1[:, CL:TT], e1[:, CL:TT], sn[:]), [be1, bsn], [be1])
                                self.V(lambda h: h.tensor_add(x_[:, CL:TT], x_[:, CL:TT], e1[:, CL:TT]), [bx_, be1], [bx_])
                            gdec = math.log1p(-2.0 ** (-((5.0 if d == 0 else 5.5) + hd)))
                            gsrc, bgs = None, None
                            qscale, kscale = 1.0, SQ
                        if mixer == 1:
                            self.V(lambda h: h.memset(e1[:], gdec), [be1], [be1])
                            gsrc, bgs = e1, be1
                        if d == 0:
                            self.V(lambda h: h.tensor_tensor_scan(cum[:], rst[:, 0:TT], gsrc[:], 0.0, ALU.mult, ALU.add), [brs, bgs], [bcum])
                            mcol, lcol = C // 2 - 1, C - 1
                        else:
                            self.V(lambda h: h.tensor_tensor_scan(cum[:, ::-1], rst[:, 1:TT + 1][:, ::-1], gsrc[:, ::-1], 0.0, ALU.mult, ALU.add), [brs, bgs], [bcum])
                            mcol, lcol = C // 2, 0
                        cum3 = c3(cum)
                        mB = cum3[:, :, mcol:mcol + 1].to_broadcast([128, NCH, C])
                        self.V(lambda h: h.tensor_sub(c3(tmp), cum3, mB), [bcum, btmp], [btmp])
                        self.A(lambda h: h.activation(e1[:], tmp[:], AF.Exp), [btmp, be1], [be1])
                        self.V(lambda h: h.scalar_tensor_tensor(qt[hd][:], q_[:], qscale, e1[:], ALU.mult, ALU.mult), [bq, be1], [bqt[hd]])
                        self.A(lambda h: h.activation(e1[:], tmp[:], AF.Exp, scale=-1.0), [btmp, be1], [be1])
                        self.V(lambda h: h.scalar_tensor_tensor(kt[hd][:], k_[:], kscale, e1[:], ALU.mult, ALU.mult), [bk, be1], [bkt[hd]])
                        self.A(lambda h: h.activation(dec[:, :, hd:hd + 1], cum3[:, :, lcol:lcol + 1], AF.Exp), [bcum], [bdec])
                        self.A(lambda h: h.activation(em[:, :, hd:hd + 1], cum3[:, :, mcol:mcol + 1], AF.Exp), [bcum], [bdec])
                        self.V(lambda h: h.tensor_sub(elm[:, :, hd:hd + 1], cum3[:, :, lcol:lcol + 1], cum3[:, :, mcol:mcol + 1]), [bcum], [bdec])
                        self.A(lambda h: h.activation(elm[:, :, hd:hd + 1], elm[:, :, hd:hd + 1], AF.Exp), [bdec], [bdec])
                    self.V(lambda h: h.memset(S_[:], 0.0), [bS], [bS])
                    self.V(lambda h: h.memset(Sb[:], 0.0), [bSb], [bSb])
                    order = list(range(NCH)) if d == 0 else (list(range(NCHC - 1, -1, -1)) + list(range(NCH - 1, NCHC - 1, -1)))
                    for ci, ch in enumerate(order):
                        i, hh = ch // NPT, ch % NPT
                        P0 = C * hh
                        cs_ = slice(ch * C, ch * C + C)
                        j = ci % 2
                        pS, pO, pK, pU = self.ps[0 + j], self.ps[2 + j], self.ps[4 + j], self.ps[6 + j]
                        bpS, bpO, bpK, bpU = self.pb[0 + j], self.pb[2 + j], self.pb[4 + j], self.pb[6 + j]
                        for hd in range(4):
                            self.PE(lambda h: h.matmul(pS[P0:P0 + C, hd * C:(hd + 1) * C], kt[hd][:, cs_], qt[hd][:, cs_], start=True, stop=True),
                                    [bkt[hd], bqt[hd]], [bpS], inc=(hd == 3))
                        self.G(lambda h: h.memset(sT[j][P0:P0 + C], 0.0), [bsT[j]], [bsT[j]])
                        self.V(lambda h: h.copy_predicated(sT[j][P0:P0 + C], cmask[P0:P0 + C, d].bitcast(mybir.dt.uint32), pS[P0:P0 + C, 0:4 * C].rearrange("p (h t) -> p h t", t=C)),
                               [bpS, self.bconst, bsT[j]], [bsT[j]])
                        self.V(lambda h: h.tensor_mul(Sb[:], S_[:], em[:, ch, :].unsqueeze(2).to_broadcast([128, 4, 128])), [bS, bdec, bSb], [bSb])
                        for hd in range(4):
                            osl = pO[P0:P0 + C, hd * 128:(hd + 1) * 128]
                            self.PE(lambda h: h.matmul(osl, sT[j][P0:P0 + C, hd, :], vbf[P0:P0 + C, i, hd * 128:(hd + 1) * 128], start=True, stop=False),
                                    [bsT[j], bv], [bpO], inc=False)
                            self.PE(lambda h: h.matmul(osl, qt[hd][:, cs_], Sb[:, hd, :], start=False, stop=True), [bqt[hd], bSb], [bpO], inc=(hd == 3))
                        if d == 0:
                            self.A(lambda h: h.copy(oacc[P0:P0 + C, i, :], pO[P0:P0 + C, :]), [bpO], [bo])
                        else:
                            self.V(lambda h: h.tensor_add(oacc[P0:P0 + C, i, :], oacc[P0:P0 + C, i, :], pO[P0:P0 + C, :]), [bpO, bo], [bo])
                        pKb = pK[:].bitcast(BF16)
                        for hd in range(4):
                            self.PE(lambda h: h.transpose(pKb[P0:P0 + C, hd * 128:(hd + 1) * 128], kt[hd][:, cs_], self.idb[:]), [bkt[hd], self.bconst], [bpK], inc=(hd == 3))
                        self.A(lambda h: h.copy(khT[j][P0:P0 + C].rearrange("p h d -> p (h d)"), pKb[P0:P0 + C, 0:512]), [bpK], [bkhT[j]])
                        for hd in range(4):
                            self.PE(lambda h: h.matmul(pU[:, hd * 128:(hd + 1) * 128], khT[j][P0:P0 + C, hd, :], vbf[P0:P0 + C, i, hd * 128:(hd + 1) * 128], start=True, stop=True),
                                    [bkhT[j], bv], [bpU], inc=(hd == 3))
                        self.V(lambda h: h.tensor_mul(Ut[:], pU[:, :].rearrange("p (h d) -> p h d", d=128), elm[:, ch, :].unsqueeze(2).to_broadcast([128, 4, 128])), [bpU, bdec, bUt], [bUt])
                        self.V(lambda h: h.tensor_mul(S_[:], S_[:], dec[:, ch, :].unsqueeze(2).to_broadcast([128, 4, 128])), [bS, bdec], [bS])
                        self.V(lambda h: h.tensor_add(S_[:], S_[:], Ut[:]), [bS, bUt], [bS])
                self.S.barrier()
            with ExitStack() as s3:
                graw = self.sb(s3, "graw", [128, NT, W]); bg = Buf()
                self.ld(graw[:], self.ptm(b, 7 if mixer == 0 else 11), [bg], q="act")
                self.A(lambda h: h.activation(graw[:], graw[:], AF.Silu), [bg], [bg])
                sq = self.sb(s3, "gsq", [128, NT, W]); ssq = self.sb(s3, "ssq", [128, NT * 4]); bsq = Buf()
                o4 = oacc[:].rearrange("p i (h d) -> p (i h) d", d=128)
                self.V(lambda h: h.tensor_mul(sq[:], oacc[:], oacc[:]), [bo], [bsq])
                self.V(lambda h: h.tensor_reduce(ssq[:], sq[:].rearrange("p i (h d) -> p (i h) d", d=128), AX.X, ALU.add), [bsq], [bsq])
                self.A(lambda h: h.activation(ssq[:], ssq[:], AF.Sqrt, scale=1.0 / 128, bias=self.epsc[:, 0:1]), [bsq, self.bconst], [bsq])
                self.V(lambda h: h.reciprocal(ssq[:], ssq[:]), [bsq], [bsq])
                self.V(lambda h: h.tensor_mul(o4, o4, ssq[:].unsqueeze(2).to_broadcast([128, NT * 4, 128])), [bo, bsq], [bo])
                self.V(lambda h: h.tensor_mul(vbf[:], oacc[:], graw[:]), [bo, bg, bv], [bv])
                mst = self.sb(s3, "mst", [128, 4, TT], BF16); bm = Buf()
                for i in range(NT):
                    p, pb = self.ps[i % 4], self.pb[i % 4]
                    pv = p[:].bitcast(BF16)
                    for hd in range(4):
                        self.PE(lambda h: h.transpose(pv[:, hd * 128:(hd + 1) * 128], vbf[:, i, hd * 128:(hd + 1) * 128], self.idb[:]), [bv, self.bconst], [pb], inc=(hd == 3))
                    self.A(lambda h: h.copy(mst[:, :, i * 128:(i + 1) * 128], pv[:, 0:512].rearrange("p (h t) -> p h t", t=128)), [pb], [bm])
                base = 2 * W + mixer * W
                self.st(self.mixT[b, base:base + W, :].rearrange("(h p) t -> p h t", p=128), mst[:], [bm])
                self.S.barrier()

    def phase_s5(self, l, b):
        c = self.c
        TT, CL = c.TT, c.CTXL
        tgs = tok_groups(TT)
        with ExitStack() as st:
            T = lambda n, dt=F32: self.sb(st, n, [128, TT], dt)
            lam = self.sb(st, "lam", [128, 3, 2, 16]); bp = Buf()
            self.ld(lam[:], self.s5lam[l], [bp])
            P = lambda n: self.sb(st, n, [128, 32])
            dt_, mag, th, thc, are, aim, den, wre, wim, t1, t2 = (P(f"p{i}") for i in range(11))
            lr = lam[:, 0].rearrange("p d s -> p (d s)"); li = lam[:, 1].rearrange("p d s -> p (d s)"); ldt = lam[:, 2].rearrange("p d s -> p (d s)")
            self.A(lambda h: h.activation(dt_[:], ldt, AF.Exp), [bp], [bp])
            self.V(lambda h: h.tensor_mul(t1[:], lr, dt_[:]), [bp], [bp])
            self.A(lambda h: h.activation(mag[:], t1[:], AF.Exp), [bp], [bp])
            self.V(lambda h: h.tensor_mul(th[:], li, dt_[:]), [bp], [bp])
            sn, bsn, cs, bcs = self.sincos(st, th, bp, 32, "ab")
            self.V(lambda h: h.tensor_mul(are[:], mag[:], cs[:]), [bp, bcs], [bp])
            self.V(lambda h: h.tensor_mul(aim[:], mag[:], sn[:]), [bp, bsn], [bp])
            self.V(lambda h: h.tensor_mul(den[:], lr, lr), [bp], [bp])
            self.V(lambda h: h.tensor_mul(t1[:], li, li), [bp], [bp])
            self.V(lambda h: h.tensor_add(den[:], den[:], t1[:]), [bp], [bp])
            self.V(lambda h: h.reciprocal(den[:], den[:]), [bp], [bp])
            self.V(lambda h: h.tensor_scalar(t2[:], are[:], -1.0, None, ALU.add), [bp], [bp])
            self.V(lambda h: h.tensor_mul(wre[:], t2[:], lr), [bp], [bp])
            self.V(lambda h: h.tensor_mul(t1[:], aim[:], li), [bp], [bp])
            self.V(lambda h: h.tensor_add(wre[:], wre[:], t1[:]), [bp], [bp])
            self.V(lambda h: h.tensor_mul(wre[:], wre[:], den[:]), [bp], [bp])
            self.V(lambda h: h.tensor_mul(wim[:], aim[:], lr), [bp], [bp])
            self.V(lambda h: h.tensor_mul(t1[:], t2[:], li), [bp], [bp])
            self.V(lambda h: h.tensor_sub(wim[:], wim[:], t1[:]), [bp], [bp])
            self.V(lambda h: h.tensor_mul(wim[:], wim[:], den[:]), [bp], [bp])
            nwim = P("nwim")
            self.V(lambda h: h.tensor_scalar(nwim[:], wim[:], -1.0, None, ALU.mult), [bp], [bp])
            thi = self.sb(st, "thi", [128, 32], I32); thf = P("thf")
            self.V(lambda h: h.tensor_scalar(thc[:], th[:], 1.0 / TWO_PI, None, ALU.mult), [bp], [bp])
            self.wrap_frac(thc, thi, thf, bp)
            dsk = self.sb(st, "dsk", [128, 4]); self.ld(dsk[:], self.s5d[l], [bp])
            with ExitStack() as s2:
                T = lambda n, dt=F32: self.sb(s2, n, [128, TT], dt)
                kid = [T("kid0"), T("kid1")]; bkid = Buf()
                self.ld(kid[0][:], self.kidx[0], [bkid]); self.ld(kid[1][:], self.kidx[1], [bkid], q="act")
                ub = self.sb(s2, "ub", [128, 4, TT], BF16); bu = Buf()
                self.ldc(ub[:], self.Pfm[b, 0:W, :].rearrange("(c p) t -> p c t", p=128), [bu])
                uf = T("uf"); buf_ = Buf()
                Bt = [self.sb(s2, f"Bt{i}", [128, 2, 128], BF16) for i in range(2)]; bB = [Buf(), Buf()]
                Cf = [self.sb(s2, f"Cf{i}", [128, 2, 128]) for i in range(2)]; bC = [Buf(), Buf()]
                Cw = [self.sb(s2, f"Cw{i}", [128, 2, 128], BF16) for i in range(2)]; bCw = [Buf(), Buf()]
                ct = self.sb(s2, "ct", [128, 128]); bct = Buf()
                y, yi = T("ty"), T("tyi", I32)
                sn_, cs_ = T("tsn"), T("tcs"); by, bsn_, bcs_ = Buf(), Buf(), Buf()
                bur, bui, gre, gim, t3 = T("bur"), T("bui"), T("gre"), T("gim"), T("t3")
                bbur, bbui, bgre, bgim, bt3 = (Buf() for _ in range(5))
                hre = [T(f"hre{i}", BF16) for i in range(2)]; him = [T(f"him{i}", BF16) for i in range(2)]
                bhre = [Buf(), Buf()]; bhim = [Buf(), Buf()]
                ytmp = T("ytmp"); bytmp = Buf()
                yo = T("yo"); byo = Buf()
                k = 0
                for cc in range(4):
                    self.ld(uf[:], self.Pfm[b, cc * 128:(cc + 1) * 128, :], [buf_])
                    pY = [self.ps[4 + g] for g in range(4)]; bpY = [self.pb[4 + g] for g in range(4)]
                    first = True
                    for d in range(2):
                        for s4 in range(4):
                            sc = cc * 4 + s4
                            col = d * 16 + sc
                            j = k % 2; k += 1
                            lastone = (d == 1 and s4 == 3)
                            self.ldc(Bt[j][:], self.s5B[l, d, sc], [bB[j]])
                            self.ld(Cf[j][:], self.s5C[l, d, sc], [bC[j]], q="act")
                            self.V(lambda h: h.tensor_scalar(ct[:], Cf[j][:, 1, :], nwim[:, col:col + 1], None, ALU.mult), [bC[j], bp, bct], [bct])
                            self.V(lambda h: h.scalar_tensor_tensor(Cw[j][:, 0, :], Cf[j][:, 0, :], wre[:, col:col + 1], ct[:], ALU.mult, ALU.add), [bC[j], bp, bct], [bCw[j]])
                            self.V(lambda h: h.tensor_scalar(ct[:], Cf[j][:, 1, :], wre[:, col:col + 1], -1.0, ALU.mult, ALU.mult), [bC[j], bp, bct], [bct])
                            self.V(lambda h: h.scalar_tensor_tensor(Cw[j][:, 1, :], Cf[j][:, 0, :], nwim[:, col:col + 1], ct[:], ALU.mult, ALU.add), [bC[j], bp, bct], [bCw[j]])
                            self.V(lambda h: h.tensor_scalar(yi[:], kid[d][:], thc[:, col:col + 1], None, ALU.mult), [bkid, bp, by], [by])
                            self.V(lambda h: h.tensor_copy(cs_[:], yi[:]), [by, bcs_], [bcs_])
                            self.V(lambda h: h.scalar_tensor_tensor(y[:], kid[d][:], thc[:, col:col + 1], cs_[:], ALU.mult, ALU.subtract), [bkid, bp, bcs_, by], [by])
                            self.A(lambda h: h.activation(sn_[:], y[:], AF.Sin, scale=TWO_PI), [by, bsn_], [bsn_])
                            self.A(lambda h: h.activation(cs_[:], y[:], AF.Abs), [by, bcs_], [bcs_])
                            self.A(lambda h: h.activation(cs_[:], cs_[:], AF.Sin, scale=-TWO_PI, bias=self.hpi[:, 0:1]), [bcs_, self.bconst], [bcs_])
                            for (ri, dst, bd) in ((0, bur, bbur), (1, bui, bbui)):
                                for gi, (t0, n) in enumerate(tgs):
                                    ps, pb = self.ps[gi % 4], self.pb[gi % 4]
                                    self.PE(lambda h: h.matmul(ps[:, 0:n], Bt[j][:, ri, :], ub[:, cc, t0:t0 + n], start=True, stop=True), [bB[j], bu], [pb])
                                    self.A(lambda h: h.copy(dst[:, t0:t0 + n], ps[:, 0:n]), [pb], [bd])
                            self.V(lambda h: h.tensor_mul(gre[:], bur[:], cs_[:]), [bbur, bcs_], [bgre])
                            self.V(lambda h: h.tensor_mul(t3[:], bui[:], sn_[:]), [bbui, bsn_], [bt3])
                            self.V(lambda h: h.tensor_add(gre[:], gre[:], t3[:]), [bgre, bt3], [bgre])
                            self.V(lambda h: h.tensor_mul(gim[:], bui[:], cs_[:]), [bbui, bcs_], [bgim])
                            self.V(lambda h: h.tensor_mul(t3[:], bur[:], sn_[:]), [bbur, bsn_], [bt3])
                            self.V(lambda h: h.tensor_sub(gim[:], gim[:], t3[:]), [bgim, bt3], [bgim])
                            for (src, dst, bs_, bd) in ((gre, bur, bgre, bbur), (gim, bui, bgim, bbui)):
                                if d == 0:
                                    self.V(lambda h: h.tensor_tensor_scan(dst[:], mag[:, col:col + 1].to_broadcast([128, TT]), src[:], 0.0, ALU.mult, ALU.add), [bs_, bp], [bd])
                                else:
                                    mC = mag[:, col:col + 1].to_broadcast([128, CL]); mL = mag[:, col:col + 1].to_broadcast([128, TT - CL])
                                    self.V(lambda h: h.tensor_tensor_scan(dst[:, 0:CL][:, ::-1], mC, src[:, 0:CL][:, ::-1], 0.0, ALU.mult, ALU.add), [bs_, bp], [bd])
                                    self.V(lambda h: h.tensor_tensor_scan(dst[:, CL:TT][:, ::-1], mL, src[:, CL:TT][:, ::-1], dst[:, 0:1], ALU.mult, ALU.add), [bs_, bp, bd], [bd])
                            self.V(lambda h: h.tensor_mul(gre[:], bur[:], cs_[:]), [bbur, bcs_], [bgre])
                            self.V(lambda h: h.tensor_mul(t3[:], bui[:], sn_[:]), [bbui, bsn_], [bt3])
                            self.V(lambda h: h.tensor_sub(hre[j][:], gre[:], t3[:]), [bgre, bt3], [bhre[j]])
                            self.V(lambda h: h.tensor_mul(gim[:], bur[:], sn_[:]), [bbur, bsn_], [bgim])
                            self.V(lambda h: h.tensor_mul(t3[:], bui[:], cs_[:]), [bbui, bcs_], [bt3])
                            self.V(lambda h: h.tensor_add(him[j][:], gim[:], t3[:]), [bgim, bt3], [bhim[j]])
                            for gi, (t0, n) in enumerate(tgs):
                                if gi < 4:
                                    self.PE(lambda h: h.matmul(pY[gi][:, 0:n], Cw[j][:, 0, :], hre[j][:, t0:t0 + n], start=first, stop=False), [bCw[j], bhre[j]], [bpY[gi]], inc=False)
                                    self.PE(lambda h: h.matmul(pY[gi][:, 0:n], Cw[j][:, 1, :], him[j][:, t0:t0 + n], start=False, stop=lastone), [bCw[j], bhim[j]], [bpY[gi]])
                                else:
                                    ps, pb = self.ps[gi % 4], self.pb[gi % 4]
                                    self.PE(lambda h: h.matmul(ps[:, 0:n], Cw[j][:, 0, :], hre[j][:, t0:t0 + n], start=True, stop=False), [bCw[j], bhre[j]], [pb], inc=False)
                                    self.PE(lambda h: h.matmul(ps[:, 0:n], Cw[j][:, 1, :], him[j][:, t0:t0 + n], start=False, stop=True), [bCw[j], bhim[j]], [pb])
                                    if first:
                                        self.V(lambda h: h.tensor_copy(ytmp[:, t0:t0 + n], ps[:, 0:n]), [pb], [bytmp])
                                    else:
                                        self.V(lambda h: h.tensor_add(ytmp[:, t0:t0 + n], ytmp[:, t0:t0 + n], ps[:, 0:n]), [pb, bytmp], [bytmp])
                            first = False
                    for gi, (t0, n) in enumerate(tgs):
                        if gi < 4:
                            self.V(lambda h: h.scalar_tensor_tensor(yo[:, t0:t0 + n], uf[:, t0:t0 + n], dsk[:, cc:cc + 1], pY[gi][:, 0:n], ALU.mult, ALU.add), [buf_, bp, bpY[gi]], [byo])
                        else:
                            self.V(lambda h: h.scalar_tensor_tensor(yo[:, t0:t0 + n], uf[:, t0:t0 + n], dsk[:, cc:cc + 1], ytmp[:, t0:t0 + n], ALU.mult, ALU.add), [buf_, bp, bytmp], [byo])
                    self.gelu(yo[:], yo[:], t3[:], [byo], byo, bt3)
                    self.st(self.ygD[b, cc * 128:(cc + 1) * 128, :], yo[:], [byo])
                self.S.barrier()
            with ExitStack() as s3:
                T = lambda n, dt=F32: self.sb(s3, n, [128, TT], dt)
                yg = self.sb(s3, "yg", [128, 4, TT]); ygb = self.sb(s3, "ygb", [128, 4, TT], BF16); byg = Buf()
                self.ld(yg[:], self.ygD[b].rearrange("(c p) t -> p c t", p=128), [byg])
                self.A(lambda h: h.copy(ygb[:], yg[:]), [byg], [byg])
                gw = self.sb(s3, "gw", [128, 4, W], BF16); gb = self.sb(s3, "gb", [128, 4]); bgw = Buf()
                self.ldc(gw[:], self.gluw[l].rearrange("(kc kp) n -> kp kc n", kp=128), [bgw])
                self.ld(gb[:], self.glub[l], [bgw])
                ao = [T("ao0", BF16), T("ao1", BF16)]; bao = [Buf(), Buf()]
                sg = T("sg"); bsg = Buf()
                for oc in range(4):
                    j = oc % 2
                    for gi, (t0, n) in enumerate(tgs):
                        ps, pb = self.ps[gi % 4], self.pb[gi % 4]
                        for kc in range(4):
                            self.PE(lambda h: h.matmul(ps[:, 0:n], gw[:, kc, oc * 128:(oc + 1) * 128], ygb[:, kc, t0:t0 + n], start=(kc == 0), stop=(kc == 3)), [bgw, byg], [pb], inc=(kc == 3))
                        self.A(lambda h: h.activation(sg[:, t0:t0 + n], ps[:, 0:n], AF.Sigmoid, bias=gb[:, oc:oc + 1]), [pb, bgw], [bsg])
                    self.V(lambda h: h.tensor_mul(ao[j][:], yg[:, oc, :], sg[:]), [byg, bsg], [bao[j]])
                    self.st(self.mixT[b, oc * 128:(oc + 1) * 128, :], ao[j][:], [bao[j]])
                self.S.barrier()

    def phase_out(self, l, b, xsrc, last):
        c = self.c
        TT, NT = c.TT, c.NT
        with ExitStack() as st:
            wo = self.sb(st, "wo", [128, KC, D], BF16); bwo = Buf()
            wsrc = self.w_out[l].rearrange("(kc kp) n -> kp kc n", kp=128)
            for g in range(4):
                self.ldc(wo[:, :, g * 512:(g + 1) * 512], wsrc[:, :, g * 512:(g + 1) * 512], [bwo])
            gB = {}
            for src in ([b] if last else [b, 2]):
                gB[src] = self.bcast_row(st, 0, src, f"gmsa{src}")
            A2 = {}; B2 = {}
            if c.sparse:
                for src in ([b] if last else [b, 2]):
                    A2[src] = self.bcast_row(st, 2, src, f"a2r{src}")
                    B2[src] = self.bcast_row(st, 3, src, f"b2r{src}")
            htk = [self.sb(st, f"htk{i}", [128, D], BF16) for i in range(2)]; bhtk = [Buf(), Buf()]
            htf = self.sb(st, "htf", [128, D]); bhtf = Buf()
            s01 = [self.sb(st, f"s01{i}", [128, NE]) for i in range(2)]; bs01 = [Buf(), Buf()]
            rk = [self.sb(st, f"rk{i}", [128, NE]) for i in range(2)]; brk = [Buf(), Buf()]
            rwt = self.sb(st, "rwt", [128, KC, NE]); rb = self.sb(st, "rb", [128, NE]); brw = Buf()
            self.ld(rwt[:], self.rw[:, :, :], [brw])
            self.ld(rb[:], self.rbias[0:1, :].partition_broadcast(128)[:, 0, :], [brw])
            nt = self.norm_tiles(st); nt["xsf"] = self.sb(st, "xsf", [128, D])
            mt = [self.sb(st, f"mt{i}", [128, KC, 128], BF16) for i in range(2)]; bmt = [Buf(), Buf()]
            xt = [self.sb(st, f"xo{i}", [128, D]) for i in range(2)]; bx = [Buf(), Buf()]
            h2f = self.sb(st, "h2f", [128, KC, 128]); h2b = [self.sb(st, f"h2b{i}", [128, KC, 128], BF16) for i in range(2)]
            bh2f = Buf(); bh2b = [Buf(), Buf()]
            R = lambda n, w=NE: self.sb(st, n, [128, w])
            scr, bia, m1, m2, gs, gmx, ing, sel, tmp4, tmp16, gsum = R("scr"), R("bia"), R("m1", 4), R("m2", 4), R("gs", 4), R("gmx", 1), R("ing", 4), R("sel"), R("tmp4", 4), R("tmp16"), R("gsum", 1)
            gout = [R("gout0"), R("gout1")]; bgo = [Buf(), Buf()]
            br_ = Buf()
            tmo = [self.sb(st, f"tmo{i}", [128, 512]) for i in range(2)]; btmo = [Buf(), Buf()]
            tiles = range(c.NTC, NT) if last else range(NT)
            for i in tiles:
                j = i % 2
                src = 2 if i < c.NTC else b
                self.ld(mt[j][:], self.mixT[b, :, i * 128:(i + 1) * 128].rearrange("(kc kp) t -> kp kc t", kp=128), [bmt[j]], q="act")
                self.ld(xt[j][:], xsrc[b, i * 128:(i + 1) * 128, :], [bx[j]])
                for g in range(4):
                    ps, pb = self.ps[g], self.pb[g]
                    for kc in range(KC):
                        self.PE(lambda h: h.matmul(ps[:, :], mt[j][:, kc, :], wo[:, kc, g * 512:(g + 1) * 512], start=(kc == 0), stop=(kc == KC - 1)), [bmt[j], bwo], [pb], inc=(kc == KC - 1))
                    gt_, bgt_ = gB[src]
                    sl = slice(g * 512, (g + 1) * 512)
                    self.V(lambda h: h.tensor_tensor(tmo[g % 2][:], ps[:, :], gt_[:, sl], ALU.mult), [pb, bgt_], [btmo[g % 2]])
                    self.V(lambda h: h.tensor_add(xt[j][:, sl], xt[j][:, sl], tmo[g % 2][:]), [btmo[g % 2], bx[j]], [bx[j]])
                self.st(self.xres[b, i * 128:(i + 1) * 128, :], xt[j][:], [bx[j]])
                self.norm_T(nt, xt[j], bx[j], 1, src, lambda kc: h2f[:, kc, :], bh2f, fp32=True)
                if not c.sparse:
                    self.A(lambda h: h.copy(h2b[j][:], h2f[:]), [bh2f], [bh2b[j]])
                    self.st(self.h2T[b, :, i * 128:(i + 1) * 128].rearrange("(kc kp) t -> kp kc t", kp=128), h2b[j][:], [bh2b[j]], q="act")
                else:
                    xsf = nt["xsf"]
                    self.V(lambda h: h.tensor_mul(htf[:], xsf[:], A2[src][0][:]), [nt["bxs"], A2[src][1], bhtf], [bhtf])
                    self.V(lambda h: h.tensor_add(htk[j][:], htf[:], B2[src][0][:]), [bhtf, B2[src][1]], [bhtk[j]])
                    self.st(self.h2tok[b * TT + i * 128: b * TT + (i + 1) * 128, :], htk[j][:], [bhtk[j]], q="act")
                pr, bpr = self.ps[4], self.pb[4]
                for kc in range(KC):
                    self.PE(lambda h: h.matmul(pr[:, 0:NE], h2f[:, kc, :], rwt[:, kc, :], start=(kc == 0), stop=(kc == KC - 1)), [bh2f, brw], [bpr], inc=(kc == KC - 1))
                self.A(lambda h: h.activation(scr[:], pr[:, 0:NE], AF.Sigmoid), [bpr], [br_])
                self.V(lambda h: h.tensor_add(bia[:], scr[:], rb[:]), [br_, brw], [br_])
                b4 = bia[:].rearrange("p (g e) -> p g e", e=4)
                self.V(lambda h: h.tensor_reduce(m1[:], b4, AX.X, ALU.max), [br_], [br_])
                self.V(lambda h: h.tensor_tensor(tmp16[:].rearrange("p (g e) -> p g e", e=4), b4, m1[:].unsqueeze(2).to_broadcast([128, 4, 4]), ALU.is_equal), [br_], [br_])
                self.V(lambda h: h.scalar_tensor_tensor(tmp16[:], tmp16[:], -1e9, bia[:], ALU.mult, ALU.add), [br_], [br_])
                self.V(lambda h: h.tensor_reduce(m2[:], tmp16[:].rearrange("p (g e) -> p g e", e=4), AX.X, ALU.max), [br_], [br_])
                self.V(lambda h: h.tensor_add(gs[:], m1[:], m2[:]), [br_], [br_])
                self.V(lambda h: h.tensor_reduce(gmx[:], gs[:], AX.X, ALU.max), [br_], [br_])
                self.V(lambda h: h.tensor_tensor(ing[:], gs[:], gmx[:].to_broadcast([128, 4]), ALU.is_equal), [br_], [br_])
                self.V(lambda h: h.tensor_tensor(sel[:].rearrange("p (g e) -> p g e", e=4), b4, m2[:].unsqueeze(2).to_broadcast([128, 4, 4]), ALU.is_ge), [br_], [br_])
                self.V(lambda h: h.tensor_mul(sel[:].rearrange("p (g e) -> p g e", e=4), sel[:].rearrange("p (g e) -> p g e", e=4), ing[:].unsqueeze(2).to_broadcast([128, 4, 4])), [br_], [br_])
                if c.sparse:
                    self.V(lambda h: h.tensor_copy(s01[j][:], sel[:]), [br_], [bs01[j]])
                    pk, bpk = self.ps[5], self.pb[5]
                    self.PE(lambda h: h.matmul(pk[:, 0:NE], self.ltt[:, 0, :], s01[j][:], start=True, stop=True), [bs01[j], self.bconst], [bpk], inc=False)
                    self.PE(lambda h: h.matmul(pk[:, NE:2 * NE], self.ltt[:, 1, :], s01[j][:], start=True, stop=True), [bs01[j], self.bconst], [bpk])
                    self.V(lambda h: h.tensor_add(rk[j][:], pk[:, 0:NE], self.run[:]), [bpk, self.brun], [brk[j]])
                    self.V(lambda h: h.tensor_add(self.run[:], self.run[:], pk[:, NE:2 * NE]), [bpk, self.brun], [self.brun])
                    self.st(self.selD[b * TT + i * 128: b * TT + (i + 1) * 128, :], s01[j][:], [bs01[j]])
                    self.st(self.rankD[b * TT + i * 128: b * TT + (i + 1) * 128, :], rk[j][:], [brk[j]], q="act")
                self.V(lambda h: h.tensor_mul(sel[:], sel[:], scr[:]), [br_], [br_])
                self.V(lambda h: h.tensor_reduce(gsum[:], sel[:], AX.X, ALU.add), [br_], [br_])
                self.V(lambda h: h.reciprocal(gsum[:], gsum[:]), [br_], [br_])
                self.V(lambda h: h.tensor_scalar(gout[j][:], sel[:], gsum[:, 0:1], None, ALU.mult), [br_], [bgo[j]])
                self.st(self.gates[b, i * 128:(i + 1) * 128, :], gout[j][:], [bgo[j]], q="act")

    def phase_moe(self, l, b, last):
        c = self.c
        NT = c.NT
        tiles = list(range(c.NTC, NT)) if last else list(range(NT))
        STM = 6
        supers = [tiles[i:i + STM] for i in range(0, len(tiles), STM)]
        with ExitStack() as st:
            h2 = self.sb(st, "h2s", [128, KC, STM * 128], BF16); bh2 = Buf()
            gt = self.sb(st, "gts", [128, STM, NE]); bgt = Buf()
            yacc = self.sb(st, "yacc", [128, STM, D]); bya = Buf()
            he = self.sb(st, "he", [128, 8, STM * 128], BF16); bhe = Buf()
            wdn = [self.sb(st, f"wdn{i}", [128, 8, D], BF16) for i in range(2)]; bwd = [Buf(), Buf()]
            wgu = [self.sb(st, f"wgu{i}", [128, 2, KC, 128], BF16) for i in range(2)]; bwgu = [Buf() for _ in range(2)]
            sg = self.sb(st, "sgm", [128, STM * 128]); bsg = Buf()
            gB = {}
            for src in ([b] if last else [b, 2]):
                gB[src] = self.bcast_row(st, 1, src, f"gmlp{src}")
            xt = [self.sb(st, "xm0", [128, D])] * 2; bx = [Buf()] * 2
            ew = 0
            for sup in supers:
                n_t = len(sup); ntok = n_t * 128
                t0 = sup[0] * 128
                self.ld(h2[:, :, 0:ntok], self.h2T[b, :, t0:t0 + ntok].rearrange("(kc kp) t -> kp kc t", kp=128), [bh2])
                self.ld(gt[:, 0:n_t, :], self.gates[b, t0:t0 + ntok, :].rearrange("(i p) e -> p i e", p=128), [bgt], q="act")
                tg = tok_groups(ntok)
                for e in range(NE):
                    jd = e % 2
                    wds = self.wd[l, e].rearrange("(fc fp) n -> fp fc n", fp=128)
                    for g in range(2):
                        self.ldc(wdn[jd][:, :, g * 1024:(g + 1) * 1024], wds[:, :, g * 1024:(g + 1) * 1024], [bwd[jd]])
                    for fc in range(8):
                        jw = ew % 2; ew += 1
                        self.ldc(wgu[jw][:, 0], self.wg[l, e][:, fc * 128:(fc + 1) * 128].rearrange("(kc kp) f -> kp kc f", kp=128), [bwgu[jw]])
                        self.ldc(wgu[jw][:, 1], self.wu[l, e][:, fc * 128:(fc + 1) * 128].rearrange("(kc kp) f -> kp kc f", kp=128), [bwgu[jw]])
                        for gi, (s0, n) in enumerate(tg):
                            pG, pU = self.ps[gi], self.ps[2 + gi]; bpG, bpU = self.pb[gi], self.pb[2 + gi]
                            for kc in range(KC):
                                self.PE(lambda h: h.matmul(pG[:, 0:n], wgu[jw][:, 0, kc, :], h2[:, kc, s0:s0 + n], start=(kc == 0), stop=(kc == KC - 1)), [bwgu[jw], bh2], [bpG], inc=(kc == KC - 1))
                            for kc in range(KC):
                                self.PE(lambda h: h.matmul(pU[:, 0:n], wgu[jw][:, 1, kc, :], h2[:, kc, s0:s0 + n], start=(kc == 0), stop=(kc == KC - 1)), [bwgu[jw], bh2], [bpU], inc=(kc == KC - 1))
                            self.A(lambda h: h.activation(sg[:, s0:s0 + n], pG[:, 0:n], AF.Silu), [bpG], [bsg])
                            self.V(lambda h: h.tensor_mul(he[:, fc, s0:s0 + n], sg[:, s0:s0 + n], pU[:, 0:n]), [bsg, bpU], [bhe])
                    for ti in range(n_t):
                        for g in range(4):
                            ps, pb = self.ps[4 + g], self.pb[4 + g]
                            for fc in range(8):
                                self.PE(lambda h: h.matmul(ps[:, :], he[:, fc, ti * 128:(ti + 1) * 128], wdn[jd][:, fc, g * 512:(g + 1) * 512], start=(fc == 0), stop=(fc == 7)), [bhe, bwd[jd]], [pb], inc=(fc == 7))
                            ysl = yacc[:, ti, g * 512:(g + 1) * 512]
                            if e == 0:
                                self.V(lambda h: h.tensor_scalar(ysl, ps[:, :], gt[:, ti, e:e + 1], None, ALU.mult), [pb, bgt], [bya])
                            else:
                                self.V(lambda h: h.scalar_tensor_tensor(ysl, ps[:, :], gt[:, ti, e:e + 1], ysl, ALU.mult, ALU.add), [pb, bgt, bya], [bya])
                for ti, i in enumerate(sup):
                    j = i % 2
                    src = 2 if i < c.NTC else b
                    gt_, bgt_ = gB[src]
                    self.ld(xt[j][:], self.xres[b, i * 128:(i + 1) * 128, :], [bx[j]])
                    self.V(lambda h: h.tensor_mul(yacc[:, ti, :], yacc[:, ti, :], gt_[:]), [bya, bgt_], [bya])
                    self.V(lambda h: h.tensor_add(xt[j][:], xt[j][:], yacc[:, ti, :]), [bya, bx[j]], [bx[j]])
                    self.st(self.xres[b, i * 128:(i + 1) * 128, :], xt[j][:], [bx[j]])


    def moe_tiles(self, last):
        c = self.c
        tl = range(c.NTC, c.NT) if last else range(c.NT)
        return [(b, i) for b in range(c.NB) for i in tl]

    def n_slots(self, last):
        c = self.c
        return (2 * len(self.moe_tiles(last)) * 128 + c.SLOT - 1) // c.SLOT + NE

    def phase_route(self, l, last):
        c = self.c
        TT = c.TT
        NS = self.n_slots(last)
        SL = float(c.SLOT)
        with ExitStack() as st:
            R = lambda n, w=NE, dt=F32: self.sb(st, n, [128, w], dt)
            x, xi, xf, nsl, cum, base, one = R("rx"), R("rxi", NE, I32), R("rxf"), R("nsl"), R("cum"), R("base"), R("one")
            bq = Buf()
            self.V(lambda h: h.tensor_scalar(x[:], self.run[:], SL - 1.0, 1.0 / SL, ALU.add, ALU.mult), [self.brun], [bq])
            self.V(lambda h: h.tensor_copy(xi[:], x[:]), [bq], [bq])
            self.V(lambda h: h.tensor_copy(xf[:], xi[:]), [bq], [bq])
            self.V(lambda h: h.tensor_tensor(nsl[:], xf[:], x[:], ALU.is_gt), [bq], [bq])
            self.V(lambda h: h.tensor_sub(nsl[:], xf[:], nsl[:]), [bq], [bq])
            self.V(lambda h: h.memset(one[:], 1.0), [], [bq])
            self.V(lambda h: h.tensor_tensor_scan(cum[:], one[:], nsl[:], 0.0, ALU.mult, ALU.add), [bq], [bq])
            self.V(lambda h: h.tensor_sub(base[:], cum[:], nsl[:]), [bq], [bq])
            self.V(lambda h: h.tensor_scalar(base[:], base[:], SL, None, ALU.mult), [bq], [bq])
            sio = R("sio", NS); ge = self.sb(st, "ge", [128, NS, NE]); es = R("es", NS)
            self.ld(sio[:], self.siota[:, 0:NS], [bq])
            self.V(lambda h: h.tensor_tensor(ge[:], sio[:].unsqueeze(2).to_broadcast([128, NS, NE]), cum[:].unsqueeze(1).to_broadcast([128, NS, NE]), ALU.is_ge), [bq], [bq])
            self.V(lambda h: h.tensor_reduce(es[:], ge[:], AX.X, ALU.add), [bq], [bq])
            self.V(lambda h: h.tensor_scalar(es[:], es[:], float(NE - 1), None, ALU.min), [bq], [bq])
            self.V(lambda h: h.tensor_scalar(self.es2[:, 0, 0:NS], es[:], float(D), None, ALU.mult), [bq], [self.bes])
            self.V(lambda h: h.tensor_scalar(self.es2[:, 1, 0:NS], es[:], float(DFF), None, ALU.mult), [bq], [self.bes])
            zg = R("zg", NS * c.SLOT // 128); bgs = Buf()
            self.V(lambda h: h.memset(zg[:], 0.0), [], [bq])
            self.st(self.gsD[0:NS * c.SLOT, :].rearrange("(p r) o -> p (r o)", p=128), zg[:], [bq])
            self.S.barrier()
            sl = [R("sl0"), R("sl1")]; gl = [R("gl0"), R("gl1")]; rl = [R("rl0"), R("rl1")]; bl = [Buf(), Buf()]
            pos, pm, t1, eq = R("pos"), R("pm"), R("t1"), R("eq")
            pp = R("pp", 2); gg = [R("gg0", 2), R("gg1", 2)]; bgg = [Buf(), Buf()]; gsum = R("gsm", 1)
            ht = [self.sb(st, f"rht{i}", [128, D], BF16) for i in range(2)]; bht = [Buf(), Buf()]
            bw_ = Buf()
            for ti, (b, i) in enumerate(self.moe_tiles(last)):
                j = ti % 2
                r0 = b * TT + i * 128
                self.ld(sl[j][:], self.selD[r0:r0 + 128, :], [bl[j]])
                self.ld(gl[j][:], self.gates[b, i * 128:(i + 1) * 128, :], [bl[j]], q="act")
                self.ld(rl[j][:], self.rankD[r0:r0 + 128, :], [bl[j]])
                self.ld(ht[j][:], self.h2tok[r0:r0 + 128, :], [bht[j]], q="act")
                self.V(lambda h: h.tensor_add(pos[:], rl[j][:], base[:]), [bl[j], bq], [bw_])
                self.V(lambda h: h.tensor_scalar(t1[:], sl[j][:], -1e6, 1e6, ALU.mult, ALU.add), [bl[j]], [bw_])
                self.V(lambda h: h.tensor_mul(pm[:], pos[:], sl[j][:]), [bw_, bl[j]], [bw_])
                self.V(lambda h: h.tensor_add(t1[:], t1[:], pm[:]), [bw_], [bw_])
                self.V(lambda h: h.tensor_reduce(pp[:, 0:1], t1[:], AX.X, ALU.min), [bw_], [bw_])
                self.V(lambda h: h.tensor_reduce(pp[:, 1:2], pm[:], AX.X, ALU.max), [bw_], [bw_])
                self.V(lambda h: h.tensor_copy(self.pidx[:, ti, :], pp[:]), [bw_], [self.bpidx])
                self.V(lambda h: h.tensor_scalar(eq[:], t1[:], pp[:, 0:1], None, ALU.is_equal), [bw_], [bw_])
                self.V(lambda h: h.tensor_mul(eq[:], eq[:], gl[j][:]), [bw_, bl[j]], [bw_])
                self.V(lambda h: h.tensor_reduce(gg[j][:, 0:1], eq[:], AX.X, ALU.add), [bw_, bgg[j]], [bgg[j]])
                self.V(lambda h: h.tensor_reduce(gsum[:], gl[j][:], AX.X, ALU.add), [bl[j]], [bw_])
                self.V(lambda h: h.tensor_sub(gg[j][:, 1:2], gsum[:], gg[j][:, 0:1]), [bw_, bgg[j]], [bgg[j]])
                for k in range(2):
                    self.S.idma(reads=[bht[j], self.bpidx], writes=[], out=self.hs[:, :], out_offset=bass.IndirectOffsetOnAxis(ap=self.pidx[:, ti, k:k + 1], axis=0),
                                in_=ht[j][:, :], in_offset=None)
                    self.S.idma(reads=[bgg[j], self.bpidx], writes=[], out=self.gsD[:, :], out_offset=bass.IndirectOffsetOnAxis(ap=self.pidx[:, ti, k:k + 1], axis=0),
                                in_=gg[j][:, k:k + 1], in_offset=None)

    def phase_moe_sparse(self, l, last):
        c = self.c
        NS = self.n_slots(last)
        SLT = c.SLOT // 128
        wgf = self.wg.rearrange("l e k (h f) -> (l e k h) f", h=2); wuf = self.wu.rearrange("l e k (h f) -> (l e k h) f", h=2)
        wdf = self.wd.rearrange("l e f n -> (l e f) n")
        with ExitStack() as st:
            wg_ = [self.sb(st, f"swg{i}", [128, KC, 512], BF16) for i in range(2)]
            wu_ = [self.sb(st, f"swu{i}", [128, KC, 512], BF16) for i in range(2)]
            wd_ = [self.sb(st, f"swd{i}", [128, 4, D], BF16) for i in range(2)]
            bwg, bwu, bwd = [Buf(), Buf()], [Buf(), Buf()], [Buf(), Buf()]
            h2s = self.sb(st, "h2s", [128, KC, c.SLOT], BF16); bh2 = Buf()
            hr = [self.sb(st, f"hr{i}", [128, D], BF16) for i in range(2)]; bhr = [Buf(), Buf()]
            he = [self.sb(st, f"she{i}", [128, 4, c.SLOT], BF16) for i in range(2)]; bhe = [Buf(), Buf()]
            ys = self.sb(st, "ys", [128, SLT, D]); bys = Buf()
            sg = self.sb(st, "ssg", [128, c.SLOT]); bsg = Buf()
            gs = [self.sb(st, f"sgs{i}", [128, SLT]) for i in range(2)]; bgs = [Buf(), Buf()]
            wi = [self.sb(st, f"swi{i}", [128, 40], I32) for i in range(2)]; bwi = [Buf(), Buf()]
            wif = self.sb(st, "swif", [128, 16]); bwif = Buf()
            hh = 0
            for s_ in range(NS):
                js = s_ % 2
                self.V(lambda h: h.tensor_scalar(wif[:], self.kiot[:, 0:16], self.es2[:, 0, s_:s_ + 1], float(l * NE * D), ALU.add, ALU.add), [self.bes, self.bconst, bwif], [bwif])
                for hf in range(2):
                    self.V(lambda h: h.tensor_scalar(wi[js][:, hf * 16:(hf + 1) * 16], wif[:], 2.0, float(hf), ALU.mult, ALU.add), [bwif, bwi[js]], [bwi[js]])
                self.V(lambda h: h.tensor_scalar(wi[js][:, 32:40], self.kiot[:, 16:24], self.es2[:, 1, s_:s_ + 1], float(l * NE * DFF), ALU.add, ALU.add), [self.bes, self.bconst, bwi[js]], [bwi[js]])
                self.S.dma("act", gs[js][:], self.gsD[s_ * c.SLOT:(s_ + 1) * c.SLOT, :].rearrange("(t p) o -> p (t o)", p=128), writes=[bgs[js]], allow_slow_non_contiguous=True)
                for t in range(SLT):
                    jr = t % 2
                    self.ld(hr[jr][:], self.hs[s_ * c.SLOT + t * 128: s_ * c.SLOT + (t + 1) * 128, :], [bhr[jr]], q=("sp" if jr == 0 else "act"))
                    for g in range(2):
                        p, pb = self.ps[4 + g], self.pb[4 + g]
                        pv = p[:].bitcast(BF16)
                        for k8 in range(8):
                            kc = g * 8 + k8
                            self.PE(lambda h: h.transpose(pv[:, k8 * 128:(k8 + 1) * 128], hr[jr][:, kc * 128:(kc + 1) * 128], self.idb[:]), [bhr[jr], self.bconst], [pb], inc=(k8 == 7))
                        dst = h2s[:, g * 8:(g + 1) * 8, t * 128:(t + 1) * 128]
                        if g == 0:
                            self.A(lambda h: h.copy(dst, pv[:, 0:1024].rearrange("p (k t) -> p k t", t=128)), [pb], [bh2])
                        else:
                            self.V(lambda h: h.tensor_copy(dst, pv[:, 0:1024].rearrange("p (k t) -> p k t", t=128)), [pb], [bh2])
                for half in range(2):
                    jw = hh % 2; hh += 1
                    cs = slice(half * 512, (half + 1) * 512)
                    for kc in range(KC):
                        self.S.idma(reads=[bwi[js]], writes=[bwg[jw]], out=wg_[jw][:, kc, :], out_offset=None, in_=wgf[:, :],
                                    in_offset=bass.IndirectOffsetOnAxis(ap=wi[js][:, half * 16 + kc:half * 16 + kc + 1], axis=0))
                        self.S.idma(reads=[bwi[js]], writes=[bwu[jw]], out=wu_[jw][:, kc, :], out_offset=None, in_=wuf[:, :],
                                    in_offset=bass.IndirectOffsetOnAxis(ap=wi[js][:, half * 16 + kc:half * 16 + kc + 1], axis=0))
                    for fc in range(4):
                        self.S.idma(reads=[bwi[js]], writes=[bwd[jw]], out=wd_[jw][:, fc, :], out_offset=None, in_=wdf[:, :],
                                    in_offset=bass.IndirectOffsetOnAxis(ap=wi[js][:, 32 + half * 4 + fc:33 + half * 4 + fc], axis=0))
                    for fc in range(4):
                        pG, pU = self.ps[fc % 2], self.ps[2 + fc % 2]; bpG, bpU = self.pb[fc % 2], self.pb[2 + fc % 2]
                        for kc in range(KC):
                            self.PE(lambda h: h.matmul(pG[:, :], wg_[jw][:, kc, fc * 128:(fc + 1) * 128], h2s[:, kc, :], start=(kc == 0), stop=(kc == KC - 1)), [bwg[jw], bh2], [bpG], inc=(kc == KC - 1))
                        for kc in range(KC):
                            self.PE(lambda h: h.matmul(pU[:, :], wu_[jw][:, kc, fc * 128:(fc + 1) * 128], h2s[:, kc, :], start=(kc == 0), stop=(kc == KC - 1)), [bwu[jw], bh2], [bpU], inc=(kc == KC - 1))
                        self.A(lambda h: h.activation(sg[:], pG[:, :], AF.Silu), [bpG, bsg], [bsg])
                        self.V(lambda h: h.tensor_mul(he[jw][:, fc, :], sg[:], pU[:, :]), [bsg, bpU], [bhe[jw]])
                    for t in range(SLT):
                        for g in range(4):
                            ps, pb = self.ps[4 + g], self.pb[4 + g]
                            for fc in range(4):
                                self.PE(lambda h: h.matmul(ps[:, :], he[jw][:, fc, t * 128:(t + 1) * 128], wd_[jw][:, fc, g * 512:(g + 1) * 512], start=(fc == 0), stop=(fc == 3)), [bhe[jw], bwd[jw]], [pb], inc=(fc == 3))
                            ysl = ys[:, t, g * 512:(g + 1) * 512]
                            if half == 0:
                                self.V(lambda h: h.tensor_scalar(ysl, ps[:, :], gs[js][:, t:t + 1], None, ALU.mult), [pb, bgs[js], bys], [bys])
                            else:
                                self.V(lambda h: h.scalar_tensor_tensor(ysl, ps[:, :], gs[js][:, t:t + 1], ysl, ALU.mult, ALU.add), [pb, bgs[js], bys], [bys])
                self.st(self.ysD[s_ * c.SLOT:(s_ + 1) * c.SLOT, :].rearrange("(t p) n -> p t n", p=128), ys[:], [bys])

    def phase_unsort(self, l, last):
        c = self.c
        TT = c.TT
        with ExitStack() as st:
            gB = {}
            for src in range(c.NB):
                gB[src] = self.bcast_row(st, 1, src, f"ugm{src}")
            if not last:
                gB[2] = self.bcast_row(st, 1, 2, "ugm2")
            ya = [self.sb(st, f"ya{i}", [128, D]) for i in range(2)]; yb = [self.sb(st, f"yb{i}", [128, D]) for i in range(2)]
            xt = [self.sb(st, f"ux{i}", [128, D]) for i in range(2)]
            bya, byb, bx = [Buf(), Buf()], [Buf(), Buf()], [Buf(), Buf()]
            for ti, (b, i) in enumerate(self.moe_tiles(last)):
                j = ti % 2
                src = 2 if i < c.NTC else b
                self.S.idma(reads=[self.bpidx], writes=[bya[j]], out=ya[j][:, :], out_offset=None, in_=self.ysD[:, :],
                            in_offset=bass.IndirectOffsetOnAxis(ap=self.pidx[:, ti, 0:1], axis=0))
                self.S.idma(reads=[self.bpidx], writes=[byb[j]], out=yb[j][:, :], out_offset=None, in_=self.ysD[:, :],
                            in_offset=bass.IndirectOffsetOnAxis(ap=self.pidx[:, ti, 1:2], axis=0))
                self.ld(xt[j][:], self.xres[b, i * 128:(i + 1) * 128, :], [bx[j]])
                self.V(lambda h: h.tensor_add(ya[j][:], ya[j][:], yb[j][:]), [bya[j], byb[j]], [bya[j]])
                self.V(lambda h: h.tensor_mul(ya[j][:], ya[j][:], gB[src][0][:]), [bya[j], gB[src][1]], [bya[j]])
                self.V(lambda h: h.tensor_add(xt[j][:], xt[j][:], ya[j][:]), [bya[j], bx[j]], [bx[j]])
                self.st(self.xres[b, i * 128:(i + 1) * 128, :], xt[j][:], [bx[j]], q="act")

    def phase_final(self):
        c = self.c
        with ExitStack() as st:
            gB = self.sb(st, "gfinB", [128, D]); bg = Buf()
            self.ld(gB[:], self.gfin[0:1, :].partition_broadcast(128)[:, 0, :], [bg])
            xt = [self.sb(st, f"xf{i}", [128, D]) for i in range(2)]; bx = [Buf(), Buf()]
            sq = self.sb(st, "fsq", [128, D], BF16); ss = self.sb(st, "fss", [128, 4]); bs = Buf()
            for b in range(c.NB):
                for i in range(c.NTC, c.NT):
                    j = i % 2
                    self.ld(xt[j][:], self.xres[b, i * 128:(i + 1) * 128, :], [bx[j]], q=("sp" if j == 0 else "act"))
                    self.A(lambda h: h.activation(sq[:], xt[j][:], AF.Square, accum_out=ss[:, 0:1]), [bx[j]], [bs])
                    self.A(lambda h: h.activation(ss[:, 1:2], ss[:, 0:1], AF.Sqrt, scale=1.0 / D, bias=self.epsc[:, 0:1]), [bs, self.bconst], [bs])
                    self.V(lambda h: h.reciprocal(ss[:, 2:3], ss[:, 1:2]), [bs], [bs])
                    self.V(lambda h: h.scalar_tensor_tensor(xt[j][:], xt[j][:], ss[:, 2:3], gB[:], ALU.mult, ALU.mult), [bx[j], bs, bg], [bx[j]])
                    self.st(self.out[b, (i - c.NTC) * 128:(i - c.NTC + 1) * 128, :], xt[j][:], [bx[j]], q=("sp" if j == 0 else "act"))


def host_shared(inp, cfg):
    L = cfg.L
    f = lambda a: np.ascontiguousarray(np.asarray(a, dtype=np.float32))
    pk = lambda v: f(np.asarray(v).reshape(v.shape[:-1] + (v.shape[-1] // 128, 128)).swapaxes(-1, -2))
    sh = {}
    sh["gmix"] = pk(inp["norm_mix_g"]); sh["gffn"] = pk(inp["norm_ffn_g"])
    sh["gfin"] = f(inp["final_norm_g"]).reshape(1, D)
    sh["w_mod"] = f(inp["w_mod"]); sh["b_mod"] = f(inp["b_mod"])
    w_in = np.asarray(inp["w_in"], np.float32).reshape(L, D, 12, W)
    def swap(p):
        x = w_in[:, :, p].reshape(L, D, 4, 2, 64)
        return x[:, :, :, ::-1, :].reshape(L, D, W)
    sh["w_in"] = f(np.concatenate([w_in.reshape(L, D, 12 * W), swap(8), swap(9)], axis=-1))
    sh["w_out"] = f(inp["w_out"])
    bre, bim = np.asarray(inp["s5_b_re"], np.float32), np.asarray(inp["s5_b_im"], np.float32)
    cre, cim = np.asarray(inp["s5_c_re"], np.float32), np.asarray(inp["s5_c_im"], np.float32)
    s5B = np.zeros((L, 2, 16, 128, 2, 128), np.float32)
    s5C = np.zeros((L, 2, 16, 128, 2, 128), np.float32)
    for sc in range(16):
        for g2 in range(2):
            g = 2 * sc + g2
            r0 = 16 * (g % 8)
            s5B[:, :, sc, r0:r0 + 16, 0, g2 * 64:(g2 + 1) * 64] = bre[:, :, g]
            s5B[:, :, sc, r0:r0 + 16, 1, g2 * 64:(g2 + 1) * 64] = bim[:, :, g]
            s5C[:, :, sc, g2 * 64:(g2 + 1) * 64, 0, r0:r0 + 16] = cre[:, :, g]
            s5C[:, :, sc, g2 * 64:(g2 + 1) * 64, 1, r0:r0 + 16] = cim[:, :, g]
    sh["s5B"], sh["s5C"] = s5B, s5C
    lam = np.zeros((L, 128, 3, 2, 16), np.float32)
    lre, lim, ldt = (np.asarray(inp[k], np.float32) for k in ("s5_lam_re", "s5_lam_im", "s5_log_dt"))
    for sc in range(16):
        for g2 in range(2):
            g = 2 * sc + g2
            lam[:, g2 * 64:(g2 + 1) * 64, 0, :, sc] = lre[:, :, g, :].transpose(0, 2, 1)
            lam[:, g2 * 64:(g2 + 1) * 64, 1, :, sc] = lim[:, :, g, :].transpose(0, 2, 1)
            lam[:, g2 * 64:(g2 + 1) * 64, 2, :, sc] = ldt[:, :, g][:, None, :]
    sh["s5lam"] = lam
    sh["s5d"] = pk(inp["s5_d"]); sh["gluw"] = f(inp["s5_glu_w"]); sh["glub"] = pk(inp["s5_glu_b"])
    TT, CL = cfg.TT, cfg.CTXL
    kf = np.arange(TT, dtype=np.float32)
    kb = np.concatenate([CL - 1 - np.arange(CL), CL + (TT - CL) - 1 - np.arange(TT - CL)]).astype(np.float32)
    sh["kidx"] = f(np.stack([np.broadcast_to(kf, (128, TT)), np.broadcast_to(kb, (128, TT))]))
    cw = np.asarray(inp["lru_conv_w"], np.float32)
    sh["convw"] = f(cw.reshape(L, 4, 4, 128).transpose(0, 3, 2, 1))
    lv = np.stack([np.asarray(inp["lru_conv_b"], np.float32)] +
                  [np.asarray(inp[k], np.float32)[:, d] for k in ("lru_ba", "lru_bx", "lru_lam") for d in range(2)], axis=-1)
    sh["lruv"] = f(lv.reshape(L, 4, 128, 7).transpose(0, 2, 1, 3))
    lw = np.zeros((L, 2, 2, 4, 128, 128), np.float32)
    for a, k in enumerate(("lru_wa", "lru_wx")):
        wsrc = np.asarray(inp[k], np.float32)
        for hd in range(8):
            cc, h2 = hd // 2, hd % 2
            lw[:, a, :, cc, h2 * 64:(h2 + 1) * 64, h2 * 64:(h2 + 1) * 64] = wsrc[:, :, hd]
    sh["lruw"] = lw
    hl = np.asarray(inp["hgrn_lb_logits"], np.float32)
    sh["hglb"] = f(hl.reshape(2, L, 4, 128).transpose(0, 1, 3, 2))
    n = cfg.SEQL
    rows = n // 64
    row = np.repeat(np.arange(rows, dtype=np.float32), 64); col = np.tile(np.arange(64, dtype=np.float32), rows)
    inv = (np.float32(10000.0) ** (-np.arange(32, dtype=np.float32) / np.float32(32))).astype(np.float32)
    ang = np.concatenate([row[:, None] * inv, col[:, None] * inv], axis=-1).astype(np.float32)
    sh["rang"] = f(np.concatenate([ang.T, ang.T], axis=0))
    sh["rw"] = f(np.asarray(inp["router_w"], np.float32).reshape(KC, 128, NE).transpose(1, 0, 2))
    sh["rbias"] = f(inp["router_bias"]).reshape(1, NE)
    sh["wg"], sh["wu"], sh["wd"] = f(inp["moe_w_gate"]), f(inp["moe_w_up"]), f(inp["moe_w_down"])
    sh["ident"] = np.eye(128, dtype=np.float32)
    s_, t_ = np.meshgrid(np.arange(64), np.arange(64), indexing="ij")
    m = np.stack([(s_ <= t_), (s_ >= t_)]).astype(np.float32)
    mm = np.concatenate([m, m], axis=1)
    sh["masks"] = f(np.broadcast_to(mm[:, :, None, :], (2, 128, 4, 64)))
    rst = np.ones((128, TT + 1), np.float32); rst[:, 0::64] = 0.0
    sh["rst"] = rst
    rst32 = np.ones((128, TT + 1), np.float32); rst32[:, 0::32] = 0.0
    sh["rst32"] = rst32
    s_, t_ = np.meshgrid(np.arange(32), np.arange(32), indexing="ij")
    m32 = np.stack([(s_ <= t_), (s_ >= t_)]).astype(np.float32)
    sh["masks32"] = f(np.broadcast_to(np.concatenate([m32] * 4, axis=1)[:, :, None, :], (2, 128, 4, 32)))
    sh["gffn_row"] = f(inp["norm_ffn_g"])
    tp_, t_ = np.meshgrid(np.arange(128), np.arange(128), indexing="ij")
    sh["ltri"] = f(np.stack([(tp_ < t_).astype(np.float32), np.ones((128, 128), np.float32)]))
    p_ = np.arange(128)[:, None]
    sh["kio"] = f(np.concatenate([np.arange(16)[None, :] * 128 + p_, np.arange(8)[None, :] * 128 + p_], axis=1))
    nsmax = (2 * cfg.NB * cfg.TT + cfg.SLOT - 1) // cfg.SLOT + NE
    sh["siota"] = f(np.broadcast_to(np.arange(nsmax, dtype=np.float32), (128, nsmax)))
    sel = np.zeros((3, 3, 128), np.float32)
    for s in range(3):
        sel[s, s, :] = 1.0
    sh["sel3"] = sel
    return sh


def host_core(inp, cfg, b0):
    NB = cfg.NB
    x = np.asarray(inp["x"], np.float32)[b0:b0 + NB]
    ctx = np.asarray(inp["ctx"], np.float32)[b0:b0 + NB]
    cvec = np.concatenate([np.asarray(inp["c"], np.float32)[b0:b0 + NB], np.asarray(inp["c_ctx"], np.float32)[None]], axis=0)
    if NB == 1:
        cvec = np.concatenate([cvec[0:1], cvec[0:1], cvec[1:2]], axis=0)
    return {"xin": np.ascontiguousarray(np.concatenate([ctx, x], axis=1)),
            "cT": np.ascontiguousarray(cvec.reshape(3, KC, 128).transpose(2, 1, 0))}


_CACHE = {}


def kernel(**inputs):
    cfg = Cfg()
    n_cores = 8
    if "nc" not in _CACHE:
        _CACHE["nc"] = Prog(cfg).build()
    nc = _CACHE["nc"]
    sh = host_shared(inputs, cfg)
    in_maps = []
    for core in range(n_cores):
        m = dict(sh)
        m.update(host_core(inputs, cfg, core * cfg.NB))
        in_maps.append(m)
    res = run_bass_kernel_spmd(nc, in_maps, core_ids=list(range(n_cores)))
    return np.concatenate([r["out"] for r in res.results], axis=0).astype(np.float32)
```

```python
import math
from contextlib import ExitStack
import numpy as np
import concourse.bass as bass
import concourse.mybir as mybir
from concourse.bass_utils import run_bass_kernel_spmd

F32 = mybir.dt.float32
BF16 = mybir.dt.bfloat16
I32 = mybir.dt.int32
ALU = mybir.AluOpType
AF = mybir.ActivationFunctionType
AX = mybir.AxisListType

D = 2048
KC = 16
W = 512
NPARTS = 14
FM_PARTS = [0, 1, 2, 3, 4, 5, 8, 9, 12, 13]
TM_PARTS = [6, 7, 10, 11]
NE = 16
DFF = 1024
EPS = 1e-6
TWO_PI = 2.0 * math.pi


class Buf:
    __slots__ = ("w", "r")

    def __init__(self):
        self.w = None
        self.r = {}


class Eng:
    def __init__(self, name, h, sem):
        self.name, self.h, self.sem = name, h, sem
        self.n = 0
        self.seen = {}
        self.dsems, self.dcnt, self.dnext = [], [], 0


class Sched:
    def __init__(self, nc, stack, n_dma_sems=16):
        self.nc = nc
        self.E = {}
        for name, h in (("pe", nc.tensor), ("dve", nc.vector), ("act", nc.scalar),
                        ("pool", nc.gpsimd), ("sp", nc.sync)):
            self.E[name] = Eng(name, h, stack.enter_context(nc.semaphore("s_" + name)))
        for name in ("sp", "act", "pool"):
            e = self.E[name]
            for i in range(n_dma_sems):
                e.dsems.append(stack.enter_context(nc.semaphore(f"d_{name}{i}")))
                e.dcnt.append(0)
        self.ninstr = 0

    def _wait(self, e, sem, val):
        if sem is e.sem and e.name == "pe":
            return
        if e.seen.get(sem, 0) < val:
            e.h.wait_ge(sem, val)
            e.seen[sem] = val

    def _deps(self, e, reads, writes):
        for b in reads:
            if b.w is not None:
                self._wait(e, *b.w)
        for b in writes:
            if b.w is not None:
                self._wait(e, *b.w)
            for s, v in b.r.items():
                self._wait(e, s, v)

    @staticmethod
    def _mark(tok, reads, writes):
        s, v = tok
        for b in reads:
            if b.r.get(s, 0) < v:
                b.r[s] = v
        for b in writes:
            b.w = tok
            b.r = {}

    def op(self, eng, fn, reads=(), writes=(), inc=True):
        e = self.E[eng]
        self._deps(e, reads, writes)
        ins = fn(e.h)
        if inc:
            e.n += 1
            ins.then_inc(e.sem, 1)
            tok = (e.sem, e.n)
            e.pend = False
        else:
            tok = (e.sem, e.n + 1)
            e.pend = True
        self._mark(tok, reads, writes)
        self.ninstr += 1
        return ins

    def dma(self, eng, out, in_, reads=(), writes=(), **kw):
        e = self.E[eng]
        self._deps(e, reads, writes)
        i = e.dnext
        e.dnext = (i + 1) % len(e.dsems)
        sem = e.dsems[i]
        if e.dcnt[i]:
            self._wait(e, sem, e.dcnt[i])
        ins = e.h.dma_start(out=out, in_=in_, **kw)
        e.dcnt[i] += 16
        ins.then_inc(sem, 16)
        self._mark((sem, e.dcnt[i]), reads, writes)
        self.ninstr += 1
        return ins

    def idma(self, reads=(), writes=(), **kw):
        e = self.E["pool"]
        self._deps(e, reads, writes)
        i = e.dnext
        e.dnext = (i + 1) % len(e.dsems)
        sem = e.dsems[i]
        if e.dcnt[i]:
            self._wait(e, sem, e.dcnt[i])
        ins = e.h.indirect_dma_start(**kw)
        e.dcnt[i] += 16
        ins.then_inc(sem, 16)
        self._mark((sem, e.dcnt[i]), reads, writes)
        self.ninstr += 1
        return ins

    def barrier(self):
        assert not any(getattr(e, "pend", False) for e in self.E.values())
        for e in self.E.values():
            for f in self.E.values():
                if f is not e and f.n:
                    self._wait(e, f.sem, f.n)
            for q in ("sp", "act", "pool"):
                qe = self.E[q]
                for sem, cnt in zip(qe.dsems, qe.dcnt):
                    if cnt:
                        self._wait(e, sem, cnt)


class Cfg:
    def __init__(self, NB=2, CTXL=256, SEQL=2048, L=2, dbg=False):
        self.NB, self.CTXL, self.SEQL, self.L, self.dbg = NB, CTXL, SEQL, L, dbg
        self.TT = CTXL + SEQL
        self.NT = self.TT // 128
        self.NTC = CTXL // 128
        self.NCH = self.TT // 64
        self.NCHC = CTXL // 64
        self.sparse = True
        self.SLOT = 640


def tok_groups(T, g=512):
    out, t = [], 0
    while t < T:
        n = min(g, T - t)
        out.append((t, n))
        t += n
    return out


class Prog:
    def __init__(self, cfg):
        self.c = cfg
        self.nc = bass.Bass("TRN2", target_bir_lowering=False)
        self.inp = {}

    def din(self, name, shape, dt=F32):
        t = self.nc.dram_tensor(name, list(shape), dt, kind="ExternalInput").ap()
        self.inp[name] = t
        return t

    def dscr(self, name, shape, dt=F32):
        kind = "ExternalOutput" if self.c.dbg else "Internal"
        return self.nc.dram_tensor(name, list(shape), dt, kind=kind).ap()

    def sb(self, st, name, shape, dt=F32):
        self._uid = getattr(self, "_uid", 0) + 1
        return st.enter_context(self.nc.sbuf_tensor(f"{name}_{self._uid}", list(shape), dt))

    def V(self, fn, R=(), Wr=()):
        return self.S.op("dve", fn, R, Wr)

    def A(self, fn, R=(), Wr=()):
        return self.S.op("act", fn, R, Wr)

    def G(self, fn, R=(), Wr=()):
        return self.S.op("pool", fn, R, Wr)

    def PE(self, fn, R=(), Wr=(), inc=True):
        return self.S.op("pe", fn, R, Wr, inc=inc)

    def ld(self, out, in_, Wr, q="sp", R=()):
        return self.S.dma(q, out, in_, reads=R, writes=Wr)

    def ldc(self, out, in_, Wr, R=()):
        return self.S.dma("pool", out, in_, reads=R, writes=Wr)

    def st(self, out, in_, R, q="sp"):
        return self.S.dma(q, out, in_, reads=R, writes=())

    def build(self):
        c, nc = self.c, self.nc
        NB, TT, NT, L = c.NB, c.TT, c.NT, c.L
        i_ = self.din
        self.xin = i_("xin", [NB, TT, D])
        self.cT = i_("cT", [128, KC, 3])
        self.gmix = i_("gmix", [L, 128, KC])
        self.gffn = i_("gffn", [L, 128, KC])
        self.gfin = i_("gfin", [1, D])
        self.w_mod = i_("w_mod", [L, D, 6 * D])
        self.b_mod = i_("b_mod", [L, 6 * D])
        self.w_in = i_("w_in", [L, D, NPARTS * W])
        self.w_out = i_("w_out", [L, D, D])
        self.s5B = i_("s5B", [L, 2, 16, 128, 2, 128])
        self.s5C = i_("s5C", [L, 2, 16, 128, 2, 128])
        self.s5lam = i_("s5lam", [L, 128, 3, 2, 16])
        self.s5d = i_("s5d", [L, 128, 4])
        self.gluw = i_("gluw", [L, W, W])
        self.glub = i_("glub", [L, 128, 4])
        self.kidx = i_("kidx", [2, 128, TT])
        self.convw = i_("convw", [L, 128, 4, 4])
        self.lruv = i_("lruv", [L, 128, 4, 7])
        self.lruw = i_("lruw", [L, 2, 2, 4, 128, 128])
        self.hglb = i_("hglb", [2, L, 128, 4])
        self.rang = i_("rang", [128, c.SEQL])
        self.rw = i_("rw", [128, KC, NE])
        self.rbias = i_("rbias", [1, NE])
        self.wg = i_("wg", [L, NE, D, DFF])
        self.wu = i_("wu", [L, NE, D, DFF])
        self.wd = i_("wd", [L, NE, DFF, D])
        self.ident = i_("ident", [128, 128])
        self.masks = i_("masks", [2, 128, 4, 64])
        self.rst = i_("rst", [128, TT + 1])
        self.rst32 = i_("rst32", [128, TT + 1])
        self.masks32 = i_("masks32", [2, 128, 4, 32])
        self.sel3 = i_("sel3", [3, 3, 128])
        self.gffn_row = i_("gffn_row", [L, D])
        self.ltri = i_("ltri", [2, 128, 128])
        self.kio = i_("kio", [128, 24])
        NSMAX = (2 * NB * TT + c.SLOT - 1) // c.SLOT + NE
        self.NSMAX = NSMAX
        self.siota = i_("siota", [128, NSMAX])
        self.out = nc.dram_tensor("out", [NB, c.SEQL, D], F32, kind="ExternalOutput").ap()

        self.xres = self.dscr("xres", [NB, TT, D])
        self.Pfm = self.dscr("Pfm", [NB, len(FM_PARTS) * W, TT])
        self.Ptm = self.dscr("Ptm", [NB, TT, len(TM_PARTS) * W])
        self.mixT = self.dscr("mixT", [NB, D, TT], BF16)
        self.h2T = self.dscr("h2T", [NB, D, TT], BF16)
        self.gates = self.dscr("gates", [NB, TT, NE])
        self.ygD = self.dscr("ygD", [NB, W, TT])
        self.modD = self.dscr("modD", [3, 4, D])
        RM = self.NSMAX * c.SLOT
        self.h2tok = self.dscr("h2tok", [NB * TT, D], BF16)
        self.selD = self.dscr("selD", [NB * TT, NE])
        self.rankD = self.dscr("rankD", [NB * TT, NE])
        self.hs = self.dscr("hs", [RM, D], BF16)
        self.gsD = self.dscr("gsD", [RM, 1])
        self.ysD = self.dscr("ysD", [RM, D])

        with ExitStack() as top:
            self.S = Sched(nc, top)
            S = self.S
            self.ps = [top.enter_context(nc.psum_tensor(f"ps{i}", [128, 512], F32)) for i in range(8)]
            self.pb = [Buf() for _ in range(8)]
            self.idf = self.sb(top, "idf", [128, 128]); self.idb = self.sb(top, "idb", [128, 128], BF16)
            self.bconst = Buf()
            self.ld(self.idf[:], self.ident[:, :], [self.bconst])
            self.ldc(self.idb[:], self.ident[:, :], [self.bconst])
            self.cmask = self.sb(top, "cmask", [128, 2, 4, 64])
            self.ld(self.cmask[:], self.masks.rearrange("a p h j -> p a h j"), [self.bconst])
            self.cmask32 = self.sb(top, "cmask32", [128, 2, 4, 32])
            self.ld(self.cmask32[:], self.masks32.rearrange("a p h j -> p a h j"), [self.bconst])
            self.epsc = self.sb(top, "epsc", [128, 1])
            self.V(lambda h: h.memset(self.epsc[:], EPS), [], [self.bconst])
            self.hpi = self.sb(top, "hpi", [128, 1])
            self.V(lambda h: h.memset(self.hpi[:], math.pi / 2), [], [self.bconst])
            self.sel3t = self.sb(top, "sel3t", [3, 3, 128])
            self.ld(self.sel3t[:], self.sel3[:, :, :], [self.bconst])
            self.modP = self.sb(top, "modP", [128, 6, KC, 3])
            self.AB = self.sb(top, "AB", [128, 4, KC, 3])
            self.bmod = Buf()
            self.run = self.sb(top, "run", [128, NE]); self.brun = Buf()
            self.pidx = self.sb(top, "pidx", [128, NB * NT, 2], I32); self.bpidx = Buf()
            self.ltt = self.sb(top, "ltt", [128, 2, 128])
            self.ld(self.ltt[:], self.ltri.rearrange("a p j -> p a j"), [self.bconst])
            self.es2 = self.sb(top, "es2", [128, 2, self.NSMAX]); self.bes = Buf()
            self.kiot = self.sb(top, "kiot", [128, 24])
            self.ld(self.kiot[:], self.kio[:, :], [self.bconst])
            if c.sparse:
                with ExitStack() as zst:
                    z = self.sb(zst, "zz", [128, D], BF16); bz = Buf()
                    self.V(lambda h: h.memset(z[:], 0.0), [], [bz])
                    for r0 in range(0, self.NSMAX * c.SLOT, 128):
                        self.st(self.hs[r0:r0 + 128, :], z[:], [bz], q=("sp" if (r0 // 128) % 2 == 0 else "act"))
                    S.barrier()
            S.barrier()
            for l in range(L):
                last = (l == L - 1)
                self.phase_mod(l)
                S.barrier()
                for b in range(NB):
                    xsrc = self.xin if l == 0 else self.xres
                    self.phase_proj(l, b, xsrc)
                    S.barrier()
                    self.phase_lru(l, b)
                    S.barrier()
                    self.phase_gla(l, b, 0)
                    S.barrier()
                    self.phase_gla(l, b, 1)
                    S.barrier()
                    self.phase_s5(l, b)
                    S.barrier()
                    self.phase_out(l, b, xsrc, last)
                    S.barrier()
                    if not c.sparse:
                        self.phase_moe(l, b, last)
                        S.barrier()
                if c.sparse:
                    self.phase_route(l, last)
                    S.barrier()
                    self.phase_moe_sparse(l, last)
                    S.barrier()
                    self.phase_unsort(l, last)
                    S.barrier()
            self.phase_final()
            S.barrier()
        return nc

    def phase_mod(self, l):
        c, S = self.c, self.S
        with ExitStack() as st:
            cT = self.sb(st, "cT", [128, KC, 3]); cs = self.sb(st, "cs", [128, KC, 3], BF16)
            bc = Buf()
            self.ld(cT[:], self.cT[:, :, :], [bc])
            self.A(lambda h: h.activation(cs[:], cT[:], AF.Silu), [bc], [bc])
            modrow = self.sb(st, "modrow", [3, 6 * D]); bmr = Buf()
            wm = [self.sb(st, f"wm{i}", [128, KC, 512], BF16) for i in range(2)]; bwm = [Buf(), Buf()]
            bt = [self.sb(st, f"bt{i}", [3, 512]) for i in range(2)]; bbt = [Buf(), Buf()]
            wsrc = self.w_mod[l].rearrange("(kc kp) n -> kp kc n", kp=128)
            for cg in range(24):
                j = cg % 2
                self.ldc(wm[j][:], wsrc[:, :, cg * 512:(cg + 1) * 512], [bwm[j]])
                self.ld(bt[j][:], self.b_mod[l:l + 1, cg * 512:(cg + 1) * 512].partition_broadcast(3)[:, 0, :], [bbt[j]], q="act")
                p = self.ps[cg % 2]; pb = self.pb[cg % 2]
                for kc in range(KC):
                    self.PE(lambda h: h.matmul(p[0:3, :], cs[:, kc, :], wm[j][:, kc, :], start=(kc == 0), stop=(kc == KC - 1)),
                            [bc, bwm[j]], [pb], inc=(kc == KC - 1))
                self.V(lambda h: h.tensor_add(modrow[:, cg * 512:(cg + 1) * 512], p[0:3, :], bt[j][:]), [pb, bbt[j]], [bmr])
            pT = self.ps[2]; pTb = self.pb[2]
            for v in range(6):
                for kc in range(KC):
                    k = v * KC + kc
                    self.PE(lambda h: h.transpose(pT[:, k * 3:k * 3 + 3], modrow[:, v * D + kc * 128: v * D + (kc + 1) * 128], self.idf[0:3, 0:3]),
                            [bmr, self.bconst], [pTb], inc=(k == 6 * KC - 1))
            self.V(lambda h: h.tensor_copy(self.modP[:].rearrange("p a k s -> p (a k s)"), pT[:, 0:288]), [pTb], [self.bmod])
            self.st(self.modD[:, 0, :], modrow[:, 2 * D:3 * D], [bmr])
            self.st(self.modD[:, 1, :], modrow[:, 5 * D:6 * D], [bmr])
            self.st(self.modD[:, 3, :], modrow[:, 3 * D:4 * D], [bmr])
            grow = self.sb(st, "grow", [3, D]); bgr = Buf()
            self.ld(grow[:], self.gffn_row[l:l + 1, :].partition_broadcast(3)[:, 0, :], [bgr])
            self.V(lambda h: h.scalar_tensor_tensor(grow[:], modrow[:, 4 * D:5 * D], 1.0, grow[:], ALU.add, ALU.mult), [bmr, bgr], [bgr])
            self.st(self.modD[:, 2, :], grow[:], [bgr])
            self.V(lambda h: h.memset(self.run[:], 0.0), [self.brun], [self.brun])
            g1 = self.sb(st, "g1", [128, KC]); g2 = self.sb(st, "g2", [128, KC]); bg = Buf()
            self.ld(g1[:], self.gmix[l], [bg]); self.ld(g2[:], self.gffn[l], [bg])
            for (gi, gt, vs, vsh) in ((0, g1, 1, 0), (2, g2, 4, 3)):
                self.V(lambda h: h.tensor_scalar(self.AB[:, gi], self.modP[:, vs], 1.0, None, ALU.add), [self.bmod], [self.bmod])
                self.V(lambda h: h.tensor_mul(self.AB[:, gi], self.AB[:, gi], gt[:].unsqueeze(2).to_broadcast([128, KC, 3])), [self.bmod, bg], [self.bmod])
                self.V(lambda h: h.tensor_copy(self.AB[:, gi + 1], self.modP[:, vsh]), [self.bmod], [self.bmod])

    def bcast_row(self, st, which, src, name):
        t = self.sb(st, name, [128, D]); b = Buf()
        self.ld(t[:], self.modD[src:src + 1, which, :].partition_broadcast(128)[:, 0, :], [b])
        return t, b

    def norm_T(self, st_tiles, xt, bx, which, src, dst_fn, bdst, fp32):
        sq, ss, xs = st_tiles["sq"], st_tiles["ss"], (st_tiles["xsf"] if fp32 else st_tiles["xsb"])
        bsq, bss, bxs = st_tiles["bsq"], st_tiles["bss"], st_tiles["bxs"]
        self.A(lambda h: h.activation(sq[:], xt[:], AF.Square, accum_out=ss[:, 0:1]), [bx], [bsq, bss])
        self.A(lambda h: h.activation(ss[:, 1:2], ss[:, 0:1], AF.Sqrt, scale=1.0 / D, bias=self.epsc[:, 0:1]), [bss, self.bconst], [bss])
        self.V(lambda h: h.reciprocal(ss[:, 2:3], ss[:, 1:2]), [bss], [bss])
        self.A(lambda h: h.activation(xs[:], xt[:], AF.Identity, scale=ss[:, 2:3]), [bx, bss], [bxs])
        a_i, b_i = (0, 1) if which == 0 else (2, 3)
        idm = self.idf if fp32 else self.idb
        ngrp = 4 if fp32 else 2
        per = KC // ngrp
        for g in range(ngrp):
            bank = 4 + g
            p = self.ps[bank]; pb = self.pb[bank]
            pv = p[:] if fp32 else p[:].bitcast(BF16)
            for j in range(per):
                kc = g * per + j
                self.PE(lambda h: h.transpose(pv[:, j * 128:(j + 1) * 128], xs[:, kc * 128:(kc + 1) * 128], idm[:]),
                        [bxs, self.bconst], [pb], inc=(j == per - 1))
            for j in range(per):
                kc = g * per + j
                sc, bi = self.AB[:, a_i, kc, src:src + 1], self.AB[:, b_i, kc, src:src + 1]
                if kc % 2 == 0:
                    self.V(lambda h: h.tensor_scalar(dst_fn(kc), pv[:, j * 128:(j + 1) * 128], sc, bi, ALU.mult, ALU.add),
                           [pb, self.bmod], [bdst])
                else:
                    self.A(lambda h: h.activation(dst_fn(kc), pv[:, j * 128:(j + 1) * 128], AF.Identity, bias=bi, scale=sc),
                           [pb, self.bmod], [bdst])

    def norm_tiles(self, st):
        d = {"sq": self.sb(st, "sq", [128, D], BF16), "ss": self.sb(st, "ss", [128, 4]),
             "xsf": None, "xsb": None, "bsq": Buf(), "bss": Buf(), "bxs": Buf()}
        return d

    def phase_proj(self, l, b, xsrc):
        c, S = self.c, self.S
        TT, NT = c.TT, c.NT
        with ExitStack() as st:
            hT = self.sb(st, "hT", [128, KC, TT], BF16); bh = Buf()
            nt = self.norm_tiles(st); nt["xsb"] = self.sb(st, "xsb", [128, D], BF16)
            xt = [self.sb(st, f"xt{i}", [128, D]) for i in range(2)]; bx = [Buf(), Buf()]
            for i in range(NT):
                j = i % 2
                self.ld(xt[j][:], xsrc[b, i * 128:(i + 1) * 128, :], [bx[j]], q=("sp" if j == 0 else "act"))
                src = 2 if i < c.NTC else b
                self.norm_T(nt, xt[j], bx[j], 0, src, lambda kc: hT[:, kc, i * 128:(i + 1) * 128], bh, fp32=False)
            wp = [self.sb(st, f"wp{i}", [128, KC, W], BF16) for i in range(2)]; bw = [Buf(), Buf()]
            stg = [self.sb(st, f"stg{i}", [128, TT]) for i in range(2)]; bs = [Buf(), Buf()]
            stg2 = [self.sb(st, f"stgb{i}", [128, W]) for i in range(2)]; bs2 = [Buf(), Buf()]
            wsrc = self.w_in[l].rearrange("(kc kp) n -> kp kc n", kp=128)
            tgs = tok_groups(TT)
            ev = 0
            for pi, p in enumerate(FM_PARTS + TM_PARTS):
                j = pi % 2
                self.ldc(wp[j][:], wsrc[:, :, p * W:(p + 1) * W], [bw[j]])
                if p in FM_PARTS:
                    fi = FM_PARTS.index(p)
                    for cb in range(4):
                        sj = cb % 2
                        for (t0, n) in tgs:
                            bank = ev % 4; ev += 1
                            ps, pb = self.ps[bank], self.pb[bank]
                            for kc in range(KC):
                                self.PE(lambda h: h.matmul(ps[:, 0:n], wp[j][:, kc, cb * 128:(cb + 1) * 128], hT[:, kc, t0:t0 + n],
                                                           start=(kc == 0), stop=(kc == KC - 1)), [bw[j], bh], [pb], inc=(kc == KC - 1))
                            if ev % 2:
                                self.A(lambda h: h.copy(stg[sj][:, t0:t0 + n], ps[:, 0:n]), [pb], [bs[sj]])
                            else:
                                self.V(lambda h: h.tensor_copy(stg[sj][:, t0:t0 + n], ps[:, 0:n]), [pb], [bs[sj]])
                        self.st(self.Pfm[b, fi * W + cb * 128: fi * W + (cb + 1) * 128, :], stg[sj][:], [bs[sj]], q=("sp" if sj == 0 else "act"))
                else:
                    ti = TM_PARTS.index(p)
                    for i in range(NT):
                        sj = i % 2
                        bank = ev % 4; ev += 1
                        ps, pb = self.ps[bank], self.pb[bank]
                        for kc in range(KC):
                            self.PE(lambda h: h.matmul(ps[:, :], hT[:, kc, i * 128:(i + 1) * 128], wp[j][:, kc, :],
                                                       start=(kc == 0), stop=(kc == KC - 1)), [bw[j], bh], [pb], inc=(kc == KC - 1))
                        if ev % 2:
                            self.A(lambda h: h.copy(stg2[sj][:], ps[:, :]), [pb], [bs2[sj]])
                        else:
                            self.V(lambda h: h.tensor_copy(stg2[sj][:], ps[:, :]), [pb], [bs2[sj]])
                        self.st(self.Ptm[b, i * 128:(i + 1) * 128, ti * W:(ti + 1) * W], stg2[sj][:], [bs2[sj]], q=("sp" if sj == 0 else "act"))

    def pfm(self, b, part, r0, n=128):
        fi = FM_PARTS.index(part)
        return self.Pfm[b, fi * W + r0: fi * W + r0 + n, :]

    def ptm(self, b, part):
        ti = TM_PARTS.index(part)
        return self.Ptm[b, :, ti * W:(ti + 1) * W].rearrange("(i p) w -> p i w", p=128)

    def scan_bidir(self, out_f, out_b, a_f, x_f, a_b, x_b, R, Wf, Wb, eng="dve"):
        c = self.c
        CL, TT = c.CTXL, c.TT
        op = self.V if eng == "dve" else self.G
        op(lambda h: h.tensor_tensor_scan(out_f[:, 0:TT], a_f[:, 0:TT], x_f[:, 0:TT], 0.0, ALU.mult, ALU.add), R, [Wf])
        op(lambda h: h.tensor_tensor_scan(out_b[:, 0:CL][:, ::-1], a_b[:, 0:CL][:, ::-1], x_b[:, 0:CL][:, ::-1], 0.0, ALU.mult, ALU.add), R, [Wb])
        op(lambda h: h.tensor_tensor_scan(out_b[:, CL:TT][:, ::-1], a_b[:, CL:TT][:, ::-1], x_b[:, CL:TT][:, ::-1], out_b[:, 0:1], ALU.mult, ALU.add),
           list(R) + [Wb], [Wb])

    def phase_lru(self, l, b):
        c = self.c
        TT, CL = c.TT, c.CTXL
        tgs = tok_groups(TT)
        with ExitStack() as st:
            cw = self.sb(st, "cw", [128, 4, 4]); lv = self.sb(st, "lv", [128, 4, 7]); bp = Buf()
            self.ld(cw[:], self.convw[l], [bp]); self.ld(lv[:], self.lruv[l], [bp])
            lw = self.sb(st, "lw", [128, 2, 2, 4, 128], BF16)
            self.ldc(lw[:], self.lruw[l].rearrange("a d c p j -> p a d c j"), [bp])
            c1 = self.sb(st, "c1", [128, 4, 2]); c2 = self.sb(st, "c2", [128, 4, 2])
            self.A(lambda h: h.activation(c1[:], lv[:, :, 5:7], AF.Exp, scale=-1.0), [bp], [bp])
            self.A(lambda h: h.activation(c1[:], c1[:], AF.Ln, bias=1.0), [bp], [bp])
            self.V(lambda h: h.tensor_scalar(c2[:], c1[:], -16.0, None, ALU.mult), [bp], [bp])
            self.V(lambda h: h.tensor_scalar(c1[:], c1[:], -8.0, None, ALU.mult), [bp], [bp])
            T = lambda n, dt=F32: self.sb(st, n, [128, TT], dt)
            xr, gt, xc, xcb = T("xr"), T("gt"), T("xc"), T("xcb", BF16)
            rr, ii, aa, bb = [T("rr0"), T("rr1")], [T("ii0"), T("ii1")], [T("aa0"), T("aa1")], [T("bb0"), T("bb1")]
            hf, hb, ob = T("hf"), T("hb"), T("ob", BF16)
            bxr, bgt, bxc, bxcb, bo = Buf(), Buf(), Buf(), Buf(), Buf()
            br, bi, ba, bbb = [Buf(), Buf()], [Buf(), Buf()], [Buf(), Buf()], [Buf(), Buf()]
            bhf, bhb = Buf(), Buf()
            for cc in range(4):
                self.ld(xr[:], self.pfm(b, 1, cc * 128), [bxr])
                self.ld(gt[:], self.pfm(b, 2, cc * 128), [bgt], q="act")
                self.V(lambda h: h.tensor_scalar(xc[:], xr[:], cw[:, cc, 2:3], lv[:, cc, 0:1], ALU.mult, ALU.add), [bxr, bp], [bxc])
                for (s0, s1) in ((0, CL), (CL, TT)):
                    for k, off in ((0, -2), (1, -1), (3, 1)):
                        o0, o1 = max(s0, s0 - off), min(s1, s1 - off)
                        self.V(lambda h: h.scalar_tensor_tensor(xc[:, o0:o1], xr[:, o0 + off:o1 + off], cw[:, cc, k:k + 1], xc[:, o0:o1], ALU.mult, ALU.add),
                               [bxr, bp, bxc], [bxc])
                self.A(lambda h: h.copy(xcb[:], xc[:]), [bxc], [bxcb])
                for d in range(2):
                    for (wi, dst, bd, bias_col) in ((0, rr[d], br[d], 1 + d), (1, ii[d], bi[d], 3 + d)):
                        for gi, (t0, n) in enumerate(tgs):
                            ps, pb = self.ps[gi % 4], self.pb[gi % 4]
                            self.PE(lambda h: h.matmul(ps[:, 0:n], lw[:, wi, d, cc, :], xcb[:, t0:t0 + n], start=True, stop=True), [bp, bxcb], [pb])
                            self.A(lambda h: h.activation(dst[:, t0:t0 + n], ps[:, 0:n], AF.Sigmoid, bias=lv[:, cc, bias_col:bias_col + 1]), [pb, bp], [bd])
                    self.A(lambda h: h.activation(aa[d][:], rr[d][:], AF.Exp, scale=c1[:, cc, d:d + 1]), [br[d], bp], [ba[d]])
                    self.A(lambda h: h.activation(bb[d][:], rr[d][:], AF.Exp, scale=c2[:, cc, d:d + 1]), [br[d], bp], [bbb[d]])
                    self.A(lambda h: h.activation(bb[d][:], bb[d][:], AF.Sqrt, scale=-1.0, bias=1.0), [bbb[d]], [bbb[d]])
                    self.V(lambda h: h.tensor_mul(ii[d][:], ii[d][:], xc[:]), [bi[d], bxc], [bi[d]])
                    self.V(lambda h: h.tensor_mul(bb[d][:], bb[d][:], ii[d][:]), [bbb[d], bi[d]], [bbb[d]])
                self.scan_bidir(hf, hb, aa[0], bb[0], aa[1], bb[1], [ba[0], ba[1], bbb[0], bbb[1]], bhf, bhb)
                self.V(lambda h: h.tensor_add(hf[:], hf[:], hb[:]), [bhf, bhb], [bhf])
                self.gelu(gt[:], gt[:], hb[:], [bgt], bgt, bhb)
                self.V(lambda h: h.tensor_mul(ob[:], hf[:], gt[:]), [bhf, bgt], [bo])
                self.st(self.mixT[b, W + cc * 128: W + (cc + 1) * 128, :], ob[:], [bo])

    def sincos(self, st, ang, bang, n, name, share=None):
        T = lambda nm, dt=F32: self.sb(st, f"{name}_{nm}", [128, n], dt)
        y, yi, sn, cs = T("y"), T("yi", I32), T("sn"), T("cs")
        b = Buf(); bs = Buf(); bcs = Buf()
        for (shift, dst, bd) in ((0.0, sn, bs), (0.25, cs, bcs)):
            self.V(lambda h: h.tensor_scalar(y[:], ang[:], 1.0 / TWO_PI, shift, ALU.mult, ALU.add), [bang, b], [b])
            self.wrap_frac(y, yi, dst, b, extra=[bd])
            self.A(lambda h: h.activation(dst[:], y[:], AF.Sin, scale=TWO_PI), [b], [bd, b])
        return sn, bs, cs, bcs

    def wrap_frac(self, y, yi, yf, b, extra=()):
        Wb = [b] + list(extra)
        self.V(lambda h: h.tensor_copy(yi[:], y[:]), [b], Wb)
        self.V(lambda h: h.tensor_copy(yf[:], yi[:]), [b], Wb)
        self.V(lambda h: h.tensor_sub(y[:], y[:], yf[:]), [b], Wb)
        self.V(lambda h: h.tensor_scalar(yf[:], y[:], 0.5, -1.0, ALU.is_gt, ALU.mult), [b], Wb)
        self.V(lambda h: h.tensor_add(y[:], y[:], yf[:]), [b], Wb)
        self.V(lambda h: h.tensor_scalar(yf[:], y[:], -0.5, None, ALU.is_lt), [b], Wb)
        self.V(lambda h: h.tensor_add(y[:], y[:], yf[:]), [b], Wb)

    def gelu(self, dst, src, tmp, R, Wd, Wt):
        self.V(lambda h: h.tensor_mul(tmp, src, src), R, [Wt])
        self.V(lambda h: h.tensor_scalar(tmp, tmp, 0.044715, 1.0, ALU.mult, ALU.add), [Wt], [Wt])
        self.V(lambda h: h.tensor_mul(tmp, tmp, src), list(R) + [Wt], [Wt])
        self.A(lambda h: h.activation(tmp, tmp, AF.Sigmoid, scale=1.5957691216057308), [Wt], [Wt])
        self.V(lambda h: h.tensor_mul(dst, src, tmp), list(R) + [Wt], [Wd])

    def phase_gla(self, l, b, mixer):
        c = self.c
        TT, NT, CL = c.TT, c.NT, c.CTXL
        C = 64
        NPT = 128 // C
        NCH, NCHC = TT // C, CL // C
        cmask = self.cmask32 if C == 32 else self.cmask
        rsrc = self.rst32 if C == 32 else self.rst
        SQ = 128 ** -0.5
        with ExitStack() as st:
            T = lambda s_, n, dt=F32: self.sb(s_, n, [128, TT], dt)
            vbf = self.sb(st, "vbf", [128, NT, W], BF16); bv = Buf()
            oacc = self.sb(st, "oacc", [128, NT, W]); bo = Buf()
            vst = [self.sb(st, f"vst{i}", [128, W]) for i in range(2)]; bvs = [Buf(), Buf()]
            vsrc = self.ptm(b, 6 if mixer == 0 else 10)
            for i in range(NT):
                j = i % 2
                self.ld(vst[j][:], vsrc[:, i, :], [bvs[j]], q=("sp" if j == 0 else "act"))
                self.A(lambda h: h.activation(vbf[:, i, :], vst[j][:], AF.Silu if mixer == 0 else AF.Identity), [bvs[j]], [bv])
            bprm = Buf()
            if mixer == 0:
                lbr = self.sb(st, "lbr", [128, 2, c.L, 4]); lbe = self.sb(st, "lbe", [128, 2, c.L, 4])
                lb = self.sb(st, "lb", [128, 2, 4]); lbs = self.sb(st, "lbs", [128, 2, 4]); oml = self.sb(st, "oml", [128, 2, 4])
                self.ld(lbr[:], self.hglb.rearrange("d l p h -> p d l h"), [bprm])
                self.A(lambda h: h.activation(lbe[:], lbr[:], AF.Exp), [bprm], [bprm])
                self.V(lambda h: h.tensor_copy(lbs[:], lbe[:, :, 0, :]), [bprm], [bprm])
                for ll in range(1, c.L):
                    self.V(lambda h: h.tensor_add(lbs[:], lbs[:], lbe[:, :, ll, :]), [bprm], [bprm])
                self.V(lambda h: h.memset(lb[:], 0.0), [], [bprm])
                for ll in range(1, l + 1):
                    self.V(lambda h: h.tensor_add(lb[:], lb[:], lbe[:, :, ll, :]), [bprm], [bprm])
                self.V(lambda h: h.reciprocal(lbs[:], lbs[:]), [bprm], [bprm])
                self.V(lambda h: h.tensor_mul(lb[:], lb[:], lbs[:]), [bprm], [bprm])
                self.V(lambda h: h.tensor_scalar(oml[:], lb[:], -1.0, 1.0, ALU.mult, ALU.add), [bprm], [bprm])
            for d in range(2):
                with ExitStack() as s2:
                    rst = self.sb(s2, "rst", [128, TT + 1]); brs = Buf()
                    self.ld(rst[:], rsrc[:, :], [brs])
                    if mixer == 1:
                        ang = self.sb(s2, "ang", [128, c.SEQL]); bang = Buf()
                        self.ld(ang[:], self.rang[:, :], [bang])
                        sn, bsn, cs, bcs = self.sincos(s2, ang, bang, c.SEQL, "rot", share=ang)
                        self.V(lambda h: h.tensor_scalar(sn[0:64, :], sn[0:64, :], -1.0, None, ALU.mult), [bsn], [bsn])
                    q_, k_, z_, cum, e1 = T(s2, "q_"), T(s2, "k_"), T(s2, "z_"), T(s2, "cum"), T(s2, "e1")
                    tmp = z_ if mixer == 1 else T(s2, "tmp")
                    bq, bk, bz, bcum, be1 = (Buf() for _ in range(5))
                    btmp = bz if mixer == 1 else Buf()
                    qt = [T(s2, f"qt{h}", BF16) for h in range(4)]; kt = [T(s2, f"kt{h}", BF16) for h in range(4)]
                    bqt = [Buf() for _ in range(4)]; bkt = [Buf() for _ in range(4)]
                    dec = self.sb(s2, "dec", [128, NCH, 4]); em = self.sb(s2, "em", [128, NCH, 4]); elm = self.sb(s2, "elm", [128, NCH, 4]); bdec = Buf()
                    S_ = self.sb(s2, "S_", [128, 4, 128]); Sb = self.sb(s2, "Sb", [128, 4, 128], BF16); Ut = self.sb(s2, "Ut", [128, 4, 128]); bS, bSb, bUt = Buf(), Buf(), Buf()
                    sT = [self.sb(s2, f"sT{i}", [128, 4, C], BF16) for i in range(2)]; bsT = [Buf(), Buf()]
                    khT = [self.sb(s2, f"khT{i}", [128, 4, 128], BF16) for i in range(2)]; bkhT = [Buf(), Buf()]
                    c3 = lambda t: t[:].rearrange("p (c j) -> p c j", j=C)
                    for hd in range(4):
                        r0 = hd * 128
                        if mixer == 0:
                            self.ld(q_[:], self.pfm(b, 3, r0), [bq])
                            self.ld(z_[:], self.pfm(b, 4 + d, r0), [bz], q="act")
                            self.A(lambda h: h.activation(k_[:], z_[:], AF.Sigmoid, scale=-1.0), [bz], [bk])
                            self.A(lambda h: h.activation(z_[:], z_[:], AF.Sigmoid), [bz, bk], [bz])
                            self.A(lambda h: h.activation(z_[:], z_[:], AF.Ln, scale=oml[:, d, hd:hd + 1], bias=lb[:, d, hd:hd + 1]), [bz, bprm], [bz])
                            self.V(lambda h: h.tensor_scalar(k_[:], k_[:], oml[:, d, hd:hd + 1], None, ALU.mult), [bk, bprm], [bk])
                            gsrc, bgs = z_, bz
                            qscale, kscale = SQ, 1.0
                        else:
                            for (x_, bx_, pa, pb_) in ((q_, bq, 8, 12), (k_, bk, 9, 13)):
                                self.ld(x_[:], self.pfm(b, pa, r0), [bx_])
                                self.ld(e1[:], self.pfm(b, pb_, r0), [be1], q="act")
                                self.V(lambda h: h.tensor_mul(x_[:, CL:TT], x_[:, CL:TT], cs[:]), [bx_, bcs], [bx_])
                                self.V(lambda h: h.tensor_mul(e1[:, CL:TT], e1[:, CL:TT], sn[:]), [be1, bsn], [be1])
                                self.V(lambda h: h.tensor_add(x_[:, CL:TT], x_[:, CL:TT], e1[:, CL:TT]), [bx_, be1], [bx_])
                            gdec = math.log1p(-2.0 ** (-((5.0 if d == 0 else 5.5) + hd)))
                            gsrc, bgs = None, None
                            qscale, kscale = 1.0, SQ
                        if mixer == 1:
                            self.V(lambda h: h.memset(e1[:], gdec), [be1], [be1])
                            gsrc, bgs = e1, be1
                        if d == 0:
                            self.V(lambda h: h.tensor_tensor_scan(cum[:], rst[:, 0:TT], gsrc[:], 0.0, ALU.mult, ALU.add), [brs, bgs], [bcum])
                            mcol, lcol = C // 2 - 1, C - 1
                        else:
                            self.V(lambda h: h.tensor_tensor_scan(cum[:, ::-1], rst[:, 1:TT + 1][:, ::-1], gsrc[:, ::-1], 0.0, ALU.mult, ALU.add), [brs, bgs], [bcum])
                            mcol, lcol = C // 2, 0
                        cum3 = c3(cum)
                        mB = cum3[:, :, mcol:mcol + 1].to_broadcast([128, NCH, C])
                        self.V(lambda h: h.tensor_sub(c3(tmp), cum3, mB), [bcum, btmp], [btmp])
                        self.A(lambda h: h.activation(e1[:], tmp[:], AF.Exp), [btmp, be1], [be1])
                        self.V(lambda h: h.scalar_tensor_tensor(qt[hd][:], q_[:], qscale, e1[:], ALU.mult, ALU.mult), [bq, be1], [bqt[hd]])
                        self.A(lambda h: h.activation(e1[:], tmp[:], AF.Exp, scale=-1.0), [btmp, be1], [be1])
                        self.V(lambda h: h.scalar_tensor_tensor(kt[hd][:], k_[:], kscale, e1[:], ALU.mult, ALU.mult), [bk, be1], [bkt[hd]])
                        self.A(lambda h: h.activation(dec[:, :, hd:hd + 1], cum3[:, :, lcol:lcol + 1], AF.Exp), [bcum], [bdec])
                        self.A(lambda h: h.activation(em[:, :, hd:hd + 1], cum3[:, :, mcol:mcol + 1], AF.Exp), [bcum], [bdec])
                        self.V(lambda h: h.tensor_sub(elm[:, :, hd:hd + 1], cum3[:, :, lcol:lcol + 1], cum3[:, :, mcol:mcol + 1]), [bcum], [bdec])
                        self.A(lambda h: h.activation(elm[:, :, hd:hd + 1], elm[:, :, hd:hd + 1], AF.Exp), [bdec], [bdec])
                    self.V(lambda h: h.memset(S_[:], 0.0), [bS], [bS])
                    self.V(lambda h: h.memset(Sb[:], 0.0), [bSb], [bSb])
                    order = list(range(NCH)) if d == 0 else (list(range(NCHC - 1, -1, -1)) + list(range(NCH - 1, NCHC - 1, -1)))
                    for ci, ch in enumerate(order):
                        i, hh = ch // NPT, ch % NPT
                        P0 = C * hh
                        cs_ = slice(ch * C, ch * C + C)
                        j = ci % 2
                        pS, pO, pK, pU = self.ps[0 + j], self.ps[2 + j], self.ps[4 + j], self.ps[6 + j]
                        bpS, bpO, bpK, bpU = self.pb[0 + j], self.pb[2 + j], self.pb[4 + j], self.pb[6 + j]
                        for hd in range(4):
                            self.PE(lambda h: h.matmul(pS[P0:P0 + C, hd * C:(hd + 1) * C], kt[hd][:, cs_], qt[hd][:, cs_], start=True, stop=True),
                                    [bkt[hd], bqt[hd]], [bpS], inc=(hd == 3))
                        self.G(lambda h: h.memset(sT[j][P0:P0 + C], 0.0), [bsT[j]], [bsT[j]])
                        self.V(lambda h: h.copy_predicated(sT[j][P0:P0 + C], cmask[P0:P0 + C, d].bitcast(mybir.dt.uint32), pS[P0:P0 + C, 0:4 * C].rearrange("p (h t) -> p h t", t=C)),
                               [bpS, self.bconst, bsT[j]], [bsT[j]])
                        self.V(lambda h: h.tensor_mul(Sb[:], S_[:], em[:, ch, :].unsqueeze(2).to_broadcast([128, 4, 128])), [bS, bdec, bSb], [bSb])
                        for hd in range(4):
                            osl = pO[P0:P0 + C, hd * 128:(hd + 1) * 128]
                            self.PE(lambda h: h.matmul(osl, sT[j][P0:P0 + C, hd, :], vbf[P0:P0 + C, i, hd * 128:(hd + 1) * 128], start=True, stop=False),
                                    [bsT[j], bv], [bpO], inc=False)
                            self.PE(lambda h: h.matmul(osl, qt[hd][:, cs_], Sb[:, hd, :], start=False, stop=True), [bqt[hd], bSb], [bpO], inc=(hd == 3))
                        if d == 0:
                            self.A(lambda h: h.copy(oacc[P0:P0 + C, i, :], pO[P0:P0 + C, :]), [bpO], [bo])
                        else:
                            self.V(lambda h: h.tensor_add(oacc[P0:P0 + C, i, :], oacc[P0:P0 + C, i, :], pO[P0:P0 + C, :]), [bpO, bo], [bo])
                        pKb = pK[:].bitcast(BF16)
                        for hd in range(4):
                            self.PE(lambda h: h.transpose(pKb[P0:P0 + C, hd * 128:(hd + 1) * 128], kt[hd][:, cs_], self.idb[:]), [bkt[hd], self.bconst], [bpK], inc=(hd == 3))
                        self.A(lambda h: h.copy(khT[j][P0:P0 + C].rearrange("p h d -> p (h d)"), pKb[P0:P0 + C, 0:512]), [bpK], [bkhT[j]])
                        for hd in range(4):
                            self.PE(lambda h: h.matmul(pU[:, hd * 128:(hd + 1) * 128], khT[j][P0:P0 + C, hd, :], vbf[P0:P0 + C, i, hd * 128:(hd + 1) * 128], start=True, stop=True),
                                    [bkhT[j], bv], [bpU], inc=(hd == 3))
                        self.V(lambda h: h.tensor_mul(Ut[:], pU[:, :].rearrange("p (h d) -> p h d", d=128), elm[:, ch, :].unsqueeze(2).to_broadcast([128, 4, 128])), [bpU, bdec, bUt], [bUt])
                        self.V(lambda h: h.tensor_mul(S_[:], S_[:], dec[:, ch, :].unsqueeze(2).to_broadcast([128, 4, 128])), [bS, bdec], [bS])
                        self.V(lambda h: h.tensor_add(S_[:], S_[:], Ut[:]), [bS, bUt], [bS])
                self.S.barrier()
            with ExitStack() as s3:
                graw = self.sb(s3, "graw", [128, NT, W]); bg = Buf()
                self.ld(graw[:], self.ptm(b, 7 if mixer == 0 else 11), [bg], q="act")
                self.A(lambda h: h.activation(graw[:], graw[:], AF.Silu), [bg], [bg])
                sq = self.sb(s3, "gsq", [128, NT, W]); ssq = self.sb(s3, "ssq", [128, NT * 4]); bsq = Buf()
                o4 = oacc[:].rearrange("p i (h d) -> p (i h) d", d=128)
                self.V(lambda h: h.tensor_mul(sq[:], oacc[:], oacc[:]), [bo], [bsq])
                self.V(lambda h: h.tensor_reduce(ssq[:], sq[:].rearrange("p i (h d) -> p (i h) d", d=128), AX.X, ALU.add), [bsq], [bsq])
                self.A(lambda h: h.activation(ssq[:], ssq[:], AF.Sqrt, scale=1.0 / 128, bias=self.epsc[:, 0:1]), [bsq, self.bconst], [bsq])
                self.V(lambda h: h.reciprocal(ssq[:], ssq[:]), [bsq], [bsq])
                self.V(lambda h: h.tensor_mul(o4, o4, ssq[:].unsqueeze(2).to_broadcast([128, NT * 4, 128])), [bo, bsq], [bo])
                self.V(lambda h: h.tensor_mul(vbf[:], oacc[:], graw[:]), [bo, bg, bv], [bv])
                mst = self.sb(s3, "mst", [128, 4, TT], BF16); bm = Buf()
                for i in range(NT):
                    p, pb = self.ps[i % 4], self.pb[i % 4]
                    pv = p[:].bitcast(BF16)
                    for hd in range(4):
                        self.PE(lambda h: h.transpose(pv[:, hd * 128:(hd + 1) * 128], vbf[:, i, hd * 128:(hd + 1) * 128], self.idb[:]), [bv, self.bconst], [pb], inc=(hd == 3))
                    self.A(lambda h: h.copy(mst[:, :, i * 128:(i + 1) * 128], pv[:, 0:512].rearrange("p (h t) -> p h t", t=128)), [pb], [bm])
                base = 2 * W + mixer * W
                self.st(self.mixT[b, base:base + W, :].rearrange("(h p) t -> p h t", p=128), mst[:], [bm])
                self.S.barrier()

    def phase_s5(self, l, b):
        c = self.c
        TT, CL = c.TT, c.CTXL
        tgs = tok_groups(TT)
        with ExitStack() as st:
            T = lambda n, dt=F32: self.sb(st, n, [128, TT], dt)
            lam = self.sb(st, "lam", [128, 3, 2, 16]); bp = Buf()
            self.ld(lam[:], self.s5lam[l], [bp])
            P = lambda n: self.sb(st, n, [128, 32])
            dt_, mag, th, thc, are, aim, den, wre, wim, t1, t2 = (P(f"p{i}") for i in range(11))
            lr = lam[:, 0].rearrange("p d s -> p (d s)"); li = lam[:, 1].rearrange("p d s -> p (d s)"); ldt = lam[:, 2].rearrange("p d s -> p (d s)")
            self.A(lambda h: h.activation(dt_[:], ldt, AF.Exp), [bp], [bp])
            self.V(lambda h: h.tensor_mul(t1[:], lr, dt_[:]), [bp], [bp])
            self.A(lambda h: h.activation(mag[:], t1[:], AF.Exp), [bp], [bp])
            self.V(lambda h: h.tensor_mul(th[:], li, dt_[:]), [bp], [bp])
            sn, bsn, cs, bcs = self.sincos(st, th, bp, 32, "ab")
            self.V(lambda h: h.tensor_mul(are[:], mag[:], cs[:]), [bp, bcs], [bp])
            self.V(lambda h: h.tensor_mul(aim[:], mag[:], sn[:]), [bp, bsn], [bp])
            self.V(lambda h: h.tensor_mul(den[:], lr, lr), [bp], [bp])
            self.V(lambda h: h.tensor_mul(t1[:], li, li), [bp], [bp])
            self.V(lambda h: h.tensor_add(den[:], den[:], t1[:]), [bp], [bp])
            self.V(lambda h: h.reciprocal(den[:], den[:]), [bp], [bp])
            self.V(lambda h: h.tensor_scalar(t2[:], are[:], -1.0, None, ALU.add), [bp], [bp])
            self.V(lambda h: h.tensor_mul(wre[:], t2[:], lr), [bp], [bp])
            self.V(lambda h: h.tensor_mul(t1[:], aim[:], li), [bp], [bp])
            self.V(lambda h: h.tensor_add(wre[:], wre[:], t1[:]), [bp], [bp])
            self.V(lambda h: h.tensor_mul(wre[:], wre[:], den[:]), [bp], [bp])
            self.V(lambda h: h.tensor_mul(wim[:], aim[:], lr), [bp], [bp])
            self.V(lambda h: h.tensor_mul(t1[:], t2[:], li), [bp], [bp])
            self.V(lambda h: h.tensor_sub(wim[:], wim[:], t1[:]), [bp], [bp])
            self.V(lambda h: h.tensor_mul(wim[:], wim[:], den[:]), [bp], [bp])
            nwim = P("nwim")
            self.V(lambda h: h.tensor_scalar(nwim[:], wim[:], -1.0, None, ALU.mult), [bp], [bp])
            thi = self.sb(st, "thi", [128, 32], I32); thf = P("thf")
            self.V(lambda h: h.tensor_scalar(thc[:], th[:], 1.0 / TWO_PI, None, ALU.mult), [bp], [bp])
            self.wrap_frac(thc, thi, thf, bp)
            dsk = self.sb(st, "dsk", [128, 4]); self.ld(dsk[:], self.s5d[l], [bp])
            with ExitStack() as s2:
                T = lambda n, dt=F32: self.sb(s2, n, [128, TT], dt)
                kid = [T("kid0"), T("kid1")]; bkid = Buf()
                self.ld(kid[0][:], self.kidx[0], [bkid]); self.ld(kid[1][:], self.kidx[1], [bkid], q="act")
                ub = self.sb(s2, "ub", [128, 4, TT], BF16); bu = Buf()
                self.ldc(ub[:], self.Pfm[b, 0:W, :].rearrange("(c p) t -> p c t", p=128), [bu])
                uf = T("uf"); buf_ = Buf()
                Bt = [self.sb(s2, f"Bt{i}", [128, 2, 128], BF16) for i in range(2)]; bB = [Buf(), Buf()]
                Cf = [self.sb(s2, f"Cf{i}", [128, 2, 128]) for i in range(2)]; bC = [Buf(), Buf()]
                Cw = [self.sb(s2, f"Cw{i}", [128, 2, 128], BF16) for i in range(2)]; bCw = [Buf(), Buf()]
                ct = self.sb(s2, "ct", [128, 128]); bct = Buf()
                y, yi = T("ty"), T("tyi", I32)
                sn_, cs_ = T("tsn"), T("tcs"); by, bsn_, bcs_ = Buf(), Buf(), Buf()
                bur, bui, gre, gim, t3 = T("bur"), T("bui"), T("gre"), T("gim"), T("t3")
                bbur, bbui, bgre, bgim, bt3 = (Buf() for _ in range(5))
                hre = [T(f"hre{i}", BF16) for i in range(2)]; him = [T(f"him{i}", BF16) for i in range(2)]
                bhre = [Buf(), Buf()]; bhim = [Buf(), Buf()]
                ytmp = T("ytmp"); bytmp = Buf()
                yo = T("yo"); byo = Buf()
                k = 0
                for cc in range(4):
                    self.ld(uf[:], self.Pfm[b, cc * 128:(cc + 1) * 128, :], [buf_])
                    pY = [self.ps[4 + g] for g in range(4)]; bpY = [self.pb[4 + g] for g in range(4)]
                    first = True
                    for d in range(2):
                        for s4 in range(4):
                            sc = cc * 4 + s4
                            col = d * 16 + sc
                            j = k % 2; k += 1
                            lastone = (d == 1 and s4 == 3)
                            self.ldc(Bt[j][:], self.s5B[l, d, sc], [bB[j]])
                            self.ld(Cf[j][:], self.s5C[l, d, sc], [bC[j]], q="act")
                            self.V(lambda h: h.tensor_scalar(ct[:], Cf[j][:, 1, :], nwim[:, col:col + 1], None, ALU.mult), [bC[j], bp, bct], [bct])
                            self.V(lambda h: h.scalar_tensor_tensor(Cw[j][:, 0, :], Cf[j][:, 0, :], wre[:, col:col + 1], ct[:], ALU.mult, ALU.add), [bC[j], bp, bct], [bCw[j]])
                            self.V(lambda h: h.tensor_scalar(ct[:], Cf[j][:, 1, :], wre[:, col:col + 1], -1.0, ALU.mult, ALU.mult), [bC[j], bp, bct], [bct])
                            self.V(lambda h: h.scalar_tensor_tensor(Cw[j][:, 1, :], Cf[j][:, 0, :], nwim[:, col:col + 1], ct[:], ALU.mult, ALU.add), [bC[j], bp, bct], [bCw[j]])
                            self.V(lambda h: h.tensor_scalar(yi[:], kid[d][:], thc[:, col:col + 1], None, ALU.mult), [bkid, bp, by], [by])
                            self.V(lambda h: h.tensor_copy(cs_[:], yi[:]), [by, bcs_], [bcs_])
                            self.V(lambda h: h.scalar_tensor_tensor(y[:], kid[d][:], thc[:, col:col + 1], cs_[:], ALU.mult, ALU.subtract), [bkid, bp, bcs_, by], [by])
                            self.A(lambda h: h.activation(sn_[:], y[:], AF.Sin, scale=TWO_PI), [by, bsn_], [bsn_])
                            self.A(lambda h: h.activation(cs_[:], y[:], AF.Abs), [by, bcs_], [bcs_])
                            self.A(lambda h: h.activation(cs_[:], cs_[:], AF.Sin, scale=-TWO_PI, bias=self.hpi[:, 0:1]), [bcs_, self.bconst], [bcs_])
                            for (ri, dst, bd) in ((0, bur, bbur), (1, bui, bbui)):
                                for gi, (t0, n) in enumerate(tgs):
                                    ps, pb = self.ps[gi % 4], self.pb[gi % 4]
                                    self.PE(lambda h: h.matmul(ps[:, 0:n], Bt[j][:, ri, :], ub[:, cc, t0:t0 + n], start=True, stop=True), [bB[j], bu], [pb])
                                    self.A(lambda h: h.copy(dst[:, t0:t0 + n], ps[:, 0:n]), [pb], [bd])
                            self.V(lambda h: h.tensor_mul(gre[:], bur[:], cs_[:]), [bbur, bcs_], [bgre])
                            self.V(lambda h: h.tensor_mul(t3[:], bui[:], sn_[:]), [bbui, bsn_], [bt3])
                            self.V(lambda h: h.tensor_add(gre[:], gre[:], t3[:]), [bgre, bt3], [bgre])
                            self.V(lambda h: h.tensor_mul(gim[:], bui[:], cs_[:]), [bbui, bcs_], [bgim])
                            self.V(lambda h: h.tensor_mul(t3[:], bur[:], sn_[:]), [bbur, bsn_], [bt3])
                            self.V(lambda h: h.tensor_sub(gim[:], gim[:], t3[:]), [bgim, bt3], [bgim])
                            for (src, dst, bs_, bd) in ((gre, bur, bgre, bbur), (gim, bui, bgim, bbui)):
                                if d == 0:
                                    self.V(lambda h: h.tensor_tensor_scan(dst[:], mag[:, col:col + 1].to_broadcast([128, TT]), src[:], 0.0, ALU.mult, ALU.add), [bs_, bp], [bd])
                                else:
                                    mC = mag[:, col:col + 1].to_broadcast([128, CL]); mL = mag[:, col:col + 1].to_broadcast([128, TT - CL])
                                    self.V(lambda h: h.tensor_tensor_scan(dst[:, 0:CL][:, ::-1], mC, src[:, 0:CL][:, ::-1], 0.0, ALU.mult, ALU.add), [bs_, bp], [bd])
                                    self.V(lambda h: h.tensor_tensor_scan(dst[:, CL:TT][:, ::-1], mL, src[:, CL:TT][:, ::-1], dst[:, 0:1], ALU.mult, ALU.add), [bs_, bp, bd], [bd])
                            self.V(lambda h: h.tensor_mul(gre[:], bur[:], cs_[:]), [bbur, bcs_], [bgre])
                            self.V(lambda h: h.tensor_mul(t3[:], bui[:], sn_[:]), [bbui, bsn_], [bt3])
                            self.V(lambda h: h.tensor_sub(hre[j][:], gre[:], t3[:]), [bgre, bt3], [bhre[j]])
                            self.V(lambda h: h.tensor_mul(gim[:], bur[:], sn_[:]), [bbur, bsn_], [bgim])
                            self.V(lambda h: h.tensor_mul(t3[:], bui[:], cs_[:]), [bbui, bcs_], [bt3])
                            self.V(lambda h: h.tensor_add(him[j][:], gim[:], t3[:]), [bgim, bt3], [bhim[j]])
                            for gi, (t0, n) in enumerate(tgs):
                                if gi < 4:
                                    self.PE(lambda h: h.matmul(pY[gi][:, 0:n], Cw[j][:, 0, :], hre[j][:, t0:t0 + n], start=first, stop=False), [bCw[j], bhre[j]], [bpY[gi]], inc=False)
                                    self.PE(lambda h: h.matmul(pY[gi][:, 0:n], Cw[j][:, 1, :], him[j][:, t0:t0 + n], start=False, stop=lastone), [bCw[j], bhim[j]], [bpY[gi]])
                                else:
                                    ps, pb = self.ps[gi % 4], self.pb[gi % 4]
                                    self.PE(lambda h: h.matmul(ps[:, 0:n], Cw[j][:, 0, :], hre[j][:, t0:t0 + n], start=True, stop=False), [bCw[j], bhre[j]], [pb], inc=False)
                                    self.PE(lambda h: h.matmul(ps[:, 0:n], Cw[j][:, 1, :], him[j][:, t0:t0 + n], start=False, stop=True), [bCw[j], bhim[j]], [pb])
                                    if first:
                                        self.V(lambda h: h.tensor_copy(ytmp[:, t0:t0 + n], ps[:, 0:n]), [pb], [bytmp])
                                    else:
                                        self.V(lambda h: h.tensor_add(ytmp[:, t0:t0 + n], ytmp[:, t0:t0 + n], ps[:, 0:n]), [pb, bytmp], [bytmp])
                            first = False
                    for gi, (t0, n) in enumerate(tgs):
                        if gi < 4:
                            self.V(lambda h: h.scalar_tensor_tensor(yo[:, t0:t0 + n], uf[:, t0:t0 + n], dsk[:, cc:cc + 1], pY[gi][:, 0:n], ALU.mult, ALU.add), [buf_, bp, bpY[gi]], [byo])
                        else:
                            self.V(lambda h: h.scalar_tensor_tensor(yo[:, t0:t0 + n], uf[:, t0:t0 + n], dsk[:, cc:cc + 1], ytmp[:, t0:t0 + n], ALU.mult, ALU.add), [buf_, bp, bytmp], [byo])
                    self.gelu(yo[:], yo[:], t3[:], [byo], byo, bt3)
                    self.st(self.ygD[b, cc * 128:(cc + 1) * 128, :], yo[:], [byo])
                self.S.barrier()
            with ExitStack() as s3:
                T = lambda n, dt=F32: self.sb(s3, n, [128, TT], dt)
                yg = self.sb(s3, "yg", [128, 4, TT]); ygb = self.sb(s3, "ygb", [128, 4, TT], BF16); byg = Buf()
                self.ld(yg[:], self.ygD[b].rearrange("(c p) t -> p c t", p=128), [byg])
                self.A(lambda h: h.copy(ygb[:], yg[:]), [byg], [byg])
                gw = self.sb(s3, "gw", [128, 4, W], BF16); gb = self.sb(s3, "gb", [128, 4]); bgw = Buf()
                self.ldc(gw[:], self.gluw[l].rearrange("(kc kp) n -> kp kc n", kp=128), [bgw])
                self.ld(gb[:], self.glub[l], [bgw])
                ao = [T("ao0", BF16), T("ao1", BF16)]; bao = [Buf(), Buf()]
                sg = T("sg"); bsg = Buf()
                for oc in range(4):
                    j = oc % 2
                    for gi, (t0, n) in enumerate(tgs):
                        ps, pb = self.ps[gi % 4], self.pb[gi % 4]
                        for kc in range(4):
                            self.PE(lambda h: h.matmul(ps[:, 0:n], gw[:, kc, oc * 128:(oc + 1) * 128], ygb[:, kc, t0:t0 + n], start=(kc == 0), stop=(kc == 3)), [bgw, byg], [pb], inc=(kc == 3))
                        self.A(lambda h: h.activation(sg[:, t0:t0 + n], ps[:, 0:n], AF.Sigmoid, bias=gb[:, oc:oc + 1]), [pb, bgw], [bsg])
                    self.V(lambda h: h.tensor_mul(ao[j][:], yg[:, oc, :], sg[:]), [byg, bsg], [bao[j]])
                    self.st(self.mixT[b, oc * 128:(oc + 1) * 128, :], ao[j][:], [bao[j]])
                self.S.barrier()

    def phase_out(self, l, b, xsrc, last):
        c = self.c
        TT, NT = c.TT, c.NT
        with ExitStack() as st:
            wo = self.sb(st, "wo", [128, KC, D], BF16); bwo = Buf()
            wsrc = self.w_out[l].rearrange("(kc kp) n -> kp kc n", kp=128)
            for g in range(4):
                self.ldc(wo[:, :, g * 512:(g + 1) * 512], wsrc[:, :, g * 512:(g + 1) * 512], [bwo])
            gB = {}
            for src in ([b] if last else [b, 2]):
                gB[src] = self.bcast_row(st, 0, src, f"gmsa{src}")
            A2 = {}; B2 = {}
            if c.sparse:
                for src in ([b] if last else [b, 2]):
                    A2[src] = self.bcast_row(st, 2, src, f"a2r{src}")
                    B2[src] = self.bcast_row(st, 3, src, f"b2r{src}")
            htk = [self.sb(st, f"htk{i}", [128, D], BF16) for i in range(2)]; bhtk = [Buf(), Buf()]
            htf = self.sb(st, "htf", [128, D]); bhtf = Buf()
            s01 = [self.sb(st, f"s01{i}", [128, NE]) for i in range(2)]; bs01 = [Buf(), Buf()]
            rk = [self.sb(st, f"rk{i}", [128, NE]) for i in range(2)]; brk = [Buf(), Buf()]
            rwt = self.sb(st, "rwt", [128, KC, NE]); rb = self.sb(st, "rb", [128, NE]); brw = Buf()
            self.ld(rwt[:], self.rw[:, :, :], [brw])
            self.ld(rb[:], self.rbias[0:1, :].partition_broadcast(128)[:, 0, :], [brw])
            nt = self.norm_tiles(st); nt["xsf"] = self.sb(st, "xsf", [128, D])
            mt = [self.sb(st, f"mt{i}", [128, KC, 128], BF16) for i in range(2)]; bmt = [Buf(), Buf()]
            xt = [self.sb(st, f"xo{i}", [128, D]) for i in range(2)]; bx = [Buf(), Buf()]
            h2f = self.sb(st, "h2f", [128, KC, 128]); h2b = [self.sb(st, f"h2b{i}", [128, KC, 128], BF16) for i in range(2)]
            bh2f = Buf(); bh2b = [Buf(), Buf()]
            R = lambda n, w=NE: self.sb(st, n, [128, w])
            scr, bia, m1, m2, gs, gmx, ing, sel, tmp4, tmp16, gsum = R("scr"), R("bia"), R("m1", 4), R("m2", 4), R("gs", 4), R("gmx", 1), R("ing", 4), R("sel"), R("tmp4", 4), R("tmp16"), R("gsum", 1)
            gout = [R("gout0"), R("gout1")]; bgo = [Buf(), Buf()]
            br_ = Buf()
            tmo = [self.sb(st, f"tmo{i}", [128, 512]) for i in range(2)]; btmo = [Buf(), Buf()]
            tiles = range(c.NTC, NT) if last else range(NT)
            for i in tiles:
                j = i % 2
                src = 2 if i < c.NTC else b
                self.ld(mt[j][:], self.mixT[b, :, i * 128:(i + 1) * 128].rearrange("(kc kp) t -> kp kc t", kp=128), [bmt[j]], q="act")
                self.ld(xt[j][:], xsrc[b, i * 128:(i + 1) * 128, :], [bx[j]])
                for g in range(4):
                    ps, pb = self.ps[g], self.pb[g]
                    for kc in range(KC):
                        self.PE(lambda h: h.matmul(ps[:, :], mt[j][:, kc, :], wo[:, kc, g * 512:(g + 1) * 512], start=(kc == 0), stop=(kc == KC - 1)), [bmt[j], bwo], [pb], inc=(kc == KC - 1))
                    gt_, bgt_ = gB[src]
                    sl = slice(g * 512, (g + 1) * 512)
                    self.V(lambda h: h.tensor_tensor(tmo[g % 2][:], ps[:, :], gt_[:, sl], ALU.mult), [pb, bgt_], [btmo[g % 2]])
                    self.V(lambda h: h.tensor_add(xt[j][:, sl], xt[j][:, sl], tmo[g % 2][:]), [btmo[g % 2], bx[j]], [bx[j]])
                self.st(self.xres[b, i * 128:(i + 1) * 128, :], xt[j][:], [bx[j]])
                self.norm_T(nt, xt[j], bx[j], 1, src, lambda kc: h2f[:, kc, :], bh2f, fp32=True)
                if not c.sparse:
                    self.A(lambda h: h.copy(h2b[j][:], h2f[:]), [bh2f], [bh2b[j]])
                    self.st(self.h2T[b, :, i * 128:(i + 1) * 128].rearrange("(kc kp) t -> kp kc t", kp=128), h2b[j][:], [bh2b[j]], q="act")
                else:
                    xsf = nt["xsf"]
                    self.V(lambda h: h.tensor_mul(htf[:], xsf[:], A2[src][0][:]), [nt["bxs"], A2[src][1], bhtf], [bhtf])
                    self.V(lambda h: h.tensor_add(htk[j][:], htf[:], B2[src][0][:]), [bhtf, B2[src][1]], [bhtk[j]])
                    self.st(self.h2tok[b * TT + i * 128: b * TT + (i + 1) * 128, :], htk[j][:], [bhtk[j]], q="act")
                pr, bpr = self.ps[4], self.pb[4]
                for kc in range(KC):
                    self.PE(lambda h: h.matmul(pr[:, 0:NE], h2f[:, kc, :], rwt[:, kc, :], start=(kc == 0), stop=(kc == KC - 1)), [bh2f, brw], [bpr], inc=(kc == KC - 1))
                self.A(lambda h: h.activation(scr[:], pr[:, 0:NE], AF.Sigmoid), [bpr], [br_])
                self.V(lambda h: h.tensor_add(bia[:], scr[:], rb[:]), [br_, brw], [br_])
                b4 = bia[:].rearrange("p (g e) -> p g e", e=4)
                self.V(lambda h: h.tensor_reduce(m1[:], b4, AX.X, ALU.max), [br_], [br_])
                self.V(lambda h: h.tensor_tensor(tmp16[:].rearrange("p (g e) -> p g e", e=4), b4, m1[:].unsqueeze(2).to_broadcast([128, 4, 4]), ALU.is_equal), [br_], [br_])
                self.V(lambda h: h.scalar_tensor_tensor(tmp16[:], tmp16[:], -1e9, bia[:], ALU.mult, ALU.add), [br_], [br_])
                self.V(lambda h: h.tensor_reduce(m2[:], tmp16[:].rearrange("p (g e) -> p g e", e=4), AX.X, ALU.max), [br_], [br_])
                self.V(lambda h: h.tensor_add(gs[:], m1[:], m2[:]), [br_], [br_])
                self.V(lambda h: h.tensor_reduce(gmx[:], gs[:], AX.X, ALU.max), [br_], [br_])
                self.V(lambda h: h.tensor_tensor(ing[:], gs[:], gmx[:].to_broadcast([128, 4]), ALU.is_equal), [br_], [br_])
                self.V(lambda h: h.tensor_tensor(sel[:].rearrange("p (g e) -> p g e", e=4), b4, m2[:].unsqueeze(2).to_broadcast([128, 4, 4]), ALU.is_ge), [br_], [br_])
                self.V(lambda h: h.tensor_mul(sel[:].rearrange("p (g e) -> p g e", e=4), sel[:].rearrange("p (g e) -> p g e", e=4), ing[:].unsqueeze(2).to_broadcast([128, 4, 4])), [br_], [br_])
                if c.sparse:
                    self.V(lambda h: h.tensor_copy(s01[j][:], sel[:]), [br_], [bs01[j]])
                    pk, bpk = self.ps[5], self.pb[5]
                    self.PE(lambda h: h.matmul(pk[:, 0:NE], self.ltt[:, 0, :], s01[j][:], start=True, stop=True), [bs01[j], self.bconst], [bpk], inc=False)
                    self.PE(lambda h: h.matmul(pk[:, NE:2 * NE], self.ltt[:, 1, :], s01[j][:], start=True, stop=True), [bs01[j], self.bconst], [bpk])
                    self.V(lambda h: h.tensor_add(rk[j][:], pk[:, 0:NE], self.run[:]), [bpk, self.brun], [brk[j]])
                    self.V(lambda h: h.tensor_add(self.run[:], self.run[:], pk[:, NE:2 * NE]), [bpk, self.brun], [self.brun])
                    self.st(self.selD[b * TT + i * 128: b * TT + (i + 1) * 128, :], s01[j][:], [bs01[j]])
                    self.st(self.rankD[b * TT + i * 128: b * TT + (i + 1) * 128, :], rk[j][:], [brk[j]], q="act")
                self.V(lambda h: h.tensor_mul(sel[:], sel[:], scr[:]), [br_], [br_])
                self.V(lambda h: h.tensor_reduce(gsum[:], sel[:], AX.X, ALU.add), [br_], [br_])
                self.V(lambda h: h.reciprocal(gsum[:], gsum[:]), [br_], [br_])
                self.V(lambda h: h.tensor_scalar(gout[j][:], sel[:], gsum[:, 0:1], None, ALU.mult), [br_], [bgo[j]])
                self.st(self.gates[b, i * 128:(i + 1) * 128, :], gout[j][:], [bgo[j]], q="act")

    def phase_moe(self, l, b, last):
        c = self.c
        NT = c.NT
        tiles = list(range(c.NTC, NT)) if last else list(range(NT))
        STM = 6
        supers = [tiles[i:i + STM] for i in range(0, len(tiles), STM)]
        with ExitStack() as st:
            h2 = self.sb(st, "h2s", [128, KC, STM * 128], BF16); bh2 = Buf()
            gt = self.sb(st, "gts", [128, STM, NE]); bgt = Buf()
            yacc = self.sb(st, "yacc", [128, STM, D]); bya = Buf()
            he = self.sb(st, "he", [128, 8, STM * 128], BF16); bhe = Buf()
            wdn = [self.sb(st, f"wdn{i}", [128, 8, D], BF16) for i in range(2)]; bwd = [Buf(), Buf()]
            wgu = [self.sb(st, f"wgu{i}", [128, 2, KC, 128], BF16) for i in range(2)]; bwgu = [Buf() for _ in range(2)]
            sg = self.sb(st, "sgm", [128, STM * 128]); bsg = Buf()
            gB = {}
            for src in ([b] if last else [b, 2]):
                gB[src] = self.bcast_row(st, 1, src, f"gmlp{src}")
            xt = [self.sb(st, "xm0", [128, D])] * 2; bx = [Buf()] * 2
            ew = 0
            for sup in supers:
                n_t = len(sup); ntok = n_t * 128
                t0 = sup[0] * 128
                self.ld(h2[:, :, 0:ntok], self.h2T[b, :, t0:t0 + ntok].rearrange("(kc kp) t -> kp kc t", kp=128), [bh2])
                self.ld(gt[:, 0:n_t, :], self.gates[b, t0:t0 + ntok, :].rearrange("(i p) e -> p i e", p=128), [bgt], q="act")
                tg = tok_groups(ntok)
                for e in range(NE):
                    jd = e % 2
                    wds = self.wd[l, e].rearrange("(fc fp) n -> fp fc n", fp=128)
                    for g in range(2):
                        self.ldc(wdn[jd][:, :, g * 1024:(g + 1) * 1024], wds[:, :, g * 1024:(g + 1) * 1024], [bwd[jd]])
                    for fc in range(8):
                        jw = ew % 2; ew += 1
                        self.ldc(wgu[jw][:, 0], self.wg[l, e][:, fc * 128:(fc + 1) * 128].rearrange("(kc kp) f -> kp kc f", kp=128), [bwgu[jw]])
                        self.ldc(wgu[jw][:, 1], self.wu[l, e][:, fc * 128:(fc + 1) * 128].rearrange("(kc kp) f -> kp kc f", kp=128), [bwgu[jw]])
                        for gi, (s0, n) in enumerate(tg):
                            pG, pU = self.ps[gi], self.ps[2 + gi]; bpG, bpU = self.pb[gi], self.pb[2 + gi]
                            for kc in range(KC):
                                self.PE(lambda h: h.matmul(pG[:, 0:n], wgu[jw][:, 0, kc, :], h2[:, kc, s0:s0 + n], start=(kc == 0), stop=(kc == KC - 1)), [bwgu[jw], bh2], [bpG], inc=(kc == KC - 1))
                            for kc in range(KC):
                                self.PE(lambda h: h.matmul(pU[:, 0:n], wgu[jw][:, 1, kc, :], h2[:, kc, s0:s0 + n], start=(kc == 0), stop=(kc == KC - 1)), [bwgu[jw], bh2], [bpU], inc=(kc == KC - 1))
                            self.A(lambda h: h.activation(sg[:, s0:s0 + n], pG[:, 0:n], AF.Silu), [bpG], [bsg])
                            self.V(lambda h: h.tensor_mul(he[:, fc, s0:s0 + n], sg[:, s0:s0 + n], pU[:, 0:n]), [bsg, bpU], [bhe])
                    for ti in range(n_t):
                        for g in range(4):
                            ps, pb = self.ps[4 + g], self.pb[4 + g]
                            for fc in range(8):
                                self.PE(lambda h: h.matmul(ps[:, :], he[:, fc, ti * 128:(ti + 1) * 128], wdn[jd][:, fc, g * 512:(g + 1) * 512], start=(fc == 0), stop=(fc == 7)), [bhe, bwd[jd]], [pb], inc=(fc == 7))
                            ysl = yacc[:, ti, g * 512:(g + 1) * 512]
                            if e == 0:
                                self.V(lambda h: h.tensor_scalar(ysl, ps[:, :], gt[:, ti, e:e + 1], None, ALU.mult), [pb, bgt], [bya])
                            else:
                                self.V(lambda h: h.scalar_tensor_tensor(ysl, ps[:, :], gt[:, ti, e:e + 1], ysl, ALU.mult, ALU.add), [pb, bgt, bya], [bya])
                for ti, i in enumerate(sup):
                    j = i % 2
                    src = 2 if i < c.NTC else b
                    gt_, bgt_ = gB[src]
                    self.ld(xt[j][:], self.xres[b, i * 128:(i + 1) * 128, :], [bx[j]])
                    self.V(lambda h: h.tensor_mul(yacc[:, ti, :], yacc[:, ti, :], gt_[:]), [bya, bgt_], [bya])
                    self.V(lambda h: h.tensor_add(xt[j][:], xt[j][:], yacc[:, ti, :]), [bya, bx[j]], [bx[j]])
                    self.st(self.xres[b, i * 128:(i + 1) * 128, :], xt[j][:], [bx[j]])


    def moe_tiles(self, last):
        c = self.c
        tl = range(c.NTC, c.NT) if last else range(c.NT)
        return [(b, i) for b in range(c.NB) for i in tl]

    def n_slots(self, last):
        c = self.c
        return (2 * len(self.moe_tiles(last)) * 128 + c.SLOT - 1) // c.SLOT + NE

    def phase_route(self, l, last):
        c = self.c
        TT = c.TT
        NS = self.n_slots(last)
        SL = float(c.SLOT)
        with ExitStack() as st:
            R = lambda n, w=NE, dt=F32: self.sb(st, n, [128, w], dt)
            x, xi, xf, nsl, cum, base, one = R("rx"), R("rxi", NE, I32), R("rxf"), R("nsl"), R("cum"), R("base"), R("one")
            bq = Buf()
            self.V(lambda h: h.tensor_scalar(x[:], self.run[:], SL - 1.0, 1.0 / SL, ALU.add, ALU.mult), [self.brun], [bq])
            self.V(lambda h: h.tensor_copy(xi[:], x[:]), [bq], [bq])
            self.V(lambda h: h.tensor_copy(xf[:], xi[:]), [bq], [bq])
            self.V(lambda h: h.tensor_tensor(nsl[:], xf[:], x[:], ALU.is_gt), [bq], [bq])
            self.V(lambda h: h.tensor_sub(nsl[:], xf[:], nsl[:]), [bq], [bq])
            self.V(lambda h: h.memset(one[:], 1.0), [], [bq])
            self.V(lambda h: h.tensor_tensor_scan(cum[:], one[:], nsl[:], 0.0, ALU.mult, ALU.add), [bq], [bq])
            self.V(lambda h: h.tensor_sub(base[:], cum[:], nsl[:]), [bq], [bq])
            self.V(lambda h: h.tensor_scalar(base[:], base[:], SL, None, ALU.mult), [bq], [bq])
            sio = R("sio", NS); ge = self.sb(st, "ge", [128, NS, NE]); es = R("es", NS)
            self.ld(sio[:], self.siota[:, 0:NS], [bq])
            self.V(lambda h: h.tensor_tensor(ge[:], sio[:].unsqueeze(2).to_broadcast([128, NS, NE]), cum[:].unsqueeze(1).to_broadcast([128, NS, NE]), ALU.is_ge), [bq], [bq])
            self.V(lambda h: h.tensor_reduce(es[:], ge[:], AX.X, ALU.add), [bq], [bq])
            self.V(lambda h: h.tensor_scalar(es[:], es[:], float(NE - 1), None, ALU.min), [bq], [bq])
            self.V(lambda h: h.tensor_scalar(self.es2[:, 0, 0:NS], es[:], float(D), None, ALU.mult), [bq], [self.bes])
            self.V(lambda h: h.tensor_scalar(self.es2[:, 1, 0:NS], es[:], float(DFF), None, ALU.mult), [bq], [self.bes])
            zg = R("zg", NS * c.SLOT // 128); bgs = Buf()
            self.V(lambda h: h.memset(zg[:], 0.0), [], [bq])
            self.st(self.gsD[0:NS * c.SLOT, :].rearrange("(p r) o -> p (r o)", p=128), zg[:], [bq])
            self.S.barrier()
            sl = [R("sl0"), R("sl1")]; gl = [R("gl0"), R("gl1")]; rl = [R("rl0"), R("rl1")]; bl = [Buf(), Buf()]
            pos, pm, t1, eq = R("pos"), R("pm"), R("t1"), R("eq")
            pp = R("pp", 2); gg = [R("gg0", 2), R("gg1", 2)]; bgg = [Buf(), Buf()]; gsum = R("gsm", 1)
            ht = [self.sb(st, f"rht{i}", [128, D], BF16) for i in range(2)]; bht = [Buf(), Buf()]
            bw_ = Buf()
            for ti, (b, i) in enumerate(self.moe_tiles(last)):
                j = ti % 2
                r0 = b * TT + i * 128
                self.ld(sl[j][:], self.selD[r0:r0 + 128, :], [bl[j]])
                self.ld(gl[j][:], self.gates[b, i * 128:(i + 1) * 128, :], [bl[j]], q="act")
                self.ld(rl[j][:], self.rankD[r0:r0 + 128, :], [bl[j]])
                self.ld(ht[j][:], self.h2tok[r0:r0 + 128, :], [bht[j]], q="act")
                self.V(lambda h: h.tensor_add(pos[:], rl[j][:], base[:]), [bl[j], bq], [bw_])
                self.V(lambda h: h.tensor_scalar(t1[:], sl[j][:], -1e6, 1e6, ALU.mult, ALU.add), [bl[j]], [bw_])
                self.V(lambda h: h.tensor_mul(pm[:], pos[:], sl[j][:]), [bw_, bl[j]], [bw_])
                self.V(lambda h: h.tensor_add(t1[:], t1[:], pm[:]), [bw_], [bw_])
                self.V(lambda h: h.tensor_reduce(pp[:, 0:1], t1[:], AX.X, ALU.min), [bw_], [bw_])
                self.V(lambda h: h.tensor_reduce(pp[:, 1:2], pm[:], AX.X, ALU.max), [bw_], [bw_])
                self.V(lambda h: h.tensor_copy(self.pidx[:, ti, :], pp[:]), [bw_], [self.bpidx])
                self.V(lambda h: h.tensor_scalar(eq[:], t1[:], pp[:, 0:1], None, ALU.is_equal), [bw_], [bw_])
                self.V(lambda h: h.tensor_mul(eq[:], eq[:], gl[j][:]), [bw_, bl[j]], [bw_])
                self.V(lambda h: h.tensor_reduce(gg[j][:, 0:1], eq[:], AX.X, ALU.add), [bw_, bgg[j]], [bgg[j]])
                self.V(lambda h: h.tensor_reduce(gsum[:], gl[j][:], AX.X, ALU.add), [bl[j]], [bw_])
                self.V(lambda h: h.tensor_sub(gg[j][:, 1:2], gsum[:], gg[j][:, 0:1]), [bw_, bgg[j]], [bgg[j]])
                for k in range(2):
                    self.S.idma(reads=[bht[j], self.bpidx], writes=[], out=self.hs[:, :], out_offset=bass.IndirectOffsetOnAxis(ap=self.pidx[:, ti, k:k + 1], axis=0),
                                in_=ht[j][:, :], in_offset=None)
                    self.S.idma(reads=[bgg[j], self.bpidx], writes=[], out=self.gsD[:, :], out_offset=bass.IndirectOffsetOnAxis(ap=self.pidx[:, ti, k:k + 1], axis=0),
                                in_=gg[j][:, k:k + 1], in_offset=None)

    def phase_moe_sparse(self, l, last):
        c = self.c
        NS = self.n_slots(last)
        SLT = c.SLOT // 128
        wgf = self.wg.rearrange("l e k (h f) -> (l e k h) f", h=2); wuf = self.wu.rearrange("l e k (h f) -> (l e k h) f", h=2)
        wdf = self.wd.rearrange("l e f n -> (l e f) n")
        with ExitStack() as st:
            wg_ = [self.sb(st, f"swg{i}", [128, KC, 512], BF16) for i in range(2)]
            wu_ = [self.sb(st, f"swu{i}", [128, KC, 512], BF16) for i in range(2)]
            wd_ = [self.sb(st, f"swd{i}", [128, 4, D], BF16) for i in range(2)]
            bwg, bwu, bwd = [Buf(), Buf()], [Buf(), Buf()], [Buf(), Buf()]
            h2s = self.sb(st, "h2s", [128, KC, c.SLOT], BF16); bh2 = Buf()
            hr = [self.sb(st, f"hr{i}", [128, D], BF16) for i in range(2)]; bhr = [Buf(), Buf()]
            he = [self.sb(st, f"she{i}", [128, 4, c.SLOT], BF16) for i in range(2)]; bhe = [Buf(), Buf()]
            ys = self.sb(st, "ys", [128, SLT, D]); bys = Buf()
            sg = self.sb(st, "ssg", [128, c.SLOT]); bsg = Buf()
            gs = [self.sb(st, f"sgs{i}", [128, SLT]) for i in range(2)]; bgs = [Buf(), Buf()]
            wi = [self.sb(st, f"swi{i}", [128, 40], I32) for i in range(2)]; bwi = [Buf(), Buf()]
            wif = self.sb(st, "swif", [128, 16]); bwif = Buf()
            hh = 0
            for s_ in range(NS):
                js = s_ % 2
                self.V(lambda h: h.tensor_scalar(wif[:], self.kiot[:, 0:16], self.es2[:, 0, s_:s_ + 1], float(l * NE * D), ALU.add, ALU.add), [self.bes, self.bconst, bwif], [bwif])
                for hf in range(2):
                    self.V(lambda h: h.tensor_scalar(wi[js][:, hf * 16:(hf + 1) * 16], wif[:], 2.0, float(hf), ALU.mult, ALU.add), [bwif, bwi[js]], [bwi[js]])
                self.V(lambda h: h.tensor_scalar(wi[js][:, 32:40], self.kiot[:, 16:24], self.es2[:, 1, s_:s_ + 1], float(l * NE * DFF), ALU.add, ALU.add), [self.bes, self.bconst, bwi[js]], [bwi[js]])
                self.S.dma("act", gs[js][:], self.gsD[s_ * c.SLOT:(s_ + 1) * c.SLOT, :].rearrange("(t p) o -> p (t o)", p=128), writes=[bgs[js]], allow_slow_non_contiguous=True)
                for t in range(SLT):
                    jr = t % 2
                    self.ld(hr[jr][:], self.hs[s_ * c.SLOT + t * 128: s_ * c.SLOT + (t + 1) * 128, :], [bhr[jr]], q=("sp" if jr == 0 else "act"))
                    for g in range(2):
                        p, pb = self.ps[4 + g], self.pb[4 + g]
                        pv = p[:].bitcast(BF16)
                        for k8 in range(8):
                            kc = g * 8 + k8
                            self.PE(lambda h: h.transpose(pv[:, k8 * 128:(k8 + 1) * 128], hr[jr][:, kc * 128:(kc + 1) * 128], self.idb[:]), [bhr[jr], self.bconst], [pb], inc=(k8 == 7))
                        dst = h2s[:, g * 8:(g + 1) * 8, t * 128:(t + 1) * 128]
                        if g == 0:
                            self.A(lambda h: h.copy(dst, pv[:, 0:1024].rearrange("p (k t) -> p k t", t=128)), [pb], [bh2])
                        else:
                            self.V(lambda h: h.tensor_copy(dst, pv[:, 0:1024].rearrange("p (k t) -> p k t", t=128)), [pb], [bh2])
                for half in range(2):
                    jw = hh % 2; hh += 1
                    cs = slice(half * 512, (half + 1) * 512)
                    for kc in range(KC):
                        self.S.idma(reads=[bwi[js]], writes=[bwg[jw]], out=wg_[jw][:, kc, :], out_offset=None, in_=wgf[:, :],
                                    in_offset=bass.IndirectOffsetOnAxis(ap=wi[js][:, half * 16 + kc:half * 16 + kc + 1], axis=0))
                        self.S.idma(reads=[bwi[js]], writes=[bwu[jw]], out=wu_[jw][:, kc, :], out_offset=None, in_=wuf[:, :],
                                    in_offset=bass.IndirectOffsetOnAxis(ap=wi[js][:, half * 16 + kc:half * 16 + kc + 1], axis=0))
                    for fc in range(4):
                        self.S.idma(reads=[bwi[js]], writes=[bwd[jw]], out=wd_[jw][:, fc, :], out_offset=None, in_=wdf[:, :],
                                    in_offset=bass.IndirectOffsetOnAxis(ap=wi[js][:, 32 + half * 4 + fc:33 + half * 4 + fc], axis=0))
                    for fc in range(4):
                        for gi, (s0, n) in enumerate(tok_groups(c.SLOT)):
                            pG, pU = self.ps[gi], self.ps[2 + gi]; bpG, bpU = self.pb[gi], self.pb[2 + gi]
                            for kc in range(KC):
                                self.PE(lambda h: h.matmul(pG[:, 0:n], wg_[jw][:, kc, fc * 128:(fc + 1) * 128], h2s[:, kc, s0:s0 + n], start=(kc == 0), stop=(kc == KC - 1)), [bwg[jw], bh2], [bpG], inc=(kc == KC - 1))
                            for kc in range(KC):
                                self.PE(lambda h: h.matmul(pU[:, 0:n], wu_[jw][:, kc, fc * 128:(fc + 1) * 128], h2s[:, kc, s0:s0 + n], start=(kc == 0), stop=(kc == KC - 1)), [bwu[jw], bh2], [bpU], inc=(kc == KC - 1))
                            self.A(lambda h: h.activation(sg[:, s0:s0 + n], pG[:, 0:n], AF.Silu), [bpG, bsg], [bsg])
                            self.V(lambda h: h.tensor_mul(he[jw][:, fc, s0:s0 + n], sg[:, s0:s0 + n], pU[:, 0:n]), [bsg, bpU], [bhe[jw]])
                    for t in range(SLT):
                        for g in range(4):
                            ps, pb = self.ps[4 + g], self.pb[4 + g]
                            for fc in range(4):
                                self.PE(lambda h: h.matmul(ps[:, :], he[jw][:, fc, t * 128:(t + 1) * 128], wd_[jw][:, fc, g * 512:(g + 1) * 512], start=(fc == 0), stop=(fc == 3)), [bhe[jw], bwd[jw]], [pb], inc=(fc == 3))
                            ysl = ys[:, t, g * 512:(g + 1) * 512]
                            if half == 0:
                                self.V(lambda h: h.tensor_scalar(ysl, ps[:, :], gs[js][:, t:t + 1], None, ALU.mult), [pb, bgs[js], bys], [bys])
                            else:
                                self.V(lambda h: h.scalar_tensor_tensor(ysl, ps[:, :], gs[js][:, t:t + 1], ysl, ALU.mult, ALU.add), [pb, bgs[js], bys], [bys])
                self.st(self.ysD[s_ * c.SLOT:(s_ + 1) * c.SLOT, :].rearrange("(t p) n -> p t n", p=128), ys[:], [bys])

    def phase_unsort(self, l, last):
        c = self.c
        TT = c.TT
        with ExitStack() as st:
            gB = {}
            for src in range(c.NB):
                gB[src] = self.bcast_row(st, 1, src, f"ugm{src}")
            if not last:
                gB[2] = self.bcast_row(st, 1, 2, "ugm2")
            ya = [self.sb(st, f"ya{i}", [128, D]) for i in range(2)]; yb = [self.sb(st, f"yb{i}", [128, D]) for i in range(2)]
            xt = [self.sb(st, f"ux{i}", [128, D]) for i in range(2)]
            bya, byb, bx = [Buf(), Buf()], [Buf(), Buf()], [Buf(), Buf()]
            for ti, (b, i) in enumerate(self.moe_tiles(last)):
                j = ti % 2
                src = 2 if i < c.NTC else b
                self.S.idma(reads=[self.bpidx], writes=[bya[j]], out=ya[j][:, :], out_offset=None, in_=self.ysD[:, :],
                            in_offset=bass.IndirectOffsetOnAxis(ap=self.pidx[:, ti, 0:1], axis=0))
                self.S.idma(reads=[self.bpidx], writes=[byb[j]], out=yb[j][:, :], out_offset=None, in_=self.ysD[:, :],
                            in_offset=bass.IndirectOffsetOnAxis(ap=self.pidx[:, ti, 1:2], axis=0))
                self.ld(xt[j][:], self.xres[b, i * 128:(i + 1) * 128, :], [bx[j]])
                self.V(lambda h: h.tensor_add(ya[j][:], ya[j][:], yb[j][:]), [bya[j], byb[j]], [bya[j]])
                self.V(lambda h: h.tensor_mul(ya[j][:], ya[j][:], gB[src][0][:]), [bya[j], gB[src][1]], [bya[j]])
                self.V(lambda h: h.tensor_add(xt[j][:], xt[j][:], ya[j][:]), [bya[j], bx[j]], [bx[j]])
                self.st(self.xres[b, i * 128:(i + 1) * 128, :], xt[j][:], [bx[j]], q="act")

    def phase_final(self):
        c = self.c
        with ExitStack() as st:
            gB = self.sb(st, "gfinB", [128, D]); bg = Buf()
            self.ld(gB[:], self.gfin[0:1, :].partition_broadcast(128)[:, 0, :], [bg])
            xt = [self.sb(st, f"xf{i}", [128, D]) for i in range(2)]; bx = [Buf(), Buf()]
            sq = self.sb(st, "fsq", [128, D], BF16); ss = self.sb(st, "fss", [128, 4]); bs = Buf()
            for b in range(c.NB):
                for i in range(c.NTC, c.NT):
                    j = i % 2
                    self.ld(xt[j][:], self.xres[b, i * 128:(i + 1) * 128, :], [bx[j]], q=("sp" if j == 0 else "act"))
                    self.A(lambda h: h.activation(sq[:], xt[j][:], AF.Square, accum_out=ss[:, 0:1]), [bx[j]], [bs])
                    self.A(lambda h: h.activation(ss[:, 1:2], ss[:, 0:1], AF.Sqrt, scale=1.0 / D, bias=self.epsc[:, 0:1]), [bs, self.bconst], [bs])
                    self.V(lambda h: h.reciprocal(ss[:, 2:3], ss[:, 1:2]), [bs], [bs])
                    self.V(lambda h: h.scalar_tensor_tensor(xt[j][:], xt[j][:], ss[:, 2:3], gB[:], ALU.mult, ALU.mult), [bx[j], bs, bg], [bx[j]])
                    self.st(self.out[b, (i - c.NTC) * 128:(i - c.NTC + 1) * 128, :], xt[j][:], [bx[j]], q=("sp" if j == 0 else "act"))


def host_shared(inp, cfg):
    L = cfg.L
    f = lambda a: np.ascontiguousarray(np.asarray(a, dtype=np.float32))
    pk = lambda v: f(np.asarray(v).reshape(v.shape[:-1] + (v.shape[-1] // 128, 128)).swapaxes(-1, -2))
    sh = {}
    sh["gmix"] = pk(inp["norm_mix_g"]); sh["gffn"] = pk(inp["norm_ffn_g"])
    sh["gfin"] = f(inp["final_norm_g"]).reshape(1, D)
    sh["w_mod"] = f(inp["w_mod"]); sh["b_mod"] = f(inp["b_mod"])
    w_in = np.asarray(inp["w_in"], np.float32).reshape(L, D, 12, W)
    def swap(p):
        x = w_in[:, :, p].reshape(L, D, 4, 2, 64)
        return x[:, :, :, ::-1, :].reshape(L, D, W)
    sh["w_in"] = f(np.concatenate([w_in.reshape(L, D, 12 * W), swap(8), swap(9)], axis=-1))
    sh["w_out"] = f(inp["w_out"])
    bre, bim = np.asarray(inp["s5_b_re"], np.float32), np.asarray(inp["s5_b_im"], np.float32)
    cre, cim = np.asarray(inp["s5_c_re"], np.float32), np.asarray(inp["s5_c_im"], np.float32)
    s5B = np.zeros((L, 2, 16, 128, 2, 128), np.float32)
    s5C = np.zeros((L, 2, 16, 128, 2, 128), np.float32)
    for sc in range(16):
        for g2 in range(2):
            g = 2 * sc + g2
            r0 = 16 * (g % 8)
            s5B[:, :, sc, r0:r0 + 16, 0, g2 * 64:(g2 + 1) * 64] = bre[:, :, g]
            s5B[:, :, sc, r0:r0 + 16, 1, g2 * 64:(g2 + 1) * 64] = bim[:, :, g]
            s5C[:, :, sc, g2 * 64:(g2 + 1) * 64, 0, r0:r0 + 16] = cre[:, :, g]
            s5C[:, :, sc, g2 * 64:(g2 + 1) * 64, 1, r0:r0 + 16] = cim[:, :, g]
    sh["s5B"], sh["s5C"] = s5B, s5C
    lam = np.zeros((L, 128, 3, 2, 16), np.float32)
    lre, lim, ldt = (np.asarray(inp[k], np.float32) for k in ("s5_lam_re", "s5_lam_im", "s5_log_dt"))
    for sc in range(16):
        for g2 in range(2):
            g = 2 * sc + g2
            lam[:, g2 * 64:(g2 + 1) * 64, 0, :, sc] = lre[:, :, g, :].transpose(0, 2, 1)
            lam[:, g2 * 64:(g2 + 1) * 64, 1, :, sc] = lim[:, :, g, :].transpose(0, 2, 1)
            lam[:, g2 * 64:(g2 + 1) * 64, 2, :, sc] = ldt[:, :, g][:, None, :]
    sh["s5lam"] = lam
    sh["s5d"] = pk(inp["s5_d"]); sh["gluw"] = f(inp["s5_glu_w"]); sh["glub"] = pk(inp["s5_glu_b"])
    TT, CL = cfg.TT, cfg.CTXL
    kf = np.arange(TT, dtype=np.float32)
    kb = np.concatenate([CL - 1 - np.arange(CL), CL + (TT - CL) - 1 - np.arange(TT - CL)]).astype(np.float32)
    sh["kidx"] = f(np.stack([np.broadcast_to(kf, (128, TT)), np.broadcast_to(kb, (128, TT))]))
    cw = np.asarray(inp["lru_conv_w"], np.float32)
    sh["convw"] = f(cw.reshape(L, 4, 4, 128).transpose(0, 3, 2, 1))
    lv = np.stack([np.asarray(inp["lru_conv_b"], np.float32)] +
                  [np.asarray(inp[k], np.float32)[:, d] for k in ("lru_ba", "lru_bx", "lru_lam") for d in range(2)], axis=-1)
    sh["lruv"] = f(lv.reshape(L, 4, 128, 7).transpose(0, 2, 1, 3))
    lw = np.zeros((L, 2, 2, 4, 128, 128), np.float32)
    for a, k in enumerate(("lru_wa", "lru_wx")):
        wsrc = np.asarray(inp[k], np.float32)
        for hd in range(8):
            cc, h2 = hd // 2, hd % 2
            lw[:, a, :, cc, h2 * 64:(h2 + 1) * 64, h2 * 64:(h2 + 1) * 64] = wsrc[:, :, hd]
    sh["lruw"] = lw
    hl = np.asarray(inp["hgrn_lb_logits"], np.float32)
    sh["hglb"] = f(hl.reshape(2, L, 4, 128).transpose(0, 1, 3, 2))
    n = cfg.SEQL
    rows = n // 64
    row = np.repeat(np.arange(rows, dtype=np.float32), 64); col = np.tile(np.arange(64, dtype=np.float32), rows)
    inv = (np.float32(10000.0) ** (-np.arange(32, dtype=np.float32) / np.float32(32))).astype(np.float32)
    ang = np.concatenate([row[:, None] * inv, col[:, None] * inv], axis=-1).astype(np.float32)
    sh["rang"] = f(np.concatenate([ang.T, ang.T], axis=0))
    sh["rw"] = f(np.asarray(inp["router_w"], np.float32).reshape(KC, 128, NE).transpose(1, 0, 2))
    sh["rbias"] = f(inp["router_bias"]).reshape(1, NE)
    sh["wg"], sh["wu"], sh["wd"] = f(inp["moe_w_gate"]), f(inp["moe_w_up"]), f(inp["moe_w_down"])
    sh["ident"] = np.eye(128, dtype=np.float32)
    s_, t_ = np.meshgrid(np.arange(64), np.arange(64), indexing="ij")
    m = np.stack([(s_ <= t_), (s_ >= t_)]).astype(np.float32)
    mm = np.concatenate([m, m], axis=1)
    sh["masks"] = f(np.broadcast_to(mm[:, :, None, :], (2, 128, 4, 64)))
    rst = np.ones((128, TT + 1), np.float32); rst[:, 0::64] = 0.0
    sh["rst"] = rst
    rst32 = np.ones((128, TT + 1), np.float32); rst32[:, 0::32] = 0.0
    sh["rst32"] = rst32
    s_, t_ = np.meshgrid(np.arange(32), np.arange(32), indexing="ij")
    m32 = np.stack([(s_ <= t_), (s_ >= t_)]).astype(np.float32)
    sh["masks32"] = f(np.broadcast_to(np.concatenate([m32] * 4, axis=1)[:, :, None, :], (2, 128, 4, 32)))
    sh["gffn_row"] = f(inp["norm_ffn_g"])
    tp_, t_ = np.meshgrid(np.arange(128), np.arange(128), indexing="ij")
    sh["ltri"] = f(np.stack([(tp_ < t_).astype(np.float32), np.ones((128, 128), np.float32)]))
    p_ = np.arange(128)[:, None]
    sh["kio"] = f(np.concatenate([np.arange(16)[None, :] * 128 + p_, np.arange(8)[None, :] * 128 + p_], axis=1))
    nsmax = (2 * cfg.NB * cfg.TT + cfg.SLOT - 1) // cfg.SLOT + NE
    sh["siota"] = f(np.broadcast_to(np.arange(nsmax, dtype=np.float32), (128, nsmax)))
    sel = np.zeros((3, 3, 128), np.float32)
    for s in range(3):
        sel[s, s, :] = 1.0
    sh["sel3"] = sel
    return sh


def host_core(inp, cfg, b0):
    NB = cfg.NB
    x = np.asarray(inp["x"], np.float32)[b0:b0 + NB]
    ctx = np.asarray(inp["ctx"], np.float32)[b0:b0 + NB]
    cvec = np.concatenate([np.asarray(inp["c"], np.float32)[b0:b0 + NB], np.asarray(inp["c_ctx"], np.float32)[None]], axis=0)
    if NB == 1:
        cvec = np.concatenate([cvec[0:1], cvec[0:1], cvec[1:2]], axis=0)
    return {"xin": np.ascontiguousarray(np.concatenate([ctx, x], axis=1)),
            "cT": np.ascontiguousarray(cvec.reshape(3, KC, 128).transpose(2, 1, 0))}


_CACHE = {}


def kernel(**inputs):
    cfg = Cfg()
    n_cores = 8
    if "nc" not in _CACHE:
        _CACHE["nc"] = Prog(cfg).build()
    nc = _CACHE["nc"]
    sh = host_shared(inputs, cfg)
    in_maps = []
    for core in range(n_cores):
        m = dict(sh)
        m.update(host_core(inputs, cfg, core * cfg.NB))
        in_maps.append(m)
    res = run_bass_kernel_spmd(nc, in_maps, core_ids=list(range(n_cores)))
    return np.concatenate([r["out"] for r in res.results], axis=0).astype(np.float32)
```

```python
import math
from contextlib import ExitStack
import numpy as np
import concourse.bass as bass
import concourse.mybir as mybir
from concourse.bass_utils import run_bass_kernel_spmd

F32 = mybir.dt.float32
BF16 = mybir.dt.bfloat16
I32 = mybir.dt.int32
ALU = mybir.AluOpType
AF = mybir.ActivationFunctionType
AX = mybir.AxisListType

D = 2048
KC = 16
W = 512
NPARTS = 14
FM_PARTS = [0, 1, 2, 3, 4, 5, 8, 9, 12, 13]
TM_PARTS = [6, 7, 10, 11]
NE = 16
DFF = 1024
EPS = 1e-6
TWO_PI = 2.0 * math.pi


class Buf:
    __slots__ = ("w", "r")

    def __init__(self):
        self.w = None
        self.r = {}


class Eng:
    def __init__(self, name, h, sem):
        self.name, self.h, self.sem = name, h, sem
        self.n = 0
        self.seen = {}
        self.dsems, self.dcnt, self.dnext = [], [], 0


class Sched:
    def __init__(self, nc, stack, n_dma_sems=16):
        self.nc = nc
        self.E = {}
        for name, h in (("pe", nc.tensor), ("dve", nc.vector), ("act", nc.scalar),
                        ("pool", nc.gpsimd), ("sp", nc.sync)):
            self.E[name] = Eng(name, h, stack.enter_context(nc.semaphore("s_" + name)))
        for name in ("sp", "act", "pool"):
            e = self.E[name]
            for i in range(n_dma_sems):
                e.dsems.append(stack.enter_context(nc.semaphore(f"d_{name}{i}")))
                e.dcnt.append(0)
        self.ninstr = 0

    def _wait(self, e, sem, val):
        if sem is e.sem and e.name == "pe":
            return
        if e.seen.get(sem, 0) < val:
            e.h.wait_ge(sem, val)
            e.seen[sem] = val

    def _deps(self, e, reads, writes):
        for b in reads:
            if b.w is not None:
                self._wait(e, *b.w)
        for b in writes:
            if b.w is not None:
                self._wait(e, *b.w)
            for s, v in b.r.items():
                self._wait(e, s, v)

    @staticmethod
    def _mark(tok, reads, writes):
        s, v = tok
        for b in reads:
            if b.r.get(s, 0) < v:
                b.r[s] = v
        for b in writes:
            b.w = tok
            b.r = {}

    def op(self, eng, fn, reads=(), writes=(), inc=True):
        e = self.E[eng]
        self._deps(e, reads, writes)
        ins = fn(e.h)
        if inc:
            e.n += 1
            ins.then_inc(e.sem, 1)
            tok = (e.sem, e.n)
            e.pend = False
        else:
            tok = (e.sem, e.n + 1)
            e.pend = True
        self._mark(tok, reads, writes)
        self.ninstr += 1
        return ins

    def dma(self, eng, out, in_, reads=(), writes=(), **kw):
        e = self.E[eng]
        self._deps(e, reads, writes)
        i = e.dnext
        e.dnext = (i + 1) % len(e.dsems)
        sem = e.dsems[i]
        if e.dcnt[i]:
            self._wait(e, sem, e.dcnt[i])
        ins = e.h.dma_start(out=out, in_=in_, **kw)
        e.dcnt[i] += 16
        ins.then_inc(sem, 16)
        self._mark((sem, e.dcnt[i]), reads, writes)
        self.ninstr += 1
        return ins

    def idma(self, reads=(), writes=(), **kw):
        e = self.E["pool"]
        self._deps(e, reads, writes)
        i = e.dnext
        e.dnext = (i + 1) % len(e.dsems)
        sem = e.dsems[i]
        if e.dcnt[i]:
            self._wait(e, sem, e.dcnt[i])
        ins = e.h.indirect_dma_start(**kw)
        e.dcnt[i] += 16
        ins.then_inc(sem, 16)
        self._mark((sem, e.dcnt[i]), reads, writes)
        self.ninstr += 1
        return ins

    def barrier(self):
        assert not any(getattr(e, "pend", False) for e in self.E.values())
        for e in self.E.values():
            for f in self.E.values():
                if f is not e and f.n:
                    self._wait(e, f.sem, f.n)
            for q in ("sp", "act", "pool"):
                qe = self.E[q]
                for sem, cnt in zip(qe.dsems, qe.dcnt):
                    if cnt:
                        self._wait(e, sem, cnt)


class Cfg:
    def __init__(self, NB=2, CTXL=256, SEQL=2048, L=2, dbg=False):
        self.NB, self.CTXL, self.SEQL, self.L, self.dbg = NB, CTXL, SEQL, L, dbg
        self.TT = CTXL + SEQL
        self.NT = self.TT // 128
        self.NTC = CTXL // 128
        self.NCH = self.TT // 64
        self.NCHC = CTXL // 64
        self.sparse = True
        self.SLOT = 640


def tok_groups(T, g=512):
    out, t = [], 0
    while t < T:
        n = min(g, T - t)
        out.append((t, n))
        t += n
    return out


class Prog:
    def __init__(self, cfg):
        self.c = cfg
        self.nc = bass.Bass("TRN2", target_bir_lowering=False)
        self.inp = {}

    def din(self, name, shape, dt=F32):
        t = self.nc.dram_tensor(name, list(shape), dt, kind="ExternalInput").ap()
        self.inp[name] = t
        return t

    def dscr(self, name, shape, dt=F32):
        kind = "ExternalOutput" if self.c.dbg else "Internal"
        return self.nc.dram_tensor(name, list(shape), dt, kind=kind).ap()

    def sb(self, st, name, shape, dt=F32):
        self._uid = getattr(self, "_uid", 0) + 1
        return st.enter_context(self.nc.sbuf_tensor(f"{name}_{self._uid}", list(shape), dt))

    def V(self, fn, R=(), Wr=()):
        return self.S.op("dve", fn, R, Wr)

    def A(self, fn, R=(), Wr=()):
        return self.S.op("act", fn, R, Wr)

    def G(self, fn, R=(), Wr=()):
        return self.S.op("pool", fn, R, Wr)

    def PE(self, fn, R=(), Wr=(), inc=True):
        return self.S.op("pe", fn, R, Wr, inc=inc)

    def ld(self, out, in_, Wr, q="sp", R=()):
        return self.S.dma(q, out, in_, reads=R, writes=Wr)

    def ldc(self, out, in_, Wr, R=()):
        return self.S.dma("pool", out, in_, reads=R, writes=Wr)

    def st(self, out, in_, R, q="sp"):
        return self.S.dma(q, out, in_, reads=R, writes=())

    def build(self):
        c, nc = self.c, self.nc
        NB, TT, NT, L = c.NB, c.TT, c.NT, c.L
        i_ = self.din
        self.xin = i_("xin", [NB, TT, D])
        self.cT = i_("cT", [128, KC, 3])
        self.gmix = i_("gmix", [L, 128, KC])
        self.gffn = i_("gffn", [L, 128, KC])
        self.gfin = i_("gfin", [1, D])
        self.w_mod = i_("w_mod", [L, D, 6 * D])
        self.b_mod = i_("b_mod", [L, 6 * D])
        self.w_in = i_("w_in", [L, D, NPARTS * W])
        self.w_out = i_("w_out", [L, D, D])
        self.s5B = i_("s5B", [L, 2, 16, 128, 2, 128])
        self.s5C = i_("s5C", [L, 2, 16, 128, 2, 128])
        self.s5lam = i_("s5lam", [L, 128, 3, 2, 16])
        self.s5d = i_("s5d", [L, 128, 4])
        self.gluw = i_("gluw", [L, W, W])
        self.glub = i_("glub", [L, 128, 4])
        self.kidx = i_("kidx", [2, 128, TT])
        self.convw = i_("convw", [L, 128, 4, 4])
        self.lruv = i_("lruv", [L, 128, 4, 7])
        self.lruw = i_("lruw", [L, 2, 2, 4, 128, 128])
        self.hglb = i_("hglb", [2, L, 128, 4])
        self.rang = i_("rang", [128, c.SEQL])
        self.rw = i_("rw", [128, KC, NE])
        self.rbias = i_("rbias", [1, NE])
        self.wg = i_("wg", [L * NE + 1, D, DFF])
        self.wu = i_("wu", [L * NE + 1, D, DFF])
        self.wd = i_("wd", [L * NE + 1, DFF, D])
        self.ident = i_("ident", [128, 128])
        self.masks = i_("masks", [2, 128, 4, 64])
        self.rst = i_("rst", [128, TT + 1])
        self.rst32 = i_("rst32", [128, TT + 1])
        self.masks32 = i_("masks32", [2, 128, 4, 32])
        self.sel3 = i_("sel3", [3, 3, 128])
        self.gffn_row = i_("gffn_row", [L, D])
        self.ltri = i_("ltri", [2, 128, 128])
        self.kio = i_("kio", [128, 24])
        NSMAX = (2 * NB * TT + c.SLOT - 1) // c.SLOT + NE
        self.NSMAX = NSMAX
        self.siota = i_("siota", [128, NSMAX])
        self.out = nc.dram_tensor("out", [NB, c.SEQL, D], F32, kind="ExternalOutput").ap()

        self.xres = self.dscr("xres", [NB, TT, D])
        self.Pfm = self.dscr("Pfm", [NB, len(FM_PARTS) * W, TT])
        self.Ptm = self.dscr("Ptm", [NB, TT, len(TM_PARTS) * W])
        self.mixT = self.dscr("mixT", [NB, D, TT], BF16)
        self.h2T = self.dscr("h2T", [NB, D, TT], BF16)
        self.gates = self.dscr("gates", [NB, TT, NE])
        self.ygD = self.dscr("ygD", [NB, W, TT])
        self.modD = self.dscr("modD", [3, 4, D])
        RM = self.NSMAX * c.SLOT
        self.h2tok = self.dscr("h2tok", [NB * TT, D], BF16)
        self.selD = self.dscr("selD", [NB * TT, NE])
        self.rankD = self.dscr("rankD", [NB * TT, NE])
        self.hs = self.dscr("hs", [RM, D], BF16)
        self.gsD = self.dscr("gsD", [RM, 1])
        self.ysD = self.dscr("ysD", [RM, D])

        with ExitStack() as top:
            self.S = Sched(nc, top)
            S = self.S
            self.ps = [top.enter_context(nc.psum_tensor(f"ps{i}", [128, 512], F32)) for i in range(8)]
            self.pb = [Buf() for _ in range(8)]
            self.idf = self.sb(top, "idf", [128, 128]); self.idb = self.sb(top, "idb", [128, 128], BF16)
            self.bconst = Buf()
            self.ld(self.idf[:], self.ident[:, :], [self.bconst])
            self.ldc(self.idb[:], self.ident[:, :], [self.bconst])
            self.cmask = self.sb(top, "cmask", [128, 2, 4, 64])
            self.ld(self.cmask[:], self.masks.rearrange("a p h j -> p a h j"), [self.bconst])
            self.cmask32 = self.sb(top, "cmask32", [128, 2, 4, 32])
            self.ld(self.cmask32[:], self.masks32.rearrange("a p h j -> p a h j"), [self.bconst])
            self.epsc = self.sb(top, "epsc", [128, 1])
            self.V(lambda h: h.memset(self.epsc[:], EPS), [], [self.bconst])
            self.hpi = self.sb(top, "hpi", [128, 1])
            self.V(lambda h: h.memset(self.hpi[:], math.pi / 2), [], [self.bconst])
            self.sel3t = self.sb(top, "sel3t", [3, 3, 128])
            self.ld(self.sel3t[:], self.sel3[:, :, :], [self.bconst])
            self.modP = self.sb(top, "modP", [128, 6, KC, 3])
            self.AB = self.sb(top, "AB", [128, 4, KC, 3])
            self.bmod = Buf()
            self.run = self.sb(top, "run", [128, NE]); self.brun = Buf()
            self.pidx = self.sb(top, "pidx", [128, NB * NT, 2], I32); self.bpidx = Buf()
            self.ltt = self.sb(top, "ltt", [128, 2, 128])
            self.ld(self.ltt[:], self.ltri.rearrange("a p j -> p a j"), [self.bconst])
            self.es2 = self.sb(top, "es2", [128, 2, self.NSMAX]); self.bes = Buf()
            self.kiot = self.sb(top, "kiot", [128, 24])
            self.ld(self.kiot[:], self.kio[:, :], [self.bconst])
            if c.sparse:
                with ExitStack() as zst:
                    z = self.sb(zst, "zz", [128, D], BF16); bz = Buf()
                    self.V(lambda h: h.memset(z[:], 0.0), [], [bz])
                    for r0 in range(0, self.NSMAX * c.SLOT, 128):
                        self.st(self.hs[r0:r0 + 128, :], z[:], [bz], q=("sp" if (r0 // 128) % 2 == 0 else "act"))
                    S.barrier()
            S.barrier()
            for l in range(L):
                last = (l == L - 1)
                self.phase_mod(l)
                S.barrier()
                for b in range(NB):
                    xsrc = self.xin if l == 0 else self.xres
                    self.phase_proj(l, b, xsrc)
                    S.barrier()
                    self.phase_lru(l, b)
                    S.barrier()
                    self.phase_gla(l, b, 0)
                    S.barrier()
                    self.phase_gla(l, b, 1)
                    S.barrier()
                    self.phase_s5(l, b)
                    S.barrier()
                    self.phase_out(l, b, xsrc, last)
                    S.barrier()
                    if not c.sparse:
                        self.phase_moe(l, b, last)
                        S.barrier()
                if c.sparse:
                    self.phase_route(l, last)
                    S.barrier()
                    self.phase_moe_sparse(l, last)
                    S.barrier()
                    self.phase_unsort(l, last)
                    S.barrier()
            self.phase_final()
            S.barrier()
        return nc

    def phase_mod(self, l):
        c, S = self.c, self.S
        with ExitStack() as st:
            cT = self.sb(st, "cT", [128, KC, 3]); cs = self.sb(st, "cs", [128, KC, 3], BF16)
            bc = Buf()
            self.ld(cT[:], self.cT[:, :, :], [bc])
            self.A(lambda h: h.activation(cs[:], cT[:], AF.Silu), [bc], [bc])
            modrow = self.sb(st, "modrow", [3, 6 * D]); bmr = Buf()
            wm = [self.sb(st, f"wm{i}", [128, KC, 512], BF16) for i in range(2)]; bwm = [Buf(), Buf()]
            bt = [self.sb(st, f"bt{i}", [3, 512]) for i in range(2)]; bbt = [Buf(), Buf()]
            wsrc = self.w_mod[l].rearrange("(kc kp) n -> kp kc n", kp=128)
            for cg in range(24):
                j = cg % 2
                self.ldc(wm[j][:], wsrc[:, :, cg * 512:(cg + 1) * 512], [bwm[j]])
                self.ld(bt[j][:], self.b_mod[l:l + 1, cg * 512:(cg + 1) * 512].partition_broadcast(3)[:, 0, :], [bbt[j]], q="act")
                p = self.ps[cg % 2]; pb = self.pb[cg % 2]
                for kc in range(KC):
                    self.PE(lambda h: h.matmul(p[0:3, :], cs[:, kc, :], wm[j][:, kc, :], start=(kc == 0), stop=(kc == KC - 1)),
                            [bc, bwm[j]], [pb], inc=(kc == KC - 1))
                self.V(lambda h: h.tensor_add(modrow[:, cg * 512:(cg + 1) * 512], p[0:3, :], bt[j][:]), [pb, bbt[j]], [bmr])
            pT = self.ps[2]; pTb = self.pb[2]
            for v in range(6):
                for kc in range(KC):
                    k = v * KC + kc
                    self.PE(lambda h: h.transpose(pT[:, k * 3:k * 3 + 3], modrow[:, v * D + kc * 128: v * D + (kc + 1) * 128], self.idf[0:3, 0:3]),
                            [bmr, self.bconst], [pTb], inc=(k == 6 * KC - 1))
            self.V(lambda h: h.tensor_copy(self.modP[:].rearrange("p a k s -> p (a k s)"), pT[:, 0:288]), [pTb], [self.bmod])
            self.st(self.modD[:, 0, :], modrow[:, 2 * D:3 * D], [bmr])
            self.st(self.modD[:, 1, :], modrow[:, 5 * D:6 * D], [bmr])
            self.st(self.modD[:, 3, :], modrow[:, 3 * D:4 * D], [bmr])
            grow = self.sb(st, "grow", [3, D]); bgr = Buf()
            self.ld(grow[:], self.gffn_row[l:l + 1, :].partition_broadcast(3)[:, 0, :], [bgr])
            self.V(lambda h: h.scalar_tensor_tensor(grow[:], modrow[:, 4 * D:5 * D], 1.0, grow[:], ALU.add, ALU.mult), [bmr, bgr], [bgr])
            self.st(self.modD[:, 2, :], grow[:], [bgr])
            self.V(lambda h: h.memset(self.run[:], 0.0), [self.brun], [self.brun])
            g1 = self.sb(st, "g1", [128, KC]); g2 = self.sb(st, "g2", [128, KC]); bg = Buf()
            self.ld(g1[:], self.gmix[l], [bg]); self.ld(g2[:], self.gffn[l], [bg])
            for (gi, gt, vs, vsh) in ((0, g1, 1, 0), (2, g2, 4, 3)):
                self.V(lambda h: h.tensor_scalar(self.AB[:, gi], self.modP[:, vs], 1.0, None, ALU.add), [self.bmod], [self.bmod])
                self.V(lambda h: h.tensor_mul(self.AB[:, gi], self.AB[:, gi], gt[:].unsqueeze(2).to_broadcast([128, KC, 3])), [self.bmod, bg], [self.bmod])
                self.V(lambda h: h.tensor_copy(self.AB[:, gi + 1], self.modP[:, vsh]), [self.bmod], [self.bmod])

    def bcast_row(self, st, which, src, name):
        t = self.sb(st, name, [128, D]); b = Buf()
        self.ld(t[:], self.modD[src:src + 1, which, :].partition_broadcast(128)[:, 0, :], [b])
        return t, b

    def norm_T(self, st_tiles, xt, bx, which, src, dst_fn, bdst, fp32):
        sq, ss, xs = st_tiles["sq"], st_tiles["ss"], (st_tiles["xsf"] if fp32 else st_tiles["xsb"])
        bsq, bss, bxs = st_tiles["bsq"], st_tiles["bss"], st_tiles["bxs"]
        self.A(lambda h: h.activation(sq[:], xt[:], AF.Square, accum_out=ss[:, 0:1]), [bx], [bsq, bss])
        self.A(lambda h: h.activation(ss[:, 1:2], ss[:, 0:1], AF.Sqrt, scale=1.0 / D, bias=self.epsc[:, 0:1]), [bss, self.bconst], [bss])
        self.V(lambda h: h.reciprocal(ss[:, 2:3], ss[:, 1:2]), [bss], [bss])
        self.A(lambda h: h.activation(xs[:], xt[:], AF.Identity, scale=ss[:, 2:3]), [bx, bss], [bxs])
        a_i, b_i = (0, 1) if which == 0 else (2, 3)
        idm = self.idf if fp32 else self.idb
        ngrp = 4 if fp32 else 2
        per = KC // ngrp
        for g in range(ngrp):
            bank = 4 + g
            p = self.ps[bank]; pb = self.pb[bank]
            pv = p[:] if fp32 else p[:].bitcast(BF16)
            for j in range(per):
                kc = g * per + j
                self.PE(lambda h: h.transpose(pv[:, j * 128:(j + 1) * 128], xs[:, kc * 128:(kc + 1) * 128], idm[:]),
                        [bxs, self.bconst], [pb], inc=(j == per - 1))
            for j in range(per):
                kc = g * per + j
                sc, bi = self.AB[:, a_i, kc, src:src + 1], self.AB[:, b_i, kc, src:src + 1]
                if kc % 2 == 0:
                    self.V(lambda h: h.tensor_scalar(dst_fn(kc), pv[:, j * 128:(j + 1) * 128], sc, bi, ALU.mult, ALU.add),
                           [pb, self.bmod], [bdst])
                else:
                    self.A(lambda h: h.activation(dst_fn(kc), pv[:, j * 128:(j + 1) * 128], AF.Identity, bias=bi, scale=sc),
                           [pb, self.bmod], [bdst])

    def norm_tiles(self, st):
        d = {"sq": self.sb(st, "sq", [128, D], BF16), "ss": self.sb(st, "ss", [128, 4]),
             "xsf": None, "xsb": None, "bsq": Buf(), "bss": Buf(), "bxs": Buf()}
        return d

    def phase_proj(self, l, b, xsrc):
        c, S = self.c, self.S
        TT, NT = c.TT, c.NT
        with ExitStack() as st:
            hT = self.sb(st, "hT", [128, KC, TT], BF16); bh = Buf()
            nt = self.norm_tiles(st); nt["xsb"] = self.sb(st, "xsb", [128, D], BF16)
            xt = [self.sb(st, f"xt{i}", [128, D]) for i in range(2)]; bx = [Buf(), Buf()]
            for i in range(NT):
                j = i % 2
                self.ld(xt[j][:], xsrc[b, i * 128:(i + 1) * 128, :], [bx[j]], q=("sp" if j == 0 else "act"))
                src = 2 if i < c.NTC else b
                self.norm_T(nt, xt[j], bx[j], 0, src, lambda kc: hT[:, kc, i * 128:(i + 1) * 128], bh, fp32=False)
            wp = [self.sb(st, f"wp{i}", [128, KC, W], BF16) for i in range(2)]; bw = [Buf(), Buf()]
            stg = [self.sb(st, f"stg{i}", [128, TT]) for i in range(2)]; bs = [Buf(), Buf()]
            stg2 = [self.sb(st, f"stgb{i}", [128, W]) for i in range(2)]; bs2 = [Buf(), Buf()]
            wsrc = self.w_in[l].rearrange("(kc kp) n -> kp kc n", kp=128)
            tgs = tok_groups(TT)
            ev = 0
            for pi, p in enumerate(FM_PARTS + TM_PARTS):
                j = pi % 2
                self.ldc(wp[j][:], wsrc[:, :, p * W:(p + 1) * W], [bw[j]])
                if p in FM_PARTS:
                    fi = FM_PARTS.index(p)
                    for cb in range(4):
                        sj = cb % 2
                        for (t0, n) in tgs:
                            bank = ev % 4; ev += 1
                            ps, pb = self.ps[bank], self.pb[bank]
                            for kc in range(KC):
                                self.PE(lambda h: h.matmul(ps[:, 0:n], wp[j][:, kc, cb * 128:(cb + 1) * 128], hT[:, kc, t0:t0 + n],
                                                           start=(kc == 0), stop=(kc == KC - 1)), [bw[j], bh], [pb], inc=(kc == KC - 1))
                            if ev % 2:
                                self.A(lambda h: h.copy(stg[sj][:, t0:t0 + n], ps[:, 0:n]), [pb], [bs[sj]])
                            else:
                                self.V(lambda h: h.tensor_copy(stg[sj][:, t0:t0 + n], ps[:, 0:n]), [pb], [bs[sj]])
                        self.st(self.Pfm[b, fi * W + cb * 128: fi * W + (cb + 1) * 128, :], stg[sj][:], [bs[sj]], q=("sp" if sj == 0 else "act"))
                else:
                    ti = TM_PARTS.index(p)
                    for i in range(NT):
                        sj = i % 2
                        bank = ev % 4; ev += 1
                        ps, pb = self.ps[bank], self.pb[bank]
                        for kc in range(KC):
                            self.PE(lambda h: h.matmul(ps[:, :], hT[:, kc, i * 128:(i + 1) * 128], wp[j][:, kc, :],
                                                       start=(kc == 0), stop=(kc == KC - 1)), [bw[j], bh], [pb], inc=(kc == KC - 1))
                        if ev % 2:
                            self.A(lambda h: h.copy(stg2[sj][:], ps[:, :]), [pb], [bs2[sj]])
                        else:
                            self.V(lambda h: h.tensor_copy(stg2[sj][:], ps[:, :]), [pb], [bs2[sj]])
                        self.st(self.Ptm[b, i * 128:(i + 1) * 128, ti * W:(ti + 1) * W], stg2[sj][:], [bs2[sj]], q=("sp" if sj == 0 else "act"))

    def pfm(self, b, part, r0, n=128):
        fi = FM_PARTS.index(part)
        return self.Pfm[b, fi * W + r0: fi * W + r0 + n, :]

    def ptm(self, b, part):
        ti = TM_PARTS.index(part)
        return self.Ptm[b, :, ti * W:(ti + 1) * W].rearrange("(i p) w -> p i w", p=128)

    def scan_bidir(self, out_f, out_b, a_f, x_f, a_b, x_b, R, Wf, Wb, eng="dve"):
        c = self.c
        CL, TT = c.CTXL, c.TT
        op = self.V if eng == "dve" else self.G
        op(lambda h: h.tensor_tensor_scan(out_f[:, 0:TT], a_f[:, 0:TT], x_f[:, 0:TT], 0.0, ALU.mult, ALU.add), R, [Wf])
        op(lambda h: h.tensor_tensor_scan(out_b[:, 0:CL][:, ::-1], a_b[:, 0:CL][:, ::-1], x_b[:, 0:CL][:, ::-1], 0.0, ALU.mult, ALU.add), R, [Wb])
        op(lambda h: h.tensor_tensor_scan(out_b[:, CL:TT][:, ::-1], a_b[:, CL:TT][:, ::-1], x_b[:, CL:TT][:, ::-1], out_b[:, 0:1], ALU.mult, ALU.add),
           list(R) + [Wb], [Wb])

    def phase_lru(self, l, b):
        c = self.c
        TT, CL = c.TT, c.CTXL
        tgs = tok_groups(TT)
        with ExitStack() as st:
            cw = self.sb(st, "cw", [128, 4, 4]); lv = self.sb(st, "lv", [128, 4, 7]); bp = Buf()
            self.ld(cw[:], self.convw[l], [bp]); self.ld(lv[:], self.lruv[l], [bp])
            lw = self.sb(st, "lw", [128, 2, 2, 4, 128], BF16)
            self.ldc(lw[:], self.lruw[l].rearrange("a d c p j -> p a d c j"), [bp])
            c1 = self.sb(st, "c1", [128, 4, 2]); c2 = self.sb(st, "c2", [128, 4, 2])
            self.A(lambda h: h.activation(c1[:], lv[:, :, 5:7], AF.Exp, scale=-1.0), [bp], [bp])
            self.A(lambda h: h.activation(c1[:], c1[:], AF.Ln, bias=1.0), [bp], [bp])
            self.V(lambda h: h.tensor_scalar(c2[:], c1[:], -16.0, None, ALU.mult), [bp], [bp])
            self.V(lambda h: h.tensor_scalar(c1[:], c1[:], -8.0, None, ALU.mult), [bp], [bp])
            T = lambda n, dt=F32: self.sb(st, n, [128, TT], dt)
            xr, gt, xc, xcb = T("xr"), T("gt"), T("xc"), T("xcb", BF16)
            rr, ii, aa, bb = [T("rr0"), T("rr1")], [T("ii0"), T("ii1")], [T("aa0"), T("aa1")], [T("bb0"), T("bb1")]
            hf, hb, ob = T("hf"), T("hb"), T("ob", BF16)
            bxr, bgt, bxc, bxcb, bo = Buf(), Buf(), Buf(), Buf(), Buf()
            br, bi, ba, bbb = [Buf(), Buf()], [Buf(), Buf()], [Buf(), Buf()], [Buf(), Buf()]
            bhf, bhb = Buf(), Buf()
            for cc in range(4):
                self.ld(xr[:], self.pfm(b, 1, cc * 128), [bxr])
                self.ld(gt[:], self.pfm(b, 2, cc * 128), [bgt], q="act")
                self.V(lambda h: h.tensor_scalar(xc[:], xr[:], cw[:, cc, 2:3], lv[:, cc, 0:1], ALU.mult, ALU.add), [bxr, bp], [bxc])
                for (s0, s1) in ((0, CL), (CL, TT)):
                    for k, off in ((0, -2), (1, -1), (3, 1)):
                        o0, o1 = max(s0, s0 - off), min(s1, s1 - off)
                        self.V(lambda h: h.scalar_tensor_tensor(xc[:, o0:o1], xr[:, o0 + off:o1 + off], cw[:, cc, k:k + 1], xc[:, o0:o1], ALU.mult, ALU.add),
                               [bxr, bp, bxc], [bxc])
                self.A(lambda h: h.copy(xcb[:], xc[:]), [bxc], [bxcb])
                for d in range(2):
                    for (wi, dst, bd, bias_col) in ((0, rr[d], br[d], 1 + d), (1, ii[d], bi[d], 3 + d)):
                        for gi, (t0, n) in enumerate(tgs):
                            ps, pb = self.ps[gi % 4], self.pb[gi % 4]
                            self.PE(lambda h: h.matmul(ps[:, 0:n], lw[:, wi, d, cc, :], xcb[:, t0:t0 + n], start=True, stop=True), [bp, bxcb], [pb])
                            self.A(lambda h: h.activation(dst[:, t0:t0 + n], ps[:, 0:n], AF.Sigmoid, bias=lv[:, cc, bias_col:bias_col + 1]), [pb, bp], [bd])
                    self.A(lambda h: h.activation(aa[d][:], rr[d][:], AF.Exp, scale=c1[:, cc, d:d + 1]), [br[d], bp], [ba[d]])
                    self.A(lambda h: h.activation(bb[d][:], rr[d][:], AF.Exp, scale=c2[:, cc, d:d + 1]), [br[d], bp], [bbb[d]])
                    self.A(lambda h: h.activation(bb[d][:], bb[d][:], AF.Sqrt, scale=-1.0, bias=1.0), [bbb[d]], [bbb[d]])
                    self.V(lambda h: h.tensor_mul(ii[d][:], ii[d][:], xc[:]), [bi[d], bxc], [bi[d]])
                    self.V(lambda h: h.tensor_mul(bb[d][:], bb[d][:], ii[d][:]), [bbb[d], bi[d]], [bbb[d]])
                self.scan_bidir(hf, hb, aa[0], bb[0], aa[1], bb[1], [ba[0], ba[1], bbb[0], bbb[1]], bhf, bhb)
                self.V(lambda h: h.tensor_add(hf[:], hf[:], hb[:]), [bhf, bhb], [bhf])
                self.gelu(gt[:], gt[:], hb[:], [bgt], bgt, bhb)
                self.V(lambda h: h.tensor_mul(ob[:], hf[:], gt[:]), [bhf, bgt], [bo])
                self.st(self.mixT[b, W + cc * 128: W + (cc + 1) * 128, :], ob[:], [bo])

    def sincos(self, st, ang, bang, n, name, share=None):
        T = lambda nm, dt=F32: self.sb(st, f"{name}_{nm}", [128, n], dt)
        y, yi, sn, cs = T("y"), T("yi", I32), T("sn"), T("cs")
        b = Buf(); bs = Buf(); bcs = Buf()
        for (shift, dst, bd) in ((0.0, sn, bs), (0.25, cs, bcs)):
            self.V(lambda h: h.tensor_scalar(y[:], ang[:], 1.0 / TWO_PI, shift, ALU.mult, ALU.add), [bang, b], [b])
            self.wrap_frac(y, yi, dst, b, extra=[bd])
            self.A(lambda h: h.activation(dst[:], y[:], AF.Sin, scale=TWO_PI), [b], [bd, b])
        return sn, bs, cs, bcs

    def wrap_frac(self, y, yi, yf, b, extra=()):
        Wb = [b] + list(extra)
        self.V(lambda h: h.tensor_copy(yi[:], y[:]), [b], Wb)
        self.V(lambda h: h.tensor_copy(yf[:], yi[:]), [b], Wb)
        self.V(lambda h: h.tensor_sub(y[:], y[:], yf[:]), [b], Wb)
        self.V(lambda h: h.tensor_scalar(yf[:], y[:], 0.5, -1.0, ALU.is_gt, ALU.mult), [b], Wb)
        self.V(lambda h: h.tensor_add(y[:], y[:], yf[:]), [b], Wb)
        self.V(lambda h: h.tensor_scalar(yf[:], y[:], -0.5, None, ALU.is_lt), [b], Wb)
        self.V(lambda h: h.tensor_add(y[:], y[:], yf[:]), [b], Wb)

    def gelu(self, dst, src, tmp, R, Wd, Wt):
        self.V(lambda h: h.tensor_mul(tmp, src, src), R, [Wt])
        self.V(lambda h: h.tensor_scalar(tmp, tmp, 0.044715, 1.0, ALU.mult, ALU.add), [Wt], [Wt])
        self.V(lambda h: h.tensor_mul(tmp, tmp, src), list(R) + [Wt], [Wt])
        self.A(lambda h: h.activation(tmp, tmp, AF.Sigmoid, scale=1.5957691216057308), [Wt], [Wt])
        self.V(lambda h: h.tensor_mul(dst, src, tmp), list(R) + [Wt], [Wd])

    def phase_gla(self, l, b, mixer):
        c = self.c
        TT, NT, CL = c.TT, c.NT, c.CTXL
        C = 64
        NPT = 128 // C
        NCH, NCHC = TT // C, CL // C
        cmask = self.cmask32 if C == 32 else self.cmask
        rsrc = self.rst32 if C == 32 else self.rst
        SQ = 128 ** -0.5
        with ExitStack() as st:
            T = lambda s_, n, dt=F32: self.sb(s_, n, [128, TT], dt)
            vbf = self.sb(st, "vbf", [128, NT, W], BF16); bv = Buf()
            oacc = self.sb(st, "oacc", [128, NT, W]); bo = Buf()
            vst = [self.sb(st, f"vst{i}", [128, W]) for i in range(2)]; bvs = [Buf(), Buf()]
            vsrc = self.ptm(b, 6 if mixer == 0 else 10)
            for i in range(NT):
                j = i % 2
                self.ld(vst[j][:], vsrc[:, i, :], [bvs[j]], q=("sp" if j == 0 else "act"))
                self.A(lambda h: h.activation(vbf[:, i, :], vst[j][:], AF.Silu if mixer == 0 else AF.Identity), [bvs[j]], [bv])
            bprm = Buf()
            if mixer == 0:
                lbr = self.sb(st, "lbr", [128, 2, c.L, 4]); lbe = self.sb(st, "lbe", [128, 2, c.L, 4])
                lb = self.sb(st, "lb", [128, 2, 4]); lbs = self.sb(st, "lbs", [128, 2, 4]); oml = self.sb(st, "oml", [128, 2, 4])
                self.ld(lbr[:], self.hglb.rearrange("d l p h -> p d l h"), [bprm])
                self.A(lambda h: h.activation(lbe[:], lbr[:], AF.Exp), [bprm], [bprm])
                self.V(lambda h: h.tensor_copy(lbs[:], lbe[:, :, 0, :]), [bprm], [bprm])
                for ll in range(1, c.L):
                    self.V(lambda h: h.tensor_add(lbs[:], lbs[:], lbe[:, :, ll, :]), [bprm], [bprm])
                self.V(lambda h: h.memset(lb[:], 0.0), [], [bprm])
                for ll in range(1, l + 1):
                    self.V(lambda h: h.tensor_add(lb[:], lb[:], lbe[:, :, ll, :]), [bprm], [bprm])
                self.V(lambda h: h.reciprocal(lbs[:], lbs[:]), [bprm], [bprm])
                self.V(lambda h: h.tensor_mul(lb[:], lb[:], lbs[:]), [bprm], [bprm])
                self.V(lambda h: h.tensor_scalar(oml[:], lb[:], -1.0, 1.0, ALU.mult, ALU.add), [bprm], [bprm])
            for d in range(2):
                with ExitStack() as s2:
                    rst = self.sb(s2, "rst", [128, TT + 1]); brs = Buf()
                    self.ld(rst[:], rsrc[:, :], [brs])
                    if mixer == 1:
                        ang = self.sb(s2, "ang", [128, c.SEQL]); bang = Buf()
                        self.ld(ang[:], self.rang[:, :], [bang])
                        sn, bsn, cs, bcs = self.sincos(s2, ang, bang, c.SEQL, "rot", share=ang)
                        self.V(lambda h: h.tensor_scalar(sn[0:64, :], sn[0:64, :], -1.0, None, ALU.mult), [bsn], [bsn])
                    q_, k_, z_, cum, e1 = T(s2, "q_"), T(s2, "k_"), T(s2, "z_"), T(s2, "cum"), T(s2, "e1")
                    tmp = z_ if mixer == 1 else T(s2, "tmp")
                    bq, bk, bz, bcum, be1 = (Buf() for _ in range(5))
                    btmp = bz if mixer == 1 else Buf()
                    qt = [T(s2, f"qt{h}", BF16) for h in range(4)]; kt = [T(s2, f"kt{h}", BF16) for h in range(4)]
                    bqt = [Buf() for _ in range(4)]; bkt = [Buf() for _ in range(4)]
                    dec = self.sb(s2, "dec", [128, NCH, 4]); em = self.sb(s2, "em", [128, NCH, 4]); elm = self.sb(s2, "elm", [128, NCH, 4]); bdec = Buf()
                    S_ = self.sb(s2, "S_", [128, 4, 128]); Sb = self.sb(s2, "Sb", [128, 4, 128], BF16); Ut = self.sb(s2, "Ut", [128, 4, 128]); bS, bSb, bUt = Buf(), Buf(), Buf()
                    sT = [self.sb(s2, f"sT{i}", [128, 4, C], BF16) for i in range(2)]; bsT = [Buf(), Buf()]
                    khT = [self.sb(s2, f"khT{i}", [128, 4, 128], BF16) for i in range(2)]; bkhT = [Buf(), Buf()]
                    c3 = lambda t: t[:].rearrange("p (c j) -> p c j", j=C)
                    for hd in range(4):
                        r0 = hd * 128
                        if mixer == 0:
                            self.ld(q_[:], self.pfm(b, 3, r0), [bq])
                            self.ld(z_[:], self.pfm(b, 4 + d, r0), [bz], q="act")
                            self.A(lambda h: h.activation(k_[:], z_[:], AF.Sigmoid, scale=-1.0), [bz], [bk])
                            self.A(lambda h: h.activation(z_[:], z_[:], AF.Sigmoid), [bz, bk], [bz])
                            self.A(lambda h: h.activation(z_[:], z_[:], AF.Ln, scale=oml[:, d, hd:hd + 1], bias=lb[:, d, hd:hd + 1]), [bz, bprm], [bz])
                            self.V(lambda h: h.tensor_scalar(k_[:], k_[:], oml[:, d, hd:hd + 1], None, ALU.mult), [bk, bprm], [bk])
                            gsrc, bgs = z_, bz
                            qscale, kscale = SQ, 1.0
                        else:
                            for (x_, bx_, pa, pb_) in ((q_, bq, 8, 12), (k_, bk, 9, 13)):
                                self.ld(x_[:], self.pfm(b, pa, r0), [bx_])
                                self.ld(e1[:], self.pfm(b, pb_, r0), [be1], q="act")
                                self.V(lambda h: h.tensor_mul(x_[:, CL:TT], x_[:, CL:TT], cs[:]), [bx_, bcs], [bx_])
                                self.V(lambda h: h.tensor_mul(e1[:, CL:TT], e1[:, CL:TT], sn[:]), [be1, bsn], [be1])
                                self.V(lambda h: h.tensor_add(x_[:, CL:TT], x_[:, CL:TT], e1[:, CL:TT]), [bx_, be1], [bx_])
                            gdec = math.log1p(-2.0 ** (-((5.0 if d == 0 else 5.5) + hd)))
                            gsrc, bgs = None, None
                            qscale, kscale = 1.0, SQ
                        if mixer == 1:
                            self.V(lambda h: h.memset(e1[:], gdec), [be1], [be1])
                            gsrc, bgs = e1, be1
                        if d == 0:
                            self.V(lambda h: h.tensor_tensor_scan(cum[:], rst[:, 0:TT], gsrc[:], 0.0, ALU.mult, ALU.add), [brs, bgs], [bcum])
                            mcol, lcol = C // 2 - 1, C - 1
                        else:
                            self.V(lambda h: h.tensor_tensor_scan(cum[:, ::-1], rst[:, 1:TT + 1][:, ::-1], gsrc[:, ::-1], 0.0, ALU.mult, ALU.add), [brs, bgs], [bcum])
                            mcol, lcol = C // 2, 0
                        cum3 = c3(cum)
                        mB = cum3[:, :, mcol:mcol + 1].to_broadcast([128, NCH, C])
                        self.V(lambda h: h.tensor_sub(c3(tmp), cum3, mB), [bcum, btmp], [btmp])
                        self.A(lambda h: h.activation(e1[:], tmp[:], AF.Exp), [btmp, be1], [be1])
                        self.V(lambda h: h.scalar_tensor_tensor(qt[hd][:], q_[:], qscale, e1[:], ALU.mult, ALU.mult), [bq, be1], [bqt[hd]])
                        self.A(lambda h: h.activation(e1[:], tmp[:], AF.Exp, scale=-1.0), [btmp, be1], [be1])
                        self.V(lambda h: h.scalar_tensor_tensor(kt[hd][:], k_[:], kscale, e1[:], ALU.mult, ALU.mult), [bk, be1], [bkt[hd]])
                        self.A(lambda h: h.activation(dec[:, :, hd:hd + 1], cum3[:, :, lcol:lcol + 1], AF.Exp), [bcum], [bdec])
                        self.A(lambda h: h.activation(em[:, :, hd:hd + 1], cum3[:, :, mcol:mcol + 1], AF.Exp), [bcum], [bdec])
                        self.V(lambda h: h.tensor_sub(elm[:, :, hd:hd + 1], cum3[:, :, lcol:lcol + 1], cum3[:, :, mcol:mcol + 1]), [bcum], [bdec])
                        self.A(lambda h: h.activation(elm[:, :, hd:hd + 1], elm[:, :, hd:hd + 1], AF.Exp), [bdec], [bdec])
                    self.V(lambda h: h.memset(S_[:], 0.0), [bS], [bS])
                    self.V(lambda h: h.memset(Sb[:], 0.0), [bSb], [bSb])
                    order = list(range(NCH)) if d == 0 else (list(range(NCHC - 1, -1, -1)) + list(range(NCH - 1, NCHC - 1, -1)))
                    for ci, ch in enumerate(order):
                        i, hh = ch // NPT, ch % NPT
                        P0 = C * hh
                        cs_ = slice(ch * C, ch * C + C)
                        j = ci % 2
                        pS, pO, pK, pU = self.ps[0 + j], self.ps[2 + j], self.ps[4 + j], self.ps[6 + j]
                        bpS, bpO, bpK, bpU = self.pb[0 + j], self.pb[2 + j], self.pb[4 + j], self.pb[6 + j]
                        for hd in range(4):
                            self.PE(lambda h: h.matmul(pS[P0:P0 + C, hd * C:(hd + 1) * C], kt[hd][:, cs_], qt[hd][:, cs_], start=True, stop=True),
                                    [bkt[hd], bqt[hd]], [bpS], inc=(hd == 3))
                        self.G(lambda h: h.memset(sT[j][P0:P0 + C], 0.0), [bsT[j]], [bsT[j]])
                        self.V(lambda h: h.copy_predicated(sT[j][P0:P0 + C], cmask[P0:P0 + C, d].bitcast(mybir.dt.uint32), pS[P0:P0 + C, 0:4 * C].rearrange("p (h t) -> p h t", t=C)),
                               [bpS, self.bconst, bsT[j]], [bsT[j]])
                        self.V(lambda h: h.tensor_mul(Sb[:], S_[:], em[:, ch, :].unsqueeze(2).to_broadcast([128, 4, 128])), [bS, bdec, bSb], [bSb])
                        for hd in range(4):
                            osl = pO[P0:P0 + C, hd * 128:(hd + 1) * 128]
                            self.PE(lambda h: h.matmul(osl, sT[j][P0:P0 + C, hd, :], vbf[P0:P0 + C, i, hd * 128:(hd + 1) * 128], start=True, stop=False),
                                    [bsT[j], bv], [bpO], inc=False)
                            self.PE(lambda h: h.matmul(osl, qt[hd][:, cs_], Sb[:, hd, :], start=False, stop=True), [bqt[hd], bSb], [bpO], inc=(hd == 3))
                        if d == 0:
                            self.A(lambda h: h.copy(oacc[P0:P0 + C, i, :], pO[P0:P0 + C, :]), [bpO], [bo])
                        else:
                            self.V(lambda h: h.tensor_add(oacc[P0:P0 + C, i, :], oacc[P0:P0 + C, i, :], pO[P0:P0 + C, :]), [bpO, bo], [bo])
                        pKb = pK[:].bitcast(BF16)
                        for hd in range(4):
                            self.PE(lambda h: h.transpose(pKb[P0:P0 + C, hd * 128:(hd + 1) * 128], kt[hd][:, cs_], self.idb[:]), [bkt[hd], self.bconst], [bpK], inc=(hd == 3))
                        self.A(lambda h: h.copy(khT[j][P0:P0 + C].rearrange("p h d -> p (h d)"), pKb[P0:P0 + C, 0:512]), [bpK], [bkhT[j]])
                        for hd in range(4):
                            self.PE(lambda h: h.matmul(pU[:, hd * 128:(hd + 1) * 128], khT[j][P0:P0 + C, hd, :], vbf[P0:P0 + C, i, hd * 128:(hd + 1) * 128], start=True, stop=True),
                                    [bkhT[j], bv], [bpU], inc=(hd == 3))
                        self.V(lambda h: h.tensor_mul(Ut[:], pU[:, :].rearrange("p (h d) -> p h d", d=128), elm[:, ch, :].unsqueeze(2).to_broadcast([128, 4, 128])), [bpU, bdec, bUt], [bUt])
                        self.V(lambda h: h.tensor_mul(S_[:], S_[:], dec[:, ch, :].unsqueeze(2).to_broadcast([128, 4, 128])), [bS, bdec], [bS])
                        self.V(lambda h: h.tensor_add(S_[:], S_[:], Ut[:]), [bS, bUt], [bS])
                self.S.barrier()
            with ExitStack() as s3:
                graw = self.sb(s3, "graw", [128, NT, W]); bg = Buf()
                self.ld(graw[:], self.ptm(b, 7 if mixer == 0 else 11), [bg], q="act")
                self.A(lambda h: h.activation(graw[:], graw[:], AF.Silu), [bg], [bg])
                sq = self.sb(s3, "gsq", [128, NT, W]); ssq = self.sb(s3, "ssq", [128, NT * 4]); bsq = Buf()
                o4 = oacc[:].rearrange("p i (h d) -> p (i h) d", d=128)
                self.V(lambda h: h.tensor_mul(sq[:], oacc[:], oacc[:]), [bo], [bsq])
                self.V(lambda h: h.tensor_reduce(ssq[:], sq[:].rearrange("p i (h d) -> p (i h) d", d=128), AX.X, ALU.add), [bsq], [bsq])
                self.A(lambda h: h.activation(ssq[:], ssq[:], AF.Sqrt, scale=1.0 / 128, bias=self.epsc[:, 0:1]), [bsq, self.bconst], [bsq])
                self.V(lambda h: h.reciprocal(ssq[:], ssq[:]), [bsq], [bsq])
                self.V(lambda h: h.tensor_mul(o4, o4, ssq[:].unsqueeze(2).to_broadcast([128, NT * 4, 128])), [bo, bsq], [bo])
                self.V(lambda h: h.tensor_mul(vbf[:], oacc[:], graw[:]), [bo, bg, bv], [bv])
                mst = self.sb(s3, "mst", [128, 4, TT], BF16); bm = Buf()
                for i in range(NT):
                    p, pb = self.ps[i % 4], self.pb[i % 4]
                    pv = p[:].bitcast(BF16)
                    for hd in range(4):
                        self.PE(lambda h: h.transpose(pv[:, hd * 128:(hd + 1) * 128], vbf[:, i, hd * 128:(hd + 1) * 128], self.idb[:]), [bv, self.bconst], [pb], inc=(hd == 3))
                    self.A(lambda h: h.copy(mst[:, :, i * 128:(i + 1) * 128], pv[:, 0:512].rearrange("p (h t) -> p h t", t=128)), [pb], [bm])
                base = 2 * W + mixer * W
                self.st(self.mixT[b, base:base + W, :].rearrange("(h p) t -> p h t", p=128), mst[:], [bm])
                self.S.barrier()

    def phase_s5(self, l, b):
        c = self.c
        TT, CL = c.TT, c.CTXL
        tgs = tok_groups(TT)
        with ExitStack() as st:
            T = lambda n, dt=F32: self.sb(st, n, [128, TT], dt)
            lam = self.sb(st, "lam", [128, 3, 2, 16]); bp = Buf()
            self.ld(lam[:], self.s5lam[l], [bp])
            P = lambda n: self.sb(st, n, [128, 32])
            dt_, mag, th, thc, are, aim, den, wre, wim, t1, t2 = (P(f"p{i}") for i in range(11))
            lr = lam[:, 0].rearrange("p d s -> p (d s)"); li = lam[:, 1].rearrange("p d s -> p (d s)"); ldt = lam[:, 2].rearrange("p d s -> p (d s)")
            self.A(lambda h: h.activation(dt_[:], ldt, AF.Exp), [bp], [bp])
            self.V(lambda h: h.tensor_mul(t1[:], lr, dt_[:]), [bp], [bp])
            self.A(lambda h: h.activation(mag[:], t1[:], AF.Exp), [bp], [bp])
            self.V(lambda h: h.tensor_mul(th[:], li, dt_[:]), [bp], [bp])
            sn, bsn, cs, bcs = self.sincos(st, th, bp, 32, "ab")
            self.V(lambda h: h.tensor_mul(are[:], mag[:], cs[:]), [bp, bcs], [bp])
            self.V(lambda h: h.tensor_mul(aim[:], mag[:], sn[:]), [bp, bsn], [bp])
            self.V(lambda h: h.tensor_mul(den[:], lr, lr), [bp], [bp])
            self.V(lambda h: h.tensor_mul(t1[:], li, li), [bp], [bp])
            self.V(lambda h: h.tensor_add(den[:], den[:], t1[:]), [bp], [bp])
            self.V(lambda h: h.reciprocal(den[:], den[:]), [bp], [bp])
            self.V(lambda h: h.tensor_scalar(t2[:], are[:], -1.0, None, ALU.add), [bp], [bp])
            self.V(lambda h: h.tensor_mul(wre[:], t2[:], lr), [bp], [bp])
            self.V(lambda h: h.tensor_mul(t1[:], aim[:], li), [bp], [bp])
            self.V(lambda h: h.tensor_add(wre[:], wre[:], t1[:]), [bp], [bp])
            self.V(lambda h: h.tensor_mul(wre[:], wre[:], den[:]), [bp], [bp])
            self.V(lambda h: h.tensor_mul(wim[:], aim[:], lr), [bp], [bp])
            self.V(lambda h: h.tensor_mul(t1[:], t2[:], li), [bp], [bp])
            self.V(lambda h: h.tensor_sub(wim[:], wim[:], t1[:]), [bp], [bp])
            self.V(lambda h: h.tensor_mul(wim[:], wim[:], den[:]), [bp], [bp])
            nwim = P("nwim")
            self.V(lambda h: h.tensor_scalar(nwim[:], wim[:], -1.0, None, ALU.mult), [bp], [bp])
            thi = self.sb(st, "thi", [128, 32], I32); thf = P("thf")
            self.V(lambda h: h.tensor_scalar(thc[:], th[:], 1.0 / TWO_PI, None, ALU.mult), [bp], [bp])
            self.wrap_frac(thc, thi, thf, bp)
            dsk = self.sb(st, "dsk", [128, 4]); self.ld(dsk[:], self.s5d[l], [bp])
            with ExitStack() as s2:
                T = lambda n, dt=F32: self.sb(s2, n, [128, TT], dt)
                kid = [T("kid0"), T("kid1")]; bkid = Buf()
                self.ld(kid[0][:], self.kidx[0], [bkid]); self.ld(kid[1][:], self.kidx[1], [bkid], q="act")
                ub = self.sb(s2, "ub", [128, 4, TT], BF16); bu = Buf()
                self.ldc(ub[:], self.Pfm[b, 0:W, :].rearrange("(c p) t -> p c t", p=128), [bu])
                uf = T("uf"); buf_ = Buf()
                Bt = [self.sb(s2, f"Bt{i}", [128, 2, 128], BF16) for i in range(2)]; bB = [Buf(), Buf()]
                Cf = [self.sb(s2, f"Cf{i}", [128, 2, 128]) for i in range(2)]; bC = [Buf(), Buf()]
                Cw = [self.sb(s2, f"Cw{i}", [128, 2, 128], BF16) for i in range(2)]; bCw = [Buf(), Buf()]
                ct = self.sb(s2, "ct", [128, 128]); bct = Buf()
                y, yi = T("ty"), T("tyi", I32)
                sn_, cs_ = T("tsn"), T("tcs"); by, bsn_, bcs_ = Buf(), Buf(), Buf()
                bur, bui, gre, gim, t3 = T("bur"), T("bui"), T("gre"), T("gim"), T("t3")
                bbur, bbui, bgre, bgim, bt3 = (Buf() for _ in range(5))
                hre = [T(f"hre{i}", BF16) for i in range(2)]; him = [T(f"him{i}", BF16) for i in range(2)]
                bhre = [Buf(), Buf()]; bhim = [Buf(), Buf()]
                ytmp = T("ytmp"); bytmp = Buf()
                yo = T("yo"); byo = Buf()
                k = 0
                for cc in range(4):
                    self.ld(uf[:], self.Pfm[b, cc * 128:(cc + 1) * 128, :], [buf_])
                    pY = [self.ps[4 + g] for g in range(4)]; bpY = [self.pb[4 + g] for g in range(4)]
                    first = True
                    for d in range(2):
                        for s4 in range(4):
                            sc = cc * 4 + s4
                            col = d * 16 + sc
                            j = k % 2; k += 1
                            lastone = (d == 1 and s4 == 3)
                            self.ldc(Bt[j][:], self.s5B[l, d, sc], [bB[j]])
                            self.ld(Cf[j][:], self.s5C[l, d, sc], [bC[j]], q="act")
                            self.V(lambda h: h.tensor_scalar(ct[:], Cf[j][:, 1, :], nwim[:, col:col + 1], None, ALU.mult), [bC[j], bp, bct], [bct])
                            self.V(lambda h: h.scalar_tensor_tensor(Cw[j][:, 0, :], Cf[j][:, 0, :], wre[:, col:col + 1], ct[:], ALU.mult, ALU.add), [bC[j], bp, bct], [bCw[j]])
                            self.V(lambda h: h.tensor_scalar(ct[:], Cf[j][:, 1, :], wre[:, col:col + 1], -1.0, ALU.mult, ALU.mult), [bC[j], bp, bct], [bct])
                            self.V(lambda h: h.scalar_tensor_tensor(Cw[j][:, 1, :], Cf[j][:, 0, :], nwim[:, col:col + 1], ct[:], ALU.mult, ALU.add), [bC[j], bp, bct], [bCw[j]])
                            self.V(lambda h: h.tensor_scalar(yi[:], kid[d][:], thc[:, col:col + 1], None, ALU.mult), [bkid, bp, by], [by])
                            self.V(lambda h: h.tensor_copy(cs_[:], yi[:]), [by, bcs_], [bcs_])
                            self.V(lambda h: h.scalar_tensor_tensor(y[:], kid[d][:], thc[:, col:col + 1], cs_[:], ALU.mult, ALU.subtract), [bkid, bp, bcs_, by], [by])
                            self.A(lambda h: h.activation(sn_[:], y[:], AF.Sin, scale=TWO_PI), [by, bsn_], [bsn_])
                            self.A(lambda h: h.activation(cs_[:], y[:], AF.Abs), [by, bcs_], [bcs_])
                            self.A(lambda h: h.activation(cs_[:], cs_[:], AF.Sin, scale=-TWO_PI, bias=self.hpi[:, 0:1]), [bcs_, self.bconst], [bcs_])
                            for (ri, dst, bd) in ((0, bur, bbur), (1, bui, bbui)):
                                for gi, (t0, n) in enumerate(tgs):
                                    ps, pb = self.ps[gi % 4], self.pb[gi % 4]
                                    self.PE(lambda h: h.matmul(ps[:, 0:n], Bt[j][:, ri, :], ub[:, cc, t0:t0 + n], start=True, stop=True), [bB[j], bu], [pb])
                                    self.A(lambda h: h.copy(dst[:, t0:t0 + n], ps[:, 0:n]), [pb], [bd])
                            self.V(lambda h: h.tensor_mul(gre[:], bur[:], cs_[:]), [bbur, bcs_], [bgre])
                            self.V(lambda h: h.tensor_mul(t3[:], bui[:], sn_[:]), [bbui, bsn_], [bt3])
                            self.V(lambda h: h.tensor_add(gre[:], gre[:], t3[:]), [bgre, bt3], [bgre])
                            self.V(lambda h: h.tensor_mul(gim[:], bui[:], cs_[:]), [bbui, bcs_], [bgim])
                            self.V(lambda h: h.tensor_mul(t3[:], bur[:], sn_[:]), [bbur, bsn_], [bt3])
                            self.V(lambda h: h.tensor_sub(gim[:], gim[:], t3[:]), [bgim, bt3], [bgim])
                            for (src, dst, bs_, bd) in ((gre, bur, bgre, bbur), (gim, bui, bgim, bbui)):
                                if d == 0:
                                    self.V(lambda h: h.tensor_tensor_scan(dst[:], mag[:, col:col + 1].to_broadcast([128, TT]), src[:], 0.0, ALU.mult, ALU.add), [bs_, bp], [bd])
                                else:
                                    mC = mag[:, col:col + 1].to_broadcast([128, CL]); mL = mag[:, col:col + 1].to_broadcast([128, TT - CL])
                                    self.V(lambda h: h.tensor_tensor_scan(dst[:, 0:CL][:, ::-1], mC, src[:, 0:CL][:, ::-1], 0.0, ALU.mult, ALU.add), [bs_, bp], [bd])
                                    self.V(lambda h: h.tensor_tensor_scan(dst[:, CL:TT][:, ::-1], mL, src[:, CL:TT][:, ::-1], dst[:, 0:1], ALU.mult, ALU.add), [bs_, bp, bd], [bd])
                            self.V(lambda h: h.tensor_mul(gre[:], bur[:], cs_[:]), [bbur, bcs_], [bgre])
                            self.V(lambda h: h.tensor_mul(t3[:], bui[:], sn_[:]), [bbui, bsn_], [bt3])
                            self.V(lambda h: h.tensor_sub(hre[j][:], gre[:], t3[:]), [bgre, bt3], [bhre[j]])
                            self.V(lambda h: h.tensor_mul(gim[:], bur[:], sn_[:]), [bbur, bsn_], [bgim])
                            self.V(lambda h: h.tensor_mul(t3[:], bui[:], cs_[:]), [bbui, bcs_], [bt3])
                            self.V(lambda h: h.tensor_add(him[j][:], gim[:], t3[:]), [bgim, bt3], [bhim[j]])
                            for gi, (t0, n) in enumerate(tgs):
                                if gi < 4:
                                    self.PE(lambda h: h.matmul(pY[gi][:, 0:n], Cw[j][:, 0, :], hre[j][:, t0:t0 + n], start=first, stop=False), [bCw[j], bhre[j]], [bpY[gi]], inc=False)
                                    self.PE(lambda h: h.matmul(pY[gi][:, 0:n], Cw[j][:, 1, :], him[j][:, t0:t0 + n], start=False, stop=lastone), [bCw[j], bhim[j]], [bpY[gi]])
                                else:
                                    ps, pb = self.ps[gi % 4], self.pb[gi % 4]
                                    self.PE(lambda h: h.matmul(ps[:, 0:n], Cw[j][:, 0, :], hre[j][:, t0:t0 + n], start=True, stop=False), [bCw[j], bhre[j]], [pb], inc=False)
                                    self.PE(lambda h: h.matmul(ps[:, 0:n], Cw[j][:, 1, :], him[j][:, t0:t0 + n], start=False, stop=True), [bCw[j], bhim[j]], [pb])
                                    if first:
                                        self.V(lambda h: h.tensor_copy(ytmp[:, t0:t0 + n], ps[:, 0:n]), [pb], [bytmp])
                                    else:
                                        self.V(lambda h: h.tensor_add(ytmp[:, t0:t0 + n], ytmp[:, t0:t0 + n], ps[:, 0:n]), [pb, bytmp], [bytmp])
                            first = False
                    for gi, (t0, n) in enumerate(tgs):
                        if gi < 4:
                            self.V(lambda h: h.scalar_tensor_tensor(yo[:, t0:t0 + n], uf[:, t0:t0 + n], dsk[:, cc:cc + 1], pY[gi][:, 0:n], ALU.mult, ALU.add), [buf_, bp, bpY[gi]], [byo])
                        else:
                            self.V(lambda h: h.scalar_tensor_tensor(yo[:, t0:t0 + n], uf[:, t0:t0 + n], dsk[:, cc:cc + 1], ytmp[:, t0:t0 + n], ALU.mult, ALU.add), [buf_, bp, bytmp], [byo])
                    self.gelu(yo[:], yo[:], t3[:], [byo], byo, bt3)
                    self.st(self.ygD[b, cc * 128:(cc + 1) * 128, :], yo[:], [byo])
                self.S.barrier()
            with ExitStack() as s3:
                T = lambda n, dt=F32: self.sb(s3, n, [128, TT], dt)
                yg = self.sb(s3, "yg", [128, 4, TT]); ygb = self.sb(s3, "ygb", [128, 4, TT], BF16); byg = Buf()
                self.ld(yg[:], self.ygD[b].rearrange("(c p) t -> p c t", p=128), [byg])
                self.A(lambda h: h.copy(ygb[:], yg[:]), [byg], [byg])
                gw = self.sb(s3, "gw", [128, 4, W], BF16); gb = self.sb(s3, "gb", [128, 4]); bgw = Buf()
                self.ldc(gw[:], self.gluw[l].rearrange("(kc kp) n -> kp kc n", kp=128), [bgw])
                self.ld(gb[:], self.glub[l], [bgw])
                ao = [T("ao0", BF16), T("ao1", BF16)]; bao = [Buf(), Buf()]
                sg = T("sg"); bsg = Buf()
                for oc in range(4):
                    j = oc % 2
                    for gi, (t0, n) in enumerate(tgs):
                        ps, pb = self.ps[gi % 4], self.pb[gi % 4]
                        for kc in range(4):
                            self.PE(lambda h: h.matmul(ps[:, 0:n], gw[:, kc, oc * 128:(oc + 1) * 128], ygb[:, kc, t0:t0 + n], start=(kc == 0), stop=(kc == 3)), [bgw, byg], [pb], inc=(kc == 3))
                        self.A(lambda h: h.activation(sg[:, t0:t0 + n], ps[:, 0:n], AF.Sigmoid, bias=gb[:, oc:oc + 1]), [pb, bgw], [bsg])
                    self.V(lambda h: h.tensor_mul(ao[j][:], yg[:, oc, :], sg[:]), [byg, bsg], [bao[j]])
                    self.st(self.mixT[b, oc * 128:(oc + 1) * 128, :], ao[j][:], [bao[j]])
                self.S.barrier()

    def phase_out(self, l, b, xsrc, last):
        c = self.c
        TT, NT = c.TT, c.NT
        with ExitStack() as st:
            wo = self.sb(st, "wo", [128, KC, D], BF16); bwo = Buf()
            wsrc = self.w_out[l].rearrange("(kc kp) n -> kp kc n", kp=128)
            for g in range(4):
                self.ldc(wo[:, :, g * 512:(g + 1) * 512], wsrc[:, :, g * 512:(g + 1) * 512], [bwo])
            gB = {}
            for src in ([b] if last else [b, 2]):
                gB[src] = self.bcast_row(st, 0, src, f"gmsa{src}")
            A2 = {}; B2 = {}
            if c.sparse:
                for src in ([b] if last else [b, 2]):
                    A2[src] = self.bcast_row(st, 2, src, f"a2r{src}")
                    B2[src] = self.bcast_row(st, 3, src, f"b2r{src}")
            htk = [self.sb(st, f"htk{i}", [128, D], BF16) for i in range(2)]; bhtk = [Buf(), Buf()]
            htf = self.sb(st, "htf", [128, D]); bhtf = Buf()
            s01 = [self.sb(st, f"s01{i}", [128, NE]) for i in range(2)]; bs01 = [Buf(), Buf()]
            rk = [self.sb(st, f"rk{i}", [128, NE]) for i in range(2)]; brk = [Buf(), Buf()]
            rwt = self.sb(st, "rwt", [128, KC, NE]); rb = self.sb(st, "rb", [128, NE]); brw = Buf()
            self.ld(rwt[:], self.rw[:, :, :], [brw])
            self.ld(rb[:], self.rbias[0:1, :].partition_broadcast(128)[:, 0, :], [brw])
            nt = self.norm_tiles(st); nt["xsf"] = self.sb(st, "xsf", [128, D])
            mt = [self.sb(st, f"mt{i}", [128, KC, 128], BF16) for i in range(2)]; bmt = [Buf(), Buf()]
            xt = [self.sb(st, f"xo{i}", [128, D]) for i in range(2)]; bx = [Buf(), Buf()]
            h2f = self.sb(st, "h2f", [128, KC, 128]); h2b = [self.sb(st, f"h2b{i}", [128, KC, 128], BF16) for i in range(2)]
            bh2f = Buf(); bh2b = [Buf(), Buf()]
            R = lambda n, w=NE: self.sb(st, n, [128, w])
            scr, bia, m1, m2, gs, gmx, ing, sel, tmp4, tmp16, gsum = R("scr"), R("bia"), R("m1", 4), R("m2", 4), R("gs", 4), R("gmx", 1), R("ing", 4), R("sel"), R("tmp4", 4), R("tmp16"), R("gsum", 1)
            gout = [R("gout0"), R("gout1")]; bgo = [Buf(), Buf()]
            br_ = Buf()
            tmo = [self.sb(st, f"tmo{i}", [128, 512]) for i in range(2)]; btmo = [Buf(), Buf()]
            tiles = range(c.NTC, NT) if last else range(NT)
            for i in tiles:
                j = i % 2
                src = 2 if i < c.NTC else b
                self.ld(mt[j][:], self.mixT[b, :, i * 128:(i + 1) * 128].rearrange("(kc kp) t -> kp kc t", kp=128), [bmt[j]], q="act")
                self.ld(xt[j][:], xsrc[b, i * 128:(i + 1) * 128, :], [bx[j]])
                for g in range(4):
                    ps, pb = self.ps[g], self.pb[g]
                    for kc in range(KC):
                        self.PE(lambda h: h.matmul(ps[:, :], mt[j][:, kc, :], wo[:, kc, g * 512:(g + 1) * 512], start=(kc == 0), stop=(kc == KC - 1)), [bmt[j], bwo], [pb], inc=(kc == KC - 1))
                    gt_, bgt_ = gB[src]
                    sl = slice(g * 512, (g + 1) * 512)
                    self.V(lambda h: h.tensor_tensor(tmo[g % 2][:], ps[:, :], gt_[:, sl], ALU.mult), [pb, bgt_], [btmo[g % 2]])
                    self.V(lambda h: h.tensor_add(xt[j][:, sl], xt[j][:, sl], tmo[g % 2][:]), [btmo[g % 2], bx[j]], [bx[j]])
                self.st(self.xres[b, i * 128:(i + 1) * 128, :], xt[j][:], [bx[j]])
                self.norm_T(nt, xt[j], bx[j], 1, src, lambda kc: h2f[:, kc, :], bh2f, fp32=True)
                if not c.sparse:
                    self.A(lambda h: h.copy(h2b[j][:], h2f[:]), [bh2f], [bh2b[j]])
                    self.st(self.h2T[b, :, i * 128:(i + 1) * 128].rearrange("(kc kp) t -> kp kc t", kp=128), h2b[j][:], [bh2b[j]], q="act")
                else:
                    xsf = nt["xsf"]
                    self.V(lambda h: h.tensor_mul(htf[:], xsf[:], A2[src][0][:]), [nt["bxs"], A2[src][1], bhtf], [bhtf])
                    self.V(lambda h: h.tensor_add(htk[j][:], htf[:], B2[src][0][:]), [bhtf, B2[src][1]], [bhtk[j]])
                    self.st(self.h2tok[b * TT + i * 128: b * TT + (i + 1) * 128, :], htk[j][:], [bhtk[j]], q="act")
                pr, bpr = self.ps[4], self.pb[4]
                for kc in range(KC):
                    self.PE(lambda h: h.matmul(pr[:, 0:NE], h2f[:, kc, :], rwt[:, kc, :], start=(kc == 0), stop=(kc == KC - 1)), [bh2f, brw], [bpr], inc=(kc == KC - 1))
                self.A(lambda h: h.activation(scr[:], pr[:, 0:NE], AF.Sigmoid), [bpr], [br_])
                self.V(lambda h: h.tensor_add(bia[:], scr[:], rb[:]), [br_, brw], [br_])
                b4 = bia[:].rearrange("p (g e) -> p g e", e=4)
                self.V(lambda h: h.tensor_reduce(m1[:], b4, AX.X, ALU.max), [br_], [br_])
                self.V(lambda h: h.tensor_tensor(tmp16[:].rearrange("p (g e) -> p g e", e=4), b4, m1[:].unsqueeze(2).to_broadcast([128, 4, 4]), ALU.is_equal), [br_], [br_])
                self.V(lambda h: h.scalar_tensor_tensor(tmp16[:], tmp16[:], -1e9, bia[:], ALU.mult, ALU.add), [br_], [br_])
                self.V(lambda h: h.tensor_reduce(m2[:], tmp16[:].rearrange("p (g e) -> p g e", e=4), AX.X, ALU.max), [br_], [br_])
                self.V(lambda h: h.tensor_add(gs[:], m1[:], m2[:]), [br_], [br_])
                self.V(lambda h: h.tensor_reduce(gmx[:], gs[:], AX.X, ALU.max), [br_], [br_])
                self.V(lambda h: h.tensor_tensor(ing[:], gs[:], gmx[:].to_broadcast([128, 4]), ALU.is_equal), [br_], [br_])
                self.V(lambda h: h.tensor_tensor(sel[:].rearrange("p (g e) -> p g e", e=4), b4, m2[:].unsqueeze(2).to_broadcast([128, 4, 4]), ALU.is_ge), [br_], [br_])
                self.V(lambda h: h.tensor_mul(sel[:].rearrange("p (g e) -> p g e", e=4), sel[:].rearrange("p (g e) -> p g e", e=4), ing[:].unsqueeze(2).to_broadcast([128, 4, 4])), [br_], [br_])
                if c.sparse:
                    self.V(lambda h: h.tensor_copy(s01[j][:], sel[:]), [br_], [bs01[j]])
                    pk, bpk = self.ps[5], self.pb[5]
                    self.PE(lambda h: h.matmul(pk[:, 0:NE], self.ltt[:, 0, :], s01[j][:], start=True, stop=True), [bs01[j], self.bconst], [bpk], inc=False)
                    self.PE(lambda h: h.matmul(pk[:, NE:2 * NE], self.ltt[:, 1, :], s01[j][:], start=True, stop=True), [bs01[j], self.bconst], [bpk])
                    self.V(lambda h: h.tensor_add(rk[j][:], pk[:, 0:NE], self.run[:]), [bpk, self.brun], [brk[j]])
                    self.V(lambda h: h.tensor_add(self.run[:], self.run[:], pk[:, NE:2 * NE]), [bpk, self.brun], [self.brun])
                    self.st(self.selD[b * TT + i * 128: b * TT + (i + 1) * 128, :], s01[j][:], [bs01[j]])
                    self.st(self.rankD[b * TT + i * 128: b * TT + (i + 1) * 128, :], rk[j][:], [brk[j]], q="act")
                self.V(lambda h: h.tensor_mul(sel[:], sel[:], scr[:]), [br_], [br_])
                self.V(lambda h: h.tensor_reduce(gsum[:], sel[:], AX.X, ALU.add), [br_], [br_])
                self.V(lambda h: h.reciprocal(gsum[:], gsum[:]), [br_], [br_])
                self.V(lambda h: h.tensor_scalar(gout[j][:], sel[:], gsum[:, 0:1], None, ALU.mult), [br_], [bgo[j]])
                self.st(self.gates[b, i * 128:(i + 1) * 128, :], gout[j][:], [bgo[j]], q="act")

    def phase_moe(self, l, b, last):
        c = self.c
        NT = c.NT
        tiles = list(range(c.NTC, NT)) if last else list(range(NT))
        STM = 6
        supers = [tiles[i:i + STM] for i in range(0, len(tiles), STM)]
        with ExitStack() as st:
            h2 = self.sb(st, "h2s", [128, KC, STM * 128], BF16); bh2 = Buf()
            gt = self.sb(st, "gts", [128, STM, NE]); bgt = Buf()
            yacc = self.sb(st, "yacc", [128, STM, D]); bya = Buf()
            he = self.sb(st, "he", [128, 8, STM * 128], BF16); bhe = Buf()
            wdn = [self.sb(st, f"wdn{i}", [128, 8, D], BF16) for i in range(2)]; bwd = [Buf(), Buf()]
            wgu = [self.sb(st, f"wgu{i}", [128, 2, KC, 128], BF16) for i in range(2)]; bwgu = [Buf() for _ in range(2)]
            sg = self.sb(st, "sgm", [128, STM * 128]); bsg = Buf()
            gB = {}
            for src in ([b] if last else [b, 2]):
                gB[src] = self.bcast_row(st, 1, src, f"gmlp{src}")
            xt = [self.sb(st, "xm0", [128, D])] * 2; bx = [Buf()] * 2
            ew = 0
            for sup in supers:
                n_t = len(sup); ntok = n_t * 128
                t0 = sup[0] * 128
                self.ld(h2[:, :, 0:ntok], self.h2T[b, :, t0:t0 + ntok].rearrange("(kc kp) t -> kp kc t", kp=128), [bh2])
                self.ld(gt[:, 0:n_t, :], self.gates[b, t0:t0 + ntok, :].rearrange("(i p) e -> p i e", p=128), [bgt], q="act")
                tg = tok_groups(ntok)
                for e in range(NE):
                    jd = e % 2
                    wds = self.wd[l * NE + e].rearrange("(fc fp) n -> fp fc n", fp=128)
                    for g in range(2):
                        self.ldc(wdn[jd][:, :, g * 1024:(g + 1) * 1024], wds[:, :, g * 1024:(g + 1) * 1024], [bwd[jd]])
                    for fc in range(8):
                        jw = ew % 2; ew += 1
                        self.ldc(wgu[jw][:, 0], self.wg[l * NE + e][:, fc * 128:(fc + 1) * 128].rearrange("(kc kp) f -> kp kc f", kp=128), [bwgu[jw]])
                        self.ldc(wgu[jw][:, 1], self.wu[l * NE + e][:, fc * 128:(fc + 1) * 128].rearrange("(kc kp) f -> kp kc f", kp=128), [bwgu[jw]])
                        for gi, (s0, n) in enumerate(tg):
                            pG, pU = self.ps[gi], self.ps[2 + gi]; bpG, bpU = self.pb[gi], self.pb[2 + gi]
                            for kc in range(KC):
                                self.PE(lambda h: h.matmul(pG[:, 0:n], wgu[jw][:, 0, kc, :], h2[:, kc, s0:s0 + n], start=(kc == 0), stop=(kc == KC - 1)), [bwgu[jw], bh2], [bpG], inc=(kc == KC - 1))
                            for kc in range(KC):
                                self.PE(lambda h: h.matmul(pU[:, 0:n], wgu[jw][:, 1, kc, :], h2[:, kc, s0:s0 + n], start=(kc == 0), stop=(kc == KC - 1)), [bwgu[jw], bh2], [bpU], inc=(kc == KC - 1))
                            self.A(lambda h: h.activation(sg[:, s0:s0 + n], pG[:, 0:n], AF.Silu), [bpG], [bsg])
                            self.V(lambda h: h.tensor_mul(he[:, fc, s0:s0 + n], sg[:, s0:s0 + n], pU[:, 0:n]), [bsg, bpU], [bhe])
                    for ti in range(n_t):
                        for g in range(4):
                            ps, pb = self.ps[4 + g], self.pb[4 + g]
                            for fc in range(8):
                                self.PE(lambda h: h.matmul(ps[:, :], he[:, fc, ti * 128:(ti + 1) * 128], wdn[jd][:, fc, g * 512:(g + 1) * 512], start=(fc == 0), stop=(fc == 7)), [bhe, bwd[jd]], [pb], inc=(fc == 7))
                            ysl = yacc[:, ti, g * 512:(g + 1) * 512]
                            if e == 0:
                                self.V(lambda h: h.tensor_scalar(ysl, ps[:, :], gt[:, ti, e:e + 1], None, ALU.mult), [pb, bgt], [bya])
                            else:
                                self.V(lambda h: h.scalar_tensor_tensor(ysl, ps[:, :], gt[:, ti, e:e + 1], ysl, ALU.mult, ALU.add), [pb, bgt, bya], [bya])
                for ti, i in enumerate(sup):
                    j = i % 2
                    src = 2 if i < c.NTC else b
                    gt_, bgt_ = gB[src]
                    self.ld(xt[j][:], self.xres[b, i * 128:(i + 1) * 128, :], [bx[j]])
                    self.V(lambda h: h.tensor_mul(yacc[:, ti, :], yacc[:, ti, :], gt_[:]), [bya, bgt_], [bya])
                    self.V(lambda h: h.tensor_add(xt[j][:], xt[j][:], yacc[:, ti, :]), [bya, bx[j]], [bx[j]])
                    self.st(self.xres[b, i * 128:(i + 1) * 128, :], xt[j][:], [bx[j]])


    def moe_tiles(self, last):
        c = self.c
        tl = range(c.NTC, c.NT) if last else range(c.NT)
        return [(b, i) for b in range(c.NB) for i in tl]

    def n_slots(self, last):
        c = self.c
        return (2 * len(self.moe_tiles(last)) * 128 + c.SLOT - 1) // c.SLOT + NE

    def phase_route(self, l, last):
        c = self.c
        TT = c.TT
        NS = self.n_slots(last)
        SL = float(c.SLOT)
        with ExitStack() as st:
            R = lambda n, w=NE, dt=F32: self.sb(st, n, [128, w], dt)
            x, xi, xf, nsl, cum, base, one = R("rx"), R("rxi", NE, I32), R("rxf"), R("nsl"), R("cum"), R("base"), R("one")
            bq = Buf()
            self.V(lambda h: h.tensor_scalar(x[:], self.run[:], SL - 1.0, 1.0 / SL, ALU.add, ALU.mult), [self.brun], [bq])
            self.V(lambda h: h.tensor_copy(xi[:], x[:]), [bq], [bq])
            self.V(lambda h: h.tensor_copy(xf[:], xi[:]), [bq], [bq])
            self.V(lambda h: h.tensor_tensor(nsl[:], xf[:], x[:], ALU.is_gt), [bq], [bq])
            self.V(lambda h: h.tensor_sub(nsl[:], xf[:], nsl[:]), [bq], [bq])
            self.V(lambda h: h.memset(one[:], 1.0), [], [bq])
            self.V(lambda h: h.tensor_tensor_scan(cum[:], one[:], nsl[:], 0.0, ALU.mult, ALU.add), [bq], [bq])
            self.V(lambda h: h.tensor_sub(base[:], cum[:], nsl[:]), [bq], [bq])
            self.V(lambda h: h.tensor_scalar(base[:], base[:], SL, None, ALU.mult), [bq], [bq])
            sio = R("sio", NS); ge = self.sb(st, "ge", [128, NS, NE]); es = R("es", NS)
            self.ld(sio[:], self.siota[:, 0:NS], [bq])
            self.V(lambda h: h.tensor_tensor(ge[:], sio[:].unsqueeze(2).to_broadcast([128, NS, NE]), cum[:].unsqueeze(1).to_broadcast([128, NS, NE]), ALU.is_ge), [bq], [bq])
            self.V(lambda h: h.tensor_reduce(es[:], ge[:], AX.X, ALU.add), [bq], [bq])
            self.V(lambda h: h.tensor_scalar(self.es2[:, 0, 0:NS], es[:], float(D), None, ALU.mult), [bq], [self.bes])
            self.V(lambda h: h.tensor_scalar(self.es2[:, 1, 0:NS], es[:], float(DFF), None, ALU.mult), [bq], [self.bes])
            zg = R("zg", NS * c.SLOT // 128); bgs = Buf()
            self.V(lambda h: h.memset(zg[:], 0.0), [], [bq])
            self.st(self.gsD[0:NS * c.SLOT, :].rearrange("(p r) o -> p (r o)", p=128), zg[:], [bq])
            self.S.barrier()
            sl = [R("sl0"), R("sl1")]; gl = [R("gl0"), R("gl1")]; rl = [R("rl0"), R("rl1")]; bl = [Buf(), Buf()]
            pos, pm, t1, eq = R("pos"), R("pm"), R("t1"), R("eq")
            pp = R("pp", 2); gg = [R("gg0", 2), R("gg1", 2)]; bgg = [Buf(), Buf()]; gsum = R("gsm", 1)
            ht = [self.sb(st, f"rht{i}", [128, D], BF16) for i in range(2)]; bht = [Buf(), Buf()]
            bw_ = Buf()
            for ti, (b, i) in enumerate(self.moe_tiles(last)):
                j = ti % 2
                r0 = b * TT + i * 128
                self.ld(sl[j][:], self.selD[r0:r0 + 128, :], [bl[j]])
                self.ld(gl[j][:], self.gates[b, i * 128:(i + 1) * 128, :], [bl[j]], q="act")
                self.ld(rl[j][:], self.rankD[r0:r0 + 128, :], [bl[j]])
                self.ld(ht[j][:], self.h2tok[r0:r0 + 128, :], [bht[j]], q="act")
                self.V(lambda h: h.tensor_add(pos[:], rl[j][:], base[:]), [bl[j], bq], [bw_])
                self.V(lambda h: h.tensor_scalar(t1[:], sl[j][:], -1e6, 1e6, ALU.mult, ALU.add), [bl[j]], [bw_])
                self.V(lambda h: h.tensor_mul(pm[:], pos[:], sl[j][:]), [bw_, bl[j]], [bw_])
                self.V(lambda h: h.tensor_add(t1[:], t1[:], pm[:]), [bw_], [bw_])
                self.V(lambda h: h.tensor_reduce(pp[:, 0:1], t1[:], AX.X, ALU.min), [bw_], [bw_])
                self.V(lambda h: h.tensor_reduce(pp[:, 1:2], pm[:], AX.X, ALU.max), [bw_], [bw_])
                self.V(lambda h: h.tensor_copy(self.pidx[:, ti, :], pp[:]), [bw_], [self.bpidx])
                self.V(lambda h: h.tensor_scalar(eq[:], t1[:], pp[:, 0:1], None, ALU.is_equal), [bw_], [bw_])
                self.V(lambda h: h.tensor_mul(eq[:], eq[:], gl[j][:]), [bw_, bl[j]], [bw_])
                self.V(lambda h: h.tensor_reduce(gg[j][:, 0:1], eq[:], AX.X, ALU.add), [bw_, bgg[j]], [bgg[j]])
                self.V(lambda h: h.tensor_reduce(gsum[:], gl[j][:], AX.X, ALU.add), [bl[j]], [bw_])
                self.V(lambda h: h.tensor_sub(gg[j][:, 1:2], gsum[:], gg[j][:, 0:1]), [bw_, bgg[j]], [bgg[j]])
                for k in range(2):
                    self.S.idma(reads=[bht[j], self.bpidx], writes=[], out=self.hs[:, :], out_offset=bass.IndirectOffsetOnAxis(ap=self.pidx[:, ti, k:k + 1], axis=0),
                                in_=ht[j][:, :], in_offset=None)
                    self.S.idma(reads=[bgg[j], self.bpidx], writes=[], out=self.gsD[:, :], out_offset=bass.IndirectOffsetOnAxis(ap=self.pidx[:, ti, k:k + 1], axis=0),
                                in_=gg[j][:, k:k + 1], in_offset=None)

    def phase_moe_sparse(self, l, last):
        c = self.c
        NS = self.n_slots(last)
        SLT = c.SLOT // 128
        wgf = self.wg.rearrange("e k (h f) -> (e k h) f", h=2); wuf = self.wu.rearrange("e k (h f) -> (e k h) f", h=2)
        bgu, bdn = 2 * (l + 1) * NE * D - 1, (l + 1) * NE * DFF - 1
        if getattr(self, "bcreg", None) is None:
            self.bcreg = self.nc.gpsimd.alloc_register("moe_bc")
        bcr = self.bcreg
        wdf = self.wd.rearrange("e f n -> (e f) n")
        with ExitStack() as st:
            wg_ = [self.sb(st, f"swg{i}", [128, KC, 512], BF16) for i in range(2)]
            wu_ = [self.sb(st, f"swu{i}", [128, KC, 512], BF16) for i in range(2)]
            wd_ = [self.sb(st, f"swd{i}", [128, 4, D], BF16) for i in range(2)]
            bwg, bwu, bwd = [Buf(), Buf()], [Buf(), Buf()], [Buf(), Buf()]
            h2s = self.sb(st, "h2s", [128, KC, c.SLOT], BF16); bh2 = Buf()
            hr = [self.sb(st, f"hr{i}", [128, D], BF16) for i in range(2)]; bhr = [Buf(), Buf()]
            he = [self.sb(st, f"she{i}", [128, 4, c.SLOT], BF16) for i in range(2)]; bhe = [Buf(), Buf()]
            ys = self.sb(st, "ys", [128, SLT, D]); bys = Buf()
            sg = self.sb(st, "ssg", [128, c.SLOT]); bsg = Buf()
            gs = [self.sb(st, f"sgs{i}", [128, SLT]) for i in range(2)]; bgs = [Buf(), Buf()]
            wi = [self.sb(st, f"swi{i}", [128, 40], I32) for i in range(2)]; bwi = [Buf(), Buf()]
            wif = self.sb(st, "swif", [128, 16]); bwif = Buf()
            hh = 0
            for s_ in range(NS):
                js = s_ % 2
                self.V(lambda h: h.tensor_scalar(wif[:], self.kiot[:, 0:16], self.es2[:, 0, s_:s_ + 1], float(l * NE * D), ALU.add, ALU.add), [self.bes, self.bconst, bwif], [bwif])
                for hf in range(2):
                    self.V(lambda h: h.tensor_scalar(wi[js][:, hf * 16:(hf + 1) * 16], wif[:], 2.0, float(hf), ALU.mult, ALU.add), [bwif, bwi[js]], [bwi[js]])
                self.V(lambda h: h.tensor_scalar(wi[js][:, 32:40], self.kiot[:, 16:24], self.es2[:, 1, s_:s_ + 1], float(l * NE * DFF), ALU.add, ALU.add), [self.bes, self.bconst, bwi[js]], [bwi[js]])
                self.S.dma("act", gs[js][:], self.gsD[s_ * c.SLOT:(s_ + 1) * c.SLOT, :].rearrange("(t p) o -> p (t o)", p=128), writes=[bgs[js]], allow_slow_non_contiguous=True)
                for t in range(SLT):
                    jr = t % 2
                    self.ld(hr[jr][:], self.hs[s_ * c.SLOT + t * 128: s_ * c.SLOT + (t + 1) * 128, :], [bhr[jr]], q=("sp" if jr == 0 else "act"))
                    for g in range(2):
                        p, pb = self.ps[4 + g], self.pb[4 + g]
                        pv = p[:].bitcast(BF16)
                        for k8 in range(8):
                            kc = g * 8 + k8
                            self.PE(lambda h: h.transpose(pv[:, k8 * 128:(k8 + 1) * 128], hr[jr][:, kc * 128:(kc + 1) * 128], self.idb[:]), [bhr[jr], self.bconst], [pb], inc=(k8 == 7))
                        dst = h2s[:, g * 8:(g + 1) * 8, t * 128:(t + 1) * 128]
                        if g == 0:
                            self.A(lambda h: h.copy(dst, pv[:, 0:1024].rearrange("p (k t) -> p k t", t=128)), [pb], [bh2])
                        else:
                            self.V(lambda h: h.tensor_copy(dst, pv[:, 0:1024].rearrange("p (k t) -> p k t", t=128)), [pb], [bh2])
                for half in range(2):
                    jw = hh % 2; hh += 1
                    cs = slice(half * 512, (half + 1) * 512)
                    self.nc.gpsimd.reg_mov(bcr, bgu)
                    for kc in range(KC):
                        self.S.idma(reads=[bwi[js]], writes=[bwg[jw]], out=wg_[jw][:, kc, :], out_offset=None, in_=wgf[:, :],
                                    in_offset=bass.IndirectOffsetOnAxis(ap=wi[js][:, half * 16 + kc:half * 16 + kc + 1], axis=0), bounds_check=bcr, oob_is_err=False)
                        self.S.idma(reads=[bwi[js]], writes=[bwu[jw]], out=wu_[jw][:, kc, :], out_offset=None, in_=wuf[:, :],
                                    in_offset=bass.IndirectOffsetOnAxis(ap=wi[js][:, half * 16 + kc:half * 16 + kc + 1], axis=0), bounds_check=bcr, oob_is_err=False)
                    self.nc.gpsimd.reg_mov(bcr, bdn)
                    for fc in range(4):
                        self.S.idma(reads=[bwi[js]], writes=[bwd[jw]], out=wd_[jw][:, fc, :], out_offset=None, in_=wdf[:, :],
                                    in_offset=bass.IndirectOffsetOnAxis(ap=wi[js][:, 32 + half * 4 + fc:33 + half * 4 + fc], axis=0), bounds_check=bcr, oob_is_err=False)
                    for fc in range(4):
                        for gi, (s0, n) in enumerate(tok_groups(c.SLOT)):
                            pG, pU = self.ps[gi], self.ps[2 + gi]; bpG, bpU = self.pb[gi], self.pb[2 + gi]
                            for kc in range(KC):
                                self.PE(lambda h: h.matmul(pG[:, 0:n], wg_[jw][:, kc, fc * 128:(fc + 1) * 128], h2s[:, kc, s0:s0 + n], start=(kc == 0), stop=(kc == KC - 1)), [bwg[jw], bh2], [bpG], inc=(kc == KC - 1))
                            for kc in range(KC):
                                self.PE(lambda h: h.matmul(pU[:, 0:n], wu_[jw][:, kc, fc * 128:(fc + 1) * 128], h2s[:, kc, s0:s0 + n], start=(kc == 0), stop=(kc == KC - 1)), [bwu[jw], bh2], [bpU], inc=(kc == KC - 1))
                            self.A(lambda h: h.activation(sg[:, s0:s0 + n], pG[:, 0:n], AF.Silu), [bpG, bsg], [bsg])
                            self.V(lambda h: h.tensor_mul(he[jw][:, fc, s0:s0 + n], sg[:, s0:s0 + n], pU[:, 0:n]), [bsg, bpU], [bhe[jw]])
                    for t in range(SLT):
                        for g in range(4):
                            ps, pb = self.ps[4 + g], self.pb[4 + g]
                            for fc in range(4):
                                self.PE(lambda h: h.matmul(ps[:, :], he[jw][:, fc, t * 128:(t + 1) * 128], wd_[jw][:, fc, g * 512:(g + 1) * 512], start=(fc == 0), stop=(fc == 3)), [bhe[jw], bwd[jw]], [pb], inc=(fc == 3))
                            ysl = ys[:, t, g * 512:(g + 1) * 512]
                            if half == 0:
                                self.V(lambda h: h.tensor_scalar(ysl, ps[:, :], gs[js][:, t:t + 1], None, ALU.mult), [pb, bgs[js], bys], [bys])
                            else:
                                self.V(lambda h: h.scalar_tensor_tensor(ysl, ps[:, :], gs[js][:, t:t + 1], ysl, ALU.mult, ALU.add), [pb, bgs[js], bys], [bys])
                self.st(self.ysD[s_ * c.SLOT:(s_ + 1) * c.SLOT, :].rearrange("(t p) n -> p t n", p=128), ys[:], [bys])

    def phase_unsort(self, l, last):
        c = self.c
        TT = c.TT
        with ExitStack() as st:
            gB = {}
            for src in range(c.NB):
                gB[src] = self.bcast_row(st, 1, src, f"ugm{src}")
            if not last:
                gB[2] = self.bcast_row(st, 1, 2, "ugm2")
            ya = [self.sb(st, f"ya{i}", [128, D]) for i in range(2)]; yb = [self.sb(st, f"yb{i}", [128, D]) for i in range(2)]
            xt = [self.sb(st, f"ux{i}", [128, D]) for i in range(2)]
            bya, byb, bx = [Buf(), Buf()], [Buf(), Buf()], [Buf(), Buf()]
            for ti, (b, i) in enumerate(self.moe_tiles(last)):
                j = ti % 2
                src = 2 if i < c.NTC else b
                self.S.idma(reads=[self.bpidx], writes=[bya[j]], out=ya[j][:, :], out_offset=None, in_=self.ysD[:, :],
                            in_offset=bass.IndirectOffsetOnAxis(ap=self.pidx[:, ti, 0:1], axis=0))
                self.S.idma(reads=[self.bpidx], writes=[byb[j]], out=yb[j][:, :], out_offset=None, in_=self.ysD[:, :],
                            in_offset=bass.IndirectOffsetOnAxis(ap=self.pidx[:, ti, 1:2], axis=0))
                self.ld(xt[j][:], self.xres[b, i * 128:(i + 1) * 128, :], [bx[j]])
                self.V(lambda h: h.tensor_add(ya[j][:], ya[j][:], yb[j][:]), [bya[j], byb[j]], [bya[j]])
                self.V(lambda h: h.tensor_mul(ya[j][:], ya[j][:], gB[src][0][:]), [bya[j], gB[src][1]], [bya[j]])
                self.V(lambda h: h.tensor_add(xt[j][:], xt[j][:], ya[j][:]), [bya[j], bx[j]], [bx[j]])
                self.st(self.xres[b, i * 128:(i + 1) * 128, :], xt[j][:], [bx[j]], q="act")

    def phase_final(self):
        c = self.c
        with ExitStack() as st:
            gB = self.sb(st, "gfinB", [128, D]); bg = Buf()
            self.ld(gB[:], self.gfin[0:1, :].partition_broadcast(128)[:, 0, :], [bg])
            xt = [self.sb(st, f"xf{i}", [128, D]) for i in range(2)]; bx = [Buf(), Buf()]
            sq = self.sb(st, "fsq", [128, D], BF16); ss = self.sb(st, "fss", [128, 4]); bs = Buf()
            for b in range(c.NB):
                for i in range(c.NTC, c.NT):
                    j = i % 2
                    self.ld(xt[j][:], self.xres[b, i * 128:(i + 1) * 128, :], [bx[j]], q=("sp" if j == 0 else "act"))
                    self.A(lambda h: h.activation(sq[:], xt[j][:], AF.Square, accum_out=ss[:, 0:1]), [bx[j]], [bs])
                    self.A(lambda h: h.activation(ss[:, 1:2], ss[:, 0:1], AF.Sqrt, scale=1.0 / D, bias=self.epsc[:, 0:1]), [bs, self.bconst], [bs])
                    self.V(lambda h: h.reciprocal(ss[:, 2:3], ss[:, 1:2]), [bs], [bs])
                    self.V(lambda h: h.scalar_tensor_tensor(xt[j][:], xt[j][:], ss[:, 2:3], gB[:], ALU.mult, ALU.mult), [bx[j], bs, bg], [bx[j]])
                    self.st(self.out[b, (i - c.NTC) * 128:(i - c.NTC + 1) * 128, :], xt[j][:], [bx[j]], q=("sp" if j == 0 else "act"))


def host_shared(inp, cfg):
    L = cfg.L
    f = lambda a: np.ascontiguousarray(np.asarray(a, dtype=np.float32))
    pk = lambda v: f(np.asarray(v).reshape(v.shape[:-1] + (v.shape[-1] // 128, 128)).swapaxes(-1, -2))
    sh = {}
    sh["gmix"] = pk(inp["norm_mix_g"]); sh["gffn"] = pk(inp["norm_ffn_g"])
    sh["gfin"] = f(inp["final_norm_g"]).reshape(1, D)
    sh["w_mod"] = f(inp["w_mod"]); sh["b_mod"] = f(inp["b_mod"])
    w_in = np.asarray(inp["w_in"], np.float32).reshape(L, D, 12, W)
    def swap(p):
        x = w_in[:, :, p].reshape(L, D, 4, 2, 64)
        return x[:, :, :, ::-1, :].reshape(L, D, W)
    sh["w_in"] = f(np.concatenate([w_in.reshape(L, D, 12 * W), swap(8), swap(9)], axis=-1))
    sh["w_out"] = f(inp["w_out"])
    bre, bim = np.asarray(inp["s5_b_re"], np.float32), np.asarray(inp["s5_b_im"], np.float32)
    cre, cim = np.asarray(inp["s5_c_re"], np.float32), np.asarray(inp["s5_c_im"], np.float32)
    s5B = np.zeros((L, 2, 16, 128, 2, 128), np.float32)
    s5C = np.zeros((L, 2, 16, 128, 2, 128), np.float32)
    for sc in range(16):
        for g2 in range(2):
            g = 2 * sc + g2
            r0 = 16 * (g % 8)
            s5B[:, :, sc, r0:r0 + 16, 0, g2 * 64:(g2 + 1) * 64] = bre[:, :, g]
            s5B[:, :, sc, r0:r0 + 16, 1, g2 * 64:(g2 + 1) * 64] = bim[:, :, g]
            s5C[:, :, sc, g2 * 64:(g2 + 1) * 64, 0, r0:r0 + 16] = cre[:, :, g]
            s5C[:, :, sc, g2 * 64:(g2 + 1) * 64, 1, r0:r0 + 16] = cim[:, :, g]
    sh["s5B"], sh["s5C"] = s5B, s5C
    lam = np.zeros((L, 128, 3, 2, 16), np.float32)
    lre, lim, ldt = (np.asarray(inp[k], np.float32) for k in ("s5_lam_re", "s5_lam_im", "s5_log_dt"))
    for sc in range(16):
        for g2 in range(2):
            g = 2 * sc + g2
            lam[:, g2 * 64:(g2 + 1) * 64, 0, :, sc] = lre[:, :, g, :].transpose(0, 2, 1)
            lam[:, g2 * 64:(g2 + 1) * 64, 1, :, sc] = lim[:, :, g, :].transpose(0, 2, 1)
            lam[:, g2 * 64:(g2 + 1) * 64, 2, :, sc] = ldt[:, :, g][:, None, :]
    sh["s5lam"] = lam
    sh["s5d"] = pk(inp["s5_d"]); sh["gluw"] = f(inp["s5_glu_w"]); sh["glub"] = pk(inp["s5_glu_b"])
    TT, CL = cfg.TT, cfg.CTXL
    kf = np.arange(TT, dtype=np.float32)
    kb = np.concatenate([CL - 1 - np.arange(CL), CL + (TT - CL) - 1 - np.arange(TT - CL)]).astype(np.float32)
    sh["kidx"] = f(np.stack([np.broadcast_to(kf, (128, TT)), np.broadcast_to(kb, (128, TT))]))
    cw = np.asarray(inp["lru_conv_w"], np.float32)
    sh["convw"] = f(cw.reshape(L, 4, 4, 128).transpose(0, 3, 2, 1))
    lv = np.stack([np.asarray(inp["lru_conv_b"], np.float32)] +
                  [np.asarray(inp[k], np.float32)[:, d] for k in ("lru_ba", "lru_bx", "lru_lam") for d in range(2)], axis=-1)
    sh["lruv"] = f(lv.reshape(L, 4, 128, 7).transpose(0, 2, 1, 3))
    lw = np.zeros((L, 2, 2, 4, 128, 128), np.float32)
    for a, k in enumerate(("lru_wa", "lru_wx")):
        wsrc = np.asarray(inp[k], np.float32)
        for hd in range(8):
            cc, h2 = hd // 2, hd % 2
            lw[:, a, :, cc, h2 * 64:(h2 + 1) * 64, h2 * 64:(h2 + 1) * 64] = wsrc[:, :, hd]
    sh["lruw"] = lw
    hl = np.asarray(inp["hgrn_lb_logits"], np.float32)
    sh["hglb"] = f(hl.reshape(2, L, 4, 128).transpose(0, 1, 3, 2))
    n = cfg.SEQL
    rows = n // 64
    row = np.repeat(np.arange(rows, dtype=np.float32), 64); col = np.tile(np.arange(64, dtype=np.float32), rows)
    inv = (np.float32(10000.0) ** (-np.arange(32, dtype=np.float32) / np.float32(32))).astype(np.float32)
    ang = np.concatenate([row[:, None] * inv, col[:, None] * inv], axis=-1).astype(np.float32)
    sh["rang"] = f(np.concatenate([ang.T, ang.T], axis=0))
    sh["rw"] = f(np.asarray(inp["router_w"], np.float32).reshape(KC, 128, NE).transpose(1, 0, 2))
    sh["rbias"] = f(inp["router_bias"]).reshape(1, NE)
    def padx(a):
        a = np.asarray(a, np.float32)
        a = a.reshape((L * NE,) + a.shape[2:])
        return np.concatenate([a, np.zeros((1,) + a.shape[1:], np.float32)], axis=0)
    sh["wg"], sh["wu"], sh["wd"] = padx(inp["moe_w_gate"]), padx(inp["moe_w_up"]), padx(inp["moe_w_down"])
    sh["ident"] = np.eye(128, dtype=np.float32)
    s_, t_ = np.meshgrid(np.arange(64), np.arange(64), indexing="ij")
    m = np.stack([(s_ <= t_), (s_ >= t_)]).astype(np.float32)
    mm = np.concatenate([m, m], axis=1)
    sh["masks"] = f(np.broadcast_to(mm[:, :, None, :], (2, 128, 4, 64)))
    rst = np.ones((128, TT + 1), np.float32); rst[:, 0::64] = 0.0
    sh["rst"] = rst
    rst32 = np.ones((128, TT + 1), np.float32); rst32[:, 0::32] = 0.0
    sh["rst32"] = rst32
    s_, t_ = np.meshgrid(np.arange(32), np.arange(32), indexing="ij")
    m32 = np.stack([(s_ <= t_), (s_ >= t_)]).astype(np.float32)
    sh["masks32"] = f(np.broadcast_to(np.concatenate([m32] * 4, axis=1)[:, :, None, :], (2, 128, 4, 32)))
    sh["gffn_row"] = f(inp["norm_ffn_g"])
    tp_, t_ = np.meshgrid(np.arange(128), np.arange(128), indexing="ij")
    sh["ltri"] = f(np.stack([(tp_ < t_).astype(np.float32), np.ones((128, 128), np.float32)]))
    p_ = np.arange(128)[:, None]
    sh["kio"] = f(np.concatenate([np.arange(16)[None, :] * 128 + p_, np.arange(8)[None, :] * 128 + p_], axis=1))
    nsmax = (2 * cfg.NB * cfg.TT + cfg.SLOT - 1) // cfg.SLOT + NE
    sh["siota"] = f(np.broadcast_to(np.arange(nsmax, dtype=np.float32), (128, nsmax)))
    sel = np.zeros((3, 3, 128), np.float32)
    for s in range(3):
        sel[s, s, :] = 1.0
    sh["sel3"] = sel
    return sh


def host_core(inp, cfg, b0):
    NB = cfg.NB
    x = np.asarray(inp["x"], np.float32)[b0:b0 + NB]
    ctx = np.asarray(inp["ctx"], np.float32)[b0:b0 + NB]
    cvec = np.concatenate([np.asarray(inp["c"], np.float32)[b0:b0 + NB], np.asarray(inp["c_ctx"], np.float32)[None]], axis=0)
    if NB == 1:
        cvec = np.concatenate([cvec[0:1], cvec[0:1], cvec[1:2]], axis=0)
    return {"xin": np.ascontiguousarray(np.concatenate([ctx, x], axis=1)),
            "cT": np.ascontiguousarray(cvec.reshape(3, KC, 128).transpose(2, 1, 0))}


_CACHE = {}


def kernel(**inputs):
    cfg = Cfg()
    n_cores = 8
    if "nc" not in _CACHE:
        _CACHE["nc"] = Prog(cfg).build()
    nc = _CACHE["nc"]
    sh = host_shared(inputs, cfg)
    in_maps = []
    for core in range(n_cores):
        m = dict(sh)
        m.update(host_core(inputs, cfg, core * cfg.NB))
        in_maps.append(m)
    res = run_bass_kernel_spmd(nc, in_maps, core_ids=list(range(n_cores)))
    return np.concatenate([r["out"] for r in res.results], axis=0).astype(np.float32)
```
